# Optimizing a Trainium2 kernel written in Bass

```python
import jax
import jax.numpy as jnp
from jax import lax
import numpy as np

D_MODEL = 1024
BATCH = 16
SEQ = 2048
DEPTH = 1

GRID_W = 64
CTX_LEN = 256
EPS = 1e-6

MLA_HEADS = 8
QK_NOPE = 64
QK_ROPE = 32
QK_HEAD = QK_NOPE + QK_ROPE
V_HEAD = 64
Q_LORA = 256
KV_LORA = 128
ROPE_BASE = 10000.0
ATTN_SCALE = QK_HEAD ** -0.5
Q_BLOCK = 128
MLA_IN = Q_LORA + KV_LORA + QK_ROPE

RW_HEADS = 8
RW_HEAD = 64
RW_DIM = RW_HEADS * RW_HEAD
DECAY_LORA = 64
AICL_LORA = 64
GATE_LORA = 160
GN_EPS = 64e-5
SHIFT_WIDTH = 3
RW_SPLITS = (RW_DIM, RW_DIM, RW_DIM, DECAY_LORA, DECAY_LORA, AICL_LORA, AICL_LORA, GATE_LORA)
RW_IN = sum(RW_SPLITS)

N_BRANCH = 2
IN_WIDTH = MLA_IN + RW_IN + N_BRANCH * D_MODEL

N_EXPERTS = 256
TOP_K = 8
N_GROUPS = 8
TOPK_GROUPS = 4
EXPERT_FF = 256
ROUTED_SCALE = 2.5
DISPATCH_BLOCK = 128

kernel_name = 'hybrid_mla_rwkv7_moe_dit_layer'


def _split(x, sizes):
    return jnp.split(x, np.cumsum(sizes)[:-1].tolist(), axis=-1)


def rms_norm(x, gain):
    xf = x.astype(jnp.float32)
    y = xf * lax.rsqrt(jnp.mean(xf * xf, axis=-1, keepdims=True) + EPS)
    return (y * gain.astype(jnp.float32)).astype(x.dtype)


def modulate(x, shift, scale):
    return x * (1 + scale) + shift


def centred_conv(x, w):
    T = x.shape[1]
    pad = SHIFT_WIDTH // 2
    xp = jnp.pad(x, ((0, 0), (pad, pad), (0, 0)))
    return sum(xp[:, j:j + T] * w[j] for j in range(SHIFT_WIDTH))


def axial_rope(n_tokens, dtype):
    rows = n_tokens // GRID_W
    row = jnp.repeat(jnp.arange(rows, dtype=jnp.float32), GRID_W)
    col = jnp.tile(jnp.arange(GRID_W, dtype=jnp.float32), rows)
    n_freq = QK_ROPE // 4
    inv_freq = ROPE_BASE ** (-jnp.arange(n_freq, dtype=jnp.float32) / n_freq)
    ang = jnp.concatenate([row[:, None] * inv_freq, col[:, None] * inv_freq], axis=-1)
    return jnp.cos(ang).astype(dtype), jnp.sin(ang).astype(dtype)


def apply_rope(x, cos, sin):
    half = QK_ROPE // 2
    x_nope, x1, x2 = x[..., :QK_NOPE], x[..., QK_NOPE:QK_NOPE + half], x[..., QK_NOPE + half:]
    c, s = cos[:, None, :], sin[:, None, :]
    return jnp.concatenate([x_nope, x1 * c - x2 * s, x1 * s + x2 * c], axis=-1)


def mla_queries(q_lat, p, rope):
    B, T, _ = q_lat.shape
    q = (rms_norm(q_lat, p['q_lat_norm']) @ p['w_uq']).reshape(B, T, MLA_HEADS, QK_HEAD)
    q = rms_norm(q, p['q_norm'])
    return q if rope is None else apply_rope(q, *rope)


def mla_keys_values(kv_lat, k_rope, p, rope):
    B, T, _ = kv_lat.shape
    kv = (rms_norm(kv_lat, p['kv_lat_norm']) @ p['w_ukv']).reshape(B, T, MLA_HEADS, QK_NOPE + V_HEAD)
    k_nope, v = kv[..., :QK_NOPE], kv[..., QK_NOPE:]
    k_shared = jnp.broadcast_to(k_rope[:, :, None, :], (B, T, MLA_HEADS, QK_ROPE))
    k = rms_norm(jnp.concatenate([k_nope, k_shared], axis=-1), p['k_norm'])
    return (k if rope is None else apply_rope(k, *rope)), v


def attend(q, k, v):
    s = jnp.einsum('bqhd,bkhd->bhqk', q, k).astype(jnp.float32) * ATTN_SCALE
    w = jax.nn.softmax(s, axis=-1).astype(v.dtype)
    return jnp.einsum('bhqk,bkhd->bqhd', w, v)


def block_attention(q, k, v):
    B, T, H, Dh = q.shape
    nb = T // Q_BLOCK
    qb = q.reshape(B, nb, Q_BLOCK, H, Dh).swapaxes(0, 1)
    o = lax.map(lambda qblk: attend(qblk, k, v), qb)
    return o.swapaxes(0, 1).reshape(B, T, H * V_HEAD)


def rwkv_operands(rw, p):
    B, T, _ = rw.shape
    heads = lambda z: z.reshape(B, T, RW_HEADS, RW_HEAD)
    r, k, v, lw_f, lw_b, la_f, la_b, lg = _split(rw, RW_SPLITS)
    kk = heads(k * p['k_k']).astype(jnp.float32)
    kk = kk * lax.rsqrt(jnp.sum(kk * kk, axis=-1, keepdims=True) + 1e-12)
    dirs = []
    for d, (lw, la) in enumerate(((lw_f, la_f), (lw_b, la_b))):
        w_log = -jax.nn.softplus(-(p['decay_w0'][d] + jnp.tanh(lw) @ p['decay_w2'][d])) - 0.5
        decay = jnp.exp(-jnp.exp(w_log.astype(jnp.float32)))
        a = jax.nn.sigmoid(p['aicl_a0'][d] + la @ p['aicl_a2'][d])
        k_d = k * (1 + (a - 1) * p['k_a'])
        dirs.append((heads(decay), heads(k_d), heads(a)))
    return heads(r), heads(v), kk, dirs, lg


def wkv7_scan(state0, r, decay, k, v, kk, a, reverse, emit):
    f32 = jnp.float32
    seqs = (decay, k, v, kk, a) + ((r,) if emit else ())
    xs = tuple(jnp.moveaxis(z.astype(f32), 1, 0) for z in seqs)

    def step(S, inp):
        w_t, k_t, v_t, kk_t, a_t = inp[:5]
        sa = jnp.einsum('bhvk,bhk->bhv', S, -kk_t)
        S = (S * w_t[:, :, None, :] + sa[..., None] * (kk_t * a_t)[:, :, None, :]
             + v_t[..., None] * k_t[:, :, None, :])
        return S, (jnp.einsum('bhvk,bhk->bhv', S, inp[5]) if emit else None)

    S, ys = lax.scan(step, state0, xs, reverse=reverse)
    return S, (jnp.moveaxis(ys, 0, 1) if emit else None)


def rwkv_bidirectional(r, v, kk, dirs, init_states, emit):
    ys, states = [], []
    for d, (decay, k_d, a) in enumerate(dirs):
        S, y = wkv7_scan(init_states[d], r, decay, k_d, v, kk, a, d == 1, emit)
        states.append(S)
        ys.append(y)
    return (ys[0] + ys[1] if emit else None), (states[0], states[1])


def rwkv_readout(y, r, v, dirs, lg, p):
    B, T = y.shape[:2]
    mu = jnp.mean(y, axis=-1, keepdims=True)
    var = jnp.mean(jnp.square(y - mu), axis=-1, keepdims=True)
    y_n = ((y - mu) * lax.rsqrt(var + GN_EPS)).reshape(B, T, RW_DIM)
    y_n = y_n * p['gn_w'].astype(jnp.float32) + p['gn_b'].astype(jnp.float32)
    k_sum = dirs[0][1] + dirs[1][1]
    bonus = jnp.sum(r * k_sum * p['r_k'], axis=-1, keepdims=True) * v
    g = jax.nn.sigmoid(lg) @ p['gate_g2']
    return (y_n.astype(v.dtype) + bonus.reshape(B, T, RW_DIM)) * g


def merge_branches(att, rw, gate_logits, p):
    g_att, g_rw = jnp.split(jax.nn.sigmoid(gate_logits), N_BRANCH, axis=-1)
    return (g_att * (att @ p['w_o_mla']) + g_rw * (rw @ p['w_o_rwkv'])) @ p['w_out']


def token_mixers(h_lat, h_ctx, p, update_ctx):
    B, T, _ = h_lat.shape
    C = h_ctx.shape[1]
    in_sizes = (MLA_IN, RW_IN, N_BRANCH * D_MODEL)
    mla_l, rw_l, gate_l = _split(h_lat @ p['w_in'], in_sizes)
    mla_c, rw_c, gate_c = _split(h_ctx @ p['w_in'], in_sizes)
    mla_sizes = (Q_LORA, KV_LORA, QK_ROPE)
    q_lat_l, kv_lat_l, kr_l = _split(mla_l, mla_sizes)
    q_lat_c, kv_lat_c, kr_c = _split(mla_c, mla_sizes)

    rope = axial_rope(T, h_lat.dtype)
    q_l = mla_queries(q_lat_l, p, rope)
    k_l, v_l = mla_keys_values(kv_lat_l, kr_l, p, rope)
    k_c, v_c = mla_keys_values(kv_lat_c, kr_c, p, None)
    att_l = block_attention(q_l, jnp.concatenate([k_l, k_c], axis=1), jnp.concatenate([v_l, v_c], axis=1))

    r_c, v_rc, kk_c, dirs_c, lg_c = rwkv_operands(centred_conv(rw_c, p['shift_conv']), p)
    r_l, v_rl, kk_l, dirs_l, lg_l = rwkv_operands(centred_conv(rw_l, p['shift_conv']), p)
    zero_state = jnp.zeros((B, RW_HEADS, RW_HEAD, RW_HEAD), jnp.float32)
    y_c, ctx_states = rwkv_bidirectional(r_c, v_rc, kk_c, dirs_c, (zero_state, zero_state), update_ctx)
    y_l, _ = rwkv_bidirectional(r_l, v_rl, kk_l, dirs_l, ctx_states, True)
    rw_out_l = rwkv_readout(y_l, r_l, v_rl, dirs_l, lg_l, p)
    mix_l = merge_branches(att_l, rw_out_l, gate_l, p)
    if not update_ctx:
        return mix_l, None
    att_c = attend(mla_queries(q_lat_c, p, None), k_c, v_c).reshape(B, C, MLA_HEADS * V_HEAD)
    rw_out_c = rwkv_readout(y_c, r_c, v_rc, dirs_c, lg_c, p)
    return mix_l, merge_branches(att_c, rw_out_c, gate_c, p)


def moe_ffn(u, p):
    B, T, D = u.shape
    n_tok = B * T
    t = u.reshape(n_tok, D)
    scores = jax.nn.sigmoid(t.astype(jnp.float32) @ p['router_w'].astype(jnp.float32))
    sel = scores + p['router_bias'].astype(jnp.float32)
    per_group = N_EXPERTS // N_GROUPS
    group_score = lax.top_k(sel.reshape(n_tok, N_GROUPS, per_group), 2)[0].sum(-1)
    _, top_groups = lax.top_k(group_score, TOPK_GROUPS)
    group_mask = jnp.any(top_groups[:, :, None] == jnp.arange(N_GROUPS)[None, None, :], axis=1)
    sel = jnp.where(jnp.repeat(group_mask, per_group, axis=1), sel, -jnp.inf)
    _, expert_idx = lax.top_k(sel, TOP_K)
    gate = jnp.take_along_axis(scores, expert_idx, axis=1)
    gate = (gate / jnp.sum(gate, axis=-1, keepdims=True) * ROUTED_SCALE).astype(u.dtype)

    n_assign = n_tok * TOP_K
    flat_e = expert_idx.reshape(-1)
    order = jnp.argsort(flat_e)
    sorted_e = flat_e[order]
    counts = jnp.bincount(flat_e, length=N_EXPERTS)
    padded = (counts + DISPATCH_BLOCK - 1) // DISPATCH_BLOCK * DISPATCH_BLOCK
    padded_end = jnp.cumsum(padded)
    rank = jnp.arange(n_assign) - (jnp.cumsum(counts) - counts)[sorted_e]
    dest = (padded_end - padded)[sorted_e] + rank
    n_blocks = -(-n_assign // DISPATCH_BLOCK) + N_EXPERTS
    slots = n_blocks * DISPATCH_BLOCK
    slot_tok = jnp.full((slots,), n_tok, jnp.int32).at[dest].set((order // TOP_K).astype(jnp.int32))
    slot_w = jnp.zeros((slots,), u.dtype).at[dest].set(gate.reshape(-1)[order])
    block_e = jnp.minimum(jnp.searchsorted(padded_end, jnp.arange(n_blocks) * DISPATCH_BLOCK, side='right'),
                          N_EXPERTS - 1)
    t_pad = jnp.concatenate([t, jnp.zeros((1, D), t.dtype)], axis=0)

    def expert_block(acc, blk):
        tok, wgt, e = blk
        xb = t_pad[tok]
        hb = jax.nn.silu(xb @ p['expert_w1'][e]) * (xb @ p['expert_w3'][e])
        return acc.at[tok].add((hb @ p['expert_w2'][e]) * wgt[:, None]), None

    acc, _ = lax.scan(expert_block, jnp.zeros((n_tok + 1, D), u.dtype),
                      (slot_tok.reshape(n_blocks, DISPATCH_BLOCK), slot_w.reshape(n_blocks, DISPATCH_BLOCK), block_e))
    shared = (jax.nn.silu(t @ p['shared_w1']) * (t @ p['shared_w3'])) @ p['shared_w2']
    return (acc[:n_tok] + shared).reshape(B, T, D)


def hybrid_layer(x, ctx, mod_lat, mod_ctx, p, update_ctx):
    sh_a, sc_a, g_a, sh_m, sc_m, g_m = jnp.split(mod_lat, 6, axis=-1)
    csh_a, csc_a, cg_a, csh_m, csc_m, cg_m = jnp.split(mod_ctx, 6, axis=-1)
    h_lat = modulate(rms_norm(x, p['norm_mix']), sh_a, sc_a)
    h_ctx = modulate(rms_norm(ctx, p['norm_mix']), csh_a, csc_a)
    mix_l, mix_c = token_mixers(h_lat, h_ctx, p, update_ctx)
    x = x + g_a * mix_l
    x = x + g_m * moe_ffn(modulate(rms_norm(x, p['norm_ffn']), sh_m, sc_m), p)
    if update_ctx:
        ctx = ctx + cg_a * mix_c
        ctx = ctx + cg_m * moe_ffn(modulate(rms_norm(ctx, p['norm_ffn']), csh_m, csc_m), p)
    return x, ctx


def setup_inputs(seed: int = 0) -> dict:
    key = jax.random.key(seed)
    ks = iter(jax.random.split(key, 40))
    nrm = lambda shape, scale: jax.random.normal(next(ks), shape, jnp.float32) * scale
    L, D, E, F = DEPTH, D_MODEL, N_EXPERTS, EXPERT_FF
    return {
        'x': nrm((BATCH, SEQ, D), 1.0),
        'c': nrm((BATCH, D), 1.0),
        'ctx': nrm((BATCH, CTX_LEN, D), 1.0),
        'c_ctx': nrm((D,), 1.0),
        'ada_w': nrm((L, D, 6 * D), 0.5 * D ** -0.5),
        'ada_b': nrm((L, 6 * D), 0.01),
        'norm_mix': 1.0 + nrm((L, D), 0.05),
        'norm_ffn': 1.0 + nrm((L, D), 0.05),
        'w_in': nrm((L, D, IN_WIDTH), D ** -0.5),
        'shift_conv': jnp.asarray([0.2, 0.6, 0.2], jnp.float32)[None, :, None] + nrm((L, SHIFT_WIDTH, RW_IN), 0.05),
        'q_lat_norm': 1.0 + nrm((L, Q_LORA), 0.05),
        'w_uq': nrm((L, Q_LORA, MLA_HEADS * QK_HEAD), Q_LORA ** -0.5),
        'kv_lat_norm': 1.0 + nrm((L, KV_LORA), 0.05),
        'w_ukv': nrm((L, KV_LORA, MLA_HEADS * (QK_NOPE + V_HEAD)), KV_LORA ** -0.5),
        'q_norm': 1.0 + nrm((L, QK_HEAD), 0.05),
        'k_norm': 1.0 + nrm((L, QK_HEAD), 0.05),
        'w_o_mla': nrm((L, MLA_HEADS * V_HEAD, D), (MLA_HEADS * V_HEAD) ** -0.5),
        'decay_w0': nrm((L, 2, RW_DIM), 0.5),
        'decay_w2': nrm((L, 2, DECAY_LORA, RW_DIM), 0.5 * DECAY_LORA ** -0.5),
        'aicl_a0': nrm((L, 2, RW_DIM), 0.5),
        'aicl_a2': nrm((L, 2, AICL_LORA, RW_DIM), 0.5 * AICL_LORA ** -0.5),
        'k_k': 0.85 + nrm((L, RW_DIM), 0.05),
        'k_a': 1.0 + nrm((L, RW_DIM), 0.05),
        'r_k': nrm((L, RW_HEADS, RW_HEAD), 0.1),
        'gn_w': 1.0 + nrm((L, RW_DIM), 0.05),
        'gn_b': nrm((L, RW_DIM), 0.01),
        'gate_g2': nrm((L, GATE_LORA, RW_DIM), GATE_LORA ** -0.5),
        'w_o_rwkv': nrm((L, RW_DIM, D), RW_DIM ** -0.5),
        'w_out': nrm((L, D, D), D ** -0.5),
        'router_w': nrm((L, D, E), D ** -0.5),
        'router_bias': nrm((L, E), 0.01),
        'expert_w1': nrm((L, E, D, F), D ** -0.5),
        'expert_w3': nrm((L, E, D, F), D ** -0.5),
        'expert_w2': nrm((L, E, F, D), F ** -0.5),
        'shared_w1': nrm((L, D, F), D ** -0.5),
        'shared_w3': nrm((L, D, F), D ** -0.5),
        'shared_w2': nrm((L, F, D), F ** -0.5),
    }


def reference(x, c, ctx, c_ctx, ada_w, ada_b, norm_mix, norm_ffn, w_in, shift_conv,
              q_lat_norm, w_uq, kv_lat_norm, w_ukv, q_norm, k_norm, w_o_mla,
              decay_w0, decay_w2, aicl_a0, aicl_a2, k_k, k_a, r_k, gn_w, gn_b, gate_g2, w_o_rwkv,
              w_out, router_w, router_bias, expert_w1, expert_w3, expert_w2,
              shared_w1, shared_w3, shared_w2):
    for i in range(DEPTH):
        p = dict(norm_mix=norm_mix[i], norm_ffn=norm_ffn[i], w_in=w_in[i], shift_conv=shift_conv[i],
                 q_lat_norm=q_lat_norm[i], w_uq=w_uq[i], kv_lat_norm=kv_lat_norm[i], w_ukv=w_ukv[i],
                 q_norm=q_norm[i], k_norm=k_norm[i], w_o_mla=w_o_mla[i],
                 decay_w0=decay_w0[i], decay_w2=decay_w2[i], aicl_a0=aicl_a0[i], aicl_a2=aicl_a2[i],
                 k_k=k_k[i], k_a=k_a[i], r_k=r_k[i], gn_w=gn_w[i], gn_b=gn_b[i], gate_g2=gate_g2[i],
                 w_o_rwkv=w_o_rwkv[i], w_out=w_out[i], router_w=router_w[i], router_bias=router_bias[i],
                 expert_w1=expert_w1[i], expert_w3=expert_w3[i], expert_w2=expert_w2[i],
                 shared_w1=shared_w1[i], shared_w3=shared_w3[i], shared_w2=shared_w2[i])
        mod_lat = (jax.nn.silu(c) @ ada_w[i] + ada_b[i])[:, None, :]
        mod_ctx = (jax.nn.silu(c_ctx) @ ada_w[i] + ada_b[i])[None, None, :]
        x, ctx = hybrid_layer(x, ctx, mod_lat, mod_ctx, p, i < DEPTH - 1)
    return x
```

```python
import numpy as np
import ml_dtypes
import concourse.bass as bass
import concourse.mybir as mybir
from concourse.bass_utils import run_bass_kernel_spmd
from contextlib import ExitStack, contextmanager

F32 = mybir.dt.float32
BF16 = mybir.dt.bfloat16
AF = mybir.ActivationFunctionType
ALU = mybir.AluOpType
AX = mybir.AxisListType

C_DEC = -0.6065306597126334
EPS = 1e-6
GN_EPS = 64e-5
ATTN_SCALE = 96 ** -0.5


NAMES = {}


class Buf:
    __slots__ = ("name", "w", "r", "excl")

    def __init__(self, name=""):
        self.name = name
        self.w = None
        self.r = {}
        self.excl = False


class Tile:
    def __init__(self, t, name):
        self.t = t
        self.b = Buf(name)

    def __getitem__(self, k):
        return self.t[k]


class Rot:
    def __init__(self, tiles):
        self.tiles = tiles
        self.i = 0

    def get(self):
        t = self.tiles[self.i % len(self.tiles)]
        self.i += 1
        return t


class KB:
    ENG = ("pe", "act", "dve", "pool", "sp")
    EPOCH = 8000
    NDMA = 40

    def __init__(self, nc, stack):
        self.nc = nc
        self.gstack = stack
        self.stack = stack
        self.e = {"pe": nc.tensor, "act": nc.scalar, "dve": nc.vector,
                  "pool": nc.gpsimd, "sp": nc.sync}
        self.cnt = {k: 0 for k in self.ENG}
        self.ep = {k: 0 for k in self.ENG}
        self.sems = {}
        self.waited = {}
        for k in self.ENG:
            self._newsem(k)
        self.dsem = []
        self.dslot = []
        self.dval = []
        for i in range(self.NDMA):
            self.dsem.append(stack.enter_context(nc.semaphore(f"dq{i}")))
            self.dslot.append(i)
            self.dval.append(0)
        self.dnext = 0
        self.dwaited = {}
        self.nins = 0
        self.uid = 0
        self.promised = False
        self.bregs = {}

    def sb(self, name, shape, dtype):
        self.uid += 1
        nm = f"{name}_{self.uid}"
        NAMES[name] = nm
        return Tile(self.stack.enter_context(self.nc.sbuf_tensor(nm, list(shape), dtype)), nm)

    def ps(self, name, shape, dtype=F32):
        t = Tile(self.gstack.enter_context(self.nc.psum_tensor(name, list(shape), dtype)), name)
        t.b.excl = True
        return t

    def dram(self, name, shape, dtype, kind="Internal"):
        return Tile(self.nc.dram_tensor(name, list(shape), dtype, kind=kind), name)

    def rot(self, name, shape, dtype, n=2):
        return Rot([self.sb(f"{name}{i}", shape, dtype) for i in range(n)])

    @contextmanager
    def scope(self):
        old = self.stack
        with ExitStack() as st:
            self.stack = st
            yield
            self.barrier()
        self.stack = old

    def _newsem(self, k):
        self.sems[(k, self.ep[k])] = self.gstack.enter_context(
            self.nc.semaphore(f"s_{k}_{self.ep[k]}"))

    def _wait(self, eng, tok):
        if tok is None:
            return
        if tok[0] == "e":
            _, k, ep, c = tok
            if eng == "pe" and k == "pe":
                return
            assert not (k == "pe" and ep == self.ep["pe"] and c > self.cnt["pe"]), "wait on a promised PE token"
            key = (eng, k, ep)
            if self.waited.get(key, 0) >= c:
                return
            self.e[eng].wait_ge(self.sems[(k, ep)], c)
            self.waited[key] = c
        else:
            _, i, v = tok
            key = (eng, i)
            if self.dwaited.get(key, 0) >= v:
                return
            self.e[eng].wait_ge(self.dsem[i], v)
            self.dwaited[key] = v

    @staticmethod
    def _bufs(xs):
        out = []
        for x in xs:
            if x is None:
                continue
            out.append(x.b if isinstance(x, Tile) else x)
        return out

    def _deps(self, eng, reads, writes):
        for b in reads:
            self._wait(eng, b.w)
        for b in writes:
            self._wait(eng, b.w)
            for t in b.r.values():
                self._wait(eng, t)

    def _commit(self, tok, reads, writes):
        for b in writes:
            b.w = tok
            b.r = {}
        for b in reads:
            if b not in writes:
                key = tok[1] if tok[0] == "e" else ("d", tok[1])
                b.r[key] = tok

    def op(self, eng, fn, reads=(), writes=(), inc=True):
        reads = self._bufs(reads)
        writes = self._bufs(writes)
        writes = writes + [b for b in reads if b.excl and b not in writes]
        self._deps(eng, reads, writes)
        if inc and self.cnt[eng] >= self.EPOCH and not (eng == "pe" and self.promised):
            self.ep[eng] += 1
            self.cnt[eng] = 0
            self._newsem(eng)
        ins = fn(self.e[eng])
        if eng == "pe":
            self.promised = not inc
        if inc:
            self.cnt[eng] += 1
            ins.then_inc(self.sems[(eng, self.ep[eng])], 1)
            tok = ("e", eng, self.ep[eng], self.cnt[eng])
        else:
            assert eng == "pe"
            tok = ("e", eng, self.ep[eng], self.cnt[eng] + 1)
        self._commit(tok, reads, writes)
        self.nins += 1

    def dma(self, eng, out, in_, reads=(), writes=(), **kw):
        reads = self._bufs(reads)
        writes = self._bufs(writes)
        self._deps(eng, reads, writes)
        s = self.dnext
        self.dnext = (self.dnext + 1) % self.NDMA
        i = self.dslot[s]
        if self.dval[i] >= 8000:
            self.dsem.append(self.gstack.enter_context(self.nc.semaphore(f"dq{len(self.dsem)}")))
            self.dval.append(0)
            prev = i
            i = len(self.dsem) - 1
            self.dslot[s] = i
            self._wait(eng, ("d", prev, self.dval[prev]))
        if self.dval[i] > 0:
            self._wait(eng, ("d", i, self.dval[i]))
        self.dval[i] += 16
        self.e[eng].dma_start(out=out, in_=in_, **kw).then_inc(self.dsem[i], 16)
        tok = ("d", i, self.dval[i])
        self._commit(tok, reads, writes)
        self.nins += 1

    def idma(self, out, in_, out_off=None, in_off=None, reads=(), writes=(), bounds=None):
        eng = "pool"
        reads = self._bufs(reads)
        writes = self._bufs(writes)
        self._deps(eng, reads, writes)
        s = self.dnext
        self.dnext = (self.dnext + 1) % self.NDMA
        i = self.dslot[s]
        if self.dval[i] >= 8000:
            self.dsem.append(self.gstack.enter_context(self.nc.semaphore(f"dq{len(self.dsem)}")))
            self.dval.append(0)
            prev = i
            i = len(self.dsem) - 1
            self.dslot[s] = i
            self._wait(eng, ("d", prev, self.dval[prev]))
        if self.dval[i] > 0:
            self._wait(eng, ("d", i, self.dval[i]))
        self.dval[i] += 16
        kw = {}
        if bounds is not None:
            if bounds not in self.bregs:
                r = self.nc.gpsimd.alloc_register(f"bnd{bounds}")
                self.nc.gpsimd.reg_mov(r, bounds)
                self.bregs[bounds] = r
            kw = dict(bounds_check=self.bregs[bounds], oob_is_err=False)
        self.e[eng].indirect_dma_start(
            out=out, out_offset=None if out_off is None else bass.IndirectOffsetOnAxis(ap=out_off, axis=0),
            in_=in_, in_offset=None if in_off is None else bass.IndirectOffsetOnAxis(ap=in_off, axis=0), **kw,
        ).then_inc(self.dsem[i], 16)
        tok = ("d", i, self.dval[i])
        self._commit(tok, reads, writes)
        self.nins += 1

    def finish(self):
        for i in range(len(self.dsem)):
            if self.dval[i] > 0:
                self._wait("sp", ("d", i, self.dval[i]))
        for k in self.ENG:
            if k != "sp" and (self.cnt[k] > 0 or self.ep[k] > 0):
                self._wait("sp", ("e", k, self.ep[k], self.cnt[k]))

    def barrier(self):
        self.finish()
        self.op("sp", lambda e: e.nop(), (), ())
        tok = ("e", "sp", self.ep["sp"], self.cnt["sp"])
        for k in ("pe", "act", "dve", "pool"):
            self._wait(k, tok)


def run_interleaved(gens, width):
    it = iter(gens)
    active = []
    exhausted = False
    while True:
        while len(active) < width and not exhausted:
            try:
                active.append(next(it))
            except StopIteration:
                exhausted = True
        if not active:
            break
        for g in list(active):
            try:
                next(g)
            except StopIteration:
                active.remove(g)


def host_consts():
    s = np.arange(128)[:, None]
    t = np.arange(128)[None, :]
    le = (s <= t).astype(np.float32)
    lt = (s < t).astype(np.float32)
    ge = (s >= t).astype(np.float32)
    gt = (s > t).astype(np.float32)
    tri = np.stack([le, lt, ge, gt], axis=1)
    mask4 = np.zeros((2, 128, 512), np.float32)
    maskt = np.zeros((2, 128, 128), np.float32)
    for d, (strict, incl) in enumerate(((lt, le), (gt, ge))):
        mask4[d, :, 0:128] = -strict
        mask4[d, :, 128:256] = -incl
        mask4[d, :, 256:384] = strict
        mask4[d, :, 384:512] = incl
        maskt[d] = -strict.T
    rows = 2048 // 64
    row = np.repeat(np.arange(rows, dtype=np.float32), 64)
    col = np.tile(np.arange(64, dtype=np.float32), rows)
    inv = (10000.0 ** (-np.arange(8, dtype=np.float32) / 8)).astype(np.float32)
    ang = np.concatenate([row[:, None] * inv, col[:, None] * inv], axis=-1).astype(np.float32)
    p_ = np.arange(128, dtype=np.float32)
    iotap = np.stack([p_, 256 * p_], axis=1)
    iotab = np.tile(np.arange(512, dtype=np.float32)[None, :], (128, 1))
    return dict(c_iotap=iotap, c_iotab=iotab, c_ident=np.eye(128, dtype=np.float32), c_tri=tri, c_mask4=mask4, c_maskt=maskt,
                c_cos=np.cos(ang).astype(np.float32), c_sin=np.sin(ang).astype(np.float32))


IN_SHAPES = dict(
    x=[2, 2048, 1024], ctx=[2, 256, 1024], cT=[1024, 3],
    ada_w=[1024, 6144], ada_b=[1, 6144], norm_mix=[1, 1024], norm_ffn=[1, 1024],
    w_in=[1024, 4416], shift_conv=[3, 1952], q_lat_norm=[1, 256], w_uq=[256, 768],
    kv_lat_norm=[1, 128], w_ukv=[128, 1024], q_norm=[1, 96], k_norm=[1, 96], w_o_mla=[512, 1024],
    decay_w0=[2, 512], decay_w2=[2, 64, 512], aicl_a0=[2, 512], aicl_a2=[2, 64, 512],
    k_k=[1, 512], k_a=[1, 512], r_k=[1, 512], gn_w=[1, 512], gn_b=[1, 512], gate_g2=[160, 512],
    w_o_rwkv=[512, 1024], w_out=[1024, 1024], router_w=[1024, 256], router_bias=[1, 256],
    expert_w1=[256, 1024, 256], expert_w3=[256, 1024, 256], expert_w2=[256, 256, 1024],
    shared_w1=[1024, 256], shared_w3=[1024, 256], shared_w2=[256, 1024],
    c_ident=[128, 128], c_tri=[128, 4, 128], c_mask4=[2, 128, 512], c_maskt=[2, 128, 128],
    c_cos=[2048, 16], c_sin=[2048, 16], c_iotap=[128, 2], c_iotab=[128, 512],
)

CT0 = 1
LT0 = 259
HW = 2308


def build(upto=99, dbg=(), n_exp=256, nbatch=2, rw_tiles=99, rw_phase=9, rw_dirs=2, blk_limit=10 ** 9):
    nc = bass.Bass("TRN2", target_bir_lowering=False)
    I = {k: nc.dram_tensor(k, list(v), F32, kind="ExternalInput") for k, v in IN_SHAPES.items()
         if not (upto < 6 and k.startswith(("expert_", "shared_")))}
    out_d = nc.dram_tensor("out", [2, 2048, 1024], F32, kind="ExternalOutput")

    with ExitStack() as gst:
        kb = KB(nc, gst)
        dbgk = lambda n: ("ExternalOutput" if n in dbg else "Internal")
        ps1 = Rot([kb.ps(f"ps1_{i}", [128, 512]) for i in range(4)])
        ps2 = Rot([kb.ps(f"ps2_{i}", [128, 1024]) for i in range(2)])

        mod_d = kb.dram("mod_d", [3, 6144], F32, dbgk("mod_d"))
        mla_d = kb.dram("mla_d", [2304, 416], F32, dbgk("mla_d"))
        mla_r = [Buf() for _ in range(18)]
        rkv_d = kb.dram("rkv_d", [2304, 1536], F32, dbgk("rkv_d"))
        rkv_r = [Buf() for _ in range(18)]
        gates_d = kb.dram("gates_d", [2048, 2048], BF16, dbgk("gates_d"))
        gates_r = [[Buf() for _ in range(4)] for _ in range(16)]
        x1_d = kb.dram("x1_d", [4096, 1024], F32, dbgk("x1_d"))
        x1_r = [Buf() for _ in range(32)]
        dbg_d = {}
        for n, shp, dt in (("fm_dbg", [128, 4, 2304], BF16), ("att_dbg", [128, 4, 2048], BF16),
                           ("y_dbg", [128, 16, 512], F32), ("rw_dbg", [128, 4, 2048], BF16),
                           ("G_dbg", [128, 16, 257], F32), ("qT_dbg", [96, 8, 2048], BF16),
                           ("kT_dbg", [96, 8, 2304], BF16), ("moe_dbg", [128, 16, 1024], F32)):
            if n in dbg:
                dbg_d[n] = kb.dram(n, shp, dt, "ExternalOutput")

        ident_f = kb.sb("ident_f", [128, 128], F32)
        ident_b = kb.sb("ident_b", [128, 128], BF16)
        ones_b = kb.sb("ones_b", [128, 128], BF16)
        kb.dma("sp", ident_f[:], I["c_ident"].ap(), writes=[ident_f])
        kb.op("dve", lambda e: e.tensor_copy(out=ident_b[:], in_=ident_f[:]), [ident_f], [ident_b])
        kb.op("pool", lambda e: e.memset(ones_b[:], 1.0), [], [ones_b])

        def bcast_load(dst_ap, dst_tile, src_ap, nparts, reads=()):
            kb.dma("sp", dst_ap, src_ap.to_broadcast([nparts, src_ap.shape[-1]]), reads=reads, writes=[dst_tile])

        def transposes(src_tile, src_aps, dst_ap, dst_tile, dt=BF16, eng="act", width=128):
            pp = ps1.get()
            pv = pp[:].bitcast(BF16) if dt == BF16 else pp[:]
            idn = ident_b if dt == BF16 else ident_f
            n = len(src_aps)
            P = src_aps[0].shape[0]
            w = src_aps[0].shape[1]
            for j, a in enumerate(src_aps):
                kb.op("pe", lambda e: e.transpose(out=pv[0:w, j * width:j * width + P], in_=a, identity=idn[0:P, 0:P]),
                      [src_tile, idn], [pp])
            src = pv[0:w, 0:n * width]
            if eng == "act":
                kb.op("act", lambda e: e.copy(out=dst_ap, in_=src.rearrange("p (j t) -> p j t", j=n) if len(dst_ap.shape) == 3 else src), [pp], [dst_tile])
            else:
                kb.op(eng, lambda e: e.tensor_copy(out=dst_ap, in_=src.rearrange("p (j t) -> p j t", j=n) if len(dst_ap.shape) == 3 else src), [pp], [dst_tile])

        def rms_rstd(ssq_ap, out_ap, tile_in, tile_out, n, eps):
            kb.op("act", lambda e: e.activation(out=out_ap, in_=ssq_ap, func=AF.Sqrt, bias=eps_t[:, 0:1] if eps == EPS else (gneps_t[:, 0:1] if eps == GN_EPS else tiny_t[:, 0:1]), scale=1.0 / n),
                  [tile_in], [tile_out])
            kb.op("dve", lambda e: e.reciprocal(out=out_ap, in_=out_ap), [tile_out], [tile_out])

        eps_t = kb.sb("eps_t", [128, 1], F32)
        gneps_t = kb.sb("gneps_t", [128, 1], F32)
        tiny_t = kb.sb("tiny_t", [128, 1], F32)
        kb.op("pool", lambda e: e.memset(eps_t[:], EPS), [], [eps_t])
        kb.op("pool", lambda e: e.memset(gneps_t[:], GN_EPS), [], [gneps_t])
        kb.op("pool", lambda e: e.memset(tiny_t[:], 1e-12), [], [tiny_t])

        with kb.scope():
            cTs = kb.sb("cTs", [128, 8, 3], F32)
            kb.dma("sp", cTs[:], I["cT"].ap().rearrange("(k p) r -> p k r", p=128), writes=[cTs])
            sT = kb.sb("sT", [128, 8, 3], F32)
            kb.op("act", lambda e: e.activation(out=sT[:], in_=cTs[:], func=AF.Silu), [cTs], [sT])
            modsb = kb.sb("modsb", [3, 6144], F32)
            adab = kb.sb("adab", [3, 6144], F32)
            bcast_load(adab[:], adab, I["ada_b"].ap()[0:1, :], 3)
            nrm = kb.sb("nrm", [3, 2, 1024], F32)
            bcast_load(nrm[:, 0, :], nrm, I["norm_mix"].ap()[0:1, :], 3)
            bcast_load(nrm[:, 1, :], nrm, I["norm_ffn"].ap()[0:1, :], 3)
            wrot = kb.rot("adaw", [128, 8, 512], F32, 2)
            for cb in range(12):
                wt = wrot.get()
                kb.dma("sp", wt[:], I["ada_w"].ap()[:, cb * 512:(cb + 1) * 512].rearrange("(k p) n -> p k n", p=128), writes=[wt])
                pp = ps1.get()
                for k in range(8):
                    kb.op("pe", lambda e: e.matmul(pp[0:3, :], lhsT=sT[:, k, :], rhs=wt[:, k, :], start=(k == 0), stop=(k == 7)),
                          [sT, wt], [pp], inc=(k == 7))
                kb.op("dve", lambda e: e.tensor_tensor(out=modsb[:, cb * 512:(cb + 1) * 512], in0=pp[0:3, :],
                                                       in1=adab[:, cb * 512:(cb + 1) * 512], op=ALU.add), [pp, adab], [modsb])
            for (c0, j) in ((1024, 0), (4096, 1)):
                kb.op("dve", lambda e: e.scalar_tensor_tensor(out=modsb[:, c0:c0 + 1024], in0=modsb[:, c0:c0 + 1024], scalar=1.0,
                                                              in1=nrm[:, j, :], op0=ALU.add, op1=ALU.mult), [modsb, nrm], [modsb])
            kb.dma("sp", mod_d[:, :], modsb[:], reads=[modsb], writes=[mod_d])
        if upto <= 0:
            kb.finish()
            return nc

        def norm_mod(xt, A, S, hb):
            junk = junk_rot.get()
            ssq = small_rot.get()
            kb.op("act", lambda e: e.activation(out=junk[:], in_=xt[:], func=AF.Square, accum_out=ssq[:, 0:1]), [xt], [junk, ssq])
            rms_rstd(ssq[:, 0:1], ssq[:, 1:2], ssq, ssq, 1024, EPS)
            kb.op("dve", lambda e: e.scalar_tensor_tensor(out=junk[:], in0=xt[:], scalar=ssq[:, 1:2], in1=A[:], op0=ALU.mult, op1=ALU.mult),
                  [xt, ssq, A], [junk])
            kb.op("pool", lambda e: e.tensor_tensor(out=hb[:], in0=junk[:], in1=S[:], op=ALU.add), [junk, S], [hb])

        for b in range(nbatch):
          with kb.scope():
            junk_rot = kb.rot("junk", [128, 1024], F32, 2)
            small_rot = kb.rot("small", [128, 16], F32, 6)
            mix_stack = ExitStack()
            fm_stack = ExitStack()
            _old = kb.stack
            kb.stack = mix_stack
            attT = kb.sb("attT", [128, 4, 2048], BF16)
            rwT = kb.sb("rwT", [128, 4, 2048], BF16)
            kb.stack = fm_stack
            fm = kb.sb("fm", [128, 4, 2304], BF16)
            kb.stack = _old

            with kb.scope():
                hT = kb.sb("hT", [128, 8, HW], BF16)
                for (c0, c1) in ((0, 1), (257, 259), (2307, 2308)):
                    kb.op("pool", lambda e: e.memset(hT[:, :, c0:c1], 0.0), [], [hT])
                with kb.scope():
                  xrot = kb.rot("xt", [128, 1024], F32, 2)
                  hbrot = kb.rot("hb", [128, 1024], BF16, 2)
                  for seg, (src, nt, off, row) in enumerate(((I["ctx"], 2, CT0, 2), (I["x"], 16, LT0, b))):
                    A1 = kb.sb("A1", [128, 1024], F32)
                    S1 = kb.sb("S1", [128, 1024], F32)
                    bcast_load(A1[:], A1, mod_d.t.ap()[row:row + 1, 1024:2048], 128, reads=[mod_d])
                    bcast_load(S1[:], S1, mod_d.t.ap()[row:row + 1, 0:1024], 128, reads=[mod_d])
                    for t in range(nt):
                        xt = xrot.get()
                        kb.dma("sp", xt[:], src.ap()[b, t * 128:(t + 1) * 128, :], writes=[xt])
                        hb = hbrot.get()
                        norm_mod(xt, A1, S1, hb)
                        transposes(hb, [hb[:, k * 128:(k + 1) * 128] for k in range(8)],
                                   hT[:, :, off + t * 128: off + (t + 1) * 128], hT)
                conv = kb.sb("conv", [128, 3, 1952], F32)
                for j in range(3):
                    bcast_load(conv[:, j, :], conv, I["shift_conv"].ap()[j:j + 1, :], 128)
                wf = kb.sb("wf", [128, 8, 512], F32)
                wbs = [kb.rot(f"wb{j}", [128, 8, 512], BF16, 2) for j in range(3)]
                osb = kb.rot("osb", [128, 512], F32, 2)
                gsb = kb.rot("gsb", [128, 512], BF16, 2)
                tok_tiles = [(CT0 + t * 128, t) for t in range(2)] + [(LT0 + t * 128, 2 + t) for t in range(16)]
                blocks = [(0, 416, "M")] + [(416 + i * 512, 512, "R") for i in range(3)] + [(1952, 416, "F")] + \
                         [(2368 + i * 512, 512, "G") for i in range(4)]
                for (c0, ncol, kind) in blocks:
                    kb.dma("sp", wf[:, :, 0:ncol], I["w_in"].ap()[:, c0:c0 + ncol].rearrange("(k p) n -> p k n", p=128), writes=[wf])
                    has_conv = kind in ("R", "F")
                    ws = []
                    for j in (range(3) if has_conv else range(1)):
                        wb = wbs[j].get()
                        if has_conv:
                            cc = c0 - 416
                            for k in range(8):
                                kb.op("pool" if k % 2 else "dve", lambda e: e.tensor_tensor(out=wb[:, k, 0:ncol], in0=wf[:, k, 0:ncol],
                                                                                           in1=conv[:, j, cc:cc + ncol], op=ALU.mult), [wf, conv], [wb])
                        else:
                            kb.op("pool", lambda e: e.tensor_copy(out=wb[:, :, 0:ncol], in_=wf[:, :, 0:ncol]), [wf], [wb])
                        ws.append(wb)
                    shifts = [(-1, 0), (0, 1), (1, 2)] if has_conv else [(0, 0)]
                    if kind in ("M", "R"):
                        for (off, ti) in tok_tiles:
                            pp = ps1.get()
                            n = len(shifts) * 8
                            i = 0
                            for (sh, j) in shifts:
                                for k in range(8):
                                    kb.op("pe", lambda e: e.matmul(pp[:, 0:ncol], lhsT=hT[:, k, off + sh:off + sh + 128], rhs=ws[j][:, k, 0:ncol],
                                                                   start=(i == 0), stop=(i == n - 1)), [hT, ws[j]], [pp], inc=(i == n - 1))
                                    i += 1
                            o = osb.get()
                            kb.op("act", lambda e: e.copy(out=o[:, 0:ncol], in_=pp[:, 0:ncol]), [pp], [o])
                            if kind == "M":
                                kb.dma("sp", mla_d[ti * 128:(ti + 1) * 128, :], o[:, 0:416], reads=[o], writes=[mla_r[ti]])
                            else:
                                cc = c0 - 416
                                kb.dma("sp", rkv_d[ti * 128:(ti + 1) * 128, cc:cc + 512], o[:, :], reads=[o], writes=[rkv_r[ti]])
                    elif kind == "F":
                        nblks = [(CT0, 0, 256)] + [(LT0 + i * 512, 256 + i * 512, 512) for i in range(4)]
                        for m, (m0, msz) in enumerate(((0, 128), (128, 128), (256, 128), (384, 32))):
                            func = (AF.Tanh, AF.Copy, AF.Sigmoid, AF.Sigmoid)[m]
                            for (off, dcol, nn) in nblks:
                                pp = ps1.get()
                                i = 0
                                for (sh, j) in shifts:
                                    for k in range(8):
                                        kb.op("pe", lambda e: e.matmul(pp[0:msz, 0:nn], lhsT=ws[j][:, k, m0:m0 + msz], rhs=hT[:, k, off + sh:off + sh + nn],
                                                                       start=(i == 0), stop=(i == 23)), [hT, ws[j]], [pp], inc=(i == 23))
                                        i += 1
                                kb.op("act", lambda e: e.activation(out=fm[0:msz, m, dcol:dcol + nn], in_=pp[0:msz, 0:nn], func=func), [pp], [fm])
                    else:
                        g0 = c0 - 2368
                        for m in range(4):
                            mt = g0 // 128 + m
                            for nb_ in range(4):
                                pp = ps1.get()
                                for k in range(8):
                                    kb.op("pe", lambda e: e.matmul(pp[:, :], lhsT=ws[0][:, k, m * 128:(m + 1) * 128],
                                                                   rhs=hT[:, k, LT0 + nb_ * 512:LT0 + (nb_ + 1) * 512], start=(k == 0), stop=(k == 7)),
                                          [hT, ws[0]], [pp], inc=(k == 7))
                                g = gsb.get()
                                kb.op("act", lambda e: e.activation(out=g[:], in_=pp[:, :], func=AF.Sigmoid), [pp], [g])
                                kb.dma("sp", gates_d[mt * 128:(mt + 1) * 128, nb_ * 512:(nb_ + 1) * 512], g[:], reads=[g], writes=[gates_r[mt][nb_]])
                if "fm_dbg" in dbg and b == 0:
                    kb.dma("sp", dbg_d["fm_dbg"][:, :, :], fm[:], reads=[fm], writes=[dbg_d["fm_dbg"]])
            if upto <= 2:
                kb.barrier(); fm_stack.close(); mix_stack.close()
                continue

            with kb.scope():
                Ybuf = kb.sb("Ybuf", [128, 16, 512], F32)
                bonus = kb.sb("bonus", [128, 16, 2, 8], F32)
                tri_f = kb.sb("tri_f", [128, 4, 128], F32)
                tri = kb.sb("tri", [128, 4, 128], BF16)
                kb.dma("sp", tri_f[:], I["c_tri"].ap(), writes=[tri_f])
                kb.op("dve", lambda e: e.tensor_copy(out=tri[:], in_=tri_f[:]), [tri_f], [tri])
                mask4 = kb.sb("mask4", [128, 2, 512], F32)
                maskt = kb.sb("maskt", [128, 2, 128], F32)
                for d in range(2):
                    kb.dma("sp", mask4[:, d, :], I["c_mask4"].ap()[d], writes=[mask4])
                    kb.dma("sp", maskt[:, d, :], I["c_maskt"].ap()[d], writes=[maskt])
                prm = kb.sb("prm", [128, 3, 512], F32)
                bcast_load(prm[:, 0, :], prm, I["k_k"].ap()[0:1, :], 128)
                bcast_load(prm[:, 1, :], prm, I["k_a"].ap()[0:1, :], 128)
                bcast_load(prm[:, 2, :], prm, I["r_k"].ap()[0:1, :], 128)
                w2f = kb.sb("w2f", [128, 2, 512], F32)
                w2b = kb.sb("w2b", [128, 2, 512], BF16)
                kb.dma("sp", w2f[:, 0, :], I["decay_w2"].ap().rearrange("d l c -> (d l) c"), writes=[w2f])
                kb.dma("sp", w2f[:, 1, :], I["aicl_a2"].ap().rearrange("d l c -> (d l) c"), writes=[w2f])
                kb.op("dve", lambda e: e.tensor_copy(out=w2b[:], in_=w2f[:]), [w2f], [w2b])
                b0f = kb.sb("b0f", [128, 2, 512], F32)
                b0b = kb.sb("b0b", [128, 2, 512], BF16)
                kb.op("pool", lambda e: e.memset(b0f[:], 0.0), [], [b0f])
                for d_ in range(2):
                    kb.dma("sp", b0f[d_ * 64:d_ * 64 + 1, 0, :], I["decay_w0"].ap()[d_:d_ + 1, :], writes=[b0f])
                    kb.dma("sp", b0f[d_ * 64:d_ * 64 + 1, 1, :], I["aicl_a0"].ap()[d_:d_ + 1, :], writes=[b0f])
                kb.op("dve", lambda e: e.tensor_copy(out=b0b[:], in_=b0f[:]), [b0f], [b0b])
                Hf = [kb.sb(f"Hf{p}", [128, 64], F32) for p in range(4)]
                Hb = [kb.sb(f"Hb{p}", [128, 64], BF16) for p in range(4)]
                rkv_rot = kb.rot("rkvt", [128, 1536], F32, 2)
                f512 = kb.rot("f512", [128, 512], F32, 10)
                b512 = kb.rot("b512", [128, 512], BF16, 14)
                fmT = kb.rot("fmT", [128, 4, 4, 128], BF16, 2)
                A4rot = kb.rot("A4", [128, 512], BF16, 8)
                XPa = kb.rot("XPa", [128, 256], BF16, 8)
                Xta = kb.rot("Xta", [128, 128], BF16, 8)
                Wsb = kb.rot("Wsb", [128, 64], BF16, 8)
                Usb = kb.rot("Usb", [128, 512], BF16, 2)
                Vb = kb.rot("Vb", [128, 512], BF16, 2)
                ptot_rot = kb.rot("ptot", [128, 4], F32, 2)

                for d in range(rw_dirs):
                    for p in range(4):
                        kb.op("pool", lambda e: e.memset(Hf[p][:], 0.0), [], [Hf[p]])
                        kb.op("pool", lambda e: e.memset(Hb[p][:], 0.0), [], [Hb[p]])
                    order = list(range(18)) if d == 0 else [1, 0] + list(range(17, 1, -1))
                    for ti in order[:rw_tiles]:
                        lat = ti >= 2
                        rt = rkv_rot.get()
                        kb.dma("sp", rt[:], rkv_d[ti * 128:(ti + 1) * 128, :], reads=[rkv_r[ti]], writes=[rt])
                        r_ = rt[:, 0:512]
                        k_ = rt[:, 512:1024]
                        v_ = rt[:, 1024:1536]
                        kkr = f512.get(); sq = f512.get(); st = small_rot.get()
                        kb.op("dve", lambda e: e.tensor_tensor(out=kkr[:], in0=k_, in1=prm[:, 0, :], op=ALU.mult), [rt, prm], [kkr])
                        kb.op("act", lambda e: e.activation(out=sq[:], in_=kkr[:], func=AF.Square), [kkr], [sq])
                        kb.op("dve", lambda e: e.tensor_reduce(out=st[:, 0:8], in_=sq[:].rearrange("p (h c) -> p h c", h=8), axis=AX.X, op=ALU.add), [sq], [st])
                        rms_rstd(st[:, 0:8], st[:, 8:16], st, st, 1.0, 1e-12)
                        kap = f512.get()
                        kb.op("dve", lambda e: e.tensor_tensor(out=kap[:].rearrange("p (h c) -> p h c", h=8), in0=kkr[:].rearrange("p (h c) -> p h c", h=8),
                                                               in1=st[:, 8:16].unsqueeze(2).to_broadcast([128, 8, 64]), op=ALU.mult), [kkr, st], [kap])
                        tcol = (ti * 128) if ti < 2 else (256 + (ti - 2) * 128)
                        pz = ps1.get(); pa = ps1.get()
                        kb.op("pe", lambda e: e.matmul(pz[:, :], lhsT=fm[d * 64:(d + 1) * 64, 0, tcol:tcol + 128], rhs=w2b[d * 64:(d + 1) * 64, 0, :], start=True, stop=False),
                              [fm, w2b], [pz], inc=False)
                        kb.op("pe", lambda e: e.matmul(pz[:, :], lhsT=ones_b[d * 64:d * 64 + 1, 0:128], rhs=b0b[d * 64:d * 64 + 1, 0, :], start=False, stop=True), [ones_b, b0b], [pz])
                        kb.op("pe", lambda e: e.matmul(pa[:, :], lhsT=fm[d * 64:(d + 1) * 64, 1, tcol:tcol + 128], rhs=w2b[d * 64:(d + 1) * 64, 1, :], start=True, stop=False),
                              [fm, w2b], [pa], inc=False)
                        kb.op("pe", lambda e: e.matmul(pa[:, :], lhsT=ones_b[d * 64:d * 64 + 1, 0:128], rhs=b0b[d * 64:d * 64 + 1, 1, :], start=False, stop=True), [ones_b, b0b], [pa])
                        sigb = b512.get()
                        kb.op("act", lambda e: e.activation(out=sigb[:], in_=pz[:, :], func=AF.Sigmoid), [pz], [sigb])
                        a_ = f512.get()
                        kb.op("act", lambda e: e.activation(out=a_[:], in_=pa[:, :], func=AF.Sigmoid), [pa], [a_])
                        t1 = f512.get(); ktl = f512.get(); beta = f512.get()
                        kb.op("dve", lambda e: e.scalar_tensor_tensor(out=t1[:], in0=a_[:], scalar=-1.0, in1=prm[:, 1, :], op0=ALU.add, op1=ALU.mult), [a_, prm], [t1])
                        kb.op("dve", lambda e: e.scalar_tensor_tensor(out=ktl[:], in0=t1[:], scalar=1.0, in1=k_, op0=ALU.add, op1=ALU.mult), [t1, rt], [ktl])
                        kb.op("pool", lambda e: e.tensor_tensor(out=beta[:], in0=a_[:], in1=kap[:], op=ALU.mult), [a_, kap], [beta])
                        if lat:
                            kb.op("pool", lambda e: e.tensor_tensor(out=t1[:], in0=r_, in1=prm[:, 2, :], op=ALU.mult), [rt, prm], [t1])
                            kb.op("pool", lambda e: e.tensor_tensor(out=t1[:], in0=t1[:], in1=ktl[:], op=ALU.mult), [t1, ktl], [t1])
                            kb.op("dve", lambda e: e.tensor_reduce(out=bonus[:, ti - 2, d, :], in_=t1[:].rearrange("p (h c) -> p h c", h=8), axis=AX.X, op=ALU.add), [t1], [bonus])
                        if rw_phase < 1:
                            continue
                        ci, ce, cr = ((0, 1, 3) if d == 0 else (2, 3, 1))
                        pI = ps1.get(); pE = ps1.get(); pR = ps1.get()
                        kb.op("pe", lambda e: e.matmul(pI[:, :], lhsT=tri[:, ci, :], rhs=sigb[:], start=True, stop=True), [tri, sigb], [pI])
                        kb.op("pe", lambda e: e.matmul(pE[:, :], lhsT=tri[:, ce, :], rhs=sigb[:], start=True, stop=True), [tri, sigb], [pE])
                        kb.op("pe", lambda e: e.matmul(pR[:, :], lhsT=tri[:, cr, :], rhs=sigb[:], start=True, stop=True), [tri, sigb], [pR])
                        eI = f512.get(); eE = f512.get(); eN = f512.get(); eR = f512.get()
                        kb.op("act", lambda e: e.activation(out=eI[:], in_=pI[:, :], func=AF.Exp, scale=C_DEC), [pI], [eI])
                        kb.op("act", lambda e: e.activation(out=eN[:], in_=pI[:, :], func=AF.Exp, scale=-C_DEC), [pI], [eN])
                        kb.op("act", lambda e: e.activation(out=eE[:], in_=pE[:, :], func=AF.Exp, scale=C_DEC), [pE], [eE])
                        kb.op("act", lambda e: e.activation(out=eR[:], in_=pR[:, :], func=AF.Exp, scale=C_DEC), [pR], [eR])
                        pT = ps1.get()
                        for p in range(4):
                            kb.op("pe", lambda e: e.matmul(pT[:, p:p + 1], lhsT=sigb[:, p * 128:(p + 1) * 128], rhs=ones_b[:, 0:1], start=True, stop=True), [sigb, ones_b], [pT])
                        ptot = ptot_rot.get()
                        kb.op("act", lambda e: e.activation(out=ptot[:], in_=pT[:, 0:4], func=AF.Exp, scale=C_DEC), [pT], [ptot])
                        Rh = b512.get(); Ka = b512.get(); Bh = b512.get(); Kh = b512.get(); NBb = b512.get(); Kbb = b512.get()
                        kb.op("dve", lambda e: e.tensor_tensor(out=Rh[:], in0=r_, in1=eI[:], op=ALU.mult), [rt, eI], [Rh])
                        kb.op("pool", lambda e: e.tensor_tensor(out=Ka[:], in0=kap[:], in1=eE[:], op=ALU.mult), [kap, eE], [Ka])
                        kb.op("dve", lambda e: e.tensor_tensor(out=Bh[:], in0=beta[:], in1=eN[:], op=ALU.mult), [beta, eN], [Bh])
                        kb.op("pool", lambda e: e.tensor_tensor(out=Kh[:], in0=ktl[:], in1=eN[:], op=ALU.mult), [ktl, eN], [Kh])
                        kb.op("dve", lambda e: e.scalar_tensor_tensor(out=NBb[:], in0=beta[:], scalar=-1.0, in1=eR[:], op0=ALU.mult, op1=ALU.mult), [beta, eR], [NBb])
                        kb.op("pool", lambda e: e.tensor_tensor(out=Kbb[:], in0=ktl[:], in1=eR[:], op=ALU.mult), [ktl, eR], [Kbb])
                        vb = Vb.get()
                        kb.op("pool", lambda e: e.tensor_copy(out=vb[:], in_=v_), [rt], [vb])
                        if rw_phase < 2:
                            continue
                        fT = fmT.get()
                        for wi, src in enumerate((Ka, Rh, Bh, Kh)):
                            transposes(src, [src[:, p * 128:(p + 1) * 128] for p in range(4)], fT[:, :, wi, :], fT, eng=("act" if wi % 2 else "dve"))
                        if rw_phase < 3:
                            continue
                        U = Usb.get()
                        pY = ps2.get() if lat else None
                        HG = 4
                        for g0 in range(0, 8, HG):
                            hs = list(range(g0, g0 + HG))
                            P_ = {h: h // 2 for h in hs}
                            LO = {h: (h % 2) * 64 for h in hs}
                            HC = {h: slice(h * 64, (h + 1) * 64) for h in hs}
                            KaT = {h: fT[LO[h]:LO[h] + 64, P_[h], 0, :] for h in hs}
                            RT = {h: fT[LO[h]:LO[h] + 64, P_[h], 1, :] for h in hs}
                            BT = {h: fT[LO[h]:LO[h] + 64, P_[h], 2, :] for h in hs}
                            KT = {h: fT[LO[h]:LO[h] + 64, P_[h], 3, :] for h in hs}
                            KaRT = {h: fT[LO[h]:LO[h] + 64, P_[h], 0:2, :].rearrange("c w t -> c (w t)") for h in hs}
                            A4 = {}; Xt = {}; XP = {}; pp_ = {}
                            for h in hs:
                                pA = ps1.get(); pp_[h] = pA
                                kb.op("pe", lambda e: e.matmul(pA[:, 0:256], lhsT=BT[h], rhs=KaRT[h], start=True, stop=True), [fT], [pA], inc=False)
                                kb.op("pe", lambda e: e.matmul(pA[:, 256:512], lhsT=KT[h], rhs=KaRT[h], start=True, stop=True), [fT], [pA])
                            for h in hs:
                                A4[h] = A4rot.get()
                                kb.op("dve", lambda e: e.tensor_tensor(out=A4[h][:], in0=pp_[h][:, :], in1=mask4[:, d, :], op=ALU.mult), [pp_[h], mask4], [A4[h]])
                            for h in hs:
                                pL = ps1.get(); pp_[h] = pL
                                kb.op("pe", lambda e: e.matmul(pL[:, 0:128], lhsT=KaT[h], rhs=BT[h], start=True, stop=True), [fT], [pL])
                            for h in hs:
                                Xt[h] = Xta.get()
                                kb.op("dve", lambda e: e.tensor_tensor(out=Xt[h][:], in0=pp_[h][:, 0:128], in1=maskt[:, d, :], op=ALU.mult), [pp_[h], maskt], [Xt[h]])
                            for h in hs:
                                XP[h] = XPa.get()
                                kb.op("pool", lambda e: e.tensor_copy(out=XP[h][:, 0:128], in_=A4[h][:, 0:128]), [A4[h]], [XP[h]])
                                kb.op("pool", lambda e: e.tensor_tensor(out=XP[h][:, 128:256], in0=A4[h][:, 0:128], in1=ident_b[:], op=ALU.add), [A4[h], ident_b], [XP[h]])
                            for h in hs:
                                pB = ps1.get(); pp_[h] = pB
                                kb.op("pe", lambda e: e.matmul(pB[:, 0:128], lhsT=Xt[h][:], rhs=XP[h][:, 0:128], start=True, stop=True), [Xt[h], XP[h]], [pB], inc=False)
                                kb.op("pe", lambda e: e.matmul(pB[:, 256:384], lhsT=XP[h][:, 0:128], rhs=Xt[h][:], start=True, stop=True), [Xt[h], XP[h]], [pB])
                            for h in hs:
                                XP2 = XPa.get(); Xt2 = Xta.get(); pB = pp_[h]
                                kb.op("act", lambda e: e.copy(out=XP2[:, 0:128], in_=pB[:, 0:128]), [pB], [XP2])
                                kb.op("dve", lambda e: e.tensor_copy(out=Xt2[:], in_=pB[:, 256:384]), [pB], [Xt2])
                                kb.op("pool", lambda e: e.tensor_copy(out=XP2[:, 128:256], in_=XP[h][:, 128:256]), [XP[h]], [XP2])
                                XP[h], Xt[h] = XP2, Xt2
                            for j in range(1, 7):
                                last = (j == 6)
                                for h in hs:
                                    pB = ps1.get(); pp_[h] = pB
                                    if not last:
                                        kb.op("pe", lambda e: e.matmul(pB[:, 0:256], lhsT=Xt[h][:], rhs=XP[h][:, 0:256], start=True, stop=True), [Xt[h], XP[h]], [pB], inc=False)
                                        kb.op("pe", lambda e: e.matmul(pB[:, 256:384], lhsT=XP[h][:, 0:128], rhs=Xt[h][:], start=True, stop=True), [Xt[h], XP[h]], [pB])
                                    else:
                                        kb.op("pe", lambda e: e.matmul(pB[:, 128:256], lhsT=Xt[h][:], rhs=XP[h][:, 128:256], start=True, stop=True), [Xt[h], XP[h]], [pB])
                                for h in hs:
                                    pB = pp_[h]
                                    XP2 = XPa.get()
                                    kb.op("dve", lambda e: e.tensor_tensor(out=XP2[:, 128:256], in0=pB[:, 128:256], in1=XP[h][:, 128:256], op=ALU.add), [pB, XP[h]], [XP2])
                                    if not last:
                                        Xt2 = Xta.get()
                                        kb.op("act", lambda e: e.copy(out=XP2[:, 0:128], in_=pB[:, 0:128]), [pB], [XP2])
                                        kb.op("act", lambda e: e.copy(out=Xt2[:], in_=pB[:, 256:384]), [pB], [Xt2])
                                        Xt[h] = Xt2
                                    XP[h] = XP2
                            Wt = {}
                            for h in hs:
                                pW = ps1.get(); pp_[h] = pW
                                kb.op("pe", lambda e: e.matmul(pW[:, 0:64], lhsT=KaT[h], rhs=Hb[P_[h]][LO[h]:LO[h] + 64, :], start=True, stop=False), [fT, Hb[P_[h]]], [pW], inc=False)
                                kb.op("pe", lambda e: e.matmul(pW[:, 0:64], lhsT=A4[h][:, 256:384], rhs=vb[:, HC[h]], start=False, stop=True), [A4[h], vb], [pW])
                            for h in hs:
                                Wt[h] = Wsb.get()
                                kb.op("act", lambda e: e.copy(out=Wt[h][:], in_=pp_[h][:, 0:64]), [pp_[h]], [Wt[h]])
                            for h in hs:
                                pU = ps1.get(); pp_[h] = pU
                                kb.op("pe", lambda e: e.matmul(pU[:, 0:64], lhsT=XP[h][:, 128:256], rhs=Wt[h][:], start=True, stop=True), [XP[h], Wt[h]], [pU])
                            for h in hs:
                                kb.op("dve" if h % 2 else "act", (lambda e: e.tensor_copy(out=U[:, HC[h]], in_=pp_[h][:, 0:64])) if h % 2 else
                                      (lambda e: e.copy(out=U[:, HC[h]], in_=pp_[h][:, 0:64])), [pp_[h]], [U])
                            if lat:
                                for h in hs:
                                    kb.op("pe", lambda e: e.matmul(pY[:, HC[h]], lhsT=RT[h], rhs=Hb[P_[h]][LO[h]:LO[h] + 64, :], start=True, stop=False), [fT, Hb[P_[h]]], [pY], inc=False)
                                    kb.op("pe", lambda e: e.matmul(pY[:, HC[h]], lhsT=A4[h][:, 128:256], rhs=U[:, HC[h]], start=False, stop=False), [A4[h], U], [pY], inc=False)
                                    kb.op("pe", lambda e: e.matmul(pY[:, HC[h]], lhsT=A4[h][:, 384:512], rhs=vb[:, HC[h]], start=False, stop=True), [A4[h], vb], [pY])
                            pHs = {}
                            for p in sorted(set(P_.values())):
                                pc = slice(p * 128, (p + 1) * 128)
                                pH = ps1.get(); pHs[p] = pH
                                kb.op("pe", lambda e: e.matmul(pH[:, 0:128], lhsT=NBb[:, pc], rhs=U[:, pc], start=True, stop=False), [NBb, U], [pH], inc=False)
                                kb.op("pe", lambda e: e.matmul(pH[:, 0:128], lhsT=Kbb[:, pc], rhs=vb[:, pc], start=False, stop=True), [Kbb, vb], [pH])
                            for p in sorted(set(P_.values())):
                                pH = pHs[p]
                                for hh in range(2):
                                    l2 = hh * 64
                                    kb.op("dve", lambda e: e.scalar_tensor_tensor(out=Hf[p][l2:l2 + 64, :], in0=Hf[p][l2:l2 + 64, :], scalar=ptot[l2:l2 + 64, p:p + 1],
                                                                                  in1=pH[l2:l2 + 64, l2:l2 + 64], op0=ALU.mult, op1=ALU.add), [Hf[p], ptot, pH], [Hf[p]])
                                kb.op("act", lambda e: e.copy(out=Hb[p][:], in_=Hf[p][:]), [Hf[p]], [Hb[p]])
                        if lat:
                            if d == 0:
                                kb.op("act", lambda e: e.copy(out=Ybuf[:, ti - 2, :], in_=pY[:, 0:512]), [pY], [Ybuf])
                            else:
                                kb.op("dve", lambda e: e.tensor_tensor(out=Ybuf[:, ti - 2, :], in0=pY[:, 0:512], in1=Ybuf[:, ti - 2, :], op=ALU.add), [pY, Ybuf], [Ybuf])
                if "y_dbg" in dbg and b == 0:
                    kb.dma("sp", dbg_d["y_dbg"][:, :, :], Ybuf[:], reads=[Ybuf], writes=[dbg_d["y_dbg"]])
                gnw = kb.sb("gnw", [128, 2, 512], F32)
                bcast_load(gnw[:, 0, :], gnw, I["gn_w"].ap()[0:1, :], 128)
                bcast_load(gnw[:, 1, :], gnw, I["gn_b"].ap()[0:1, :], 128)
                g2f = kb.sb("g2f", [128, 2, 512], F32)
                g2b = kb.sb("g2b", [128, 2, 512], BF16)
                kb.dma("sp", g2f[:, 0, :], I["gate_g2"].ap()[0:128, :], writes=[g2f])
                kb.dma("sp", g2f[0:32, 1, :], I["gate_g2"].ap()[128:160, :], writes=[g2f])
                kb.op("dve", lambda e: e.tensor_copy(out=g2b[:, 0, :], in_=g2f[:, 0, :]), [g2f], [g2b])
                kb.op("dve", lambda e: e.tensor_copy(out=g2b[0:32, 1, :], in_=g2f[0:32, 1, :]), [g2f], [g2b])
                rwb = kb.rot("rwb", [128, 512], BF16, 2)
                def tileRO(t):
                        ti = t + 2
                        rt = rkv_rot.get()
                        kb.dma("sp", rt[:], rkv_d[ti * 128:(ti + 1) * 128, :], reads=[rkv_r[ti]], writes=[rt])
                        v_ = rt[:, 1024:1536]
                        y3 = Ybuf[:, t, :].rearrange("p (h c) -> p h c", h=8)
                        st = small_rot.get(); st2 = small_rot.get()
                        cen = f512.get(); sq = f512.get(); yn = f512.get()
                        cen3 = cen[:].rearrange("p (h c) -> p h c", h=8)
                        kb.op("dve", lambda e: e.tensor_reduce(out=st[:, 0:8], in_=y3, axis=AX.X, op=ALU.add), [Ybuf], [st])
                        kb.op("dve", lambda e: e.tensor_scalar(out=st[:, 0:8], in0=st[:, 0:8], scalar1=-1.0 / 64, scalar2=None, op0=ALU.mult), [st], [st])
                        kb.op("dve", lambda e: e.tensor_tensor(out=cen3, in0=y3, in1=st[:, 0:8].unsqueeze(2).to_broadcast([128, 8, 64]), op=ALU.add), [Ybuf, st], [cen])
                        yield
                        kb.op("act", lambda e: e.activation(out=sq[:], in_=cen[:], func=AF.Square), [cen], [sq])
                        kb.op("dve", lambda e: e.tensor_reduce(out=st2[:, 0:8], in_=sq[:].rearrange("p (h c) -> p h c", h=8), axis=AX.X, op=ALU.add), [sq], [st2])
                        yield
                        rms_rstd(st2[:, 0:8], st2[:, 8:16], st2, st2, 64, GN_EPS)
                        kb.op("dve", lambda e: e.tensor_tensor(out=yn[:].rearrange("p (h c) -> p h c", h=8), in0=cen3, in1=st2[:, 8:16].unsqueeze(2).to_broadcast([128, 8, 64]), op=ALU.mult),
                              [cen, st2], [yn])
                        kb.op("pool", lambda e: e.tensor_tensor(out=yn[:], in0=yn[:], in1=gnw[:, 0, :], op=ALU.mult), [yn, gnw], [yn])
                        kb.op("pool", lambda e: e.tensor_tensor(out=yn[:], in0=yn[:], in1=gnw[:, 1, :], op=ALU.add), [yn, gnw], [yn])
                        yield
                        kb.op("dve", lambda e: e.tensor_tensor(out=st[:, 8:16], in0=bonus[:, t, 0, :], in1=bonus[:, t, 1, :], op=ALU.add), [bonus], [st])
                        kb.op("dve", lambda e: e.tensor_tensor(out=sq[:].rearrange("p (h c) -> p h c", h=8), in0=v_.rearrange("p (h c) -> p h c", h=8),
                                                               in1=st[:, 8:16].unsqueeze(2).to_broadcast([128, 8, 64]), op=ALU.mult), [rt, st], [sq])
                        kb.op("pool", lambda e: e.tensor_tensor(out=yn[:], in0=yn[:], in1=sq[:], op=ALU.add), [yn, sq], [yn])
                        yield
                        pg = ps1.get()
                        tcol = 256 + t * 128
                        kb.op("pe", lambda e: e.matmul(pg[:, :], lhsT=fm[:, 2, tcol:tcol + 128], rhs=g2b[:, 0, :], start=True, stop=False), [fm, g2b], [pg], inc=False)
                        kb.op("pe", lambda e: e.matmul(pg[:, :], lhsT=fm[0:32, 3, tcol:tcol + 128], rhs=g2b[0:32, 1, :], start=False, stop=True), [fm, g2b], [pg])
                        ro = rwb.get()
                        kb.op("dve", lambda e: e.tensor_tensor(out=ro[:], in0=pg[:, :], in1=yn[:], op=ALU.mult), [pg, yn], [ro])
                        transposes(ro, [ro[:, k * 128:(k + 1) * 128] for k in range(4)], rwT[:, :, t * 128:(t + 1) * 128], rwT)
                run_interleaved((tileRO(t) for t in range(16)), 2)
                if "rw_dbg" in dbg and b == 0:
                    kb.dma("sp", dbg_d["rw_dbg"][:, :, :], rwT[:], reads=[rwT], writes=[dbg_d["rw_dbg"]])
            kb.barrier()
            fm_stack.close()
            if upto <= 3:
                mix_stack.close()
                continue

            with kb.scope():
                qT = kb.sb("qT", [96, 8, 2048], BF16)
                kT = kb.sb("kT", [96, 8, 2304], BF16)
                Vall = kb.sb("Vall", [128, 18, 8, 65], BF16)
                kb.op("pool", lambda e: e.memset(Vall[:, :, :, 64:65], 1.0), [], [Vall])
                qln = kb.sb("qln", [128, 256], F32)
                kvln = kb.sb("kvln", [128, 128], F32)
                qnw = kb.sb("qnw", [128, 8, 96], F32)
                knw = kb.sb("knw", [128, 8, 96], F32)
                bcast_load(qln[:], qln, I["q_lat_norm"].ap()[0:1, :], 128)
                bcast_load(kvln[:], kvln, I["kv_lat_norm"].ap()[0:1, :], 128)
                for h in range(8):
                    bcast_load(qnw[:, h, :], qnw, I["q_norm"].ap()[0:1, :], 128)
                    bcast_load(knw[:, h, :], knw, I["k_norm"].ap()[0:1, :], 128)
                kb.op("dve", lambda e: e.tensor_scalar(out=qnw[:], in0=qnw[:], scalar1=ATTN_SCALE, scalar2=None, op0=ALU.mult), [qnw], [qnw])
                wuq_f = kb.sb("wuq_f", [128, 2, 768], F32)
                wuq = kb.sb("wuq", [128, 2, 768], BF16)
                kb.dma("sp", wuq_f[:], I["w_uq"].ap().rearrange("(k p) n -> p k n", p=128), writes=[wuq_f])
                kb.op("pool", lambda e: e.tensor_copy(out=wuq[:], in_=wuq_f[:]), [wuq_f], [wuq])
                wukv_f = kb.sb("wukv_f", [128, 1024], F32)
                wukv = kb.sb("wukv", [128, 1024], BF16)
                kb.dma("sp", wukv_f[:], I["w_ukv"].ap(), writes=[wukv_f])
                kb.op("pool", lambda e: e.tensor_copy(out=wukv[:], in_=wukv_f[:]), [wukv_f], [wukv])
                mrot = kb.rot("mt", [128, 416], F32, 2)
                cs_rot = kb.rot("cs", [128, 2, 16], F32, 2)
                t768 = kb.rot("t768", [128, 8, 96], F32, 8)
                small_m = kb.rot("small_m", [128, 16], F32, 10)
                tb768 = kb.rot("tb768", [128, 8, 96], BF16, 4)
                tbn = kb.rot("tbn", [128, 256], BF16, 4)
                tTn = kb.rot("tTn", [128, 2, 128], BF16, 4)
                rope_t = kb.rot("rope_t", [128, 8, 16], F32, 16)

                def head_norm_rope(src, dst_b, gain, cs, n_extra_ssq=None):
                    sq = t768.get()
                    st = small_m.get()
                    kb.op("act", lambda e: e.activation(out=sq[:], in_=src[:], func=AF.Square), [src], [sq])
                    kb.op("dve", lambda e: e.tensor_reduce(out=st[:, 0:8], in_=sq[:], axis=AX.X, op=ALU.add), [sq], [st])
                    yield
                    rms_rstd(st[:, 0:8], st[:, 8:16], st, st, 96, EPS)
                    kb.op("dve", lambda e: e.tensor_tensor(out=sq[:], in0=src[:], in1=st[:, 8:16].unsqueeze(2).to_broadcast([128, 8, 96]), op=ALU.mult),
                          [src, st], [sq])
                    yield
                    if cs is None:
                        kb.op("pool", lambda e: e.tensor_tensor(out=dst_b[:], in0=sq[:], in1=gain[:], op=ALU.mult), [sq, gain], [dst_b])
                        return
                    kb.op("pool", lambda e: e.tensor_tensor(out=sq[:], in0=sq[:], in1=gain[:], op=ALU.mult), [sq, gain], [sq])
                    kb.op("pool", lambda e: e.tensor_copy(out=dst_b[:, :, 0:64], in_=sq[:, :, 0:64]), [sq], [dst_b])
                    cb_ = cs[:, 0, :].unsqueeze(1).to_broadcast([128, 8, 16])
                    sb_ = cs[:, 1, :].unsqueeze(1).to_broadcast([128, 8, 16])
                    x1 = sq[:, :, 64:80]
                    x2 = sq[:, :, 80:96]
                    ta = rope_t.get(); tb_ = rope_t.get(); tc_ = rope_t.get(); td = rope_t.get()
                    yield
                    kb.op("dve", lambda e: e.tensor_tensor(out=ta[:], in0=x1, in1=cb_, op=ALU.mult), [sq, cs], [ta])
                    kb.op("dve", lambda e: e.tensor_tensor(out=tb_[:], in0=x2, in1=sb_, op=ALU.mult), [sq, cs], [tb_])
                    kb.op("pool", lambda e: e.tensor_tensor(out=tc_[:], in0=x1, in1=sb_, op=ALU.mult), [sq, cs], [tc_])
                    kb.op("pool", lambda e: e.tensor_tensor(out=td[:], in0=x2, in1=cb_, op=ALU.mult), [sq, cs], [td])
                    kb.op("dve", lambda e: e.tensor_tensor(out=dst_b[:, :, 64:80], in0=ta[:], in1=tb_[:], op=ALU.subtract), [ta, tb_], [dst_b])
                    kb.op("pool", lambda e: e.tensor_tensor(out=dst_b[:, :, 80:96], in0=tc_[:], in1=td[:], op=ALU.add), [tc_, td], [dst_b])

                def mla_tile(ti):
                        lat = ti >= 2
                        mt_ = mrot.get()
                        kb.dma("sp", mt_[:], mla_d[ti * 128:(ti + 1) * 128, :], reads=[mla_r[ti]], writes=[mt_])
                        cs = None
                        if lat:
                            cs = cs_rot.get()
                            lt_ = ti - 2
                            kb.dma("sp", cs[:, 0, :], I["c_cos"].ap()[lt_ * 128:(lt_ + 1) * 128, :], writes=[cs])
                            kb.dma("sp", cs[:, 1, :], I["c_sin"].ap()[lt_ * 128:(lt_ + 1) * 128, :], writes=[cs])
                        junk = junk_rot.get()
                        st = small_m.get()
                        kb.op("act", lambda e: e.activation(out=junk[:, 0:128], in_=mt_[:, 256:384], func=AF.Square, accum_out=st[:, 0:1]), [mt_], [junk, st])
                        kb.op("act", lambda e: e.activation(out=junk[:, 128:160], in_=mt_[:, 384:416], func=AF.Square, accum_out=st[:, 2:3]), [mt_], [junk, st])
                        rms_rstd(st[:, 0:1], st[:, 1:2], st, st, 128, EPS)
                        kvn = tbn.get()
                        kb.op("dve", lambda e: e.scalar_tensor_tensor(out=kvn[:, 0:128], in0=mt_[:, 256:384], scalar=st[:, 1:2], in1=kvln[:], op0=ALU.mult, op1=ALU.mult),
                              [mt_, st, kvln], [kvn])
                        yield
                        kvT = tTn.get()
                        transposes(kvn, [kvn[:, 0:128]], kvT[:, 0, :], kvT)
                        pk = ps2.get()
                        for nb_ in range(2):
                            kb.op("pe", lambda e: e.matmul(pk[:, nb_ * 512:(nb_ + 1) * 512], lhsT=kvT[:, 0, :], rhs=wukv[:, nb_ * 512:(nb_ + 1) * 512], start=True, stop=True),
                                  [kvT, wukv], [pk])
                        pk3 = pk[:].rearrange("p (h c) -> p h c", h=8)
                        kb.op("act", lambda e: e.copy(out=Vall[:, ti, :, 0:64], in_=pk3[:, :, 64:128]), [pk], [Vall])
                        kf = t768.get()
                        kb.op("dve", lambda e: e.tensor_copy(out=kf[:, :, 0:64], in_=pk3[:, :, 0:64]), [pk], [kf])
                        kb.op("pool", lambda e: e.tensor_copy(out=kf[:, :, 64:96], in_=mt_[:, 384:416].unsqueeze(1).to_broadcast([128, 8, 32])), [mt_], [kf])
                        yield
                        kbf = tb768.get()
                        yield from head_norm_rope(kf, kbf, knw, cs)
                        yield
                        transposes(kbf, [kbf[:, h, :] for h in range(8)], kT[:, :, ti * 128:(ti + 1) * 128], kT, eng="dve")
                        yield
                        if lat:
                            st2 = small_m.get()
                            kb.op("act", lambda e: e.activation(out=junk[:, 256:512], in_=mt_[:, 0:256], func=AF.Square, accum_out=st2[:, 0:1]), [mt_], [junk, st2])
                            rms_rstd(st2[:, 0:1], st2[:, 1:2], st2, st2, 256, EPS)
                            qn = tbn.get()
                            kb.op("dve", lambda e: e.scalar_tensor_tensor(out=qn[:], in0=mt_[:, 0:256], scalar=st2[:, 1:2], in1=qln[:], op0=ALU.mult, op1=ALU.mult),
                                  [mt_, st2, qln], [qn])
                            yield
                            qnT = tTn.get()
                            transposes(qn, [qn[:, 0:128], qn[:, 128:256]], qnT[:], qnT)
                            pq = ps2.get()
                            for (n0, n1) in ((0, 512), (512, 768)):
                                for k in range(2):
                                    kb.op("pe", lambda e: e.matmul(pq[:, n0:n1], lhsT=qnT[:, k, :], rhs=wuq[:, k, n0:n1], start=(k == 0), stop=(k == 1)),
                                          [qnT, wuq], [pq], inc=(k == 1))
                            qf = t768.get()
                            kb.op("act", lambda e: e.copy(out=qf[:].rearrange("p h c -> p (h c)"), in_=pq[:, 0:768]), [pq], [qf])
                            yield
                            qbf = tb768.get()
                            yield from head_norm_rope(qf, qbf, qnw, cs)
                            yield
                            transposes(qbf, [qbf[:, h, :] for h in range(8)], qT[:, :, lt_ * 128:(lt_ + 1) * 128], qT, eng="dve")

                run_interleaved((mla_tile(ti) for ti in range(18)), 2)
                if "qT_dbg" in dbg and b == 0:
                    kb.dma("sp", dbg_d["qT_dbg"][:, :, :], qT[:], reads=[qT], writes=[dbg_d["qT_dbg"]])
                    kb.dma("sp", dbg_d["kT_dbg"][:, :, :], kT[:], reads=[kT], writes=[dbg_d["kT_dbg"]])
                Erot = kb.rot("E", [128, 512], BF16, 3)
                apair = kb.rot("apair", [128, 4, 128], BF16, 2)
                for hp in range(4):
                    for qb in range(4):
                        ap_ = apair.get()
                        for hh in range(2):
                            h = hp * 2 + hh
                            po = [ps2.get(), ps2.get()]
                            for kt in range(18):
                                pS = ps1.get()
                                kb.op("pe", lambda e: e.matmul(pS[:, :], lhsT=kT[:, h, kt * 128:(kt + 1) * 128], rhs=qT[:, h, qb * 512:(qb + 1) * 512], start=True, stop=True),
                                      [kT, qT], [pS])
                                E = Erot.get()
                                kb.op("act", lambda e: e.activation(out=E[:], in_=pS[:, :], func=AF.Exp), [pS], [E])
                                for qs in range(4):
                                    p_ = po[qs // 2]
                                    c0 = (qs % 2) * 512
                                    kb.op("pe", lambda e: e.matmul(p_[:, c0:c0 + 65], lhsT=E[:, qs * 128:(qs + 1) * 128], rhs=Vall[:, kt, h, :],
                                                                   start=(kt == 0), stop=(kt == 17)), [E, Vall], [p_], inc=(kt == 17))
                            for qs in range(4):
                                p_ = po[qs // 2]
                                c0 = (qs % 2) * 512
                                st = small_rot.get()
                                kb.op("dve", lambda e: e.reciprocal(out=st[:, 0:1], in_=p_[:, c0 + 64:c0 + 65]), [p_], [st])
                                kb.op("dve", lambda e: e.tensor_scalar(out=ap_[:, qs, hh * 64:(hh + 1) * 64], in0=p_[:, c0:c0 + 64], scalar1=st[:, 0:1], scalar2=None,
                                                                       op0=ALU.mult), [p_, st], [ap_])
                        transposes(ap_, [ap_[:, qs, :] for qs in range(4)], attT[:, hp, qb * 512:(qb + 1) * 512], attT)
                if "att_dbg" in dbg and b == 0:
                    kb.dma("sp", dbg_d["att_dbg"][:, :, :], attT[:], reads=[attT], writes=[dbg_d["att_dbg"]])
            if upto <= 4:
                mix_stack.close()
                continue

            with kb.scope():
                wo_f = kb.sb("wo_f", [128, 8, 1024], F32)
                wo1 = kb.sb("wo1", [128, 4, 1024], BF16)
                wo2 = kb.sb("wo2", [128, 4, 1024], BF16)
                wo3 = kb.sb("wo3", [128, 8, 1024], BF16)
                kb.dma("sp", wo_f[:, 0:4, :], I["w_o_mla"].ap().rearrange("(k p) n -> p k n", p=128), writes=[wo_f])
                kb.op("pool", lambda e: e.tensor_copy(out=wo1[:], in_=wo_f[:, 0:4, :]), [wo_f], [wo1])
                kb.dma("sp", wo_f[:, 0:4, :], I["w_o_rwkv"].ap().rearrange("(k p) n -> p k n", p=128), reads=[wo_f], writes=[wo_f])
                kb.op("pool", lambda e: e.tensor_copy(out=wo2[:], in_=wo_f[:, 0:4, :]), [wo_f], [wo2])
                kb.dma("sp", wo_f[:], I["w_out"].ap().rearrange("(k p) n -> p k n", p=128), reads=[wo_f], writes=[wo_f])
                kb.op("pool", lambda e: e.tensor_copy(out=wo3[:], in_=wo_f[:]), [wo_f], [wo3])
                mT = kb.sb("mT", [128, 8, 2048], BF16)
                grot = kb.rot("gt", [128, 2, 512], BF16, 2)
                trot = kb.rot("tm", [128, 512], F32, 2)
                for m in range(8):
                    for nb_ in range(4):
                        g = grot.get()
                        kb.dma("sp", g[:, 0, :], gates_d[m * 128:(m + 1) * 128, nb_ * 512:(nb_ + 1) * 512], reads=[gates_r[m][nb_]], writes=[g])
                        kb.dma("sp", g[:, 1, :], gates_d[(8 + m) * 128:(9 + m) * 128, nb_ * 512:(nb_ + 1) * 512], reads=[gates_r[8 + m][nb_]], writes=[g])
                        p1 = ps1.get(); p2 = ps1.get()
                        for k in range(4):
                            kb.op("pe", lambda e: e.matmul(p1[:, :], lhsT=wo1[:, k, m * 128:(m + 1) * 128], rhs=attT[:, k, nb_ * 512:(nb_ + 1) * 512], start=(k == 0), stop=(k == 3)),
                                  [wo1, attT], [p1], inc=(k == 3))
                        for k in range(4):
                            kb.op("pe", lambda e: e.matmul(p2[:, :], lhsT=wo2[:, k, m * 128:(m + 1) * 128], rhs=rwT[:, k, nb_ * 512:(nb_ + 1) * 512], start=(k == 0), stop=(k == 3)),
                                  [wo2, rwT], [p2], inc=(k == 3))
                        t1 = trot.get(); t2 = trot.get()
                        kb.op("dve", lambda e: e.tensor_tensor(out=t1[:], in0=p1[:, :], in1=g[:, 0, :], op=ALU.mult), [p1, g], [t1])
                        kb.op("dve", lambda e: e.tensor_tensor(out=t2[:], in0=p2[:, :], in1=g[:, 1, :], op=ALU.mult), [p2, g], [t2])
                        kb.op("pool", lambda e: e.tensor_tensor(out=mT[:, m, nb_ * 512:(nb_ + 1) * 512], in0=t1[:], in1=t2[:], op=ALU.add), [t1, t2], [mT])
                Ga = kb.sb("Ga", [128, 1024], F32)
                bcast_load(Ga[:], Ga, mod_d.t.ap()[b:b + 1, 2048:3072], 128, reads=[mod_d])
                xrot = kb.rot("xt5", [128, 1024], F32, 2)
                for t in range(16):
                    xt = xrot.get()
                    kb.dma("sp", xt[:], I["x"].ap()[b, t * 128:(t + 1) * 128, :], writes=[xt])
                    pm = ps2.get()
                    for nb_ in range(2):
                        for k in range(8):
                            kb.op("pe", lambda e: e.matmul(pm[:, nb_ * 512:(nb_ + 1) * 512], lhsT=mT[:, k, t * 128:(t + 1) * 128], rhs=wo3[:, k, nb_ * 512:(nb_ + 1) * 512],
                                                           start=(k == 0), stop=(k == 7)), [mT, wo3], [pm], inc=(k == 7))
                    junk = junk_rot.get()
                    kb.op("dve", lambda e: e.tensor_tensor(out=junk[:], in0=pm[:, :], in1=Ga[:], op=ALU.mult), [pm, Ga], [junk])
                    kb.op("pool", lambda e: e.tensor_tensor(out=xt[:], in0=junk[:], in1=xt[:], op=ALU.add), [junk, xt], [xt])
                    kb.dma("sp", x1_d[(b * 16 + t) * 128:(b * 16 + t + 1) * 128, :], xt[:], reads=[xt], writes=[x1_r[b * 16 + t]])
            kb.barrier()
            mix_stack.close()
            if upto <= 5:
                continue

            pass

        if upto > 5:
          NT = 16 * nbatch
          NBLK = NT * 4 + 256
          with kb.scope():
            I32 = mybir.dt.int32
            u_d = kb.dram("u_d", [NT * 128, 1024], BF16)
            u_r = [Buf() for _ in range(NT)]
            xs_d = kb.dram("xs_d", [NBLK * 256, 1024], BF16)
            Y_d = kb.dram("Y_d", [NBLK * 256, 1024], BF16)
            junk_rot = kb.rot("junk6", [128, 1024], F32, 3)
            small_rot = kb.rot("small6", [128, 16], F32, 8)
            dest_all = kb.sb("dest_all", [128, NT, 8], I32)
            gate_all = kb.sb("gate_all", [128, NT, 8], F32)
            IDXi = kb.sb("IDXi", [128, NBLK], I32)
            tri_f = kb.sb("tri6_f", [128, 4, 128], F32)
            tri = kb.sb("tri6", [128, 4, 128], BF16)
            kb.dma("sp", tri_f[:], I["c_tri"].ap(), writes=[tri_f])
            kb.op("dve", lambda e: e.tensor_copy(out=tri[:], in_=tri_f[:]), [tri_f], [tri])
            iota_p = kb.sb("iota_p", [128, 2], F32)
            kb.dma("sp", iota_p[:], I["c_iotap"].ap(), writes=[iota_p])
            with kb.scope():
                G_all = kb.sb("G_all", [128, NT, 256], F32)
                POS_all = kb.sb("POS_all", [128, NT, 256], F32)
                mask_all = kb.sb("mask_all", [128, NT, 256], BF16)
                cnt = kb.sb("cnt", [128, 256], F32)
                kb.op("pool", lambda e: e.memset(cnt[:], 0.0), [], [cnt])
                with kb.scope():
                    rw_f = kb.sb("rw_f", [128, 8, 256], F32)
                    kb.dma("sp", rw_f[:], I["router_w"].ap().rearrange("(k p) n -> p k n", p=128), writes=[rw_f])
                    rbias = kb.sb("rbias", [128, 256], F32)
                    bcast_load(rbias[:], rbias, I["router_bias"].ap()[0:1, :], 128)
                    xrot = kb.rot("xt6", [128, 1024], F32, 3)
                    ufr = kb.rot("uf", [128, 1024], F32, 3)
                    ubr = kb.rot("ub", [128, 1024], BF16, 3)
                    uTf = kb.rot("uTf", [128, 8, 128], F32, 3)
                    s256 = kb.rot("s256", [128, 256], F32, 12)
                    m8r = kb.rot("m8", [128, 8], F32, 16)
                    A2s = []
                    for b in range(nbatch):
                        A2 = kb.sb("A2", [128, 1024], F32)
                        S2 = kb.sb("S2", [128, 1024], F32)
                        bcast_load(A2[:], A2, mod_d.t.ap()[b:b + 1, 4096:5120], 128, reads=[mod_d])
                        bcast_load(S2[:], S2, mod_d.t.ap()[b:b + 1, 3072:4096], 128, reads=[mod_d])
                        A2s.append((A2, S2))
                    def tileA(i):
                            b = i // 16
                            A2, S2 = A2s[b]
                            xt = xrot.get()
                            kb.dma("sp", xt[:], x1_d[i * 128:(i + 1) * 128, :], reads=[x1_r[i]], writes=[xt])
                            junk = junk_rot.get(); ssq = small_rot.get()
                            kb.op("act", lambda e: e.activation(out=junk[:], in_=xt[:], func=AF.Square, accum_out=ssq[:, 0:1]), [xt], [junk, ssq])
                            rms_rstd(ssq[:, 0:1], ssq[:, 1:2], ssq, ssq, 1024, EPS)
                            yield
                            uf = ufr.get()
                            kb.op("dve", lambda e: e.scalar_tensor_tensor(out=junk[:], in0=xt[:], scalar=ssq[:, 1:2], in1=A2[:], op0=ALU.mult, op1=ALU.mult), [xt, ssq, A2], [junk])
                            kb.op("pool", lambda e: e.tensor_tensor(out=uf[:], in0=junk[:], in1=S2[:], op=ALU.add), [junk, S2], [uf])
                            ub = ubr.get()
                            kb.op("act", lambda e: e.copy(out=ub[:], in_=uf[:]), [uf], [ub])
                            kb.dma("sp", u_d[i * 128:(i + 1) * 128, :], ub[:], reads=[ub], writes=[u_r[i]])
                            yield
                            utf = uTf.get()
                            for half in range(2):
                                transposes(uf, [uf[:, (half * 4 + k) * 128:(half * 4 + k + 1) * 128] for k in range(4)], utf[:, half * 4:(half + 1) * 4, :], utf, dt=F32, eng="dve")
                            pr = ps1.get()
                            for k in range(8):
                                kb.op("pe", lambda e: e.matmul(pr[:, 0:256], lhsT=utf[:, k, :], rhs=rw_f[:, k, :], start=(k == 0), stop=(k == 7)), [utf, rw_f], [pr], inc=(k == 7))
                            sc = s256.get(); sel = s256.get(); w1_ = s256.get(); w2_ = s256.get()
                            kb.op("act", lambda e: e.activation(out=sc[:], in_=pr[:, 0:256], func=AF.Sigmoid), [pr], [sc])
                            kb.op("dve", lambda e: e.tensor_tensor(out=sel[:], in0=sc[:], in1=rbias[:], op=ALU.add), [sc, rbias], [sel])
                            yield
                            sel3 = sel[:].rearrange("p (g c) -> p g c", g=8)
                            mx = m8r.get(); mx2 = m8r.get(); gs = m8r.get(); gsort = m8r.get()
                            kb.op("dve", lambda e: e.tensor_reduce(out=mx[:], in_=sel3, axis=AX.X, op=ALU.max), [sel], [mx])
                            kb.op("dve", lambda e: e.tensor_tensor(out=w1_[:].rearrange("p (g c) -> p g c", g=8), in0=sel3, in1=mx[:].unsqueeze(2).to_broadcast([128, 8, 32]), op=ALU.is_ge),
                                  [sel, mx], [w1_])
                            kb.op("dve", lambda e: e.scalar_tensor_tensor(out=w2_[:], in0=w1_[:], scalar=-10.0, in1=sel[:], op0=ALU.mult, op1=ALU.add), [w1_, sel], [w2_])
                            kb.op("dve", lambda e: e.tensor_reduce(out=mx2[:], in_=w2_[:].rearrange("p (g c) -> p g c", g=8), axis=AX.X, op=ALU.max), [w2_], [mx2])
                            yield
                            kb.op("dve", lambda e: e.tensor_tensor(out=gs[:], in0=mx[:], in1=mx2[:], op=ALU.add), [mx, mx2], [gs])
                            kb.op("dve", lambda e: e.max(out=gsort[:], in_=gs[:]), [gs], [gsort])
                            kb.op("dve", lambda e: e.tensor_scalar(out=gs[:], in0=gs[:], scalar1=gsort[:, 3:4], scalar2=None, op0=ALU.is_ge), [gs, gsort], [gs])
                            kb.op("dve", lambda e: e.scalar_tensor_tensor(out=w1_[:].rearrange("p (g c) -> p g c", g=8), in0=sel3, scalar=2.0,
                                                                          in1=gs[:].unsqueeze(2).to_broadcast([128, 8, 32]), op0=ALU.add, op1=ALU.mult), [sel, gs], [w1_])
                            yield
                            top8 = m8r.get()
                            kb.op("dve", lambda e: e.max(out=top8[:], in_=w1_[:]), [w1_], [top8])
                            kb.op("dve", lambda e: e.tensor_scalar(out=w2_[:], in0=w1_[:], scalar1=top8[:, 7:8], scalar2=None, op0=ALU.is_ge), [w1_, top8], [w2_])
                            kb.op("pool", lambda e: e.tensor_copy(out=mask_all[:, i, :], in_=w2_[:]), [w2_], [mask_all])
                            yield
                            st = small_rot.get()
                            kb.op("dve", lambda e: e.tensor_tensor(out=w1_[:], in0=w2_[:], in1=sc[:], op=ALU.mult), [w2_, sc], [w1_])
                            kb.op("dve", lambda e: e.tensor_reduce(out=st[:, 0:1], in_=w1_[:], axis=AX.X, op=ALU.add), [w1_], [st])
                            kb.op("dve", lambda e: e.reciprocal(out=st[:, 1:2], in_=st[:, 0:1]), [st], [st])
                            kb.op("dve", lambda e: e.tensor_scalar(out=G_all[:, i, :], in0=w1_[:], scalar1=st[:, 1:2], scalar2=2.5, op0=ALU.mult, op1=ALU.mult), [w1_, st], [G_all])
                            yield
                            pc = ps1.get()
                            kb.op("pe", lambda e: e.matmul(pc[:, 0:256], lhsT=tri[:, 1, :], rhs=mask_all[:, i, :], start=True, stop=True), [tri, mask_all], [pc])
                            kb.op("pe", lambda e: e.matmul(pc[:, 256:512], lhsT=ones_b[:], rhs=mask_all[:, i, :], start=True, stop=True), [ones_b, mask_all], [pc])
                            kb.op("dve", lambda e: e.tensor_tensor(out=POS_all[:, i, :], in0=pc[:, 0:256], in1=cnt[:], op=ALU.add), [pc, cnt], [POS_all])
                            kb.op("dve", lambda e: e.tensor_tensor(out=cnt[:], in0=pc[:, 256:512], in1=cnt[:], op=ALU.add), [pc, cnt], [cnt])
                    run_interleaved((tileA(i) for i in range(NT)), 3)
                if "G_dbg" in dbg:
                    kb.dma("sp", dbg_d["G_dbg"][:, :, 0:256], G_all[:, 0:16, :], reads=[G_all], writes=[dbg_d["G_dbg"]])
                base_r = kb.sb("base_r", [128, 256], F32)
                with kb.scope():
                    ind = kb.sb("ind", [128, 256], BF16)
                    kb.op("dve", lambda e: e.tensor_scalar(out=ind[:], in0=cnt[:], scalar1=iota_p[:, 1:2], scalar2=None, op0=ALU.is_gt), [cnt, iota_p], [ind])
                    pn = ps1.get()
                    kb.op("pe", lambda e: e.matmul(pn[:, 0:256], lhsT=ones_b[:], rhs=ind[:], start=True, stop=True), [ones_b, ind], [pn])
                    nblk = kb.sb("nblk", [128, 256], BF16)
                    kb.op("dve", lambda e: e.tensor_copy(out=nblk[:], in_=pn[:, 0:256]), [pn], [nblk])
                    nbT = kb.sb("nbT", [128, 2, 128], BF16)
                    transposes(nblk, [nblk[:, 0:128], nblk[:, 128:256]], nbT[:], nbT)
                    pb = ps1.get()
                    kb.op("pe", lambda e: e.matmul(pb[:, 0:128], lhsT=nbT[:, 0, :], rhs=tri[:, 1, :], start=True, stop=True), [nbT, tri], [pb])
                    kb.op("pe", lambda e: e.matmul(pb[:, 128:256], lhsT=nbT[:, 0, :], rhs=ones_b[:], start=True, stop=False), [nbT, ones_b], [pb], inc=False)
                    kb.op("pe", lambda e: e.matmul(pb[:, 128:256], lhsT=nbT[:, 1, :], rhs=tri[:, 1, :], start=False, stop=True), [nbT, tri], [pb])
                    kb.op("dve", lambda e: e.tensor_copy(out=base_r[:], in_=pb[:, 0:256]), [pb], [base_r])
                    pe_ = ps1.get()
                    kb.op("pe", lambda e: e.matmul(pe_[:, 0:1], lhsT=tri[:, 0, :], rhs=nbT[:, 0, 0:1], start=True, stop=True), [nbT, tri], [pe_])
                    kb.op("pe", lambda e: e.matmul(pe_[:, 1:2], lhsT=ones_b[:], rhs=nbT[:, 0, 0:1], start=True, stop=False), [nbT, ones_b], [pe_], inc=False)
                    kb.op("pe", lambda e: e.matmul(pe_[:, 1:2], lhsT=tri[:, 0, :], rhs=nbT[:, 1, 0:1], start=False, stop=True), [nbT, tri], [pe_])
                    endT = kb.sb("endT", [128, 2], F32)
                    kb.op("dve", lambda e: e.tensor_copy(out=endT[:], in_=pe_[:, 0:2]), [pe_], [endT])
                    iota_b = kb.sb("iota_b", [128, NBLK], F32)
                    kb.dma("sp", iota_b[:], I["c_iotab"].ap()[:, 0:NBLK], writes=[iota_b])
                    ind2 = kb.sb("ind2", [128, 2, NBLK], BF16)
                    for c_ in range(2):
                        kb.op("dve", lambda e: e.tensor_scalar(out=ind2[:, c_, :], in0=iota_b[:], scalar1=endT[:, c_:c_ + 1], scalar2=None, op0=ALU.is_ge), [iota_b, endT], [ind2])
                    BEf = kb.sb("BEf", [128, NBLK], F32)
                    for n0 in range(0, NBLK, 512):
                        n1 = min(n0 + 512, NBLK)
                        pbe = ps1.get()
                        for c_ in range(2):
                            kb.op("pe", lambda e: e.matmul(pbe[:, 0:n1 - n0], lhsT=ones_b[:], rhs=ind2[:, c_, n0:n1], start=(c_ == 0), stop=(c_ == 1)), [ones_b, ind2], [pbe], inc=(c_ == 1))
                        kb.op("dve", lambda e: e.tensor_scalar(out=BEf[:, n0:n1], in0=pbe[:, 0:n1 - n0], scalar1=128.0, scalar2=iota_p[:, 0:1], op0=ALU.mult, op1=ALU.add), [pbe, iota_p], [BEf])
                    kb.op("dve", lambda e: e.tensor_copy(out=IDXi[:], in_=BEf[:]), [BEf], [IDXi])
                with kb.scope():
                    s256 = kb.rot("s256b", [128, 256], F32, 9)
                    m8r = kb.rot("m8b", [128, 8], F32, 6)
                    ubr = kb.rot("ubB", [128, 1024], BF16, 3)
                    def tileB(i):
                            t_ = s256.get(); V = s256.get(); eq = s256.get()
                            kb.op("dve", lambda e: e.scalar_tensor_tensor(out=t_[:], in0=base_r[:], scalar=256.0, in1=POS_all[:, i, :], op0=ALU.mult, op1=ALU.add), [base_r, POS_all], [t_])
                            kb.op("dve", lambda e: e.scalar_tensor_tensor(out=V[:], in0=t_[:], scalar=1.0, in1=mask_all[:, i, :], op0=ALU.add, op1=ALU.mult), [t_, mask_all], [V])
                            yield
                            top8 = m8r.get(); d8 = m8r.get()
                            kb.op("dve", lambda e: e.max(out=top8[:], in_=V[:]), [V], [top8])
                            kb.op("dve", lambda e: e.tensor_scalar(out=d8[:], in0=top8[:], scalar1=-1.0, scalar2=None, op0=ALU.add), [top8], [d8])
                            kb.op("dve", lambda e: e.tensor_copy(out=dest_all[:, i, :], in_=d8[:]), [d8], [dest_all])
                            yield
                            for k in range(8):
                                kb.op("dve", lambda e: e.tensor_scalar(out=eq[:], in0=V[:], scalar1=top8[:, k:k + 1], scalar2=None, op0=ALU.is_equal), [V, top8], [eq])
                                kb.op("pool", lambda e: e.tensor_tensor(out=eq[:], in0=eq[:], in1=G_all[:, i, :], op=ALU.mult), [eq, G_all], [eq])
                                kb.op("dve", lambda e: e.tensor_reduce(out=gate_all[:, i, k:k + 1], in_=eq[:], axis=AX.X, op=ALU.add), [eq], [gate_all])
                                if k % 2 == 1:
                                    yield
                            yield
                            ub = ubr.get()
                            kb.dma("sp", ub[:], u_d[i * 128:(i + 1) * 128, :], reads=[u_r[i]], writes=[ub])
                            for k in range(8):
                                kb.idma(out=xs_d[:, :], in_=ub[:], out_off=dest_all[:, i, k:k + 1], reads=[ub, dest_all], writes=[xs_d])
                    run_interleaved((tileB(i) for i in range(NT)), 3)
            xT_rot = kb.rot("xT", [128, 8, 128], BF16, 3)
            silu_rot = kb.rot("silu", [128, 256], F32, 2)
            hb_rot = kb.rot("hb", [128, 256], BF16, 4)
            hT_rot = kb.rot("hT6", [128, 2, 128], BF16, 3)

            def ffn_a(xsb, wb13, strided):
                if strided:
                    srcs = [xsb[:].rearrange("s (p k) -> s k p", k=8)[:, k, :] for k in range(8)]
                else:
                    srcs = [xsb[:, k * 128:(k + 1) * 128] for k in range(8)]
                xT = xT_rot.get()
                transposes(xsb, srcs, xT[:], xT)
                ph = ps1.get()
                for k in range(8):
                    kb.op("pe", lambda e: e.matmul(ph[:, :], lhsT=xT[:, k, :], rhs=wb13[:, k, :, :].rearrange("p w n -> p (w n)"), start=(k == 0), stop=(k == 7)),
                          [xT, wb13], [ph], inc=(k == 7))
                sl = silu_rot.get(); hb = hb_rot.get()
                kb.op("act", lambda e: e.activation(out=sl[:], in_=ph[:, 0:256], func=AF.Silu), [ph], [sl])
                kb.op("dve", lambda e: e.tensor_tensor(out=hb[:], in0=ph[:, 256:512], in1=sl[:], op=ALU.mult), [ph, sl], [hb])
                return hb

            def ffn_b(hb, wb2, strided):
                if strided:
                    hs = [hb[:].rearrange("s (p j) -> s j p", j=2)[:, j, :] for j in range(2)]
                else:
                    hs = [hb[:, j * 128:(j + 1) * 128] for j in range(2)]
                hT = hT_rot.get()
                transposes(hb, hs, hT[:], hT, eng="dve")
                po_ = ps2.get()
                for nb_ in range(2):
                    for j in range(2):
                        kb.op("pe", lambda e: e.matmul(po_[:, nb_ * 512:(nb_ + 1) * 512], lhsT=hT[:, j, :], rhs=wb2[:, j, nb_ * 512:(nb_ + 1) * 512],
                                                       start=(j == 0), stop=(j == 1)), [hT, wb2], [po_], inc=(j == 1))
                return po_

            def ffn_block(xsb, wb13, wb2, strided):
                return ffn_b(ffn_a(xsb, wb13, strided), wb2, strided)

            with kb.scope():
                w1v = I["expert_w1"].ap().rearrange("e (p k) n -> (e p) (k n)", k=8)
                w3v = I["expert_w3"].ap().rearrange("e (p k) n -> (e p) (k n)", k=8)
                w2v = I["expert_w2"].ap().rearrange("e (p j) n -> (e p) (j n)", j=2)
                wf13 = kb.rot("wf13", [128, 2, 2048], F32, 3)
                wf2 = kb.rot("wf2", [128, 2048], F32, 3)
                wb13r = kb.rot("wb13", [128, 8, 2, 256], BF16, 3)
                wb2r = kb.rot("wb2", [128, 2, 1024], BF16, 4)
                xsr = kb.rot("xsb", [128, 1024], BF16, 8)
                ybr = kb.rot("yb", [128, 1024], BF16, 3)
                for t_ in wf13.tiles + wf2.tiles:
                    kb.op("pool", lambda e: e.memset(t_[:], 0.0), [], [t_])
                pend = {}

                def issue(bk):
                    f13 = wf13.get(); f2 = wf2.get()
                    idx = IDXi[:, bk:bk + 1]
                    kb.idma(out=f13[:, 0, :], in_=w1v, in_off=idx, reads=[IDXi], writes=[f13], bounds=32767)
                    kb.idma(out=f13[:, 1, :], in_=w3v, in_off=idx, reads=[IDXi], writes=[f13], bounds=32767)
                    kb.idma(out=f2[:], in_=w2v, in_off=idx, reads=[IDXi], writes=[f2], bounds=32767)
                    xs2 = []
                    for sbk in range(2):
                        xsb = xsr.get()
                        r0 = (bk * 2 + sbk) * 128
                        kb.dma("sp", xsb[:], xs_d[r0:r0 + 128, :], reads=[xs_d], writes=[xsb])
                        xs2.append(xsb)
                    pend[bk] = (f13, f2, xs2)

                nrun = min(NBLK, blk_limit)
                NSUB = nrun * 2
                ctx_ = {}

                def st0(n):
                    bk = n // 2
                    if n % 2 == 0:
                        if bk + 2 < nrun:
                            issue(bk + 2)
                        f13, f2, xs2 = pend.pop(bk)
                        wb13 = wb13r.get(); wb2 = wb2r.get()
                        kb.op("act", lambda e: e.copy(out=wb13[:, :, 0, :], in_=f13[:, 0, :].rearrange("p (k n) -> p k n", k=8)), [f13], [wb13])
                        kb.op("dve", lambda e: e.tensor_copy(out=wb13[:, :, 1, :], in_=f13[:, 1, :].rearrange("p (k n) -> p k n", k=8)), [f13], [wb13])
                        kb.op("act", lambda e: e.copy(out=wb2[:, 0, :], in_=f2[:, 0:1024]), [f2], [wb2])
                        kb.op("dve", lambda e: e.tensor_copy(out=wb2[:, 1, :], in_=f2[:, 1024:2048]), [f2], [wb2])
                        ctx_[("w", bk)] = (wb13, wb2, xs2)
                    wb13, wb2, xs2 = ctx_[("w", bk)]
                    xsb = xs2[n % 2]
                    srcs = [xsb[:].rearrange("s (p k) -> s k p", k=8)[:, k, :] for k in range(8)]
                    xT = xT_rot.get()
                    transposes(xsb, srcs, xT[:], xT)
                    ctx_[n] = dict(xT=xT, wb13=wb13, wb2=wb2)

                def st1(n):
                    c = ctx_[n]
                    xT, wb13 = c["xT"], c["wb13"]
                    ph = ps1.get()
                    for k in range(8):
                        kb.op("pe", lambda e: e.matmul(ph[:, :], lhsT=xT[:, k, :], rhs=wb13[:, k, :, :].rearrange("p w n -> p (w n)"), start=(k == 0), stop=(k == 7)),
                              [xT, wb13], [ph], inc=(k == 7))
                    sl = silu_rot.get(); hb = hb_rot.get()
                    kb.op("act", lambda e: e.activation(out=sl[:], in_=ph[:, 0:256], func=AF.Silu), [ph], [sl])
                    kb.op("dve", lambda e: e.tensor_tensor(out=hb[:], in0=ph[:, 256:512], in1=sl[:], op=ALU.mult), [ph, sl], [hb])
                    c["hb"] = hb

                def st2(n):
                    c = ctx_[n]
                    hb = c["hb"]
                    hs = [hb[:].rearrange("s (p j) -> s j p", j=2)[:, j, :] for j in range(2)]
                    hT = hT_rot.get()
                    transposes(hb, hs, hT[:], hT, eng="dve")
                    c["hT"] = hT

                def st3(n):
                    c = ctx_.pop(n)
                    hT, wb2 = c["hT"], c["wb2"]
                    po_ = ps2.get()
                    for nb_ in range(2):
                        for j in range(2):
                            kb.op("pe", lambda e: e.matmul(po_[:, nb_ * 512:(nb_ + 1) * 512], lhsT=hT[:, j, :], rhs=wb2[:, j, nb_ * 512:(nb_ + 1) * 512],
                                                           start=(j == 0), stop=(j == 1)), [hT, wb2], [po_], inc=(j == 1))
                    yb = ybr.get()
                    kb.op("act", lambda e: e.copy(out=yb[:, 0:512], in_=po_[:, 0:512]), [po_], [yb])
                    kb.op("dve", lambda e: e.tensor_copy(out=yb[:, 512:1024], in_=po_[:, 512:1024]), [po_], [yb])
                    kb.dma("sp", Y_d[n * 128:(n + 1) * 128, :], yb[:], reads=[yb], writes=[Y_d])

                for bk in range(min(2, nrun)):
                    issue(bk)
                stages = (st0, st1, st2, st3)
                for step in range(NSUB + 3):
                    for k_, fn_ in enumerate(stages):
                        n = step - k_
                        if 0 <= n < NSUB:
                            fn_(n)
            with kb.scope():
                wsf = kb.sb("wsf", [128, 8, 256], F32)
                ws13 = kb.sb("ws13", [128, 8, 2, 256], BF16)
                ws2f = kb.sb("ws2f", [128, 2, 1024], F32)
                ws2 = kb.sb("ws2", [128, 2, 1024], BF16)
                for w_, nm in enumerate(("shared_w1", "shared_w3")):
                    kb.dma("sp", wsf[:], I[nm].ap().rearrange("(k p) n -> p k n", p=128), reads=[wsf], writes=[wsf])
                    kb.op("dve", lambda e: e.tensor_copy(out=ws13[:, :, w_, :], in_=wsf[:]), [wsf], [ws13])
                kb.dma("sp", ws2f[:], I["shared_w2"].ap().rearrange("(j p) n -> p j n", p=128), writes=[ws2f])
                kb.op("dve", lambda e: e.tensor_copy(out=ws2[:], in_=ws2f[:]), [ws2f], [ws2])
                Gms = []
                for b in range(nbatch):
                    Gm = kb.sb("Gm", [128, 1024], F32)
                    bcast_load(Gm[:], Gm, mod_d.t.ap()[b:b + 1, 5120:6144], 128, reads=[mod_d])
                    Gms.append(Gm)
                ubr = kb.rot("ubD", [128, 1024], BF16, 3)
                ygr = kb.rot("yg", [128, 1024], BF16, 8)
                accr = kb.rot("acc", [128, 1024], F32, 3)
                xrot = kb.rot("xtD", [128, 1024], F32, 3)
                def tileD(i):
                        b = i // 16
                        ub = ubr.get()
                        kb.dma("sp", ub[:], u_d[i * 128:(i + 1) * 128, :], reads=[u_r[i]], writes=[ub])
                        yield
                        hb_ = ffn_a(ub, ws13, False)
                        yield
                        po_ = ffn_b(hb_, ws2, False)
                        acc = accr.get()
                        kb.op("act", lambda e: e.copy(out=acc[:], in_=po_[:, :]), [po_], [acc])
                        yield
                        for k in range(8):
                            yg = ygr.get()
                            kb.idma(out=yg[:], in_=Y_d[:, :], in_off=dest_all[:, i, k:k + 1], reads=[dest_all, Y_d], writes=[yg])
                            kb.op("dve", lambda e: e.scalar_tensor_tensor(out=acc[:], in0=yg[:], scalar=gate_all[:, i, k:k + 1], in1=acc[:], op0=ALU.mult, op1=ALU.add),
                                  [yg, gate_all, acc], [acc])
                            if k % 2 == 1:
                                yield
                        if "moe_dbg" in dbg and i < 16:
                            kb.dma("sp", dbg_d["moe_dbg"][:, i, :], acc[:], reads=[acc], writes=[dbg_d["moe_dbg"]])
                        yield
                        xt = xrot.get()
                        kb.dma("sp", xt[:], x1_d[i * 128:(i + 1) * 128, :], reads=[x1_r[i]], writes=[xt])
                        kb.op("dve", lambda e: e.tensor_tensor(out=acc[:], in0=acc[:], in1=Gms[b][:], op=ALU.mult), [acc, Gms[b]], [acc])
                        kb.op("pool", lambda e: e.tensor_tensor(out=xt[:], in0=xt[:], in1=acc[:], op=ALU.add), [xt, acc], [xt])
                        kb.dma("sp", out_d.ap()[b, (i % 16) * 128:(i % 16 + 1) * 128, :], xt[:], reads=[xt])
                run_interleaved((tileD(i) for i in range(NT)), 3)
        kb.finish()
        print("instructions:", kb.nins, "sems:", len(kb.sems) + len(kb.dsem))
    return nc


_CACHE = {}


def kernel(**inputs):
    n = 8
    consts = host_consts()
    sq = lambda k: np.ascontiguousarray(np.asarray(inputs[k], dtype=np.float32)[0])
    shared = {}
    for k in IN_SHAPES:
        if k in ("x", "ctx", "cT") or k.startswith("c_"):
            continue
        a = sq(k)
        shared[k] = a.reshape(IN_SHAPES[k])
    shared.update(consts)
    x = np.asarray(inputs["x"], np.float32)
    ctx = np.asarray(inputs["ctx"], np.float32)
    c = np.asarray(inputs["c"], np.float32)
    c_ctx = np.asarray(inputs["c_ctx"], np.float32)
    in_maps = []
    for i in range(n):
        m = dict(shared)
        m["x"] = np.ascontiguousarray(x[2 * i:2 * i + 2])
        m["ctx"] = np.ascontiguousarray(ctx[2 * i:2 * i + 2])
        m["cT"] = np.ascontiguousarray(np.stack([c[2 * i], c[2 * i + 1], c_ctx], axis=1))
        in_maps.append(m)
    if "nc" not in _CACHE:
        _CACHE["nc"] = build()
    res = run_bass_kernel_spmd(_CACHE["nc"], in_maps, core_ids=list(range(n)))
    return np.concatenate([np.asarray(r["out"], np.float32) for r in res.results], axis=0)
```

```python
import numpy as np
import ml_dtypes
import concourse.bass as bass
import concourse.mybir as mybir
from concourse.bass_utils import run_bass_kernel_spmd
from contextlib import ExitStack, contextmanager

F32 = mybir.dt.float32
BF16 = mybir.dt.bfloat16
AF = mybir.ActivationFunctionType
ALU = mybir.AluOpType
AX = mybir.AxisListType

C_DEC = -0.6065306597126334
EPS = 1e-6
GN_EPS = 64e-5
ATTN_SCALE = 96 ** -0.5


NAMES = {}


class Buf:
    __slots__ = ("name", "w", "r", "excl")

    def __init__(self, name=""):
        self.name = name
        self.w = None
        self.r = {}
        self.excl = False


class Tile:
    def __init__(self, t, name):
        self.t = t
        self.b = Buf(name)

    def __getitem__(self, k):
        return self.t[k]


class Rot:
    def __init__(self, tiles):
        self.tiles = tiles
        self.i = 0

    def get(self):
        t = self.tiles[self.i % len(self.tiles)]
        self.i += 1
        return t


class KB:
    ENG = ("pe", "act", "dve", "pool", "sp")
    EPOCH = 8000
    NDMA = 40

    def __init__(self, nc, stack):
        self.nc = nc
        self.gstack = stack
        self.stack = stack
        self.e = {"pe": nc.tensor, "act": nc.scalar, "dve": nc.vector,
                  "pool": nc.gpsimd, "sp": nc.sync}
        self.cnt = {k: 0 for k in self.ENG}
        self.ep = {k: 0 for k in self.ENG}
        self.sems = {}
        self.waited = {}
        for k in self.ENG:
            self._newsem(k)
        self.dsem = []
        self.dslot = []
        self.dval = []
        for i in range(self.NDMA):
            self.dsem.append(stack.enter_context(nc.semaphore(f"dq{i}")))
            self.dslot.append(i)
            self.dval.append(0)
        self.dnext = 0
        self.dwaited = {}
        self.nins = 0
        self.uid = 0
        self.promised = False
        self.bregs = {}

    def sb(self, name, shape, dtype):
        self.uid += 1
        nm = f"{name}_{self.uid}"
        NAMES[name] = nm
        return Tile(self.stack.enter_context(self.nc.sbuf_tensor(nm, list(shape), dtype)), nm)

    def ps(self, name, shape, dtype=F32):
        t = Tile(self.gstack.enter_context(self.nc.psum_tensor(name, list(shape), dtype)), name)
        t.b.excl = True
        return t

    def dram(self, name, shape, dtype, kind="Internal"):
        return Tile(self.nc.dram_tensor(name, list(shape), dtype, kind=kind), name)

    def rot(self, name, shape, dtype, n=2):
        return Rot([self.sb(f"{name}{i}", shape, dtype) for i in range(n)])

    @contextmanager
    def scope(self):
        old = self.stack
        with ExitStack() as st:
            self.stack = st
            yield
            self.barrier()
        self.stack = old

    def _newsem(self, k):
        self.sems[(k, self.ep[k])] = self.gstack.enter_context(
            self.nc.semaphore(f"s_{k}_{self.ep[k]}"))

    def _wait(self, eng, tok):
        if tok is None:
            return
        if tok[0] == "e":
            _, k, ep, c = tok
            if eng == "pe" and k == "pe":
                return
            assert not (k == "pe" and ep == self.ep["pe"] and c > self.cnt["pe"]), "wait on a promised PE token"
            key = (eng, k, ep)
            if self.waited.get(key, 0) >= c:
                return
            self.e[eng].wait_ge(self.sems[(k, ep)], c)
            self.waited[key] = c
        else:
            _, i, v = tok
            key = (eng, i)
            if self.dwaited.get(key, 0) >= v:
                return
            self.e[eng].wait_ge(self.dsem[i], v)
            self.dwaited[key] = v

    @staticmethod
    def _bufs(xs):
        out = []
        for x in xs:
            if x is None:
                continue
            out.append(x.b if isinstance(x, Tile) else x)
        return out

    def _deps(self, eng, reads, writes):
        for b in reads:
            self._wait(eng, b.w)
        for b in writes:
            self._wait(eng, b.w)
            for t in b.r.values():
                self._wait(eng, t)

    def _commit(self, tok, reads, writes):
        for b in writes:
            b.w = tok
            b.r = {}
        for b in reads:
            if b not in writes:
                key = tok[1] if tok[0] == "e" else ("d", tok[1])
                b.r[key] = tok

    def op(self, eng, fn, reads=(), writes=(), inc=True):
        reads = self._bufs(reads)
        writes = self._bufs(writes)
        writes = writes + [b for b in reads if b.excl and b not in writes]
        self._deps(eng, reads, writes)
        if inc and self.cnt[eng] >= self.EPOCH and not (eng == "pe" and self.promised):
            self.ep[eng] += 1
            self.cnt[eng] = 0
            self._newsem(eng)
        ins = fn(self.e[eng])
        if eng == "pe":
            self.promised = not inc
        if inc:
            self.cnt[eng] += 1
            ins.then_inc(self.sems[(eng, self.ep[eng])], 1)
            tok = ("e", eng, self.ep[eng], self.cnt[eng])
        else:
            assert eng == "pe"
            tok = ("e", eng, self.ep[eng], self.cnt[eng] + 1)
        self._commit(tok, reads, writes)
        self.nins += 1

    def dma(self, eng, out, in_, reads=(), writes=(), **kw):
        reads = self._bufs(reads)
        writes = self._bufs(writes)
        self._deps(eng, reads, writes)
        s = self.dnext
        self.dnext = (self.dnext + 1) % self.NDMA
        i = self.dslot[s]
        if self.dval[i] >= 8000:
            self.dsem.append(self.gstack.enter_context(self.nc.semaphore(f"dq{len(self.dsem)}")))
            self.dval.append(0)
            prev = i
            i = len(self.dsem) - 1
            self.dslot[s] = i
            self._wait(eng, ("d", prev, self.dval[prev]))
        if self.dval[i] > 0:
            self._wait(eng, ("d", i, self.dval[i]))
        self.dval[i] += 16
        self.e[eng].dma_start(out=out, in_=in_, **kw).then_inc(self.dsem[i], 16)
        tok = ("d", i, self.dval[i])
        self._commit(tok, reads, writes)
        self.nins += 1

    def idma(self, out, in_, out_off=None, in_off=None, reads=(), writes=(), bounds=None):
        eng = "pool"
        reads = self._bufs(reads)
        writes = self._bufs(writes)
        self._deps(eng, reads, writes)
        s = self.dnext
        self.dnext = (self.dnext + 1) % self.NDMA
        i = self.dslot[s]
        if self.dval[i] >= 8000:
            self.dsem.append(self.gstack.enter_context(self.nc.semaphore(f"dq{len(self.dsem)}")))
            self.dval.append(0)
            prev = i
            i = len(self.dsem) - 1
            self.dslot[s] = i
            self._wait(eng, ("d", prev, self.dval[prev]))
        if self.dval[i] > 0:
            self._wait(eng, ("d", i, self.dval[i]))
        self.dval[i] += 16
        kw = {}
        if bounds is not None:
            if bounds not in self.bregs:
                r = self.nc.gpsimd.alloc_register(f"bnd{bounds}")
                self.nc.gpsimd.reg_mov(r, bounds)
                self.bregs[bounds] = r
            kw = dict(bounds_check=self.bregs[bounds], oob_is_err=False)
        self.e[eng].indirect_dma_start(
            out=out, out_offset=None if out_off is None else bass.IndirectOffsetOnAxis(ap=out_off, axis=0),
            in_=in_, in_offset=None if in_off is None else bass.IndirectOffsetOnAxis(ap=in_off, axis=0), **kw,
        ).then_inc(self.dsem[i], 16)
        tok = ("d", i, self.dval[i])
        self._commit(tok, reads, writes)
        self.nins += 1

    def finish(self):
        for i in range(len(self.dsem)):
            if self.dval[i] > 0:
                self._wait("sp", ("d", i, self.dval[i]))
        for k in self.ENG:
            if k != "sp" and (self.cnt[k] > 0 or self.ep[k] > 0):
                self._wait("sp", ("e", k, self.ep[k], self.cnt[k]))

    def barrier(self):
        self.finish()
        self.op("sp", lambda e: e.nop(), (), ())
        tok = ("e", "sp", self.ep["sp"], self.cnt["sp"])
        for k in ("pe", "act", "dve", "pool"):
            self._wait(k, tok)


def run_interleaved(gens, width):
    it = iter(gens)
    active = []
    exhausted = False
    while True:
        while len(active) < width and not exhausted:
            try:
                active.append(next(it))
            except StopIteration:
                exhausted = True
        if not active:
            break
        for g in list(active):
            try:
                next(g)
            except StopIteration:
                active.remove(g)


def host_consts():
    s = np.arange(128)[:, None]
    t = np.arange(128)[None, :]
    le = (s <= t).astype(np.float32)
    lt = (s < t).astype(np.float32)
    ge = (s >= t).astype(np.float32)
    gt = (s > t).astype(np.float32)
    tri = np.stack([le, lt, ge, gt], axis=1)
    mask4 = np.zeros((2, 128, 512), np.float32)
    maskt = np.zeros((2, 128, 128), np.float32)
    for d, (strict, incl) in enumerate(((lt, le), (gt, ge))):
        mask4[d, :, 0:128] = -strict
        mask4[d, :, 128:256] = -incl
        mask4[d, :, 256:384] = strict
        mask4[d, :, 384:512] = incl
        maskt[d] = -strict.T
    rows = 2048 // 64
    row = np.repeat(np.arange(rows, dtype=np.float32), 64)
    col = np.tile(np.arange(64, dtype=np.float32), rows)
    inv = (10000.0 ** (-np.arange(8, dtype=np.float32) / 8)).astype(np.float32)
    ang = np.concatenate([row[:, None] * inv, col[:, None] * inv], axis=-1).astype(np.float32)
    p_ = np.arange(128, dtype=np.float32)
    iotap = np.stack([p_, 256 * p_], axis=1)
    iotab = np.tile(np.arange(512, dtype=np.float32)[None, :], (128, 1))
    return dict(c_iotap=iotap, c_iotab=iotab, c_ident=np.eye(128, dtype=np.float32), c_tri=tri, c_mask4=mask4, c_maskt=maskt,
                c_cos=np.cos(ang).astype(np.float32), c_sin=np.sin(ang).astype(np.float32))


IN_SHAPES = dict(
    x=[2, 2048, 1024], ctx=[2, 256, 1024], cT=[1024, 3],
    ada_w=[1024, 6144], ada_b=[1, 6144], norm_mix=[1, 1024], norm_ffn=[1, 1024],
    w_in=[1024, 4416], shift_conv=[3, 1952], q_lat_norm=[1, 256], w_uq=[256, 768],
    kv_lat_norm=[1, 128], w_ukv=[128, 1024], q_norm=[1, 96], k_norm=[1, 96], w_o_mla=[512, 1024],
    decay_w0=[2, 512], decay_w2=[2, 64, 512], aicl_a0=[2, 512], aicl_a2=[2, 64, 512],
    k_k=[1, 512], k_a=[1, 512], r_k=[1, 512], gn_w=[1, 512], gn_b=[1, 512], gate_g2=[160, 512],
    w_o_rwkv=[512, 1024], w_out=[1024, 1024], router_w=[1024, 256], router_bias=[1, 256],
    expert_w1=[256, 1024, 256], expert_w3=[256, 1024, 256], expert_w2=[256, 256, 1024],
    shared_w1=[1024, 256], shared_w3=[1024, 256], shared_w2=[256, 1024],
    c_ident=[128, 128], c_tri=[128, 4, 128], c_mask4=[2, 128, 512], c_maskt=[2, 128, 128],
    c_cos=[2048, 16], c_sin=[2048, 16], c_iotap=[128, 2], c_iotab=[128, 512],
)

CT0 = 1
LT0 = 259
HW = 2308


def build(upto=99, dbg=(), n_exp=256, nbatch=2, rw_tiles=99, rw_phase=9, rw_dirs=2, blk_limit=10 ** 9):
    nc = bass.Bass("TRN2", target_bir_lowering=False)
    I = {k: nc.dram_tensor(k, list(v), F32, kind="ExternalInput") for k, v in IN_SHAPES.items()
         if not (upto < 6 and k.startswith(("expert_", "shared_")))}
    out_d = nc.dram_tensor("out", [2, 2048, 1024], F32, kind="ExternalOutput")

    with ExitStack() as gst:
        kb = KB(nc, gst)
        dbgk = lambda n: ("ExternalOutput" if n in dbg else "Internal")
        ps1 = Rot([kb.ps(f"ps1_{i}", [128, 512]) for i in range(4)])
        ps2 = Rot([kb.ps(f"ps2_{i}", [128, 1024]) for i in range(2)])

        mod_d = kb.dram("mod_d", [3, 6144], F32, dbgk("mod_d"))
        mla_d = kb.dram("mla_d", [2304, 416], F32, dbgk("mla_d"))
        mla_r = [Buf() for _ in range(18)]
        rkv_d = kb.dram("rkv_d", [2304, 1536], F32, dbgk("rkv_d"))
        rkv_r = [Buf() for _ in range(18)]
        gates_d = kb.dram("gates_d", [2048, 2048], BF16, dbgk("gates_d"))
        gates_r = [[Buf() for _ in range(4)] for _ in range(16)]
        x1_d = kb.dram("x1_d", [4096, 1024], F32, dbgk("x1_d"))
        x1_r = [Buf() for _ in range(32)]
        dbg_d = {}
        for n, shp, dt in (("fm_dbg", [128, 4, 2304], BF16), ("att_dbg", [128, 4, 2048], BF16),
                           ("y_dbg", [128, 16, 512], F32), ("rw_dbg", [128, 4, 2048], BF16),
                           ("G_dbg", [128, 16, 257], F32), ("qT_dbg", [96, 8, 2048], BF16),
                           ("kT_dbg", [96, 8, 2304], BF16), ("moe_dbg", [128, 16, 1024], F32)):
            if n in dbg:
                dbg_d[n] = kb.dram(n, shp, dt, "ExternalOutput")

        ident_f = kb.sb("ident_f", [128, 128], F32)
        ident_b = kb.sb("ident_b", [128, 128], BF16)
        ones_b = kb.sb("ones_b", [128, 128], BF16)
        kb.dma("sp", ident_f[:], I["c_ident"].ap(), writes=[ident_f])
        kb.op("dve", lambda e: e.tensor_copy(out=ident_b[:], in_=ident_f[:]), [ident_f], [ident_b])
        kb.op("pool", lambda e: e.memset(ones_b[:], 1.0), [], [ones_b])

        def bcast_load(dst_ap, dst_tile, src_ap, nparts, reads=()):
            kb.dma("sp", dst_ap, src_ap.to_broadcast([nparts, src_ap.shape[-1]]), reads=reads, writes=[dst_tile])

        def transposes(src_tile, src_aps, dst_ap, dst_tile, dt=BF16, eng="act", width=128):
            pp = ps1.get()
            pv = pp[:].bitcast(BF16) if dt == BF16 else pp[:]
            idn = ident_b if dt == BF16 else ident_f
            n = len(src_aps)
            P = src_aps[0].shape[0]
            w = src_aps[0].shape[1]
            for j, a in enumerate(src_aps):
                kb.op("pe", lambda e: e.transpose(out=pv[0:w, j * width:j * width + P], in_=a, identity=idn[0:P, 0:P]),
                      [src_tile, idn], [pp])
            src = pv[0:w, 0:n * width]
            if eng == "act":
                kb.op("act", lambda e: e.copy(out=dst_ap, in_=src.rearrange("p (j t) -> p j t", j=n) if len(dst_ap.shape) == 3 else src), [pp], [dst_tile])
            else:
                kb.op(eng, lambda e: e.tensor_copy(out=dst_ap, in_=src.rearrange("p (j t) -> p j t", j=n) if len(dst_ap.shape) == 3 else src), [pp], [dst_tile])

        def rms_rstd(ssq_ap, out_ap, tile_in, tile_out, n, eps):
            kb.op("act", lambda e: e.activation(out=out_ap, in_=ssq_ap, func=AF.Sqrt, bias=eps_t[:, 0:1] if eps == EPS else (gneps_t[:, 0:1] if eps == GN_EPS else tiny_t[:, 0:1]), scale=1.0 / n),
                  [tile_in], [tile_out])
            kb.op("dve", lambda e: e.reciprocal(out=out_ap, in_=out_ap), [tile_out], [tile_out])

        eps_t = kb.sb("eps_t", [128, 1], F32)
        gneps_t = kb.sb("gneps_t", [128, 1], F32)
        tiny_t = kb.sb("tiny_t", [128, 1], F32)
        kb.op("pool", lambda e: e.memset(eps_t[:], EPS), [], [eps_t])
        kb.op("pool", lambda e: e.memset(gneps_t[:], GN_EPS), [], [gneps_t])
        kb.op("pool", lambda e: e.memset(tiny_t[:], 1e-12), [], [tiny_t])

        with kb.scope():
            cTs = kb.sb("cTs", [128, 8, 3], F32)
            kb.dma("sp", cTs[:], I["cT"].ap().rearrange("(k p) r -> p k r", p=128), writes=[cTs])
            sT = kb.sb("sT", [128, 8, 3], F32)
            kb.op("act", lambda e: e.activation(out=sT[:], in_=cTs[:], func=AF.Silu), [cTs], [sT])
            modsb = kb.sb("modsb", [3, 6144], F32)
            adab = kb.sb("adab", [3, 6144], F32)
            bcast_load(adab[:], adab, I["ada_b"].ap()[0:1, :], 3)
            nrm = kb.sb("nrm", [3, 2, 1024], F32)
            bcast_load(nrm[:, 0, :], nrm, I["norm_mix"].ap()[0:1, :], 3)
            bcast_load(nrm[:, 1, :], nrm, I["norm_ffn"].ap()[0:1, :], 3)
            wrot = kb.rot("adaw", [128, 8, 512], F32, 2)
            for cb in range(12):
                wt = wrot.get()
                kb.dma("sp", wt[:], I["ada_w"].ap()[:, cb * 512:(cb + 1) * 512].rearrange("(k p) n -> p k n", p=128), writes=[wt])
                pp = ps1.get()
                for k in range(8):
                    kb.op("pe", lambda e: e.matmul(pp[0:3, :], lhsT=sT[:, k, :], rhs=wt[:, k, :], start=(k == 0), stop=(k == 7)),
                          [sT, wt], [pp], inc=(k == 7))
                kb.op("dve", lambda e: e.tensor_tensor(out=modsb[:, cb * 512:(cb + 1) * 512], in0=pp[0:3, :],
                                                       in1=adab[:, cb * 512:(cb + 1) * 512], op=ALU.add), [pp, adab], [modsb])
            for (c0, j) in ((1024, 0), (4096, 1)):
                kb.op("dve", lambda e: e.scalar_tensor_tensor(out=modsb[:, c0:c0 + 1024], in0=modsb[:, c0:c0 + 1024], scalar=1.0,
                                                              in1=nrm[:, j, :], op0=ALU.add, op1=ALU.mult), [modsb, nrm], [modsb])
            kb.dma("sp", mod_d[:, :], modsb[:], reads=[modsb], writes=[mod_d])
        if upto <= 0:
            kb.finish()
            return nc

        def norm_mod(xt, A, S, hb):
            junk = junk_rot.get()
            ssq = small_rot.get()
            kb.op("act", lambda e: e.activation(out=junk[:], in_=xt[:], func=AF.Square, accum_out=ssq[:, 0:1]), [xt], [junk, ssq])
            rms_rstd(ssq[:, 0:1], ssq[:, 1:2], ssq, ssq, 1024, EPS)
            kb.op("dve", lambda e: e.scalar_tensor_tensor(out=junk[:], in0=xt[:], scalar=ssq[:, 1:2], in1=A[:], op0=ALU.mult, op1=ALU.mult),
                  [xt, ssq, A], [junk])
            kb.op("pool", lambda e: e.tensor_tensor(out=hb[:], in0=junk[:], in1=S[:], op=ALU.add), [junk, S], [hb])

        for b in range(nbatch):
          with kb.scope():
            junk_rot = kb.rot("junk", [128, 1024], F32, 2)
            small_rot = kb.rot("small", [128, 16], F32, 6)
            mix_stack = ExitStack()
            fm_stack = ExitStack()
            _old = kb.stack
            kb.stack = mix_stack
            attT = kb.sb("attT", [128, 4, 2048], BF16)
            rwT = kb.sb("rwT", [128, 4, 2048], BF16)
            kb.stack = fm_stack
            fm = kb.sb("fm", [128, 4, 2304], BF16)
            kb.stack = _old

            with kb.scope():
                hT = kb.sb("hT", [128, 8, HW], BF16)
                for (c0, c1) in ((0, 1), (257, 259), (2307, 2308)):
                    kb.op("pool", lambda e: e.memset(hT[:, :, c0:c1], 0.0), [], [hT])
                with kb.scope():
                  xrot = kb.rot("xt", [128, 1024], F32, 2)
                  hbrot = kb.rot("hb", [128, 1024], BF16, 2)
                  for seg, (src, nt, off, row) in enumerate(((I["ctx"], 2, CT0, 2), (I["x"], 16, LT0, b))):
                    A1 = kb.sb("A1", [128, 1024], F32)
                    S1 = kb.sb("S1", [128, 1024], F32)
                    bcast_load(A1[:], A1, mod_d.t.ap()[row:row + 1, 1024:2048], 128, reads=[mod_d])
                    bcast_load(S1[:], S1, mod_d.t.ap()[row:row + 1, 0:1024], 128, reads=[mod_d])
                    for t in range(nt):
                        xt = xrot.get()
                        kb.dma("sp", xt[:], src.ap()[b, t * 128:(t + 1) * 128, :], writes=[xt])
                        hb = hbrot.get()
                        norm_mod(xt, A1, S1, hb)
                        transposes(hb, [hb[:, k * 128:(k + 1) * 128] for k in range(8)],
                                   hT[:, :, off + t * 128: off + (t + 1) * 128], hT)
                conv = kb.sb("conv", [128, 3, 1952], F32)
                for j in range(3):
                    bcast_load(conv[:, j, :], conv, I["shift_conv"].ap()[j:j + 1, :], 128)
                wf = kb.sb("wf", [128, 8, 512], F32)
                wbs = [kb.rot(f"wb{j}", [128, 8, 512], BF16, 2) for j in range(3)]
                osb = kb.rot("osb", [128, 512], F32, 2)
                gsb = kb.rot("gsb", [128, 512], BF16, 2)
                tok_tiles = [(CT0 + t * 128, t) for t in range(2)] + [(LT0 + t * 128, 2 + t) for t in range(16)]
                blocks = [(0, 416, "M")] + [(416 + i * 512, 512, "R") for i in range(3)] + [(1952, 416, "F")] + \
                         [(2368 + i * 512, 512, "G") for i in range(4)]
                for (c0, ncol, kind) in blocks:
                    kb.dma("sp", wf[:, :, 0:ncol], I["w_in"].ap()[:, c0:c0 + ncol].rearrange("(k p) n -> p k n", p=128), writes=[wf])
                    has_conv = kind in ("R", "F")
                    ws = []
                    for j in (range(3) if has_conv else range(1)):
                        wb = wbs[j].get()
                        if has_conv:
                            cc = c0 - 416
                            for k in range(8):
                                kb.op("pool" if k % 2 else "dve", lambda e: e.tensor_tensor(out=wb[:, k, 0:ncol], in0=wf[:, k, 0:ncol],
                                                                                           in1=conv[:, j, cc:cc + ncol], op=ALU.mult), [wf, conv], [wb])
                        else:
                            kb.op("pool", lambda e: e.tensor_copy(out=wb[:, :, 0:ncol], in_=wf[:, :, 0:ncol]), [wf], [wb])
                        ws.append(wb)
                    shifts = [(-1, 0), (0, 1), (1, 2)] if has_conv else [(0, 0)]
                    if kind in ("M", "R"):
                        for (off, ti) in tok_tiles:
                            pp = ps1.get()
                            n = len(shifts) * 8
                            i = 0
                            for (sh, j) in shifts:
                                for k in range(8):
                                    kb.op("pe", lambda e: e.matmul(pp[:, 0:ncol], lhsT=hT[:, k, off + sh:off + sh + 128], rhs=ws[j][:, k, 0:ncol],
                                                                   start=(i == 0), stop=(i == n - 1)), [hT, ws[j]], [pp], inc=(i == n - 1))
                                    i += 1
                            o = osb.get()
                            kb.op("act", lambda e: e.copy(out=o[:, 0:ncol], in_=pp[:, 0:ncol]), [pp], [o])
                            if kind == "M":
                                kb.dma("sp", mla_d[ti * 128:(ti + 1) * 128, :], o[:, 0:416], reads=[o], writes=[mla_r[ti]])
                            else:
                                cc = c0 - 416
                                kb.dma("sp", rkv_d[ti * 128:(ti + 1) * 128, cc:cc + 512], o[:, :], reads=[o], writes=[rkv_r[ti]])
                    elif kind == "F":
                        nblks = [(CT0, 0, 256)] + [(LT0 + i * 512, 256 + i * 512, 512) for i in range(4)]
                        for m, (m0, msz) in enumerate(((0, 128), (128, 128), (256, 128), (384, 32))):
                            func = (AF.Tanh, AF.Copy, AF.Sigmoid, AF.Sigmoid)[m]
                            for (off, dcol, nn) in nblks:
                                pp = ps1.get()
                                i = 0
                                for (sh, j) in shifts:
                                    for k in range(8):
                                        kb.op("pe", lambda e: e.matmul(pp[0:msz, 0:nn], lhsT=ws[j][:, k, m0:m0 + msz], rhs=hT[:, k, off + sh:off + sh + nn],
                                                                       start=(i == 0), stop=(i == 23)), [hT, ws[j]], [pp], inc=(i == 23))
                                        i += 1
                                kb.op("act", lambda e: e.activation(out=fm[0:msz, m, dcol:dcol + nn], in_=pp[0:msz, 0:nn], func=func), [pp], [fm])
                    else:
                        g0 = c0 - 2368
                        for m in range(4):
                            mt = g0 // 128 + m
                            for nb_ in range(4):
                                pp = ps1.get()
                                for k in range(8):
                                    kb.op("pe", lambda e: e.matmul(pp[:, :], lhsT=ws[0][:, k, m * 128:(m + 1) * 128],
                                                                   rhs=hT[:, k, LT0 + nb_ * 512:LT0 + (nb_ + 1) * 512], start=(k == 0), stop=(k == 7)),
                                          [hT, ws[0]], [pp], inc=(k == 7))
                                g = gsb.get()
                                kb.op("act", lambda e: e.activation(out=g[:], in_=pp[:, :], func=AF.Sigmoid), [pp], [g])
                                kb.dma("sp", gates_d[mt * 128:(mt + 1) * 128, nb_ * 512:(nb_ + 1) * 512], g[:], reads=[g], writes=[gates_r[mt][nb_]])
                if "fm_dbg" in dbg and b == 0:
                    kb.dma("sp", dbg_d["fm_dbg"][:, :, :], fm[:], reads=[fm], writes=[dbg_d["fm_dbg"]])
            if upto <= 2:
                kb.barrier(); fm_stack.close(); mix_stack.close()
                continue

            with kb.scope():
                Ybuf = kb.sb("Ybuf", [128, 16, 512], F32)
                halves = []
                for t2 in ps2.tiles:
                    for c0_ in (0, 512):
                        ht = Tile(t2.t[:, c0_:c0_ + 512], f"{t2.b.name}_h{c0_}")
                        ht.b.excl = True
                        halves.append(ht)
                pY_bank = halves[0]
                psR = Rot(ps1.tiles + halves[1:])
                bonus = kb.sb("bonus", [128, 16, 2, 8], F32)
                tri_f = kb.sb("tri_f", [128, 4, 128], F32)
                tri = kb.sb("tri", [128, 4, 128], BF16)
                kb.dma("sp", tri_f[:], I["c_tri"].ap(), writes=[tri_f])
                kb.op("dve", lambda e: e.tensor_copy(out=tri[:], in_=tri_f[:]), [tri_f], [tri])
                mask4 = kb.sb("mask4", [128, 2, 512], F32)
                maskt = kb.sb("maskt", [128, 2, 128], F32)
                for d in range(2):
                    kb.dma("sp", mask4[:, d, :], I["c_mask4"].ap()[d], writes=[mask4])
                    kb.dma("sp", maskt[:, d, :], I["c_maskt"].ap()[d], writes=[maskt])
                prm = kb.sb("prm", [128, 3, 512], F32)
                bcast_load(prm[:, 0, :], prm, I["k_k"].ap()[0:1, :], 128)
                bcast_load(prm[:, 1, :], prm, I["k_a"].ap()[0:1, :], 128)
                bcast_load(prm[:, 2, :], prm, I["r_k"].ap()[0:1, :], 128)
                w2f = kb.sb("w2f", [128, 2, 512], F32)
                w2b = kb.sb("w2b", [128, 2, 512], BF16)
                kb.dma("sp", w2f[:, 0, :], I["decay_w2"].ap().rearrange("d l c -> (d l) c"), writes=[w2f])
                kb.dma("sp", w2f[:, 1, :], I["aicl_a2"].ap().rearrange("d l c -> (d l) c"), writes=[w2f])
                kb.op("dve", lambda e: e.tensor_copy(out=w2b[:], in_=w2f[:]), [w2f], [w2b])
                b0f = kb.sb("b0f", [128, 2, 512], F32)
                b0b = kb.sb("b0b", [128, 2, 512], BF16)
                kb.op("pool", lambda e: e.memset(b0f[:], 0.0), [], [b0f])
                for d_ in range(2):
                    kb.dma("sp", b0f[d_ * 64:d_ * 64 + 1, 0, :], I["decay_w0"].ap()[d_:d_ + 1, :], writes=[b0f])
                    kb.dma("sp", b0f[d_ * 64:d_ * 64 + 1, 1, :], I["aicl_a0"].ap()[d_:d_ + 1, :], writes=[b0f])
                kb.op("dve", lambda e: e.tensor_copy(out=b0b[:], in_=b0f[:]), [b0f], [b0b])
                Hf = [kb.sb(f"Hf{p}", [128, 64], F32) for p in range(4)]
                Hb = [kb.sb(f"Hb{p}", [128, 64], BF16) for p in range(4)]
                rkv_rot = kb.rot("rkvt", [128, 1536], F32, 2)
                f512 = kb.rot("f512", [128, 512], F32, 9)
                b512 = kb.rot("b512", [128, 512], BF16, 14)
                fmT = kb.rot("fmT", [128, 4, 4, 128], BF16, 2)
                A4rot = kb.rot("A4", [128, 512], BF16, 8)
                XPa = kb.rot("XPa", [128, 256], BF16, 12)
                Xta = kb.rot("Xta", [128, 128], BF16, 12)
                Wsb = kb.rot("Wsb", [128, 64], BF16, 10)
                Usb = kb.rot("Usb", [128, 512], BF16, 2)
                Vb = kb.rot("Vb", [128, 512], BF16, 2)
                ptot_rot = kb.rot("ptot", [128, 4], F32, 2)

                for d in range(rw_dirs):
                    for p in range(4):
                        kb.op("pool", lambda e: e.memset(Hf[p][:], 0.0), [], [Hf[p]])
                        kb.op("pool", lambda e: e.memset(Hb[p][:], 0.0), [], [Hb[p]])
                    order = list(range(18)) if d == 0 else [1, 0] + list(range(17, 1, -1))
                    for ti in order[:rw_tiles]:
                        lat = ti >= 2
                        rt = rkv_rot.get()
                        kb.dma("sp", rt[:], rkv_d[ti * 128:(ti + 1) * 128, :], reads=[rkv_r[ti]], writes=[rt])
                        r_ = rt[:, 0:512]
                        k_ = rt[:, 512:1024]
                        v_ = rt[:, 1024:1536]
                        kkr = f512.get(); sq = f512.get(); st = small_rot.get()
                        kb.op("dve", lambda e: e.tensor_tensor(out=kkr[:], in0=k_, in1=prm[:, 0, :], op=ALU.mult), [rt, prm], [kkr])
                        kb.op("act", lambda e: e.activation(out=sq[:], in_=kkr[:], func=AF.Square), [kkr], [sq])
                        kb.op("dve", lambda e: e.tensor_reduce(out=st[:, 0:8], in_=sq[:].rearrange("p (h c) -> p h c", h=8), axis=AX.X, op=ALU.add), [sq], [st])
                        rms_rstd(st[:, 0:8], st[:, 8:16], st, st, 1.0, 1e-12)
                        kap = f512.get()
                        kb.op("dve", lambda e: e.tensor_tensor(out=kap[:].rearrange("p (h c) -> p h c", h=8), in0=kkr[:].rearrange("p (h c) -> p h c", h=8),
                                                               in1=st[:, 8:16].unsqueeze(2).to_broadcast([128, 8, 64]), op=ALU.mult), [kkr, st], [kap])
                        tcol = (ti * 128) if ti < 2 else (256 + (ti - 2) * 128)
                        pz = psR.get(); pa = psR.get()
                        kb.op("pe", lambda e: e.matmul(pz[:, :], lhsT=fm[d * 64:(d + 1) * 64, 0, tcol:tcol + 128], rhs=w2b[d * 64:(d + 1) * 64, 0, :], start=True, stop=False),
                              [fm, w2b], [pz], inc=False)
                        kb.op("pe", lambda e: e.matmul(pz[:, :], lhsT=ones_b[d * 64:d * 64 + 1, 0:128], rhs=b0b[d * 64:d * 64 + 1, 0, :], start=False, stop=True), [ones_b, b0b], [pz])
                        kb.op("pe", lambda e: e.matmul(pa[:, :], lhsT=fm[d * 64:(d + 1) * 64, 1, tcol:tcol + 128], rhs=w2b[d * 64:(d + 1) * 64, 1, :], start=True, stop=False),
                              [fm, w2b], [pa], inc=False)
                        kb.op("pe", lambda e: e.matmul(pa[:, :], lhsT=ones_b[d * 64:d * 64 + 1, 0:128], rhs=b0b[d * 64:d * 64 + 1, 1, :], start=False, stop=True), [ones_b, b0b], [pa])
                        sigb = b512.get()
                        kb.op("act", lambda e: e.activation(out=sigb[:], in_=pz[:, :], func=AF.Sigmoid), [pz], [sigb])
                        a_ = f512.get()
                        kb.op("act", lambda e: e.activation(out=a_[:], in_=pa[:, :], func=AF.Sigmoid), [pa], [a_])
                        t1 = f512.get(); ktl = f512.get(); beta = f512.get()
                        kb.op("dve", lambda e: e.scalar_tensor_tensor(out=t1[:], in0=a_[:], scalar=-1.0, in1=prm[:, 1, :], op0=ALU.add, op1=ALU.mult), [a_, prm], [t1])
                        kb.op("dve", lambda e: e.scalar_tensor_tensor(out=ktl[:], in0=t1[:], scalar=1.0, in1=k_, op0=ALU.add, op1=ALU.mult), [t1, rt], [ktl])
                        kb.op("pool", lambda e: e.tensor_tensor(out=beta[:], in0=a_[:], in1=kap[:], op=ALU.mult), [a_, kap], [beta])
                        if lat:
                            kb.op("pool", lambda e: e.tensor_tensor(out=t1[:], in0=r_, in1=prm[:, 2, :], op=ALU.mult), [rt, prm], [t1])
                            kb.op("pool", lambda e: e.tensor_tensor(out=t1[:], in0=t1[:], in1=ktl[:], op=ALU.mult), [t1, ktl], [t1])
                            kb.op("dve", lambda e: e.tensor_reduce(out=bonus[:, ti - 2, d, :], in_=t1[:].rearrange("p (h c) -> p h c", h=8), axis=AX.X, op=ALU.add), [t1], [bonus])
                        if rw_phase < 1:
                            continue
                        ci, ce, cr = ((0, 1, 3) if d == 0 else (2, 3, 1))
                        pI = psR.get(); pE = psR.get(); pR = psR.get()
                        kb.op("pe", lambda e: e.matmul(pI[:, :], lhsT=tri[:, ci, :], rhs=sigb[:], start=True, stop=True), [tri, sigb], [pI])
                        kb.op("pe", lambda e: e.matmul(pE[:, :], lhsT=tri[:, ce, :], rhs=sigb[:], start=True, stop=True), [tri, sigb], [pE])
                        kb.op("pe", lambda e: e.matmul(pR[:, :], lhsT=tri[:, cr, :], rhs=sigb[:], start=True, stop=True), [tri, sigb], [pR])
                        eI = f512.get(); eE = f512.get(); eN = f512.get(); eR = f512.get()
                        kb.op("act", lambda e: e.activation(out=eI[:], in_=pI[:, :], func=AF.Exp, scale=C_DEC), [pI], [eI])
                        kb.op("act", lambda e: e.activation(out=eN[:], in_=pI[:, :], func=AF.Exp, scale=-C_DEC), [pI], [eN])
                        kb.op("act", lambda e: e.activation(out=eE[:], in_=pE[:, :], func=AF.Exp, scale=C_DEC), [pE], [eE])
                        kb.op("act", lambda e: e.activation(out=eR[:], in_=pR[:, :], func=AF.Exp, scale=C_DEC), [pR], [eR])
                        pT = psR.get()
                        for p in range(4):
                            kb.op("pe", lambda e: e.matmul(pT[:, p:p + 1], lhsT=sigb[:, p * 128:(p + 1) * 128], rhs=ones_b[:, 0:1], start=True, stop=True), [sigb, ones_b], [pT])
                        ptot = ptot_rot.get()
                        kb.op("act", lambda e: e.activation(out=ptot[:], in_=pT[:, 0:4], func=AF.Exp, scale=C_DEC), [pT], [ptot])
                        Rh = b512.get(); Ka = b512.get(); Bh = b512.get(); Kh = b512.get(); NBb = b512.get(); Kbb = b512.get()
                        kb.op("dve", lambda e: e.tensor_tensor(out=Rh[:], in0=r_, in1=eI[:], op=ALU.mult), [rt, eI], [Rh])
                        kb.op("pool", lambda e: e.tensor_tensor(out=Ka[:], in0=kap[:], in1=eE[:], op=ALU.mult), [kap, eE], [Ka])
                        kb.op("dve", lambda e: e.tensor_tensor(out=Bh[:], in0=beta[:], in1=eN[:], op=ALU.mult), [beta, eN], [Bh])
                        kb.op("pool", lambda e: e.tensor_tensor(out=Kh[:], in0=ktl[:], in1=eN[:], op=ALU.mult), [ktl, eN], [Kh])
                        kb.op("dve", lambda e: e.scalar_tensor_tensor(out=NBb[:], in0=beta[:], scalar=-1.0, in1=eR[:], op0=ALU.mult, op1=ALU.mult), [beta, eR], [NBb])
                        kb.op("pool", lambda e: e.tensor_tensor(out=Kbb[:], in0=ktl[:], in1=eR[:], op=ALU.mult), [ktl, eR], [Kbb])
                        vb = Vb.get()
                        kb.op("pool", lambda e: e.tensor_copy(out=vb[:], in_=v_), [rt], [vb])
                        if rw_phase < 2:
                            continue
                        fT = fmT.get()
                        for wi, src in enumerate((Ka, Rh, Bh, Kh)):
                            transposes(src, [src[:, p * 128:(p + 1) * 128] for p in range(4)], fT[:, :, wi, :], fT, eng=("act" if wi % 2 else "dve"))
                        if rw_phase < 3:
                            continue
                        U = Usb.get()
                        pY = pY_bank if lat else None
                        HG = 8
                        for g0 in range(0, 8, HG):
                            hs = list(range(g0, g0 + HG))
                            P_ = {h: h // 2 for h in hs}
                            LO = {h: (h % 2) * 64 for h in hs}
                            HC = {h: slice(h * 64, (h + 1) * 64) for h in hs}
                            KaT = {h: fT[LO[h]:LO[h] + 64, P_[h], 0, :] for h in hs}
                            RT = {h: fT[LO[h]:LO[h] + 64, P_[h], 1, :] for h in hs}
                            BT = {h: fT[LO[h]:LO[h] + 64, P_[h], 2, :] for h in hs}
                            KT = {h: fT[LO[h]:LO[h] + 64, P_[h], 3, :] for h in hs}
                            KaRT = {h: fT[LO[h]:LO[h] + 64, P_[h], 0:2, :].rearrange("c w t -> c (w t)") for h in hs}
                            A4 = {}; Xt = {}; XP = {}; pp_ = {}; Wt = {}
                            NBK = len(psR.tiles)

                            def lockstep(pe_fn, ev_fn):
                                pend_ = []
                                for h_ in hs:
                                    if len(pend_) >= NBK:
                                        ev_fn(pend_.pop(0))
                                    pe_fn(h_)
                                    pend_.append(h_)
                                for h_ in pend_:
                                    ev_fn(h_)

                            def pe_A(h):
                                pA = psR.get(); pp_[h] = pA
                                kb.op("pe", lambda e: e.matmul(pA[:, 0:256], lhsT=BT[h], rhs=KaRT[h], start=True, stop=True), [fT], [pA], inc=False)
                                kb.op("pe", lambda e: e.matmul(pA[:, 256:512], lhsT=KT[h], rhs=KaRT[h], start=True, stop=True), [fT], [pA])

                            def ev_A(h):
                                A4[h] = A4rot.get()
                                kb.op("dve", lambda e: e.tensor_tensor(out=A4[h][:], in0=pp_[h][:, :], in1=mask4[:, d, :], op=ALU.mult), [pp_[h], mask4], [A4[h]])
                            lockstep(pe_A, ev_A)

                            def pe_L(h):
                                pL = psR.get(); pp_[h] = pL
                                kb.op("pe", lambda e: e.matmul(pL[:, 0:128], lhsT=KaT[h], rhs=BT[h], start=True, stop=True), [fT], [pL])

                            def ev_L(h):
                                Xt[h] = Xta.get()
                                kb.op("dve", lambda e: e.tensor_tensor(out=Xt[h][:], in0=pp_[h][:, 0:128], in1=maskt[:, d, :], op=ALU.mult), [pp_[h], maskt], [Xt[h]])
                            lockstep(pe_L, ev_L)
                            for h in hs:
                                XP[h] = XPa.get()
                                kb.op("pool", lambda e: e.tensor_copy(out=XP[h][:, 0:128], in_=A4[h][:, 0:128]), [A4[h]], [XP[h]])
                                kb.op("pool", lambda e: e.tensor_tensor(out=XP[h][:, 128:256], in0=A4[h][:, 0:128], in1=ident_b[:], op=ALU.add), [A4[h], ident_b], [XP[h]])

                            def pe_B0(h):
                                pB = psR.get(); pp_[h] = pB
                                kb.op("pe", lambda e: e.matmul(pB[:, 0:128], lhsT=Xt[h][:], rhs=XP[h][:, 0:128], start=True, stop=True), [Xt[h], XP[h]], [pB], inc=False)
                                kb.op("pe", lambda e: e.matmul(pB[:, 256:384], lhsT=XP[h][:, 0:128], rhs=Xt[h][:], start=True, stop=True), [Xt[h], XP[h]], [pB])

                            def ev_B0(h):
                                XP2 = XPa.get(); Xt2 = Xta.get(); pB = pp_[h]
                                kb.op("act", lambda e: e.copy(out=XP2[:, 0:128], in_=pB[:, 0:128]), [pB], [XP2])
                                kb.op("dve", lambda e: e.tensor_copy(out=Xt2[:], in_=pB[:, 256:384]), [pB], [Xt2])
                                kb.op("pool", lambda e: e.tensor_copy(out=XP2[:, 128:256], in_=XP[h][:, 128:256]), [XP[h]], [XP2])
                                XP[h], Xt[h] = XP2, Xt2
                            lockstep(pe_B0, ev_B0)
                            for j in range(1, 7):
                                last = (j == 6)

                                def pe_Bj(h):
                                    pB = psR.get(); pp_[h] = pB
                                    if not last:
                                        kb.op("pe", lambda e: e.matmul(pB[:, 0:256], lhsT=Xt[h][:], rhs=XP[h][:, 0:256], start=True, stop=True), [Xt[h], XP[h]], [pB], inc=False)
                                        kb.op("pe", lambda e: e.matmul(pB[:, 256:384], lhsT=XP[h][:, 0:128], rhs=Xt[h][:], start=True, stop=True), [Xt[h], XP[h]], [pB])
                                    else:
                                        kb.op("pe", lambda e: e.matmul(pB[:, 128:256], lhsT=Xt[h][:], rhs=XP[h][:, 128:256], start=True, stop=True), [Xt[h], XP[h]], [pB])

                                def ev_Bj(h):
                                    pB = pp_[h]
                                    XP2 = XPa.get()
                                    kb.op("dve", lambda e: e.tensor_tensor(out=XP2[:, 128:256], in0=pB[:, 128:256], in1=XP[h][:, 128:256], op=ALU.add), [pB, XP[h]], [XP2])
                                    if not last:
                                        Xt2 = Xta.get()
                                        kb.op("act", lambda e: e.copy(out=XP2[:, 0:128], in_=pB[:, 0:128]), [pB], [XP2])
                                        kb.op("act", lambda e: e.copy(out=Xt2[:], in_=pB[:, 256:384]), [pB], [Xt2])
                                        Xt[h] = Xt2
                                    XP[h] = XP2
                                lockstep(pe_Bj, ev_Bj)

                            def pe_W(h):
                                pW = psR.get(); pp_[h] = pW
                                kb.op("pe", lambda e: e.matmul(pW[:, 0:64], lhsT=KaT[h], rhs=Hb[P_[h]][LO[h]:LO[h] + 64, :], start=True, stop=False), [fT, Hb[P_[h]]], [pW], inc=False)
                                kb.op("pe", lambda e: e.matmul(pW[:, 0:64], lhsT=A4[h][:, 256:384], rhs=vb[:, HC[h]], start=False, stop=True), [A4[h], vb], [pW])

                            def ev_W(h):
                                Wt[h] = Wsb.get()
                                kb.op("act", lambda e: e.copy(out=Wt[h][:], in_=pp_[h][:, 0:64]), [pp_[h]], [Wt[h]])
                            lockstep(pe_W, ev_W)

                            def pe_U(h):
                                pU = psR.get(); pp_[h] = pU
                                kb.op("pe", lambda e: e.matmul(pU[:, 0:64], lhsT=XP[h][:, 128:256], rhs=Wt[h][:], start=True, stop=True), [XP[h], Wt[h]], [pU])

                            def ev_U(h):
                                kb.op("dve" if h % 2 else "act", (lambda e: e.tensor_copy(out=U[:, HC[h]], in_=pp_[h][:, 0:64])) if h % 2 else
                                      (lambda e: e.copy(out=U[:, HC[h]], in_=pp_[h][:, 0:64])), [pp_[h]], [U])
                            lockstep(pe_U, ev_U)
                            if lat:
                                for h in hs:
                                    kb.op("pe", lambda e: e.matmul(pY[:, HC[h]], lhsT=RT[h], rhs=Hb[P_[h]][LO[h]:LO[h] + 64, :], start=True, stop=False), [fT, Hb[P_[h]]], [pY], inc=False)
                                    kb.op("pe", lambda e: e.matmul(pY[:, HC[h]], lhsT=A4[h][:, 128:256], rhs=U[:, HC[h]], start=False, stop=False), [A4[h], U], [pY], inc=False)
                                    kb.op("pe", lambda e: e.matmul(pY[:, HC[h]], lhsT=A4[h][:, 384:512], rhs=vb[:, HC[h]], start=False, stop=True), [A4[h], vb], [pY])
                            pHs = {}
                            for p in sorted(set(P_.values())):
                                pc = slice(p * 128, (p + 1) * 128)
                                pH = psR.get(); pHs[p] = pH
                                kb.op("pe", lambda e: e.matmul(pH[:, 0:128], lhsT=NBb[:, pc], rhs=U[:, pc], start=True, stop=False), [NBb, U], [pH], inc=False)
                                kb.op("pe", lambda e: e.matmul(pH[:, 0:128], lhsT=Kbb[:, pc], rhs=vb[:, pc], start=False, stop=True), [Kbb, vb], [pH])
                            for p in sorted(set(P_.values())):
                                pH = pHs[p]
                                for hh in range(2):
                                    l2 = hh * 64
                                    kb.op("dve", lambda e: e.scalar_tensor_tensor(out=Hf[p][l2:l2 + 64, :], in0=Hf[p][l2:l2 + 64, :], scalar=ptot[l2:l2 + 64, p:p + 1],
                                                                                  in1=pH[l2:l2 + 64, l2:l2 + 64], op0=ALU.mult, op1=ALU.add), [Hf[p], ptot, pH], [Hf[p]])
                                kb.op("act", lambda e: e.copy(out=Hb[p][:], in_=Hf[p][:]), [Hf[p]], [Hb[p]])
                        if lat:
                            if d == 0:
                                kb.op("act", lambda e: e.copy(out=Ybuf[:, ti - 2, :], in_=pY[:, 0:512]), [pY], [Ybuf])
                            else:
                                kb.op("dve", lambda e: e.tensor_tensor(out=Ybuf[:, ti - 2, :], in0=pY[:, 0:512], in1=Ybuf[:, ti - 2, :], op=ALU.add), [pY, Ybuf], [Ybuf])
                if "y_dbg" in dbg and b == 0:
                    kb.dma("sp", dbg_d["y_dbg"][:, :, :], Ybuf[:], reads=[Ybuf], writes=[dbg_d["y_dbg"]])
                gnw = kb.sb("gnw", [128, 2, 512], F32)
                bcast_load(gnw[:, 0, :], gnw, I["gn_w"].ap()[0:1, :], 128)
                bcast_load(gnw[:, 1, :], gnw, I["gn_b"].ap()[0:1, :], 128)
                g2f = w2f
                g2b = kb.sb("g2b", [128, 2, 512], BF16)
                kb.dma("sp", g2f[:, 0, :], I["gate_g2"].ap()[0:128, :], reads=[], writes=[g2f])
                kb.dma("sp", g2f[0:32, 1, :], I["gate_g2"].ap()[128:160, :], writes=[g2f])
                kb.op("dve", lambda e: e.tensor_copy(out=g2b[:, 0, :], in_=g2f[:, 0, :]), [g2f], [g2b])
                kb.op("dve", lambda e: e.tensor_copy(out=g2b[0:32, 1, :], in_=g2f[0:32, 1, :]), [g2f], [g2b])
                rwb = kb.rot("rwb", [128, 512], BF16, 2)
                def tileRO(t):
                        ti = t + 2
                        rt = rkv_rot.get()
                        kb.dma("sp", rt[:], rkv_d[ti * 128:(ti + 1) * 128, :], reads=[rkv_r[ti]], writes=[rt])
                        v_ = rt[:, 1024:1536]
                        y3 = Ybuf[:, t, :].rearrange("p (h c) -> p h c", h=8)
                        st = small_rot.get(); st2 = small_rot.get()
                        cen = f512.get(); sq = f512.get(); yn = f512.get()
                        cen3 = cen[:].rearrange("p (h c) -> p h c", h=8)
                        kb.op("dve", lambda e: e.tensor_reduce(out=st[:, 0:8], in_=y3, axis=AX.X, op=ALU.add), [Ybuf], [st])
                        kb.op("dve", lambda e: e.tensor_scalar(out=st[:, 0:8], in0=st[:, 0:8], scalar1=-1.0 / 64, scalar2=None, op0=ALU.mult), [st], [st])
                        kb.op("dve", lambda e: e.tensor_tensor(out=cen3, in0=y3, in1=st[:, 0:8].unsqueeze(2).to_broadcast([128, 8, 64]), op=ALU.add), [Ybuf, st], [cen])
                        yield
                        kb.op("act", lambda e: e.activation(out=sq[:], in_=cen[:], func=AF.Square), [cen], [sq])
                        kb.op("dve", lambda e: e.tensor_reduce(out=st2[:, 0:8], in_=sq[:].rearrange("p (h c) -> p h c", h=8), axis=AX.X, op=ALU.add), [sq], [st2])
                        yield
                        rms_rstd(st2[:, 0:8], st2[:, 8:16], st2, st2, 64, GN_EPS)
                        kb.op("dve", lambda e: e.tensor_tensor(out=yn[:].rearrange("p (h c) -> p h c", h=8), in0=cen3, in1=st2[:, 8:16].unsqueeze(2).to_broadcast([128, 8, 64]), op=ALU.mult),
                              [cen, st2], [yn])
                        kb.op("pool", lambda e: e.tensor_tensor(out=yn[:], in0=yn[:], in1=gnw[:, 0, :], op=ALU.mult), [yn, gnw], [yn])
                        kb.op("pool", lambda e: e.tensor_tensor(out=yn[:], in0=yn[:], in1=gnw[:, 1, :], op=ALU.add), [yn, gnw], [yn])
                        yield
                        kb.op("dve", lambda e: e.tensor_tensor(out=st[:, 8:16], in0=bonus[:, t, 0, :], in1=bonus[:, t, 1, :], op=ALU.add), [bonus], [st])
                        kb.op("dve", lambda e: e.tensor_tensor(out=sq[:].rearrange("p (h c) -> p h c", h=8), in0=v_.rearrange("p (h c) -> p h c", h=8),
                                                               in1=st[:, 8:16].unsqueeze(2).to_broadcast([128, 8, 64]), op=ALU.mult), [rt, st], [sq])
                        kb.op("pool", lambda e: e.tensor_tensor(out=yn[:], in0=yn[:], in1=sq[:], op=ALU.add), [yn, sq], [yn])
                        yield
                        pg = ps1.get()
                        tcol = 256 + t * 128
                        kb.op("pe", lambda e: e.matmul(pg[:, :], lhsT=fm[:, 2, tcol:tcol + 128], rhs=g2b[:, 0, :], start=True, stop=False), [fm, g2b], [pg], inc=False)
                        kb.op("pe", lambda e: e.matmul(pg[:, :], lhsT=fm[0:32, 3, tcol:tcol + 128], rhs=g2b[0:32, 1, :], start=False, stop=True), [fm, g2b], [pg])
                        ro = rwb.get()
                        kb.op("dve", lambda e: e.tensor_tensor(out=ro[:], in0=pg[:, :], in1=yn[:], op=ALU.mult), [pg, yn], [ro])
                        transposes(ro, [ro[:, k * 128:(k + 1) * 128] for k in range(4)], rwT[:, :, t * 128:(t + 1) * 128], rwT)
                run_interleaved((tileRO(t) for t in range(16)), 2)
                if "rw_dbg" in dbg and b == 0:
                    kb.dma("sp", dbg_d["rw_dbg"][:, :, :], rwT[:], reads=[rwT], writes=[dbg_d["rw_dbg"]])
            kb.barrier()
            fm_stack.close()
            if upto <= 3:
                mix_stack.close()
                continue

            with kb.scope():
                qT = kb.sb("qT", [96, 8, 2048], BF16)
                kT = kb.sb("kT", [96, 8, 2304], BF16)
                Vall = kb.sb("Vall", [128, 18, 8, 65], BF16)
                kb.op("pool", lambda e: e.memset(Vall[:, :, :, 64:65], 1.0), [], [Vall])
                qln = kb.sb("qln", [128, 256], F32)
                kvln = kb.sb("kvln", [128, 128], F32)
                qnw = kb.sb("qnw", [128, 8, 96], F32)
                knw = kb.sb("knw", [128, 8, 96], F32)
                bcast_load(qln[:], qln, I["q_lat_norm"].ap()[0:1, :], 128)
                bcast_load(kvln[:], kvln, I["kv_lat_norm"].ap()[0:1, :], 128)
                for h in range(8):
                    bcast_load(qnw[:, h, :], qnw, I["q_norm"].ap()[0:1, :], 128)
                    bcast_load(knw[:, h, :], knw, I["k_norm"].ap()[0:1, :], 128)
                kb.op("dve", lambda e: e.tensor_scalar(out=qnw[:], in0=qnw[:], scalar1=ATTN_SCALE, scalar2=None, op0=ALU.mult), [qnw], [qnw])
                wuq_f = kb.sb("wuq_f", [128, 2, 768], F32)
                wuq = kb.sb("wuq", [128, 2, 768], BF16)
                kb.dma("sp", wuq_f[:], I["w_uq"].ap().rearrange("(k p) n -> p k n", p=128), writes=[wuq_f])
                kb.op("pool", lambda e: e.tensor_copy(out=wuq[:], in_=wuq_f[:]), [wuq_f], [wuq])
                wukv_f = kb.sb("wukv_f", [128, 1024], F32)
                wukv = kb.sb("wukv", [128, 1024], BF16)
                kb.dma("sp", wukv_f[:], I["w_ukv"].ap(), writes=[wukv_f])
                kb.op("pool", lambda e: e.tensor_copy(out=wukv[:], in_=wukv_f[:]), [wukv_f], [wukv])
                mrot = kb.rot("mt", [128, 416], F32, 2)
                cs_rot = kb.rot("cs", [128, 2, 16], F32, 2)
                t768 = kb.rot("t768", [128, 8, 96], F32, 8)
                small_m = kb.rot("small_m", [128, 16], F32, 10)
                tb768 = kb.rot("tb768", [128, 8, 96], BF16, 4)
                tbn = kb.rot("tbn", [128, 256], BF16, 4)
                tTn = kb.rot("tTn", [128, 2, 128], BF16, 4)
                rope_t = kb.rot("rope_t", [128, 8, 16], F32, 16)

                def head_norm_rope(src, dst_b, gain, cs, n_extra_ssq=None):
                    sq = t768.get()
                    st = small_m.get()
                    kb.op("act", lambda e: e.activation(out=sq[:], in_=src[:], func=AF.Square), [src], [sq])
                    kb.op("dve", lambda e: e.tensor_reduce(out=st[:, 0:8], in_=sq[:], axis=AX.X, op=ALU.add), [sq], [st])
                    yield
                    rms_rstd(st[:, 0:8], st[:, 8:16], st, st, 96, EPS)
                    kb.op("dve", lambda e: e.tensor_tensor(out=sq[:], in0=src[:], in1=st[:, 8:16].unsqueeze(2).to_broadcast([128, 8, 96]), op=ALU.mult),
                          [src, st], [sq])
                    yield
                    if cs is None:
                        kb.op("pool", lambda e: e.tensor_tensor(out=dst_b[:], in0=sq[:], in1=gain[:], op=ALU.mult), [sq, gain], [dst_b])
                        return
                    kb.op("pool", lambda e: e.tensor_tensor(out=sq[:], in0=sq[:], in1=gain[:], op=ALU.mult), [sq, gain], [sq])
                    kb.op("pool", lambda e: e.tensor_copy(out=dst_b[:, :, 0:64], in_=sq[:, :, 0:64]), [sq], [dst_b])
                    cb_ = cs[:, 0, :].unsqueeze(1).to_broadcast([128, 8, 16])
                    sb_ = cs[:, 1, :].unsqueeze(1).to_broadcast([128, 8, 16])
                    x1 = sq[:, :, 64:80]
                    x2 = sq[:, :, 80:96]
                    ta = rope_t.get(); tb_ = rope_t.get(); tc_ = rope_t.get(); td = rope_t.get()
                    yield
                    kb.op("dve", lambda e: e.tensor_tensor(out=ta[:], in0=x1, in1=cb_, op=ALU.mult), [sq, cs], [ta])
                    kb.op("dve", lambda e: e.tensor_tensor(out=tb_[:], in0=x2, in1=sb_, op=ALU.mult), [sq, cs], [tb_])
                    kb.op("pool", lambda e: e.tensor_tensor(out=tc_[:], in0=x1, in1=sb_, op=ALU.mult), [sq, cs], [tc_])
                    kb.op("pool", lambda e: e.tensor_tensor(out=td[:], in0=x2, in1=cb_, op=ALU.mult), [sq, cs], [td])
                    kb.op("dve", lambda e: e.tensor_tensor(out=dst_b[:, :, 64:80], in0=ta[:], in1=tb_[:], op=ALU.subtract), [ta, tb_], [dst_b])
                    kb.op("pool", lambda e: e.tensor_tensor(out=dst_b[:, :, 80:96], in0=tc_[:], in1=td[:], op=ALU.add), [tc_, td], [dst_b])

                def mla_tile(ti):
                        lat = ti >= 2
                        mt_ = mrot.get()
                        kb.dma("sp", mt_[:], mla_d[ti * 128:(ti + 1) * 128, :], reads=[mla_r[ti]], writes=[mt_])
                        cs = None
                        if lat:
                            cs = cs_rot.get()
                            lt_ = ti - 2
                            kb.dma("sp", cs[:, 0, :], I["c_cos"].ap()[lt_ * 128:(lt_ + 1) * 128, :], writes=[cs])
                            kb.dma("sp", cs[:, 1, :], I["c_sin"].ap()[lt_ * 128:(lt_ + 1) * 128, :], writes=[cs])
                        junk = junk_rot.get()
                        st = small_m.get()
                        kb.op("act", lambda e: e.activation(out=junk[:, 0:128], in_=mt_[:, 256:384], func=AF.Square, accum_out=st[:, 0:1]), [mt_], [junk, st])
                        kb.op("act", lambda e: e.activation(out=junk[:, 128:160], in_=mt_[:, 384:416], func=AF.Square, accum_out=st[:, 2:3]), [mt_], [junk, st])
                        rms_rstd(st[:, 0:1], st[:, 1:2], st, st, 128, EPS)
                        kvn = tbn.get()
                        kb.op("dve", lambda e: e.scalar_tensor_tensor(out=kvn[:, 0:128], in0=mt_[:, 256:384], scalar=st[:, 1:2], in1=kvln[:], op0=ALU.mult, op1=ALU.mult),
                              [mt_, st, kvln], [kvn])
                        yield
                        kvT = tTn.get()
                        transposes(kvn, [kvn[:, 0:128]], kvT[:, 0, :], kvT)
                        pk = ps2.get()
                        for nb_ in range(2):
                            kb.op("pe", lambda e: e.matmul(pk[:, nb_ * 512:(nb_ + 1) * 512], lhsT=kvT[:, 0, :], rhs=wukv[:, nb_ * 512:(nb_ + 1) * 512], start=True, stop=True),
                                  [kvT, wukv], [pk])
                        pk3 = pk[:].rearrange("p (h c) -> p h c", h=8)
                        kb.op("act", lambda e: e.copy(out=Vall[:, ti, :, 0:64], in_=pk3[:, :, 64:128]), [pk], [Vall])
                        kf = t768.get()
                        kb.op("dve", lambda e: e.tensor_copy(out=kf[:, :, 0:64], in_=pk3[:, :, 0:64]), [pk], [kf])
                        kb.op("pool", lambda e: e.tensor_copy(out=kf[:, :, 64:96], in_=mt_[:, 384:416].unsqueeze(1).to_broadcast([128, 8, 32])), [mt_], [kf])
                        yield
                        kbf = tb768.get()
                        yield from head_norm_rope(kf, kbf, knw, cs)
                        yield
                        transposes(kbf, [kbf[:, h, :] for h in range(8)], kT[:, :, ti * 128:(ti + 1) * 128], kT, eng="dve")
                        yield
                        if lat:
                            st2 = small_m.get()
                            kb.op("act", lambda e: e.activation(out=junk[:, 256:512], in_=mt_[:, 0:256], func=AF.Square, accum_out=st2[:, 0:1]), [mt_], [junk, st2])
                            rms_rstd(st2[:, 0:1], st2[:, 1:2], st2, st2, 256, EPS)
                            qn = tbn.get()
                            kb.op("dve", lambda e: e.scalar_tensor_tensor(out=qn[:], in0=mt_[:, 0:256], scalar=st2[:, 1:2], in1=qln[:], op0=ALU.mult, op1=ALU.mult),
                                  [mt_, st2, qln], [qn])
                            yield
                            qnT = tTn.get()
                            transposes(qn, [qn[:, 0:128], qn[:, 128:256]], qnT[:], qnT)
                            pq = ps2.get()
                            for (n0, n1) in ((0, 512), (512, 768)):
                                for k in range(2):
                                    kb.op("pe", lambda e: e.matmul(pq[:, n0:n1], lhsT=qnT[:, k, :], rhs=wuq[:, k, n0:n1], start=(k == 0), stop=(k == 1)),
                                          [qnT, wuq], [pq], inc=(k == 1))
                            qf = t768.get()
                            kb.op("act", lambda e: e.copy(out=qf[:].rearrange("p h c -> p (h c)"), in_=pq[:, 0:768]), [pq], [qf])
                            yield
                            qbf = tb768.get()
                            yield from head_norm_rope(qf, qbf, qnw, cs)
                            yield
                            transposes(qbf, [qbf[:, h, :] for h in range(8)], qT[:, :, lt_ * 128:(lt_ + 1) * 128], qT, eng="dve")

                run_interleaved((mla_tile(ti) for ti in range(18)), 2)
                if "qT_dbg" in dbg and b == 0:
                    kb.dma("sp", dbg_d["qT_dbg"][:, :, :], qT[:], reads=[qT], writes=[dbg_d["qT_dbg"]])
                    kb.dma("sp", dbg_d["kT_dbg"][:, :, :], kT[:], reads=[kT], writes=[dbg_d["kT_dbg"]])
                Erot = kb.rot("E", [128, 512], BF16, 3)
                apair = kb.rot("apair", [128, 4, 128], BF16, 2)
                for hp in range(4):
                    for qb in range(4):
                        ap_ = apair.get()
                        for hh in range(2):
                            h = hp * 2 + hh
                            po = [ps2.get(), ps2.get()]
                            for kt in range(18):
                                pS = ps1.get()
                                kb.op("pe", lambda e: e.matmul(pS[:, :], lhsT=kT[:, h, kt * 128:(kt + 1) * 128], rhs=qT[:, h, qb * 512:(qb + 1) * 512], start=True, stop=True),
                                      [kT, qT], [pS])
                                E = Erot.get()
                                kb.op("act", lambda e: e.activation(out=E[:], in_=pS[:, :], func=AF.Exp), [pS], [E])
                                for qs in range(4):
                                    p_ = po[qs // 2]
                                    c0 = (qs % 2) * 512
                                    kb.op("pe", lambda e: e.matmul(p_[:, c0:c0 + 65], lhsT=E[:, qs * 128:(qs + 1) * 128], rhs=Vall[:, kt, h, :],
                                                                   start=(kt == 0), stop=(kt == 17)), [E, Vall], [p_], inc=(kt == 17))
                            for qs in range(4):
                                p_ = po[qs // 2]
                                c0 = (qs % 2) * 512
                                st = small_rot.get()
                                kb.op("dve", lambda e: e.reciprocal(out=st[:, 0:1], in_=p_[:, c0 + 64:c0 + 65]), [p_], [st])
                                kb.op("dve", lambda e: e.tensor_scalar(out=ap_[:, qs, hh * 64:(hh + 1) * 64], in0=p_[:, c0:c0 + 64], scalar1=st[:, 0:1], scalar2=None,
                                                                       op0=ALU.mult), [p_, st], [ap_])
                        transposes(ap_, [ap_[:, qs, :] for qs in range(4)], attT[:, hp, qb * 512:(qb + 1) * 512], attT)
                if "att_dbg" in dbg and b == 0:
                    kb.dma("sp", dbg_d["att_dbg"][:, :, :], attT[:], reads=[attT], writes=[dbg_d["att_dbg"]])
            if upto <= 4:
                mix_stack.close()
                continue

            with kb.scope():
                wo_f = kb.sb("wo_f", [128, 8, 1024], F32)
                wo1 = kb.sb("wo1", [128, 4, 1024], BF16)
                wo2 = kb.sb("wo2", [128, 4, 1024], BF16)
                wo3 = kb.sb("wo3", [128, 8, 1024], BF16)
                kb.dma("sp", wo_f[:, 0:4, :], I["w_o_mla"].ap().rearrange("(k p) n -> p k n", p=128), writes=[wo_f])
                kb.op("pool", lambda e: e.tensor_copy(out=wo1[:], in_=wo_f[:, 0:4, :]), [wo_f], [wo1])
                kb.dma("sp", wo_f[:, 0:4, :], I["w_o_rwkv"].ap().rearrange("(k p) n -> p k n", p=128), reads=[wo_f], writes=[wo_f])
                kb.op("pool", lambda e: e.tensor_copy(out=wo2[:], in_=wo_f[:, 0:4, :]), [wo_f], [wo2])
                kb.dma("sp", wo_f[:], I["w_out"].ap().rearrange("(k p) n -> p k n", p=128), reads=[wo_f], writes=[wo_f])
                kb.op("pool", lambda e: e.tensor_copy(out=wo3[:], in_=wo_f[:]), [wo_f], [wo3])
                mT = kb.sb("mT", [128, 8, 2048], BF16)
                grot = kb.rot("gt", [128, 2, 512], BF16, 2)
                trot = kb.rot("tm", [128, 512], F32, 2)
                for m in range(8):
                    for nb_ in range(4):
                        g = grot.get()
                        kb.dma("sp", g[:, 0, :], gates_d[m * 128:(m + 1) * 128, nb_ * 512:(nb_ + 1) * 512], reads=[gates_r[m][nb_]], writes=[g])
                        kb.dma("sp", g[:, 1, :], gates_d[(8 + m) * 128:(9 + m) * 128, nb_ * 512:(nb_ + 1) * 512], reads=[gates_r[8 + m][nb_]], writes=[g])
                        p1 = ps1.get(); p2 = ps1.get()
                        for k in range(4):
                            kb.op("pe", lambda e: e.matmul(p1[:, :], lhsT=wo1[:, k, m * 128:(m + 1) * 128], rhs=attT[:, k, nb_ * 512:(nb_ + 1) * 512], start=(k == 0), stop=(k == 3)),
                                  [wo1, attT], [p1], inc=(k == 3))
                        for k in range(4):
                            kb.op("pe", lambda e: e.matmul(p2[:, :], lhsT=wo2[:, k, m * 128:(m + 1) * 128], rhs=rwT[:, k, nb_ * 512:(nb_ + 1) * 512], start=(k == 0), stop=(k == 3)),
                                  [wo2, rwT], [p2], inc=(k == 3))
                        t1 = trot.get(); t2 = trot.get()
                        kb.op("dve", lambda e: e.tensor_tensor(out=t1[:], in0=p1[:, :], in1=g[:, 0, :], op=ALU.mult), [p1, g], [t1])
                        kb.op("dve", lambda e: e.tensor_tensor(out=t2[:], in0=p2[:, :], in1=g[:, 1, :], op=ALU.mult), [p2, g], [t2])
                        kb.op("pool", lambda e: e.tensor_tensor(out=mT[:, m, nb_ * 512:(nb_ + 1) * 512], in0=t1[:], in1=t2[:], op=ALU.add), [t1, t2], [mT])
                Ga = kb.sb("Ga", [128, 1024], F32)
                bcast_load(Ga[:], Ga, mod_d.t.ap()[b:b + 1, 2048:3072], 128, reads=[mod_d])
                xrot = kb.rot("xt5", [128, 1024], F32, 2)
                for t in range(16):
                    xt = xrot.get()
                    kb.dma("sp", xt[:], I["x"].ap()[b, t * 128:(t + 1) * 128, :], writes=[xt])
                    pm = ps2.get()
                    for nb_ in range(2):
                        for k in range(8):
                            kb.op("pe", lambda e: e.matmul(pm[:, nb_ * 512:(nb_ + 1) * 512], lhsT=mT[:, k, t * 128:(t + 1) * 128], rhs=wo3[:, k, nb_ * 512:(nb_ + 1) * 512],
                                                           start=(k == 0), stop=(k == 7)), [mT, wo3], [pm], inc=(k == 7))
                    junk = junk_rot.get()
                    kb.op("dve", lambda e: e.tensor_tensor(out=junk[:], in0=pm[:, :], in1=Ga[:], op=ALU.mult), [pm, Ga], [junk])
                    kb.op("pool", lambda e: e.tensor_tensor(out=xt[:], in0=junk[:], in1=xt[:], op=ALU.add), [junk, xt], [xt])
                    kb.dma("sp", x1_d[(b * 16 + t) * 128:(b * 16 + t + 1) * 128, :], xt[:], reads=[xt], writes=[x1_r[b * 16 + t]])
            kb.barrier()
            mix_stack.close()
            if upto <= 5:
                continue

            pass

        if upto > 5:
          NT = 16 * nbatch
          NBLK = NT * 4 + 256
          with kb.scope():
            I32 = mybir.dt.int32
            u_d = kb.dram("u_d", [NT * 128, 1024], BF16)
            u_r = [Buf() for _ in range(NT)]
            xs_d = kb.dram("xs_d", [NBLK * 256, 1024], BF16)
            Y_d = kb.dram("Y_d", [NBLK * 256, 1024], BF16)
            junk_rot = kb.rot("junk6", [128, 1024], F32, 3)
            small_rot = kb.rot("small6", [128, 16], F32, 8)
            dest_all = kb.sb("dest_all", [128, NT, 8], I32)
            gate_all = kb.sb("gate_all", [128, NT, 8], F32)
            IDXi = kb.sb("IDXi", [128, NBLK], I32)
            tri_f = kb.sb("tri6_f", [128, 4, 128], F32)
            tri = kb.sb("tri6", [128, 4, 128], BF16)
            kb.dma("sp", tri_f[:], I["c_tri"].ap(), writes=[tri_f])
            kb.op("dve", lambda e: e.tensor_copy(out=tri[:], in_=tri_f[:]), [tri_f], [tri])
            iota_p = kb.sb("iota_p", [128, 2], F32)
            kb.dma("sp", iota_p[:], I["c_iotap"].ap(), writes=[iota_p])
            with kb.scope():
                G_all = kb.sb("G_all", [128, NT, 256], F32)
                POS_all = kb.sb("POS_all", [128, NT, 256], F32)
                mask_all = kb.sb("mask_all", [128, NT, 256], BF16)
                cnt = kb.sb("cnt", [128, 256], F32)
                kb.op("pool", lambda e: e.memset(cnt[:], 0.0), [], [cnt])
                with kb.scope():
                    rw_f = kb.sb("rw_f", [128, 8, 256], F32)
                    kb.dma("sp", rw_f[:], I["router_w"].ap().rearrange("(k p) n -> p k n", p=128), writes=[rw_f])
                    rbias = kb.sb("rbias", [128, 256], F32)
                    bcast_load(rbias[:], rbias, I["router_bias"].ap()[0:1, :], 128)
                    xrot = kb.rot("xt6", [128, 1024], F32, 3)
                    ufr = kb.rot("uf", [128, 1024], F32, 3)
                    ubr = kb.rot("ub", [128, 1024], BF16, 3)
                    uTf = kb.rot("uTf", [128, 8, 128], F32, 3)
                    s256 = kb.rot("s256", [128, 256], F32, 12)
                    m8r = kb.rot("m8", [128, 8], F32, 16)
                    A2s = []
                    for b in range(nbatch):
                        A2 = kb.sb("A2", [128, 1024], F32)
                        S2 = kb.sb("S2", [128, 1024], F32)
                        bcast_load(A2[:], A2, mod_d.t.ap()[b:b + 1, 4096:5120], 128, reads=[mod_d])
                        bcast_load(S2[:], S2, mod_d.t.ap()[b:b + 1, 3072:4096], 128, reads=[mod_d])
                        A2s.append((A2, S2))
                    def tileA(i):
                            b = i // 16
                            A2, S2 = A2s[b]
                            xt = xrot.get()
                            kb.dma("sp", xt[:], x1_d[i * 128:(i + 1) * 128, :], reads=[x1_r[i]], writes=[xt])
                            junk = junk_rot.get(); ssq = small_rot.get()
                            kb.op("act", lambda e: e.activation(out=junk[:], in_=xt[:], func=AF.Square, accum_out=ssq[:, 0:1]), [xt], [junk, ssq])
                            rms_rstd(ssq[:, 0:1], ssq[:, 1:2], ssq, ssq, 1024, EPS)
                            yield
                            uf = ufr.get()
                            kb.op("dve", lambda e: e.scalar_tensor_tensor(out=junk[:], in0=xt[:], scalar=ssq[:, 1:2], in1=A2[:], op0=ALU.mult, op1=ALU.mult), [xt, ssq, A2], [junk])
                            kb.op("pool", lambda e: e.tensor_tensor(out=uf[:], in0=junk[:], in1=S2[:], op=ALU.add), [junk, S2], [uf])
                            ub = ubr.get()
                            kb.op("act", lambda e: e.copy(out=ub[:], in_=uf[:]), [uf], [ub])
                            kb.dma("sp", u_d[i * 128:(i + 1) * 128, :], ub[:], reads=[ub], writes=[u_r[i]])
                            yield
                            utf = uTf.get()
                            for half in range(2):
                                transposes(uf, [uf[:, (half * 4 + k) * 128:(half * 4 + k + 1) * 128] for k in range(4)], utf[:, half * 4:(half + 1) * 4, :], utf, dt=F32, eng="dve")
                            pr = ps1.get()
                            for k in range(8):
                                kb.op("pe", lambda e: e.matmul(pr[:, 0:256], lhsT=utf[:, k, :], rhs=rw_f[:, k, :], start=(k == 0), stop=(k == 7)), [utf, rw_f], [pr], inc=(k == 7))
                            sc = s256.get(); sel = s256.get(); w1_ = s256.get(); w2_ = s256.get()
                            kb.op("act", lambda e: e.activation(out=sc[:], in_=pr[:, 0:256], func=AF.Sigmoid), [pr], [sc])
                            kb.op("dve", lambda e: e.tensor_tensor(out=sel[:], in0=sc[:], in1=rbias[:], op=ALU.add), [sc, rbias], [sel])
                            yield
                            sel3 = sel[:].rearrange("p (g c) -> p g c", g=8)
                            mx = m8r.get(); mx2 = m8r.get(); gs = m8r.get(); gsort = m8r.get()
                            kb.op("dve", lambda e: e.tensor_reduce(out=mx[:], in_=sel3, axis=AX.X, op=ALU.max), [sel], [mx])
                            kb.op("dve", lambda e: e.tensor_tensor(out=w1_[:].rearrange("p (g c) -> p g c", g=8), in0=sel3, in1=mx[:].unsqueeze(2).to_broadcast([128, 8, 32]), op=ALU.is_ge),
                                  [sel, mx], [w1_])
                            kb.op("dve", lambda e: e.scalar_tensor_tensor(out=w2_[:], in0=w1_[:], scalar=-10.0, in1=sel[:], op0=ALU.mult, op1=ALU.add), [w1_, sel], [w2_])
                            kb.op("dve", lambda e: e.tensor_reduce(out=mx2[:], in_=w2_[:].rearrange("p (g c) -> p g c", g=8), axis=AX.X, op=ALU.max), [w2_], [mx2])
                            yield
                            kb.op("dve", lambda e: e.tensor_tensor(out=gs[:], in0=mx[:], in1=mx2[:], op=ALU.add), [mx, mx2], [gs])
                            kb.op("dve", lambda e: e.max(out=gsort[:], in_=gs[:]), [gs], [gsort])
                            kb.op("dve", lambda e: e.tensor_scalar(out=gs[:], in0=gs[:], scalar1=gsort[:, 3:4], scalar2=None, op0=ALU.is_ge), [gs, gsort], [gs])
                            kb.op("dve", lambda e: e.scalar_tensor_tensor(out=w1_[:].rearrange("p (g c) -> p g c", g=8), in0=sel3, scalar=2.0,
                                                                          in1=gs[:].unsqueeze(2).to_broadcast([128, 8, 32]), op0=ALU.add, op1=ALU.mult), [sel, gs], [w1_])
                            yield
                            top8 = m8r.get()
                            kb.op("dve", lambda e: e.max(out=top8[:], in_=w1_[:]), [w1_], [top8])
                            kb.op("dve", lambda e: e.tensor_scalar(out=w2_[:], in0=w1_[:], scalar1=top8[:, 7:8], scalar2=None, op0=ALU.is_ge), [w1_, top8], [w2_])
                            kb.op("pool", lambda e: e.tensor_copy(out=mask_all[:, i, :], in_=w2_[:]), [w2_], [mask_all])
                            yield
                            st = small_rot.get()
                            kb.op("dve", lambda e: e.tensor_tensor(out=w1_[:], in0=w2_[:], in1=sc[:], op=ALU.mult), [w2_, sc], [w1_])
                            kb.op("dve", lambda e: e.tensor_reduce(out=st[:, 0:1], in_=w1_[:], axis=AX.X, op=ALU.add), [w1_], [st])
                            kb.op("dve", lambda e: e.reciprocal(out=st[:, 1:2], in_=st[:, 0:1]), [st], [st])
                            kb.op("dve", lambda e: e.tensor_scalar(out=G_all[:, i, :], in0=w1_[:], scalar1=st[:, 1:2], scalar2=2.5, op0=ALU.mult, op1=ALU.mult), [w1_, st], [G_all])
                            yield
                            pc = ps1.get()
                            kb.op("pe", lambda e: e.matmul(pc[:, 0:256], lhsT=tri[:, 1, :], rhs=mask_all[:, i, :], start=True, stop=True), [tri, mask_all], [pc])
                            kb.op("pe", lambda e: e.matmul(pc[:, 256:512], lhsT=ones_b[:], rhs=mask_all[:, i, :], start=True, stop=True), [ones_b, mask_all], [pc])
                            kb.op("dve", lambda e: e.tensor_tensor(out=POS_all[:, i, :], in0=pc[:, 0:256], in1=cnt[:], op=ALU.add), [pc, cnt], [POS_all])
                            kb.op("dve", lambda e: e.tensor_tensor(out=cnt[:], in0=pc[:, 256:512], in1=cnt[:], op=ALU.add), [pc, cnt], [cnt])
                    run_interleaved((tileA(i) for i in range(NT)), 3)
                if "G_dbg" in dbg:
                    kb.dma("sp", dbg_d["G_dbg"][:, :, 0:256], G_all[:, 0:16, :], reads=[G_all], writes=[dbg_d["G_dbg"]])
                base_r = kb.sb("base_r", [128, 256], F32)
                with kb.scope():
                    ind = kb.sb("ind", [128, 256], BF16)
                    kb.op("dve", lambda e: e.tensor_scalar(out=ind[:], in0=cnt[:], scalar1=iota_p[:, 1:2], scalar2=None, op0=ALU.is_gt), [cnt, iota_p], [ind])
                    pn = ps1.get()
                    kb.op("pe", lambda e: e.matmul(pn[:, 0:256], lhsT=ones_b[:], rhs=ind[:], start=True, stop=True), [ones_b, ind], [pn])
                    nblk = kb.sb("nblk", [128, 256], BF16)
                    kb.op("dve", lambda e: e.tensor_copy(out=nblk[:], in_=pn[:, 0:256]), [pn], [nblk])
                    nbT = kb.sb("nbT", [128, 2, 128], BF16)
                    transposes(nblk, [nblk[:, 0:128], nblk[:, 128:256]], nbT[:], nbT)
                    pb = ps1.get()
                    kb.op("pe", lambda e: e.matmul(pb[:, 0:128], lhsT=nbT[:, 0, :], rhs=tri[:, 1, :], start=True, stop=True), [nbT, tri], [pb])
                    kb.op("pe", lambda e: e.matmul(pb[:, 128:256], lhsT=nbT[:, 0, :], rhs=ones_b[:], start=True, stop=False), [nbT, ones_b], [pb], inc=False)
                    kb.op("pe", lambda e: e.matmul(pb[:, 128:256], lhsT=nbT[:, 1, :], rhs=tri[:, 1, :], start=False, stop=True), [nbT, tri], [pb])
                    kb.op("dve", lambda e: e.tensor_copy(out=base_r[:], in_=pb[:, 0:256]), [pb], [base_r])
                    pe_ = ps1.get()
                    kb.op("pe", lambda e: e.matmul(pe_[:, 0:1], lhsT=tri[:, 0, :], rhs=nbT[:, 0, 0:1], start=True, stop=True), [nbT, tri], [pe_])
                    kb.op("pe", lambda e: e.matmul(pe_[:, 1:2], lhsT=ones_b[:], rhs=nbT[:, 0, 0:1], start=True, stop=False), [nbT, ones_b], [pe_], inc=False)
                    kb.op("pe", lambda e: e.matmul(pe_[:, 1:2], lhsT=tri[:, 0, :], rhs=nbT[:, 1, 0:1], start=False, stop=True), [nbT, tri], [pe_])
                    endT = kb.sb("endT", [128, 2], F32)
                    kb.op("dve", lambda e: e.tensor_copy(out=endT[:], in_=pe_[:, 0:2]), [pe_], [endT])
                    iota_b = kb.sb("iota_b", [128, NBLK], F32)
                    kb.dma("sp", iota_b[:], I["c_iotab"].ap()[:, 0:NBLK], writes=[iota_b])
                    ind2 = kb.sb("ind2", [128, 2, NBLK], BF16)
                    for c_ in range(2):
                        kb.op("dve", lambda e: e.tensor_scalar(out=ind2[:, c_, :], in0=iota_b[:], scalar1=endT[:, c_:c_ + 1], scalar2=None, op0=ALU.is_ge), [iota_b, endT], [ind2])
                    BEf = kb.sb("BEf", [128, NBLK], F32)
                    for n0 in range(0, NBLK, 512):
                        n1 = min(n0 + 512, NBLK)
                        pbe = ps1.get()
                        for c_ in range(2):
                            kb.op("pe", lambda e: e.matmul(pbe[:, 0:n1 - n0], lhsT=ones_b[:], rhs=ind2[:, c_, n0:n1], start=(c_ == 0), stop=(c_ == 1)), [ones_b, ind2], [pbe], inc=(c_ == 1))
                        kb.op("dve", lambda e: e.tensor_scalar(out=BEf[:, n0:n1], in0=pbe[:, 0:n1 - n0], scalar1=128.0, scalar2=iota_p[:, 0:1], op0=ALU.mult, op1=ALU.add), [pbe, iota_p], [BEf])
                    kb.op("dve", lambda e: e.tensor_copy(out=IDXi[:], in_=BEf[:]), [BEf], [IDXi])
                with kb.scope():
                    s256 = kb.rot("s256b", [128, 256], F32, 9)
                    m8r = kb.rot("m8b", [128, 8], F32, 6)
                    ubr = kb.rot("ubB", [128, 1024], BF16, 3)
                    def tileB(i):
                            t_ = s256.get(); V = s256.get(); eq = s256.get()
                            kb.op("dve", lambda e: e.scalar_tensor_tensor(out=t_[:], in0=base_r[:], scalar=256.0, in1=POS_all[:, i, :], op0=ALU.mult, op1=ALU.add), [base_r, POS_all], [t_])
                            kb.op("dve", lambda e: e.scalar_tensor_tensor(out=V[:], in0=t_[:], scalar=1.0, in1=mask_all[:, i, :], op0=ALU.add, op1=ALU.mult), [t_, mask_all], [V])
                            yield
                            top8 = m8r.get(); d8 = m8r.get()
                            kb.op("dve", lambda e: e.max(out=top8[:], in_=V[:]), [V], [top8])
                            kb.op("dve", lambda e: e.tensor_scalar(out=d8[:], in0=top8[:], scalar1=-1.0, scalar2=None, op0=ALU.add), [top8], [d8])
                            kb.op("dve", lambda e: e.tensor_copy(out=dest_all[:, i, :], in_=d8[:]), [d8], [dest_all])
                            yield
                            for k in range(8):
                                kb.op("dve", lambda e: e.tensor_scalar(out=eq[:], in0=V[:], scalar1=top8[:, k:k + 1], scalar2=None, op0=ALU.is_equal), [V, top8], [eq])
                                kb.op("pool", lambda e: e.tensor_tensor(out=eq[:], in0=eq[:], in1=G_all[:, i, :], op=ALU.mult), [eq, G_all], [eq])
                                kb.op("dve", lambda e: e.tensor_reduce(out=gate_all[:, i, k:k + 1], in_=eq[:], axis=AX.X, op=ALU.add), [eq], [gate_all])
                                if k % 2 == 1:
                                    yield
                            yield
                            ub = ubr.get()
                            kb.dma("sp", ub[:], u_d[i * 128:(i + 1) * 128, :], reads=[u_r[i]], writes=[ub])
                            for k in range(8):
                                kb.idma(out=xs_d[:, :], in_=ub[:], out_off=dest_all[:, i, k:k + 1], reads=[ub, dest_all], writes=[xs_d])
                    run_interleaved((tileB(i) for i in range(NT)), 3)
            xT_rot = kb.rot("xT", [128, 8, 128], BF16, 3)
            silu_rot = kb.rot("silu", [128, 256], F32, 2)
            hb_rot = kb.rot("hb", [128, 256], BF16, 4)
            hT_rot = kb.rot("hT6", [128, 2, 128], BF16, 3)

            def ffn_a(xsb, wb13, strided):
                if strided:
                    srcs = [xsb[:].rearrange("s (p k) -> s k p", k=8)[:, k, :] for k in range(8)]
                else:
                    srcs = [xsb[:, k * 128:(k + 1) * 128] for k in range(8)]
                xT = xT_rot.get()
                transposes(xsb, srcs, xT[:], xT)
                ph = ps1.get()
                for k in range(8):
                    kb.op("pe", lambda e: e.matmul(ph[:, :], lhsT=xT[:, k, :], rhs=wb13[:, k, :, :].rearrange("p w n -> p (w n)"), start=(k == 0), stop=(k == 7)),
                          [xT, wb13], [ph], inc=(k == 7))
                sl = silu_rot.get(); hb = hb_rot.get()
                kb.op("act", lambda e: e.activation(out=sl[:], in_=ph[:, 0:256], func=AF.Silu), [ph], [sl])
                kb.op("dve", lambda e: e.tensor_tensor(out=hb[:], in0=ph[:, 256:512], in1=sl[:], op=ALU.mult), [ph, sl], [hb])
                return hb

            def ffn_b(hb, wb2, strided):
                if strided:
                    hs = [hb[:].rearrange("s (p j) -> s j p", j=2)[:, j, :] for j in range(2)]
                else:
                    hs = [hb[:, j * 128:(j + 1) * 128] for j in range(2)]
                hT = hT_rot.get()
                transposes(hb, hs, hT[:], hT, eng="dve")
                po_ = ps2.get()
                for nb_ in range(2):
                    for j in range(2):
                        kb.op("pe", lambda e: e.matmul(po_[:, nb_ * 512:(nb_ + 1) * 512], lhsT=hT[:, j, :], rhs=wb2[:, j, nb_ * 512:(nb_ + 1) * 512],
                                                       start=(j == 0), stop=(j == 1)), [hT, wb2], [po_], inc=(j == 1))
                return po_

            def ffn_block(xsb, wb13, wb2, strided):
                return ffn_b(ffn_a(xsb, wb13, strided), wb2, strided)

            with kb.scope():
                w1v = I["expert_w1"].ap().rearrange("e (p k) n -> (e p) (k n)", k=8)
                w3v = I["expert_w3"].ap().rearrange("e (p k) n -> (e p) (k n)", k=8)
                w2v = I["expert_w2"].ap().rearrange("e (p j) n -> (e p) (j n)", j=2)
                wf13 = kb.rot("wf13", [128, 2, 2048], F32, 3)
                wf2 = kb.rot("wf2", [128, 2048], F32, 3)
                wb13r = kb.rot("wb13", [128, 8, 2, 256], BF16, 3)
                wb2r = kb.rot("wb2", [128, 2, 1024], BF16, 4)
                xsr = kb.rot("xsb", [128, 1024], BF16, 8)
                ybr = kb.rot("yb", [128, 1024], BF16, 3)
                for t_ in wf13.tiles + wf2.tiles:
                    kb.op("pool", lambda e: e.memset(t_[:], 0.0), [], [t_])
                pend = {}

                def issue(bk):
                    f13 = wf13.get(); f2 = wf2.get()
                    idx = IDXi[:, bk:bk + 1]
                    kb.idma(out=f13[:, 0, :], in_=w1v, in_off=idx, reads=[IDXi], writes=[f13], bounds=32767)
                    kb.idma(out=f13[:, 1, :], in_=w3v, in_off=idx, reads=[IDXi], writes=[f13], bounds=32767)
                    kb.idma(out=f2[:], in_=w2v, in_off=idx, reads=[IDXi], writes=[f2], bounds=32767)
                    xs2 = []
                    for sbk in range(2):
                        xsb = xsr.get()
                        r0 = (bk * 2 + sbk) * 128
                        kb.dma("sp", xsb[:], xs_d[r0:r0 + 128, :], reads=[xs_d], writes=[xsb])
                        xs2.append(xsb)
                    pend[bk] = (f13, f2, xs2)

                nrun = min(NBLK, blk_limit)
                NSUB = nrun * 2
                ctx_ = {}

                def st0(n):
                    bk = n // 2
                    if n % 2 == 0:
                        if bk + 2 < nrun:
                            issue(bk + 2)
                        f13, f2, xs2 = pend.pop(bk)
                        wb13 = wb13r.get(); wb2 = wb2r.get()
                        kb.op("act", lambda e: e.copy(out=wb13[:, :, 0, :], in_=f13[:, 0, :].rearrange("p (k n) -> p k n", k=8)), [f13], [wb13])
                        kb.op("dve", lambda e: e.tensor_copy(out=wb13[:, :, 1, :], in_=f13[:, 1, :].rearrange("p (k n) -> p k n", k=8)), [f13], [wb13])
                        kb.op("act", lambda e: e.copy(out=wb2[:, 0, :], in_=f2[:, 0:1024]), [f2], [wb2])
                        kb.op("dve", lambda e: e.tensor_copy(out=wb2[:, 1, :], in_=f2[:, 1024:2048]), [f2], [wb2])
                        ctx_[("w", bk)] = (wb13, wb2, xs2)
                    wb13, wb2, xs2 = ctx_[("w", bk)]
                    xsb = xs2[n % 2]
                    srcs = [xsb[:].rearrange("s (p k) -> s k p", k=8)[:, k, :] for k in range(8)]
                    xT = xT_rot.get()
                    transposes(xsb, srcs, xT[:], xT)
                    ctx_[n] = dict(xT=xT, wb13=wb13, wb2=wb2)

                def st1(n):
                    c = ctx_[n]
                    xT, wb13 = c["xT"], c["wb13"]
                    ph = ps1.get()
                    for k in range(8):
                        kb.op("pe", lambda e: e.matmul(ph[:, :], lhsT=xT[:, k, :], rhs=wb13[:, k, :, :].rearrange("p w n -> p (w n)"), start=(k == 0), stop=(k == 7)),
                              [xT, wb13], [ph], inc=(k == 7))
                    sl = silu_rot.get(); hb = hb_rot.get()
                    kb.op("act", lambda e: e.activation(out=sl[:], in_=ph[:, 0:256], func=AF.Silu), [ph], [sl])
                    kb.op("dve", lambda e: e.tensor_tensor(out=hb[:], in0=ph[:, 256:512], in1=sl[:], op=ALU.mult), [ph, sl], [hb])
                    c["hb"] = hb

                def st2(n):
                    c = ctx_[n]
                    hb = c["hb"]
                    hs = [hb[:].rearrange("s (p j) -> s j p", j=2)[:, j, :] for j in range(2)]
                    hT = hT_rot.get()
                    transposes(hb, hs, hT[:], hT, eng="dve")
                    c["hT"] = hT

                def st3(n):
                    c = ctx_.pop(n)
                    hT, wb2 = c["hT"], c["wb2"]
                    po_ = ps2.get()
                    for nb_ in range(2):
                        for j in range(2):
                            kb.op("pe", lambda e: e.matmul(po_[:, nb_ * 512:(nb_ + 1) * 512], lhsT=hT[:, j, :], rhs=wb2[:, j, nb_ * 512:(nb_ + 1) * 512],
                                                           start=(j == 0), stop=(j == 1)), [hT, wb2], [po_], inc=(j == 1))
                    yb = ybr.get()
                    kb.op("act", lambda e: e.copy(out=yb[:, 0:512], in_=po_[:, 0:512]), [po_], [yb])
                    kb.op("dve", lambda e: e.tensor_copy(out=yb[:, 512:1024], in_=po_[:, 512:1024]), [po_], [yb])
                    kb.dma("sp", Y_d[n * 128:(n + 1) * 128, :], yb[:], reads=[yb], writes=[Y_d])

                for bk in range(min(2, nrun)):
                    issue(bk)
                stages = (st0, st1, st2, st3)
                for step in range(NSUB + 3):
                    for k_, fn_ in enumerate(stages):
                        n = step - k_
                        if 0 <= n < NSUB:
                            fn_(n)
            with kb.scope():
                wsf = kb.sb("wsf", [128, 8, 256], F32)
                ws13 = kb.sb("ws13", [128, 8, 2, 256], BF16)
                ws2f = kb.sb("ws2f", [128, 2, 1024], F32)
                ws2 = kb.sb("ws2", [128, 2, 1024], BF16)
                for w_, nm in enumerate(("shared_w1", "shared_w3")):
                    kb.dma("sp", wsf[:], I[nm].ap().rearrange("(k p) n -> p k n", p=128), reads=[wsf], writes=[wsf])
                    kb.op("dve", lambda e: e.tensor_copy(out=ws13[:, :, w_, :], in_=wsf[:]), [wsf], [ws13])
                kb.dma("sp", ws2f[:], I["shared_w2"].ap().rearrange("(j p) n -> p j n", p=128), writes=[ws2f])
                kb.op("dve", lambda e: e.tensor_copy(out=ws2[:], in_=ws2f[:]), [ws2f], [ws2])
                Gms = []
                for b in range(nbatch):
                    Gm = kb.sb("Gm", [128, 1024], F32)
                    bcast_load(Gm[:], Gm, mod_d.t.ap()[b:b + 1, 5120:6144], 128, reads=[mod_d])
                    Gms.append(Gm)
                ubr = kb.rot("ubD", [128, 1024], BF16, 3)
                ygr = kb.rot("yg", [128, 1024], BF16, 8)
                accr = kb.rot("acc", [128, 1024], F32, 3)
                xrot = kb.rot("xtD", [128, 1024], F32, 3)
                def tileD(i):
                        b = i // 16
                        ub = ubr.get()
                        kb.dma("sp", ub[:], u_d[i * 128:(i + 1) * 128, :], reads=[u_r[i]], writes=[ub])
                        yield
                        hb_ = ffn_a(ub, ws13, False)
                        yield
                        po_ = ffn_b(hb_, ws2, False)
                        acc = accr.get()
                        kb.op("act", lambda e: e.copy(out=acc[:], in_=po_[:, :]), [po_], [acc])
                        yield
                        for k in range(8):
                            yg = ygr.get()
                            kb.idma(out=yg[:], in_=Y_d[:, :], in_off=dest_all[:, i, k:k + 1], reads=[dest_all, Y_d], writes=[yg])
                            kb.op("dve", lambda e: e.scalar_tensor_tensor(out=acc[:], in0=yg[:], scalar=gate_all[:, i, k:k + 1], in1=acc[:], op0=ALU.mult, op1=ALU.add),
                                  [yg, gate_all, acc], [acc])
                            if k % 2 == 1:
                                yield
                        if "moe_dbg" in dbg and i < 16:
                            kb.dma("sp", dbg_d["moe_dbg"][:, i, :], acc[:], reads=[acc], writes=[dbg_d["moe_dbg"]])
                        yield
                        xt = xrot.get()
                        kb.dma("sp", xt[:], x1_d[i * 128:(i + 1) * 128, :], reads=[x1_r[i]], writes=[xt])
                        kb.op("dve", lambda e: e.tensor_tensor(out=acc[:], in0=acc[:], in1=Gms[b][:], op=ALU.mult), [acc, Gms[b]], [acc])
                        kb.op("pool", lambda e: e.tensor_tensor(out=xt[:], in0=xt[:], in1=acc[:], op=ALU.add), [xt, acc], [xt])
                        kb.dma("sp", out_d.ap()[b, (i % 16) * 128:(i % 16 + 1) * 128, :], xt[:], reads=[xt])
                run_interleaved((tileD(i) for i in range(NT)), 3)
        kb.finish()
        print("instructions:", kb.nins, "sems:", len(kb.sems) + len(kb.dsem))
    return nc


_CACHE = {}


def kernel(**inputs):
    n = 8
    consts = host_consts()
    sq = lambda k: np.ascontiguousarray(np.asarray(inputs[k], dtype=np.float32)[0])
    shared = {}
    for k in IN_SHAPES:
        if k in ("x", "ctx", "cT") or k.startswith("c_"):
            continue
        a = sq(k)
        shared[k] = a.reshape(IN_SHAPES[k])
    shared.update(consts)
    x = np.asarray(inputs["x"], np.float32)
    ctx = np.asarray(inputs["ctx"], np.float32)
    c = np.asarray(inputs["c"], np.float32)
    c_ctx = np.asarray(inputs["c_ctx"], np.float32)
    in_maps = []
    for i in range(n):
        m = dict(shared)
        m["x"] = np.ascontiguousarray(x[2 * i:2 * i + 2])
        m["ctx"] = np.ascontiguousarray(ctx[2 * i:2 * i + 2])
        m["cT"] = np.ascontiguousarray(np.stack([c[2 * i], c[2 * i + 1], c_ctx], axis=1))
        in_maps.append(m)
    if "nc" not in _CACHE:
        _CACHE["nc"] = build()
    res = run_bass_kernel_spmd(_CACHE["nc"], in_maps, core_ids=list(range(n)))
    return np.concatenate([np.asarray(r["out"], np.float32) for r in res.results], axis=0)
```

```python
import numpy as np
import ml_dtypes
import concourse.bass as bass
import concourse.mybir as mybir
from concourse.bass_utils import run_bass_kernel_spmd
from contextlib import ExitStack, contextmanager

F32 = mybir.dt.float32
BF16 = mybir.dt.bfloat16
AF = mybir.ActivationFunctionType
ALU = mybir.AluOpType
AX = mybir.AxisListType

C_DEC = -0.6065306597126334
EPS = 1e-6
GN_EPS = 64e-5
ATTN_SCALE = 96 ** -0.5


NAMES = {}


class Buf:
    __slots__ = ("name", "w", "r", "excl")

    def __init__(self, name=""):
        self.name = name
        self.w = None
        self.r = {}
        self.excl = False


class Tile:
    def __init__(self, t, name):
        self.t = t
        self.b = Buf(name)

    def __getitem__(self, k):
        return self.t[k]


class Rot:
    def __init__(self, tiles):
        self.tiles = tiles
        self.i = 0

    def get(self):
        t = self.tiles[self.i % len(self.tiles)]
        self.i += 1
        return t


class KB:
    ENG = ("pe", "act", "dve", "pool", "sp")
    EPOCH = 8000
    NDMA = 40

    def __init__(self, nc, stack):
        self.nc = nc
        self.gstack = stack
        self.stack = stack
        self.e = {"pe": nc.tensor, "act": nc.scalar, "dve": nc.vector,
                  "pool": nc.gpsimd, "sp": nc.sync}
        self.cnt = {k: 0 for k in self.ENG}
        self.ep = {k: 0 for k in self.ENG}
        self.sems = {}
        self.waited = {}
        for k in self.ENG:
            self._newsem(k)
        self.dsem = []
        self.dslot = []
        self.dval = []
        for i in range(self.NDMA):
            self.dsem.append(stack.enter_context(nc.semaphore(f"dq{i}")))
            self.dslot.append(i)
            self.dval.append(0)
        self.dnext = 0
        self.dwaited = {}
        self.nins = 0
        self.uid = 0
        self.promised = False
        self.bregs = {}

    def sb(self, name, shape, dtype):
        self.uid += 1
        nm = f"{name}_{self.uid}"
        NAMES[name] = nm
        return Tile(self.stack.enter_context(self.nc.sbuf_tensor(nm, list(shape), dtype)), nm)

    def ps(self, name, shape, dtype=F32):
        t = Tile(self.gstack.enter_context(self.nc.psum_tensor(name, list(shape), dtype)), name)
        t.b.excl = True
        return t

    def dram(self, name, shape, dtype, kind="Internal"):
        return Tile(self.nc.dram_tensor(name, list(shape), dtype, kind=kind), name)

    def rot(self, name, shape, dtype, n=2):
        return Rot([self.sb(f"{name}{i}", shape, dtype) for i in range(n)])

    @contextmanager
    def scope(self):
        old = self.stack
        with ExitStack() as st:
            self.stack = st
            yield
            self.barrier()
        self.stack = old

    def _newsem(self, k):
        self.sems[(k, self.ep[k])] = self.gstack.enter_context(
            self.nc.semaphore(f"s_{k}_{self.ep[k]}"))

    def _wait(self, eng, tok):
        if tok is None:
            return
        if tok[0] == "e":
            _, k, ep, c = tok
            if eng == "pe" and k == "pe":
                return
            assert not (k == "pe" and ep == self.ep["pe"] and c > self.cnt["pe"]), "wait on a promised PE token"
            key = (eng, k, ep)
            if self.waited.get(key, 0) >= c:
                return
            self.e[eng].wait_ge(self.sems[(k, ep)], c)
            self.waited[key] = c
        else:
            _, i, v = tok
            key = (eng, i)
            if self.dwaited.get(key, 0) >= v:
                return
            self.e[eng].wait_ge(self.dsem[i], v)
            self.dwaited[key] = v

    @staticmethod
    def _bufs(xs):
        out = []
        for x in xs:
            if x is None:
                continue
            out.append(x.b if isinstance(x, Tile) else x)
        return out

    def _deps(self, eng, reads, writes):
        for b in reads:
            self._wait(eng, b.w)
        for b in writes:
            self._wait(eng, b.w)
            for t in b.r.values():
                self._wait(eng, t)

    def _commit(self, tok, reads, writes):
        for b in writes:
            b.w = tok
            b.r = {}
        for b in reads:
            if b not in writes:
                key = tok[1] if tok[0] == "e" else ("d", tok[1])
                b.r[key] = tok

    def op(self, eng, fn, reads=(), writes=(), inc=True):
        reads = self._bufs(reads)
        writes = self._bufs(writes)
        writes = writes + [b for b in reads if b.excl and b not in writes]
        self._deps(eng, reads, writes)
        if inc and self.cnt[eng] >= self.EPOCH and not (eng == "pe" and self.promised):
            self.ep[eng] += 1
            self.cnt[eng] = 0
            self._newsem(eng)
        ins = fn(self.e[eng])
        if eng == "pe":
            self.promised = not inc
        if inc:
            self.cnt[eng] += 1
            ins.then_inc(self.sems[(eng, self.ep[eng])], 1)
            tok = ("e", eng, self.ep[eng], self.cnt[eng])
        else:
            assert eng == "pe"
            tok = ("e", eng, self.ep[eng], self.cnt[eng] + 1)
        self._commit(tok, reads, writes)
        self.nins += 1

    def dma(self, eng, out, in_, reads=(), writes=(), **kw):
        reads = self._bufs(reads)
        writes = self._bufs(writes)
        self._deps(eng, reads, writes)
        s = self.dnext
        self.dnext = (self.dnext + 1) % self.NDMA
        i = self.dslot[s]
        if self.dval[i] >= 8000:
            self.dsem.append(self.gstack.enter_context(self.nc.semaphore(f"dq{len(self.dsem)}")))
            self.dval.append(0)
            prev = i
            i = len(self.dsem) - 1
            self.dslot[s] = i
            self._wait(eng, ("d", prev, self.dval[prev]))
        if self.dval[i] > 0:
            self._wait(eng, ("d", i, self.dval[i]))
        self.dval[i] += 16
        self.e[eng].dma_start(out=out, in_=in_, **kw).then_inc(self.dsem[i], 16)
        tok = ("d", i, self.dval[i])
        self._commit(tok, reads, writes)
        self.nins += 1

    def idma(self, out, in_, out_off=None, in_off=None, reads=(), writes=(), bounds=None):
        eng = "pool"
        reads = self._bufs(reads)
        writes = self._bufs(writes)
        self._deps(eng, reads, writes)
        s = self.dnext
        self.dnext = (self.dnext + 1) % self.NDMA
        i = self.dslot[s]
        if self.dval[i] >= 8000:
            self.dsem.append(self.gstack.enter_context(self.nc.semaphore(f"dq{len(self.dsem)}")))
            self.dval.append(0)
            prev = i
            i = len(self.dsem) - 1
            self.dslot[s] = i
            self._wait(eng, ("d", prev, self.dval[prev]))
        if self.dval[i] > 0:
            self._wait(eng, ("d", i, self.dval[i]))
        self.dval[i] += 16
        kw = {}
        if bounds is not None:
            if bounds not in self.bregs:
                r = self.nc.gpsimd.alloc_register(f"bnd{bounds}")
                self.nc.gpsimd.reg_mov(r, bounds)
                self.bregs[bounds] = r
            kw = dict(bounds_check=self.bregs[bounds], oob_is_err=False)
        self.e[eng].indirect_dma_start(
            out=out, out_offset=None if out_off is None else bass.IndirectOffsetOnAxis(ap=out_off, axis=0),
            in_=in_, in_offset=None if in_off is None else bass.IndirectOffsetOnAxis(ap=in_off, axis=0), **kw,
        ).then_inc(self.dsem[i], 16)
        tok = ("d", i, self.dval[i])
        self._commit(tok, reads, writes)
        self.nins += 1

    def finish(self):
        for i in range(len(self.dsem)):
            if self.dval[i] > 0:
                self._wait("sp", ("d", i, self.dval[i]))
        for k in self.ENG:
            if k != "sp" and (self.cnt[k] > 0 or self.ep[k] > 0):
                self._wait("sp", ("e", k, self.ep[k], self.cnt[k]))

    def barrier(self):
        self.finish()
        self.op("sp", lambda e: e.nop(), (), ())
        tok = ("e", "sp", self.ep["sp"], self.cnt["sp"])
        for k in ("pe", "act", "dve", "pool"):
            self._wait(k, tok)


def run_interleaved(gens, width):
    it = iter(gens)
    active = []
    exhausted = False
    while True:
        while len(active) < width and not exhausted:
            try:
                active.append(next(it))
            except StopIteration:
                exhausted = True
        if not active:
            break
        for g in list(active):
            try:
                next(g)
            except StopIteration:
                active.remove(g)


def host_consts():
    s = np.arange(128)[:, None]
    t = np.arange(128)[None, :]
    le = (s <= t).astype(np.float32)
    lt = (s < t).astype(np.float32)
    ge = (s >= t).astype(np.float32)
    gt = (s > t).astype(np.float32)
    tri = np.stack([le, lt, ge, gt], axis=1)
    mask4 = np.zeros((2, 128, 512), np.float32)
    maskt = np.zeros((2, 128, 128), np.float32)
    for d, (strict, incl) in enumerate(((lt, le), (gt, ge))):
        mask4[d, :, 0:128] = -strict
        mask4[d, :, 128:256] = -incl
        mask4[d, :, 256:384] = strict
        mask4[d, :, 384:512] = incl
        maskt[d] = -strict.T
    rows = 2048 // 64
    row = np.repeat(np.arange(rows, dtype=np.float32), 64)
    col = np.tile(np.arange(64, dtype=np.float32), rows)
    inv = (10000.0 ** (-np.arange(8, dtype=np.float32) / 8)).astype(np.float32)
    ang = np.concatenate([row[:, None] * inv, col[:, None] * inv], axis=-1).astype(np.float32)
    p_ = np.arange(128, dtype=np.float32)
    iotap = np.stack([p_, 256 * p_], axis=1)
    iotab = np.tile(np.arange(512, dtype=np.float32)[None, :], (128, 1))
    return dict(c_iotap=iotap, c_iotab=iotab, c_ident=np.eye(128, dtype=np.float32), c_tri=tri, c_mask4=mask4, c_maskt=maskt,
                c_cos=np.cos(ang).astype(np.float32), c_sin=np.sin(ang).astype(np.float32))


IN_SHAPES = dict(
    x=[2, 2048, 1024], ctx=[2, 256, 1024], cT=[1024, 3],
    ada_w=[1024, 6144], ada_b=[1, 6144], norm_mix=[1, 1024], norm_ffn=[1, 1024],
    w_in=[1024, 4416], shift_conv=[3, 1952], q_lat_norm=[1, 256], w_uq=[256, 768],
    kv_lat_norm=[1, 128], w_ukv=[128, 1024], q_norm=[1, 96], k_norm=[1, 96], w_o_mla=[512, 1024],
    decay_w0=[2, 512], decay_w2=[2, 64, 512], aicl_a0=[2, 512], aicl_a2=[2, 64, 512],
    k_k=[1, 512], k_a=[1, 512], r_k=[1, 512], gn_w=[1, 512], gn_b=[1, 512], gate_g2=[160, 512],
    w_o_rwkv=[512, 1024], w_out=[1024, 1024], router_w=[1024, 256], router_bias=[1, 256],
    expert_w1=[256, 1024, 256], expert_w3=[256, 1024, 256], expert_w2=[256, 256, 1024],
    shared_w1=[1024, 256], shared_w3=[1024, 256], shared_w2=[256, 1024],
    c_ident=[128, 128], c_tri=[128, 4, 128], c_mask4=[2, 128, 512], c_maskt=[2, 128, 128],
    c_cos=[2048, 16], c_sin=[2048, 16], c_iotap=[128, 2], c_iotab=[128, 512],
)

CT0 = 1
LT0 = 259
HW = 2308


def build(upto=99, dbg=(), n_exp=256, nbatch=2, rw_tiles=99, rw_phase=9, rw_dirs=2, blk_limit=10 ** 9):
    nc = bass.Bass("TRN2", target_bir_lowering=False)
    I = {k: nc.dram_tensor(k, list(v), F32, kind="ExternalInput") for k, v in IN_SHAPES.items()
         if not (upto < 6 and k.startswith(("expert_", "shared_")))}
    out_d = nc.dram_tensor("out", [2, 2048, 1024], F32, kind="ExternalOutput")

    with ExitStack() as gst:
        kb = KB(nc, gst)
        dbgk = lambda n: ("ExternalOutput" if n in dbg else "Internal")
        ps1 = Rot([kb.ps(f"ps1_{i}", [128, 512]) for i in range(4)])
        ps2 = Rot([kb.ps(f"ps2_{i}", [128, 1024]) for i in range(2)])

        mod_d = kb.dram("mod_d", [3, 6144], F32, dbgk("mod_d"))
        mla_d = kb.dram("mla_d", [2304, 416], F32, dbgk("mla_d"))
        mla_r = [Buf() for _ in range(18)]
        rkv_d = kb.dram("rkv_d", [2304, 1536], F32, dbgk("rkv_d"))
        rkv_r = [Buf() for _ in range(18)]
        gates_d = kb.dram("gates_d", [2048, 2048], BF16, dbgk("gates_d"))
        gates_r = [[Buf() for _ in range(4)] for _ in range(16)]
        x1_d = kb.dram("x1_d", [4096, 1024], F32, dbgk("x1_d"))
        x1_r = [Buf() for _ in range(32)]
        dbg_d = {}
        for n, shp, dt in (("fm_dbg", [128, 4, 2304], BF16), ("att_dbg", [128, 4, 2048], BF16),
                           ("y_dbg", [128, 16, 512], F32), ("rw_dbg", [128, 4, 2048], BF16),
                           ("G_dbg", [128, 16, 257], F32), ("qT_dbg", [96, 8, 2048], BF16),
                           ("kT_dbg", [96, 8, 2304], BF16), ("moe_dbg", [128, 16, 1024], F32)):
            if n in dbg:
                dbg_d[n] = kb.dram(n, shp, dt, "ExternalOutput")

        ident_f = kb.sb("ident_f", [128, 128], F32)
        ident_b = kb.sb("ident_b", [128, 128], BF16)
        ones_b = kb.sb("ones_b", [128, 128], BF16)
        kb.dma("sp", ident_f[:], I["c_ident"].ap(), writes=[ident_f])
        kb.op("dve", lambda e: e.tensor_copy(out=ident_b[:], in_=ident_f[:]), [ident_f], [ident_b])
        kb.op("pool", lambda e: e.memset(ones_b[:], 1.0), [], [ones_b])

        def bcast_load(dst_ap, dst_tile, src_ap, nparts, reads=()):
            kb.dma("sp", dst_ap, src_ap.to_broadcast([nparts, src_ap.shape[-1]]), reads=reads, writes=[dst_tile])

        def transposes(src_tile, src_aps, dst_ap, dst_tile, dt=BF16, eng="act", width=128):
            pp = ps1.get()
            pv = pp[:].bitcast(BF16) if dt == BF16 else pp[:]
            idn = ident_b if dt == BF16 else ident_f
            n = len(src_aps)
            P = src_aps[0].shape[0]
            w = src_aps[0].shape[1]
            for j, a in enumerate(src_aps):
                kb.op("pe", lambda e: e.transpose(out=pv[0:w, j * width:j * width + P], in_=a, identity=idn[0:P, 0:P]),
                      [src_tile, idn], [pp])
            src = pv[0:w, 0:n * width]
            if eng == "act":
                kb.op("act", lambda e: e.copy(out=dst_ap, in_=src.rearrange("p (j t) -> p j t", j=n) if len(dst_ap.shape) == 3 else src), [pp], [dst_tile])
            else:
                kb.op(eng, lambda e: e.tensor_copy(out=dst_ap, in_=src.rearrange("p (j t) -> p j t", j=n) if len(dst_ap.shape) == 3 else src), [pp], [dst_tile])

        def rms_rstd(ssq_ap, out_ap, tile_in, tile_out, n, eps):
            kb.op("act", lambda e: e.activation(out=out_ap, in_=ssq_ap, func=AF.Sqrt, bias=eps_t[:, 0:1] if eps == EPS else (gneps_t[:, 0:1] if eps == GN_EPS else tiny_t[:, 0:1]), scale=1.0 / n),
                  [tile_in], [tile_out])
            kb.op("dve", lambda e: e.reciprocal(out=out_ap, in_=out_ap), [tile_out], [tile_out])

        eps_t = kb.sb("eps_t", [128, 1], F32)
        gneps_t = kb.sb("gneps_t", [128, 1], F32)
        tiny_t = kb.sb("tiny_t", [128, 1], F32)
        kb.op("pool", lambda e: e.memset(eps_t[:], EPS), [], [eps_t])
        kb.op("pool", lambda e: e.memset(gneps_t[:], GN_EPS), [], [gneps_t])
        kb.op("pool", lambda e: e.memset(tiny_t[:], 1e-12), [], [tiny_t])

        with kb.scope():
            cTs = kb.sb("cTs", [128, 8, 3], F32)
            kb.dma("sp", cTs[:], I["cT"].ap().rearrange("(k p) r -> p k r", p=128), writes=[cTs])
            sT = kb.sb("sT", [128, 8, 3], F32)
            kb.op("act", lambda e: e.activation(out=sT[:], in_=cTs[:], func=AF.Silu), [cTs], [sT])
            modsb = kb.sb("modsb", [3, 6144], F32)
            adab = kb.sb("adab", [3, 6144], F32)
            bcast_load(adab[:], adab, I["ada_b"].ap()[0:1, :], 3)
            nrm = kb.sb("nrm", [3, 2, 1024], F32)
            bcast_load(nrm[:, 0, :], nrm, I["norm_mix"].ap()[0:1, :], 3)
            bcast_load(nrm[:, 1, :], nrm, I["norm_ffn"].ap()[0:1, :], 3)
            wrot = kb.rot("adaw", [128, 8, 512], F32, 2)
            for cb in range(12):
                wt = wrot.get()
                kb.dma("sp", wt[:], I["ada_w"].ap()[:, cb * 512:(cb + 1) * 512].rearrange("(k p) n -> p k n", p=128), writes=[wt])
                pp = ps1.get()
                for k in range(8):
                    kb.op("pe", lambda e: e.matmul(pp[0:3, :], lhsT=sT[:, k, :], rhs=wt[:, k, :], start=(k == 0), stop=(k == 7)),
                          [sT, wt], [pp], inc=(k == 7))
                kb.op("dve", lambda e: e.tensor_tensor(out=modsb[:, cb * 512:(cb + 1) * 512], in0=pp[0:3, :],
                                                       in1=adab[:, cb * 512:(cb + 1) * 512], op=ALU.add), [pp, adab], [modsb])
            for (c0, j) in ((1024, 0), (4096, 1)):
                kb.op("dve", lambda e: e.scalar_tensor_tensor(out=modsb[:, c0:c0 + 1024], in0=modsb[:, c0:c0 + 1024], scalar=1.0,
                                                              in1=nrm[:, j, :], op0=ALU.add, op1=ALU.mult), [modsb, nrm], [modsb])
            kb.dma("sp", mod_d[:, :], modsb[:], reads=[modsb], writes=[mod_d])
        if upto <= 0:
            kb.finish()
            return nc

        def norm_mod(xt, A, S, hb):
            junk = junk_rot.get()
            ssq = small_rot.get()
            kb.op("act", lambda e: e.activation(out=junk[:], in_=xt[:], func=AF.Square, accum_out=ssq[:, 0:1]), [xt], [junk, ssq])
            rms_rstd(ssq[:, 0:1], ssq[:, 1:2], ssq, ssq, 1024, EPS)
            kb.op("dve", lambda e: e.scalar_tensor_tensor(out=junk[:], in0=xt[:], scalar=ssq[:, 1:2], in1=A[:], op0=ALU.mult, op1=ALU.mult),
                  [xt, ssq, A], [junk])
            kb.op("pool", lambda e: e.tensor_tensor(out=hb[:], in0=junk[:], in1=S[:], op=ALU.add), [junk, S], [hb])

        for b in range(nbatch):
          with kb.scope():
            junk_rot = kb.rot("junk", [128, 1024], F32, 2)
            small_rot = kb.rot("small", [128, 16], F32, 6)
            mix_stack = ExitStack()
            fm_stack = ExitStack()
            _old = kb.stack
            kb.stack = mix_stack
            attT = kb.sb("attT", [128, 4, 2048], BF16)
            rwT = kb.sb("rwT", [128, 4, 2048], BF16)
            kb.stack = fm_stack
            fm = kb.sb("fm", [128, 4, 2304], BF16)
            kb.stack = _old

            with kb.scope():
                hT = kb.sb("hT", [128, 8, HW], BF16)
                for (c0, c1) in ((0, 1), (257, 259), (2307, 2308)):
                    kb.op("pool", lambda e: e.memset(hT[:, :, c0:c1], 0.0), [], [hT])
                with kb.scope():
                  xrot = kb.rot("xt", [128, 1024], F32, 2)
                  hbrot = kb.rot("hb", [128, 1024], BF16, 2)
                  for seg, (src, nt, off, row) in enumerate(((I["ctx"], 2, CT0, 2), (I["x"], 16, LT0, b))):
                    A1 = kb.sb("A1", [128, 1024], F32)
                    S1 = kb.sb("S1", [128, 1024], F32)
                    bcast_load(A1[:], A1, mod_d.t.ap()[row:row + 1, 1024:2048], 128, reads=[mod_d])
                    bcast_load(S1[:], S1, mod_d.t.ap()[row:row + 1, 0:1024], 128, reads=[mod_d])
                    for t in range(nt):
                        xt = xrot.get()
                        kb.dma("sp", xt[:], src.ap()[b, t * 128:(t + 1) * 128, :], writes=[xt])
                        hb = hbrot.get()
                        norm_mod(xt, A1, S1, hb)
                        transposes(hb, [hb[:, k * 128:(k + 1) * 128] for k in range(8)],
                                   hT[:, :, off + t * 128: off + (t + 1) * 128], hT)
                conv = kb.sb("conv", [128, 3, 1952], F32)
                for j in range(3):
                    bcast_load(conv[:, j, :], conv, I["shift_conv"].ap()[j:j + 1, :], 128)
                wf = kb.sb("wf", [128, 8, 512], F32)
                wbs = [kb.rot(f"wb{j}", [128, 8, 512], BF16, 2) for j in range(3)]
                osb = kb.rot("osb", [128, 512], F32, 2)
                gsb = kb.rot("gsb", [128, 512], BF16, 2)
                tok_tiles = [(CT0 + t * 128, t) for t in range(2)] + [(LT0 + t * 128, 2 + t) for t in range(16)]
                blocks = [(0, 416, "M")] + [(416 + i * 512, 512, "R") for i in range(3)] + [(1952, 416, "F")] + \
                         [(2368 + i * 512, 512, "G") for i in range(4)]
                for (c0, ncol, kind) in blocks:
                    kb.dma("sp", wf[:, :, 0:ncol], I["w_in"].ap()[:, c0:c0 + ncol].rearrange("(k p) n -> p k n", p=128), writes=[wf])
                    has_conv = kind in ("R", "F")
                    ws = []
                    for j in (range(3) if has_conv else range(1)):
                        wb = wbs[j].get()
                        if has_conv:
                            cc = c0 - 416
                            for k in range(8):
                                kb.op("pool" if k % 2 else "dve", lambda e: e.tensor_tensor(out=wb[:, k, 0:ncol], in0=wf[:, k, 0:ncol],
                                                                                           in1=conv[:, j, cc:cc + ncol], op=ALU.mult), [wf, conv], [wb])
                        else:
                            kb.op("pool", lambda e: e.tensor_copy(out=wb[:, :, 0:ncol], in_=wf[:, :, 0:ncol]), [wf], [wb])
                        ws.append(wb)
                    shifts = [(-1, 0), (0, 1), (1, 2)] if has_conv else [(0, 0)]
                    if kind in ("M", "R"):
                        for (off, ti) in tok_tiles:
                            pp = ps1.get()
                            n = len(shifts) * 8
                            i = 0
                            for (sh, j) in shifts:
                                for k in range(8):
                                    kb.op("pe", lambda e: e.matmul(pp[:, 0:ncol], lhsT=hT[:, k, off + sh:off + sh + 128], rhs=ws[j][:, k, 0:ncol],
                                                                   start=(i == 0), stop=(i == n - 1)), [hT, ws[j]], [pp], inc=(i == n - 1))
                                    i += 1
                            o = osb.get()
                            kb.op("act", lambda e: e.copy(out=o[:, 0:ncol], in_=pp[:, 0:ncol]), [pp], [o])
                            if kind == "M":
                                kb.dma("sp", mla_d[ti * 128:(ti + 1) * 128, :], o[:, 0:416], reads=[o], writes=[mla_r[ti]])
                            else:
                                cc = c0 - 416
                                kb.dma("sp", rkv_d[ti * 128:(ti + 1) * 128, cc:cc + 512], o[:, :], reads=[o], writes=[rkv_r[ti]])
                    elif kind == "F":
                        nblks = [(CT0, 0, 256)] + [(LT0 + i * 512, 256 + i * 512, 512) for i in range(4)]
                        for m, (m0, msz) in enumerate(((0, 128), (128, 128), (256, 128), (384, 32))):
                            func = (AF.Tanh, AF.Copy, AF.Sigmoid, AF.Sigmoid)[m]
                            for (off, dcol, nn) in nblks:
                                pp = ps1.get()
                                i = 0
                                for (sh, j) in shifts:
                                    for k in range(8):
                                        kb.op("pe", lambda e: e.matmul(pp[0:msz, 0:nn], lhsT=ws[j][:, k, m0:m0 + msz], rhs=hT[:, k, off + sh:off + sh + nn],
                                                                       start=(i == 0), stop=(i == 23)), [hT, ws[j]], [pp], inc=(i == 23))
                                        i += 1
                                kb.op("act", lambda e: e.activation(out=fm[0:msz, m, dcol:dcol + nn], in_=pp[0:msz, 0:nn], func=func), [pp], [fm])
                    else:
                        g0 = c0 - 2368
                        for m in range(4):
                            mt = g0 // 128 + m
                            for nb_ in range(4):
                                pp = ps1.get()
                                for k in range(8):
                                    kb.op("pe", lambda e: e.matmul(pp[:, :], lhsT=ws[0][:, k, m * 128:(m + 1) * 128],
                                                                   rhs=hT[:, k, LT0 + nb_ * 512:LT0 + (nb_ + 1) * 512], start=(k == 0), stop=(k == 7)),
                                          [hT, ws[0]], [pp], inc=(k == 7))
                                g = gsb.get()
                                kb.op("act", lambda e: e.activation(out=g[:], in_=pp[:, :], func=AF.Sigmoid), [pp], [g])
                                kb.dma("sp", gates_d[mt * 128:(mt + 1) * 128, nb_ * 512:(nb_ + 1) * 512], g[:], reads=[g], writes=[gates_r[mt][nb_]])
                if "fm_dbg" in dbg and b == 0:
                    kb.dma("sp", dbg_d["fm_dbg"][:, :, :], fm[:], reads=[fm], writes=[dbg_d["fm_dbg"]])
            if upto <= 2:
                kb.barrier(); fm_stack.close(); mix_stack.close()
                continue

            with kb.scope():
                Ybuf = kb.sb("Ybuf", [128, 16, 512], F32)
                halves = []
                for t2 in ps2.tiles:
                    for c0_ in (0, 512):
                        ht = Tile(t2.t[:, c0_:c0_ + 512], f"{t2.b.name}_h{c0_}")
                        ht.b.excl = True
                        halves.append(ht)
                pY_bank = halves[0]
                psR = Rot(ps1.tiles + halves[1:])
                bonus = kb.sb("bonus", [128, 16, 2, 8], F32)
                tri_f = kb.sb("tri_f", [128, 4, 128], F32)
                tri = kb.sb("tri", [128, 4, 128], BF16)
                kb.dma("sp", tri_f[:], I["c_tri"].ap(), writes=[tri_f])
                kb.op("dve", lambda e: e.tensor_copy(out=tri[:], in_=tri_f[:]), [tri_f], [tri])
                mask4 = kb.sb("mask4", [128, 2, 512], F32)
                maskt = kb.sb("maskt", [128, 2, 128], F32)
                for d in range(2):
                    kb.dma("sp", mask4[:, d, :], I["c_mask4"].ap()[d], writes=[mask4])
                    kb.dma("sp", maskt[:, d, :], I["c_maskt"].ap()[d], writes=[maskt])
                prm = kb.sb("prm", [128, 3, 512], F32)
                bcast_load(prm[:, 0, :], prm, I["k_k"].ap()[0:1, :], 128)
                bcast_load(prm[:, 1, :], prm, I["k_a"].ap()[0:1, :], 128)
                bcast_load(prm[:, 2, :], prm, I["r_k"].ap()[0:1, :], 128)
                w2f = kb.sb("w2f", [128, 2, 512], F32)
                w2b = kb.sb("w2b", [128, 2, 512], BF16)
                kb.dma("sp", w2f[:, 0, :], I["decay_w2"].ap().rearrange("d l c -> (d l) c"), writes=[w2f])
                kb.dma("sp", w2f[:, 1, :], I["aicl_a2"].ap().rearrange("d l c -> (d l) c"), writes=[w2f])
                kb.op("dve", lambda e: e.tensor_copy(out=w2b[:], in_=w2f[:]), [w2f], [w2b])
                b0f = kb.sb("b0f", [128, 2, 512], F32)
                b0b = kb.sb("b0b", [128, 2, 512], BF16)
                kb.op("pool", lambda e: e.memset(b0f[:], 0.0), [], [b0f])
                for d_ in range(2):
                    kb.dma("sp", b0f[d_ * 64:d_ * 64 + 1, 0, :], I["decay_w0"].ap()[d_:d_ + 1, :], writes=[b0f])
                    kb.dma("sp", b0f[d_ * 64:d_ * 64 + 1, 1, :], I["aicl_a0"].ap()[d_:d_ + 1, :], writes=[b0f])
                kb.op("dve", lambda e: e.tensor_copy(out=b0b[:], in_=b0f[:]), [b0f], [b0b])
                Hf = [kb.sb(f"Hf{p}", [128, 64], F32) for p in range(4)]
                Hb = [kb.sb(f"Hb{p}", [128, 64], BF16) for p in range(4)]
                rkv_rot = kb.rot("rkvt", [128, 1536], F32, 2)
                f512 = kb.rot("f512", [128, 512], F32, 9)
                b512 = kb.rot("b512", [128, 512], BF16, 14)
                fmT = kb.rot("fmT", [128, 4, 4, 128], BF16, 2)
                A4rot = kb.rot("A4", [128, 512], BF16, 8)
                XPa = kb.rot("XPa", [128, 512], BF16, 12)
                Wsb = kb.rot("Wsb", [128, 64], BF16, 10)
                Usb = kb.rot("Usb", [128, 512], BF16, 2)
                Vb = kb.rot("Vb", [128, 512], BF16, 2)
                ptot_rot = kb.rot("ptot", [128, 4], F32, 2)

                for d in range(rw_dirs):
                    for p in range(4):
                        kb.op("pool", lambda e: e.memset(Hf[p][:], 0.0), [], [Hf[p]])
                        kb.op("pool", lambda e: e.memset(Hb[p][:], 0.0), [], [Hb[p]])
                    order = list(range(18)) if d == 0 else [1, 0] + list(range(17, 1, -1))
                    for ti in order[:rw_tiles]:
                        lat = ti >= 2
                        rt = rkv_rot.get()
                        kb.dma("sp", rt[:], rkv_d[ti * 128:(ti + 1) * 128, :], reads=[rkv_r[ti]], writes=[rt])
                        r_ = rt[:, 0:512]
                        k_ = rt[:, 512:1024]
                        v_ = rt[:, 1024:1536]
                        kkr = f512.get(); sq = f512.get(); st = small_rot.get()
                        kb.op("dve", lambda e: e.tensor_tensor(out=kkr[:], in0=k_, in1=prm[:, 0, :], op=ALU.mult), [rt, prm], [kkr])
                        kb.op("act", lambda e: e.activation(out=sq[:], in_=kkr[:], func=AF.Square), [kkr], [sq])
                        kb.op("dve", lambda e: e.tensor_reduce(out=st[:, 0:8], in_=sq[:].rearrange("p (h c) -> p h c", h=8), axis=AX.X, op=ALU.add), [sq], [st])
                        rms_rstd(st[:, 0:8], st[:, 8:16], st, st, 1.0, 1e-12)
                        kap = f512.get()
                        kb.op("dve", lambda e: e.tensor_tensor(out=kap[:].rearrange("p (h c) -> p h c", h=8), in0=kkr[:].rearrange("p (h c) -> p h c", h=8),
                                                               in1=st[:, 8:16].unsqueeze(2).to_broadcast([128, 8, 64]), op=ALU.mult), [kkr, st], [kap])
                        tcol = (ti * 128) if ti < 2 else (256 + (ti - 2) * 128)
                        pz = psR.get(); pa = psR.get()
                        kb.op("pe", lambda e: e.matmul(pz[:, :], lhsT=fm[d * 64:(d + 1) * 64, 0, tcol:tcol + 128], rhs=w2b[d * 64:(d + 1) * 64, 0, :], start=True, stop=False),
                              [fm, w2b], [pz], inc=False)
                        kb.op("pe", lambda e: e.matmul(pz[:, :], lhsT=ones_b[d * 64:d * 64 + 1, 0:128], rhs=b0b[d * 64:d * 64 + 1, 0, :], start=False, stop=True), [ones_b, b0b], [pz])
                        kb.op("pe", lambda e: e.matmul(pa[:, :], lhsT=fm[d * 64:(d + 1) * 64, 1, tcol:tcol + 128], rhs=w2b[d * 64:(d + 1) * 64, 1, :], start=True, stop=False),
                              [fm, w2b], [pa], inc=False)
                        kb.op("pe", lambda e: e.matmul(pa[:, :], lhsT=ones_b[d * 64:d * 64 + 1, 0:128], rhs=b0b[d * 64:d * 64 + 1, 1, :], start=False, stop=True), [ones_b, b0b], [pa])
                        sigb = b512.get()
                        kb.op("act", lambda e: e.activation(out=sigb[:], in_=pz[:, :], func=AF.Sigmoid), [pz], [sigb])
                        a_ = f512.get()
                        kb.op("act", lambda e: e.activation(out=a_[:], in_=pa[:, :], func=AF.Sigmoid), [pa], [a_])
                        t1 = f512.get(); ktl = f512.get(); beta = f512.get()
                        kb.op("dve", lambda e: e.scalar_tensor_tensor(out=t1[:], in0=a_[:], scalar=-1.0, in1=prm[:, 1, :], op0=ALU.add, op1=ALU.mult), [a_, prm], [t1])
                        kb.op("dve", lambda e: e.scalar_tensor_tensor(out=ktl[:], in0=t1[:], scalar=1.0, in1=k_, op0=ALU.add, op1=ALU.mult), [t1, rt], [ktl])
                        kb.op("pool", lambda e: e.tensor_tensor(out=beta[:], in0=a_[:], in1=kap[:], op=ALU.mult), [a_, kap], [beta])
                        if lat:
                            kb.op("pool", lambda e: e.tensor_tensor(out=t1[:], in0=r_, in1=prm[:, 2, :], op=ALU.mult), [rt, prm], [t1])
                            kb.op("pool", lambda e: e.tensor_tensor(out=t1[:], in0=t1[:], in1=ktl[:], op=ALU.mult), [t1, ktl], [t1])
                            kb.op("dve", lambda e: e.tensor_reduce(out=bonus[:, ti - 2, d, :], in_=t1[:].rearrange("p (h c) -> p h c", h=8), axis=AX.X, op=ALU.add), [t1], [bonus])
                        if rw_phase < 1:
                            continue
                        ci, ce, cr = ((0, 1, 3) if d == 0 else (2, 3, 1))
                        pI = psR.get(); pE = psR.get(); pR = psR.get()
                        kb.op("pe", lambda e: e.matmul(pI[:, :], lhsT=tri[:, ci, :], rhs=sigb[:], start=True, stop=True), [tri, sigb], [pI])
                        kb.op("pe", lambda e: e.matmul(pE[:, :], lhsT=tri[:, ce, :], rhs=sigb[:], start=True, stop=True), [tri, sigb], [pE])
                        kb.op("pe", lambda e: e.matmul(pR[:, :], lhsT=tri[:, cr, :], rhs=sigb[:], start=True, stop=True), [tri, sigb], [pR])
                        eI = f512.get(); eE = f512.get(); eN = f512.get(); eR = f512.get()
                        kb.op("act", lambda e: e.activation(out=eI[:], in_=pI[:, :], func=AF.Exp, scale=C_DEC), [pI], [eI])
                        kb.op("act", lambda e: e.activation(out=eN[:], in_=pI[:, :], func=AF.Exp, scale=-C_DEC), [pI], [eN])
                        kb.op("act", lambda e: e.activation(out=eE[:], in_=pE[:, :], func=AF.Exp, scale=C_DEC), [pE], [eE])
                        kb.op("act", lambda e: e.activation(out=eR[:], in_=pR[:, :], func=AF.Exp, scale=C_DEC), [pR], [eR])
                        pT = psR.get()
                        for p in range(4):
                            kb.op("pe", lambda e: e.matmul(pT[:, p:p + 1], lhsT=sigb[:, p * 128:(p + 1) * 128], rhs=ones_b[:, 0:1], start=True, stop=True), [sigb, ones_b], [pT])
                        ptot = ptot_rot.get()
                        kb.op("act", lambda e: e.activation(out=ptot[:], in_=pT[:, 0:4], func=AF.Exp, scale=C_DEC), [pT], [ptot])
                        Rh = b512.get(); Ka = b512.get(); Bh = b512.get(); Kh = b512.get(); NBb = b512.get(); Kbb = b512.get()
                        kb.op("dve", lambda e: e.tensor_tensor(out=Rh[:], in0=r_, in1=eI[:], op=ALU.mult), [rt, eI], [Rh])
                        kb.op("pool", lambda e: e.tensor_tensor(out=Ka[:], in0=kap[:], in1=eE[:], op=ALU.mult), [kap, eE], [Ka])
                        kb.op("dve", lambda e: e.tensor_tensor(out=Bh[:], in0=beta[:], in1=eN[:], op=ALU.mult), [beta, eN], [Bh])
                        kb.op("pool", lambda e: e.tensor_tensor(out=Kh[:], in0=ktl[:], in1=eN[:], op=ALU.mult), [ktl, eN], [Kh])
                        kb.op("dve", lambda e: e.scalar_tensor_tensor(out=NBb[:], in0=beta[:], scalar=-1.0, in1=eR[:], op0=ALU.mult, op1=ALU.mult), [beta, eR], [NBb])
                        kb.op("pool", lambda e: e.tensor_tensor(out=Kbb[:], in0=ktl[:], in1=eR[:], op=ALU.mult), [ktl, eR], [Kbb])
                        vb = Vb.get()
                        kb.op("pool", lambda e: e.tensor_copy(out=vb[:], in_=v_), [rt], [vb])
                        if rw_phase < 2:
                            continue
                        fT = fmT.get()
                        for wi, src in enumerate((Ka, Rh, Bh, Kh)):
                            transposes(src, [src[:, p * 128:(p + 1) * 128] for p in range(4)], fT[:, :, wi, :], fT, eng=("act" if wi % 2 else "dve"))
                        if rw_phase < 3:
                            continue
                        U = Usb.get()
                        pY = pY_bank if lat else None
                        HG = 8
                        for g0 in range(0, 8, HG):
                            hs = list(range(g0, g0 + HG))
                            P_ = {h: h // 2 for h in hs}
                            LO = {h: (h % 2) * 64 for h in hs}
                            HC = {h: slice(h * 64, (h + 1) * 64) for h in hs}
                            KaT = {h: fT[LO[h]:LO[h] + 64, P_[h], 0, :] for h in hs}
                            RT = {h: fT[LO[h]:LO[h] + 64, P_[h], 1, :] for h in hs}
                            BT = {h: fT[LO[h]:LO[h] + 64, P_[h], 2, :] for h in hs}
                            KT = {h: fT[LO[h]:LO[h] + 64, P_[h], 3, :] for h in hs}
                            KaRT = {h: fT[LO[h]:LO[h] + 64, P_[h], 0:2, :].rearrange("c w t -> c (w t)") for h in hs}
                            A4 = {}; Xt = {}; XP = {}; pp_ = {}; Wt = {}
                            NBK = len(psR.tiles)

                            def lockstep(pe_fn, ev_fn):
                                pend_ = []
                                for h_ in hs:
                                    if len(pend_) >= NBK:
                                        ev_fn(pend_.pop(0))
                                    pe_fn(h_)
                                    pend_.append(h_)
                                for h_ in pend_:
                                    ev_fn(h_)

                            def pe_A(h):
                                pA = psR.get(); pp_[h] = pA
                                kb.op("pe", lambda e: e.matmul(pA[:, 0:256], lhsT=BT[h], rhs=KaRT[h], start=True, stop=True), [fT], [pA], inc=False)
                                kb.op("pe", lambda e: e.matmul(pA[:, 256:512], lhsT=KT[h], rhs=KaRT[h], start=True, stop=True), [fT], [pA])

                            def ev_A(h):
                                A4[h] = A4rot.get()
                                kb.op("dve", lambda e: e.tensor_tensor(out=A4[h][:], in0=pp_[h][:, :], in1=mask4[:, d, :], op=ALU.mult), [pp_[h], mask4], [A4[h]])
                            lockstep(pe_A, ev_A)

                            def pe_L(h):
                                pL = psR.get(); pp_[h] = pL
                                kb.op("pe", lambda e: e.matmul(pL[:, 0:128], lhsT=KaT[h], rhs=BT[h], start=True, stop=True), [fT], [pL])

                            def ev_L(h):
                                XP[h] = XPa.get()
                                kb.op("dve", lambda e: e.tensor_tensor(out=XP[h][:, 256:384], in0=pp_[h][:, 0:128], in1=maskt[:, d, :], op=ALU.mult), [pp_[h], maskt], [XP[h]])
                                kb.op("pool", lambda e: e.tensor_copy(out=XP[h][:, 0:128], in_=A4[h][:, 0:128]), [A4[h]], [XP[h]])
                                kb.op("pool", lambda e: e.tensor_tensor(out=XP[h][:, 128:256], in0=A4[h][:, 0:128], in1=ident_b[:], op=ALU.add), [A4[h], ident_b], [XP[h]])
                            lockstep(pe_L, ev_L)

                            def xxt(ap_):
                                return ap_[:, 0:512].rearrange("p (a c) -> p a c", c=256)[:, :, 0:128] if ap_.shape[-1] >= 512 else None

                            for j in range(0, 7):
                                first = (j == 0)
                                last = (j == 6)

                                def pe_Bj(h):
                                    pB = psR.get(); pp_[h] = pB
                                    if first:
                                        kb.op("pe", lambda e: e.matmul(pB[:, 0:128], lhsT=XP[h][:, 256:384], rhs=XP[h][:, 0:128], start=True, stop=True), [XP[h]], [pB], inc=False)
                                        kb.op("pe", lambda e: e.matmul(pB[:, 256:384], lhsT=XP[h][:, 0:128], rhs=XP[h][:, 256:384], start=True, stop=True), [XP[h]], [pB])
                                    elif not last:
                                        kb.op("pe", lambda e: e.matmul(pB[:, 0:256], lhsT=XP[h][:, 256:384], rhs=XP[h][:, 0:256], start=True, stop=True), [XP[h]], [pB], inc=False)
                                        kb.op("pe", lambda e: e.matmul(pB[:, 256:384], lhsT=XP[h][:, 0:128], rhs=XP[h][:, 256:384], start=True, stop=True), [XP[h]], [pB])
                                    else:
                                        kb.op("pe", lambda e: e.matmul(pB[:, 128:256], lhsT=XP[h][:, 256:384], rhs=XP[h][:, 128:256], start=True, stop=True), [XP[h]], [pB])

                                def ev_Bj(h):
                                    pB = pp_[h]
                                    XP2 = XPa.get()
                                    if first:
                                        kb.op("pool", lambda e: e.tensor_copy(out=XP2[:, 128:256], in_=XP[h][:, 128:256]), [XP[h]], [XP2])
                                    else:
                                        kb.op("dve", lambda e: e.tensor_tensor(out=XP2[:, 128:256], in0=pB[:, 128:256], in1=XP[h][:, 128:256], op=ALU.add), [pB, XP[h]], [XP2])
                                    if not last:
                                        src_ = pB[:, 0:512].rearrange("p (a c) -> p a c", c=256)[:, :, 0:128]
                                        dst_ = XP2[:, 0:512].rearrange("p (a c) -> p a c", c=256)[:, :, 0:128]
                                        kb.op("act", lambda e: e.copy(out=dst_, in_=src_), [pB], [XP2])
                                    XP[h] = XP2
                                lockstep(pe_Bj, ev_Bj)

                            def pe_W(h):
                                pW = psR.get(); pp_[h] = pW
                                kb.op("pe", lambda e: e.matmul(pW[:, 0:64], lhsT=KaT[h], rhs=Hb[P_[h]][LO[h]:LO[h] + 64, :], start=True, stop=False), [fT, Hb[P_[h]]], [pW], inc=False)
                                kb.op("pe", lambda e: e.matmul(pW[:, 0:64], lhsT=A4[h][:, 256:384], rhs=vb[:, HC[h]], start=False, stop=True), [A4[h], vb], [pW])

                            def ev_W(h):
                                Wt[h] = Wsb.get()
                                kb.op("act", lambda e: e.copy(out=Wt[h][:], in_=pp_[h][:, 0:64]), [pp_[h]], [Wt[h]])
                            lockstep(pe_W, ev_W)

                            def pe_U(h):
                                pU = psR.get(); pp_[h] = pU
                                kb.op("pe", lambda e: e.matmul(pU[:, 0:64], lhsT=XP[h][:, 128:256], rhs=Wt[h][:], start=True, stop=True), [XP[h], Wt[h]], [pU])

                            def ev_U(h):
                                kb.op("dve" if h % 2 else "act", (lambda e: e.tensor_copy(out=U[:, HC[h]], in_=pp_[h][:, 0:64])) if h % 2 else
                                      (lambda e: e.copy(out=U[:, HC[h]], in_=pp_[h][:, 0:64])), [pp_[h]], [U])
                            lockstep(pe_U, ev_U)
                            if lat:
                                for h in hs:
                                    kb.op("pe", lambda e: e.matmul(pY[:, HC[h]], lhsT=RT[h], rhs=Hb[P_[h]][LO[h]:LO[h] + 64, :], start=True, stop=False), [fT, Hb[P_[h]]], [pY], inc=False)
                                    kb.op("pe", lambda e: e.matmul(pY[:, HC[h]], lhsT=A4[h][:, 128:256], rhs=U[:, HC[h]], start=False, stop=False), [A4[h], U], [pY], inc=False)
                                    kb.op("pe", lambda e: e.matmul(pY[:, HC[h]], lhsT=A4[h][:, 384:512], rhs=vb[:, HC[h]], start=False, stop=True), [A4[h], vb], [pY])
                            pHs = {}
                            for p in sorted(set(P_.values())):
                                pc = slice(p * 128, (p + 1) * 128)
                                pH = psR.get(); pHs[p] = pH
                                kb.op("pe", lambda e: e.matmul(pH[:, 0:128], lhsT=NBb[:, pc], rhs=U[:, pc], start=True, stop=False), [NBb, U], [pH], inc=False)
                                kb.op("pe", lambda e: e.matmul(pH[:, 0:128], lhsT=Kbb[:, pc], rhs=vb[:, pc], start=False, stop=True), [Kbb, vb], [pH])
                            for p in sorted(set(P_.values())):
                                pH = pHs[p]
                                for hh in range(2):
                                    l2 = hh * 64
                                    kb.op("dve", lambda e: e.scalar_tensor_tensor(out=Hf[p][l2:l2 + 64, :], in0=Hf[p][l2:l2 + 64, :], scalar=ptot[l2:l2 + 64, p:p + 1],
                                                                                  in1=pH[l2:l2 + 64, l2:l2 + 64], op0=ALU.mult, op1=ALU.add), [Hf[p], ptot, pH], [Hf[p]])
                                kb.op("act", lambda e: e.copy(out=Hb[p][:], in_=Hf[p][:]), [Hf[p]], [Hb[p]])
                        if lat:
                            if d == 0:
                                kb.op("act", lambda e: e.copy(out=Ybuf[:, ti - 2, :], in_=pY[:, 0:512]), [pY], [Ybuf])
                            else:
                                kb.op("dve", lambda e: e.tensor_tensor(out=Ybuf[:, ti - 2, :], in0=pY[:, 0:512], in1=Ybuf[:, ti - 2, :], op=ALU.add), [pY, Ybuf], [Ybuf])
                if "y_dbg" in dbg and b == 0:
                    kb.dma("sp", dbg_d["y_dbg"][:, :, :], Ybuf[:], reads=[Ybuf], writes=[dbg_d["y_dbg"]])
                gnw = kb.sb("gnw", [128, 2, 512], F32)
                bcast_load(gnw[:, 0, :], gnw, I["gn_w"].ap()[0:1, :], 128)
                bcast_load(gnw[:, 1, :], gnw, I["gn_b"].ap()[0:1, :], 128)
                g2f = w2f
                g2b = kb.sb("g2b", [128, 2, 512], BF16)
                kb.dma("sp", g2f[:, 0, :], I["gate_g2"].ap()[0:128, :], reads=[], writes=[g2f])
                kb.dma("sp", g2f[0:32, 1, :], I["gate_g2"].ap()[128:160, :], writes=[g2f])
                kb.op("dve", lambda e: e.tensor_copy(out=g2b[:, 0, :], in_=g2f[:, 0, :]), [g2f], [g2b])
                kb.op("dve", lambda e: e.tensor_copy(out=g2b[0:32, 1, :], in_=g2f[0:32, 1, :]), [g2f], [g2b])
                rwb = kb.rot("rwb", [128, 512], BF16, 2)
                def tileRO(t):
                        ti = t + 2
                        rt = rkv_rot.get()
                        kb.dma("sp", rt[:], rkv_d[ti * 128:(ti + 1) * 128, :], reads=[rkv_r[ti]], writes=[rt])
                        v_ = rt[:, 1024:1536]
                        y3 = Ybuf[:, t, :].rearrange("p (h c) -> p h c", h=8)
                        st = small_rot.get(); st2 = small_rot.get()
                        cen = f512.get(); sq = f512.get(); yn = f512.get()
                        cen3 = cen[:].rearrange("p (h c) -> p h c", h=8)
                        kb.op("dve", lambda e: e.tensor_reduce(out=st[:, 0:8], in_=y3, axis=AX.X, op=ALU.add), [Ybuf], [st])
                        kb.op("dve", lambda e: e.tensor_scalar(out=st[:, 0:8], in0=st[:, 0:8], scalar1=-1.0 / 64, scalar2=None, op0=ALU.mult), [st], [st])
                        kb.op("dve", lambda e: e.tensor_tensor(out=cen3, in0=y3, in1=st[:, 0:8].unsqueeze(2).to_broadcast([128, 8, 64]), op=ALU.add), [Ybuf, st], [cen])
                        yield
                        kb.op("act", lambda e: e.activation(out=sq[:], in_=cen[:], func=AF.Square), [cen], [sq])
                        kb.op("dve", lambda e: e.tensor_reduce(out=st2[:, 0:8], in_=sq[:].rearrange("p (h c) -> p h c", h=8), axis=AX.X, op=ALU.add), [sq], [st2])
                        yield
                        rms_rstd(st2[:, 0:8], st2[:, 8:16], st2, st2, 64, GN_EPS)
                        kb.op("dve", lambda e: e.tensor_tensor(out=yn[:].rearrange("p (h c) -> p h c", h=8), in0=cen3, in1=st2[:, 8:16].unsqueeze(2).to_broadcast([128, 8, 64]), op=ALU.mult),
                              [cen, st2], [yn])
                        kb.op("pool", lambda e: e.tensor_tensor(out=yn[:], in0=yn[:], in1=gnw[:, 0, :], op=ALU.mult), [yn, gnw], [yn])
                        kb.op("pool", lambda e: e.tensor_tensor(out=yn[:], in0=yn[:], in1=gnw[:, 1, :], op=ALU.add), [yn, gnw], [yn])
                        yield
                        kb.op("dve", lambda e: e.tensor_tensor(out=st[:, 8:16], in0=bonus[:, t, 0, :], in1=bonus[:, t, 1, :], op=ALU.add), [bonus], [st])
                        kb.op("dve", lambda e: e.tensor_tensor(out=sq[:].rearrange("p (h c) -> p h c", h=8), in0=v_.rearrange("p (h c) -> p h c", h=8),
                                                               in1=st[:, 8:16].unsqueeze(2).to_broadcast([128, 8, 64]), op=ALU.mult), [rt, st], [sq])
                        kb.op("pool", lambda e: e.tensor_tensor(out=yn[:], in0=yn[:], in1=sq[:], op=ALU.add), [yn, sq], [yn])
                        yield
                        pg = ps1.get()
                        tcol = 256 + t * 128
                        kb.op("pe", lambda e: e.matmul(pg[:, :], lhsT=fm[:, 2, tcol:tcol + 128], rhs=g2b[:, 0, :], start=True, stop=False), [fm, g2b], [pg], inc=False)
                        kb.op("pe", lambda e: e.matmul(pg[:, :], lhsT=fm[0:32, 3, tcol:tcol + 128], rhs=g2b[0:32, 1, :], start=False, stop=True), [fm, g2b], [pg])
                        ro = rwb.get()
                        kb.op("dve", lambda e: e.tensor_tensor(out=ro[:], in0=pg[:, :], in1=yn[:], op=ALU.mult), [pg, yn], [ro])
                        transposes(ro, [ro[:, k * 128:(k + 1) * 128] for k in range(4)], rwT[:, :, t * 128:(t + 1) * 128], rwT)
                run_interleaved((tileRO(t) for t in range(16)), 2)
                if "rw_dbg" in dbg and b == 0:
                    kb.dma("sp", dbg_d["rw_dbg"][:, :, :], rwT[:], reads=[rwT], writes=[dbg_d["rw_dbg"]])
            kb.barrier()
            fm_stack.close()
            if upto <= 3:
                mix_stack.close()
                continue

            with kb.scope():
                qT = kb.sb("qT", [96, 8, 2048], BF16)
                kT = kb.sb("kT", [96, 8, 2304], BF16)
                Vall = kb.sb("Vall", [128, 18, 8, 65], BF16)
                kb.op("pool", lambda e: e.memset(Vall[:, :, :, 64:65], 1.0), [], [Vall])
                qln = kb.sb("qln", [128, 256], F32)
                kvln = kb.sb("kvln", [128, 128], F32)
                qnw = kb.sb("qnw", [128, 8, 96], F32)
                knw = kb.sb("knw", [128, 8, 96], F32)
                bcast_load(qln[:], qln, I["q_lat_norm"].ap()[0:1, :], 128)
                bcast_load(kvln[:], kvln, I["kv_lat_norm"].ap()[0:1, :], 128)
                for h in range(8):
                    bcast_load(qnw[:, h, :], qnw, I["q_norm"].ap()[0:1, :], 128)
                    bcast_load(knw[:, h, :], knw, I["k_norm"].ap()[0:1, :], 128)
                kb.op("dve", lambda e: e.tensor_scalar(out=qnw[:], in0=qnw[:], scalar1=ATTN_SCALE, scalar2=None, op0=ALU.mult), [qnw], [qnw])
                wuq_f = kb.sb("wuq_f", [128, 2, 768], F32)
                wuq = kb.sb("wuq", [128, 2, 768], BF16)
                kb.dma("sp", wuq_f[:], I["w_uq"].ap().rearrange("(k p) n -> p k n", p=128), writes=[wuq_f])
                kb.op("pool", lambda e: e.tensor_copy(out=wuq[:], in_=wuq_f[:]), [wuq_f], [wuq])
                wukv_f = kb.sb("wukv_f", [128, 1024], F32)
                wukv = kb.sb("wukv", [128, 1024], BF16)
                kb.dma("sp", wukv_f[:], I["w_ukv"].ap(), writes=[wukv_f])
                kb.op("pool", lambda e: e.tensor_copy(out=wukv[:], in_=wukv_f[:]), [wukv_f], [wukv])
                mrot = kb.rot("mt", [128, 416], F32, 2)
                cs_rot = kb.rot("cs", [128, 2, 16], F32, 2)
                t768 = kb.rot("t768", [128, 8, 96], F32, 8)
                small_m = kb.rot("small_m", [128, 16], F32, 10)
                tb768 = kb.rot("tb768", [128, 8, 96], BF16, 4)
                tbn = kb.rot("tbn", [128, 256], BF16, 4)
                tTn = kb.rot("tTn", [128, 2, 128], BF16, 4)
                rope_t = kb.rot("rope_t", [128, 8, 16], F32, 16)

                def head_norm_rope(src, dst_b, gain, cs, n_extra_ssq=None):
                    sq = t768.get()
                    st = small_m.get()
                    kb.op("act", lambda e: e.activation(out=sq[:], in_=src[:], func=AF.Square), [src], [sq])
                    kb.op("dve", lambda e: e.tensor_reduce(out=st[:, 0:8], in_=sq[:], axis=AX.X, op=ALU.add), [sq], [st])
                    yield
                    rms_rstd(st[:, 0:8], st[:, 8:16], st, st, 96, EPS)
                    kb.op("dve", lambda e: e.tensor_tensor(out=sq[:], in0=src[:], in1=st[:, 8:16].unsqueeze(2).to_broadcast([128, 8, 96]), op=ALU.mult),
                          [src, st], [sq])
                    yield
                    if cs is None:
                        kb.op("pool", lambda e: e.tensor_tensor(out=dst_b[:], in0=sq[:], in1=gain[:], op=ALU.mult), [sq, gain], [dst_b])
                        return
                    kb.op("pool", lambda e: e.tensor_tensor(out=sq[:], in0=sq[:], in1=gain[:], op=ALU.mult), [sq, gain], [sq])
                    kb.op("pool", lambda e: e.tensor_copy(out=dst_b[:, :, 0:64], in_=sq[:, :, 0:64]), [sq], [dst_b])
                    cb_ = cs[:, 0, :].unsqueeze(1).to_broadcast([128, 8, 16])
                    sb_ = cs[:, 1, :].unsqueeze(1).to_broadcast([128, 8, 16])
                    x1 = sq[:, :, 64:80]
                    x2 = sq[:, :, 80:96]
                    ta = rope_t.get(); tb_ = rope_t.get(); tc_ = rope_t.get(); td = rope_t.get()
                    yield
                    kb.op("dve", lambda e: e.tensor_tensor(out=ta[:], in0=x1, in1=cb_, op=ALU.mult), [sq, cs], [ta])
                    kb.op("dve", lambda e: e.tensor_tensor(out=tb_[:], in0=x2, in1=sb_, op=ALU.mult), [sq, cs], [tb_])
                    kb.op("pool", lambda e: e.tensor_tensor(out=tc_[:], in0=x1, in1=sb_, op=ALU.mult), [sq, cs], [tc_])
                    kb.op("pool", lambda e: e.tensor_tensor(out=td[:], in0=x2, in1=cb_, op=ALU.mult), [sq, cs], [td])
                    kb.op("dve", lambda e: e.tensor_tensor(out=dst_b[:, :, 64:80], in0=ta[:], in1=tb_[:], op=ALU.subtract), [ta, tb_], [dst_b])
                    kb.op("pool", lambda e: e.tensor_tensor(out=dst_b[:, :, 80:96], in0=tc_[:], in1=td[:], op=ALU.add), [tc_, td], [dst_b])

                def mla_tile(ti):
                        lat = ti >= 2
                        mt_ = mrot.get()
                        kb.dma("sp", mt_[:], mla_d[ti * 128:(ti + 1) * 128, :], reads=[mla_r[ti]], writes=[mt_])
                        cs = None
                        if lat:
                            cs = cs_rot.get()
                            lt_ = ti - 2
                            kb.dma("sp", cs[:, 0, :], I["c_cos"].ap()[lt_ * 128:(lt_ + 1) * 128, :], writes=[cs])
                            kb.dma("sp", cs[:, 1, :], I["c_sin"].ap()[lt_ * 128:(lt_ + 1) * 128, :], writes=[cs])
                        junk = junk_rot.get()
                        st = small_m.get()
                        kb.op("act", lambda e: e.activation(out=junk[:, 0:128], in_=mt_[:, 256:384], func=AF.Square, accum_out=st[:, 0:1]), [mt_], [junk, st])
                        kb.op("act", lambda e: e.activation(out=junk[:, 128:160], in_=mt_[:, 384:416], func=AF.Square, accum_out=st[:, 2:3]), [mt_], [junk, st])
                        rms_rstd(st[:, 0:1], st[:, 1:2], st, st, 128, EPS)
                        kvn = tbn.get()
                        kb.op("dve", lambda e: e.scalar_tensor_tensor(out=kvn[:, 0:128], in0=mt_[:, 256:384], scalar=st[:, 1:2], in1=kvln[:], op0=ALU.mult, op1=ALU.mult),
                              [mt_, st, kvln], [kvn])
                        yield
                        kvT = tTn.get()
                        transposes(kvn, [kvn[:, 0:128]], kvT[:, 0, :], kvT)
                        pk = ps2.get()
                        for nb_ in range(2):
                            kb.op("pe", lambda e: e.matmul(pk[:, nb_ * 512:(nb_ + 1) * 512], lhsT=kvT[:, 0, :], rhs=wukv[:, nb_ * 512:(nb_ + 1) * 512], start=True, stop=True),
                                  [kvT, wukv], [pk])
                        pk3 = pk[:].rearrange("p (h c) -> p h c", h=8)
                        kb.op("act", lambda e: e.copy(out=Vall[:, ti, :, 0:64], in_=pk3[:, :, 64:128]), [pk], [Vall])
                        kf = t768.get()
                        kb.op("dve", lambda e: e.tensor_copy(out=kf[:, :, 0:64], in_=pk3[:, :, 0:64]), [pk], [kf])
                        kb.op("pool", lambda e: e.tensor_copy(out=kf[:, :, 64:96], in_=mt_[:, 384:416].unsqueeze(1).to_broadcast([128, 8, 32])), [mt_], [kf])
                        yield
                        kbf = tb768.get()
                        yield from head_norm_rope(kf, kbf, knw, cs)
                        yield
                        transposes(kbf, [kbf[:, h, :] for h in range(8)], kT[:, :, ti * 128:(ti + 1) * 128], kT, eng="dve")
                        yield
                        if lat:
                            st2 = small_m.get()
                            kb.op("act", lambda e: e.activation(out=junk[:, 256:512], in_=mt_[:, 0:256], func=AF.Square, accum_out=st2[:, 0:1]), [mt_], [junk, st2])
                            rms_rstd(st2[:, 0:1], st2[:, 1:2], st2, st2, 256, EPS)
                            qn = tbn.get()
                            kb.op("dve", lambda e: e.scalar_tensor_tensor(out=qn[:], in0=mt_[:, 0:256], scalar=st2[:, 1:2], in1=qln[:], op0=ALU.mult, op1=ALU.mult),
                                  [mt_, st2, qln], [qn])
                            yield
                            qnT = tTn.get()
                            transposes(qn, [qn[:, 0:128], qn[:, 128:256]], qnT[:], qnT)
                            pq = ps2.get()
                            for (n0, n1) in ((0, 512), (512, 768)):
                                for k in range(2):
                                    kb.op("pe", lambda e: e.matmul(pq[:, n0:n1], lhsT=qnT[:, k, :], rhs=wuq[:, k, n0:n1], start=(k == 0), stop=(k == 1)),
                                          [qnT, wuq], [pq], inc=(k == 1))
                            qf = t768.get()
                            kb.op("act", lambda e: e.copy(out=qf[:].rearrange("p h c -> p (h c)"), in_=pq[:, 0:768]), [pq], [qf])
                            yield
                            qbf = tb768.get()
                            yield from head_norm_rope(qf, qbf, qnw, cs)
                            yield
                            transposes(qbf, [qbf[:, h, :] for h in range(8)], qT[:, :, lt_ * 128:(lt_ + 1) * 128], qT, eng="dve")

                run_interleaved((mla_tile(ti) for ti in range(18)), 2)
                if "qT_dbg" in dbg and b == 0:
                    kb.dma("sp", dbg_d["qT_dbg"][:, :, :], qT[:], reads=[qT], writes=[dbg_d["qT_dbg"]])
                    kb.dma("sp", dbg_d["kT_dbg"][:, :, :], kT[:], reads=[kT], writes=[dbg_d["kT_dbg"]])
                Erot = kb.rot("E", [128, 512], BF16, 3)
                apair = kb.rot("apair", [128, 4, 128], BF16, 2)
                for hp in range(4):
                    for qb in range(4):
                        ap_ = apair.get()
                        for hh in range(2):
                            h = hp * 2 + hh
                            po = [ps2.get(), ps2.get()]
                            for kt in range(18):
                                pS = ps1.get()
                                kb.op("pe", lambda e: e.matmul(pS[:, :], lhsT=kT[:, h, kt * 128:(kt + 1) * 128], rhs=qT[:, h, qb * 512:(qb + 1) * 512], start=True, stop=True),
                                      [kT, qT], [pS])
                                E = Erot.get()
                                kb.op("act", lambda e: e.activation(out=E[:], in_=pS[:, :], func=AF.Exp), [pS], [E])
                                for qs in range(4):
                                    p_ = po[qs // 2]
                                    c0 = (qs % 2) * 512
                                    kb.op("pe", lambda e: e.matmul(p_[:, c0:c0 + 65], lhsT=E[:, qs * 128:(qs + 1) * 128], rhs=Vall[:, kt, h, :],
                                                                   start=(kt == 0), stop=(kt == 17)), [E, Vall], [p_], inc=(kt == 17))
                            for qs in range(4):
                                p_ = po[qs // 2]
                                c0 = (qs % 2) * 512
                                st = small_rot.get()
                                kb.op("dve", lambda e: e.reciprocal(out=st[:, 0:1], in_=p_[:, c0 + 64:c0 + 65]), [p_], [st])
                                kb.op("dve", lambda e: e.tensor_scalar(out=ap_[:, qs, hh * 64:(hh + 1) * 64], in0=p_[:, c0:c0 + 64], scalar1=st[:, 0:1], scalar2=None,
                                                                       op0=ALU.mult), [p_, st], [ap_])
                        transposes(ap_, [ap_[:, qs, :] for qs in range(4)], attT[:, hp, qb * 512:(qb + 1) * 512], attT)
                if "att_dbg" in dbg and b == 0:
                    kb.dma("sp", dbg_d["att_dbg"][:, :, :], attT[:], reads=[attT], writes=[dbg_d["att_dbg"]])
            if upto <= 4:
                mix_stack.close()
                continue

            with kb.scope():
                wo_f = kb.sb("wo_f", [128, 8, 1024], F32)
                wo1 = kb.sb("wo1", [128, 4, 1024], BF16)
                wo2 = kb.sb("wo2", [128, 4, 1024], BF16)
                wo3 = kb.sb("wo3", [128, 8, 1024], BF16)
                kb.dma("sp", wo_f[:, 0:4, :], I["w_o_mla"].ap().rearrange("(k p) n -> p k n", p=128), writes=[wo_f])
                kb.op("pool", lambda e: e.tensor_copy(out=wo1[:], in_=wo_f[:, 0:4, :]), [wo_f], [wo1])
                kb.dma("sp", wo_f[:, 0:4, :], I["w_o_rwkv"].ap().rearrange("(k p) n -> p k n", p=128), reads=[wo_f], writes=[wo_f])
                kb.op("pool", lambda e: e.tensor_copy(out=wo2[:], in_=wo_f[:, 0:4, :]), [wo_f], [wo2])
                kb.dma("sp", wo_f[:], I["w_out"].ap().rearrange("(k p) n -> p k n", p=128), reads=[wo_f], writes=[wo_f])
                kb.op("pool", lambda e: e.tensor_copy(out=wo3[:], in_=wo_f[:]), [wo_f], [wo3])
                mT = kb.sb("mT", [128, 8, 2048], BF16)
                grot = kb.rot("gt", [128, 2, 512], BF16, 2)
                trot = kb.rot("tm", [128, 512], F32, 2)
                for m in range(8):
                    for nb_ in range(4):
                        g = grot.get()
                        kb.dma("sp", g[:, 0, :], gates_d[m * 128:(m + 1) * 128, nb_ * 512:(nb_ + 1) * 512], reads=[gates_r[m][nb_]], writes=[g])
                        kb.dma("sp", g[:, 1, :], gates_d[(8 + m) * 128:(9 + m) * 128, nb_ * 512:(nb_ + 1) * 512], reads=[gates_r[8 + m][nb_]], writes=[g])
                        p1 = ps1.get(); p2 = ps1.get()
                        for k in range(4):
                            kb.op("pe", lambda e: e.matmul(p1[:, :], lhsT=wo1[:, k, m * 128:(m + 1) * 128], rhs=attT[:, k, nb_ * 512:(nb_ + 1) * 512], start=(k == 0), stop=(k == 3)),
                                  [wo1, attT], [p1], inc=(k == 3))
                        for k in range(4):
                            kb.op("pe", lambda e: e.matmul(p2[:, :], lhsT=wo2[:, k, m * 128:(m + 1) * 128], rhs=rwT[:, k, nb_ * 512:(nb_ + 1) * 512], start=(k == 0), stop=(k == 3)),
                                  [wo2, rwT], [p2], inc=(k == 3))
                        t1 = trot.get(); t2 = trot.get()
                        kb.op("dve", lambda e: e.tensor_tensor(out=t1[:], in0=p1[:, :], in1=g[:, 0, :], op=ALU.mult), [p1, g], [t1])
                        kb.op("dve", lambda e: e.tensor_tensor(out=t2[:], in0=p2[:, :], in1=g[:, 1, :], op=ALU.mult), [p2, g], [t2])
                        kb.op("pool", lambda e: e.tensor_tensor(out=mT[:, m, nb_ * 512:(nb_ + 1) * 512], in0=t1[:], in1=t2[:], op=ALU.add), [t1, t2], [mT])
                Ga = kb.sb("Ga", [128, 1024], F32)
                bcast_load(Ga[:], Ga, mod_d.t.ap()[b:b + 1, 2048:3072], 128, reads=[mod_d])
                xrot = kb.rot("xt5", [128, 1024], F32, 2)
                for t in range(16):
                    xt = xrot.get()
                    kb.dma("sp", xt[:], I["x"].ap()[b, t * 128:(t + 1) * 128, :], writes=[xt])
                    pm = ps2.get()
                    for nb_ in range(2):
                        for k in range(8):
                            kb.op("pe", lambda e: e.matmul(pm[:, nb_ * 512:(nb_ + 1) * 512], lhsT=mT[:, k, t * 128:(t + 1) * 128], rhs=wo3[:, k, nb_ * 512:(nb_ + 1) * 512],
                                                           start=(k == 0), stop=(k == 7)), [mT, wo3], [pm], inc=(k == 7))
                    junk = junk_rot.get()
                    kb.op("dve", lambda e: e.tensor_tensor(out=junk[:], in0=pm[:, :], in1=Ga[:], op=ALU.mult), [pm, Ga], [junk])
                    kb.op("pool", lambda e: e.tensor_tensor(out=xt[:], in0=junk[:], in1=xt[:], op=ALU.add), [junk, xt], [xt])
                    kb.dma("sp", x1_d[(b * 16 + t) * 128:(b * 16 + t + 1) * 128, :], xt[:], reads=[xt], writes=[x1_r[b * 16 + t]])
            kb.barrier()
            mix_stack.close()
            if upto <= 5:
                continue

            pass

        if upto > 5:
          NT = 16 * nbatch
          NBLK = NT * 4 + 256
          with kb.scope():
            I32 = mybir.dt.int32
            u_d = kb.dram("u_d", [NT * 128, 1024], BF16)
            u_r = [Buf() for _ in range(NT)]
            xs_d = kb.dram("xs_d", [NBLK * 256, 1024], BF16)
            Y_d = kb.dram("Y_d", [NBLK * 256, 1024], BF16)
            junk_rot = kb.rot("junk6", [128, 1024], F32, 3)
            small_rot = kb.rot("small6", [128, 16], F32, 8)
            dest_all = kb.sb("dest_all", [128, NT, 8], I32)
            gate_all = kb.sb("gate_all", [128, NT, 8], F32)
            IDXi = kb.sb("IDXi", [128, NBLK], I32)
            tri_f = kb.sb("tri6_f", [128, 4, 128], F32)
            tri = kb.sb("tri6", [128, 4, 128], BF16)
            kb.dma("sp", tri_f[:], I["c_tri"].ap(), writes=[tri_f])
            kb.op("dve", lambda e: e.tensor_copy(out=tri[:], in_=tri_f[:]), [tri_f], [tri])
            iota_p = kb.sb("iota_p", [128, 2], F32)
            kb.dma("sp", iota_p[:], I["c_iotap"].ap(), writes=[iota_p])
            with kb.scope():
                G_all = kb.sb("G_all", [128, NT, 256], F32)
                POS_all = kb.sb("POS_all", [128, NT, 256], F32)
                mask_all = kb.sb("mask_all", [128, NT, 256], BF16)
                cnt = kb.sb("cnt", [128, 256], F32)
                kb.op("pool", lambda e: e.memset(cnt[:], 0.0), [], [cnt])
                with kb.scope():
                    rw_f = kb.sb("rw_f", [128, 8, 256], F32)
                    kb.dma("sp", rw_f[:], I["router_w"].ap().rearrange("(k p) n -> p k n", p=128), writes=[rw_f])
                    rbias = kb.sb("rbias", [128, 256], F32)
                    bcast_load(rbias[:], rbias, I["router_bias"].ap()[0:1, :], 128)
                    xrot = kb.rot("xt6", [128, 1024], F32, 3)
                    ufr = kb.rot("uf", [128, 1024], F32, 3)
                    ubr = kb.rot("ub", [128, 1024], BF16, 3)
                    uTf = kb.rot("uTf", [128, 8, 128], F32, 3)
                    s256 = kb.rot("s256", [128, 256], F32, 12)
                    m8r = kb.rot("m8", [128, 8], F32, 16)
                    A2s = []
                    for b in range(nbatch):
                        A2 = kb.sb("A2", [128, 1024], F32)
                        S2 = kb.sb("S2", [128, 1024], F32)
                        bcast_load(A2[:], A2, mod_d.t.ap()[b:b + 1, 4096:5120], 128, reads=[mod_d])
                        bcast_load(S2[:], S2, mod_d.t.ap()[b:b + 1, 3072:4096], 128, reads=[mod_d])
                        A2s.append((A2, S2))
                    def tileA(i):
                            b = i // 16
                            A2, S2 = A2s[b]
                            xt = xrot.get()
                            kb.dma("sp", xt[:], x1_d[i * 128:(i + 1) * 128, :], reads=[x1_r[i]], writes=[xt])
                            junk = junk_rot.get(); ssq = small_rot.get()
                            kb.op("act", lambda e: e.activation(out=junk[:], in_=xt[:], func=AF.Square, accum_out=ssq[:, 0:1]), [xt], [junk, ssq])
                            rms_rstd(ssq[:, 0:1], ssq[:, 1:2], ssq, ssq, 1024, EPS)
                            yield
                            uf = ufr.get()
                            kb.op("dve", lambda e: e.scalar_tensor_tensor(out=junk[:], in0=xt[:], scalar=ssq[:, 1:2], in1=A2[:], op0=ALU.mult, op1=ALU.mult), [xt, ssq, A2], [junk])
                            kb.op("pool", lambda e: e.tensor_tensor(out=uf[:], in0=junk[:], in1=S2[:], op=ALU.add), [junk, S2], [uf])
                            ub = ubr.get()
                            kb.op("act", lambda e: e.copy(out=ub[:], in_=uf[:]), [uf], [ub])
                            kb.dma("sp", u_d[i * 128:(i + 1) * 128, :], ub[:], reads=[ub], writes=[u_r[i]])
                            yield
                            utf = uTf.get()
                            for half in range(2):
                                transposes(uf, [uf[:, (half * 4 + k) * 128:(half * 4 + k + 1) * 128] for k in range(4)], utf[:, half * 4:(half + 1) * 4, :], utf, dt=F32, eng="dve")
                            pr = ps1.get()
                            for k in range(8):
                                kb.op("pe", lambda e: e.matmul(pr[:, 0:256], lhsT=utf[:, k, :], rhs=rw_f[:, k, :], start=(k == 0), stop=(k == 7)), [utf, rw_f], [pr], inc=(k == 7))
                            sc = s256.get(); sel = s256.get(); w1_ = s256.get(); w2_ = s256.get()
                            kb.op("act", lambda e: e.activation(out=sc[:], in_=pr[:, 0:256], func=AF.Sigmoid), [pr], [sc])
                            kb.op("dve", lambda e: e.tensor_tensor(out=sel[:], in0=sc[:], in1=rbias[:], op=ALU.add), [sc, rbias], [sel])
                            yield
                            sel3 = sel[:].rearrange("p (g c) -> p g c", g=8)
                            mx = m8r.get(); mx2 = m8r.get(); gs = m8r.get(); gsort = m8r.get()
                            kb.op("dve", lambda e: e.tensor_reduce(out=mx[:], in_=sel3, axis=AX.X, op=ALU.max), [sel], [mx])
                            kb.op("dve", lambda e: e.tensor_tensor(out=w1_[:].rearrange("p (g c) -> p g c", g=8), in0=sel3, in1=mx[:].unsqueeze(2).to_broadcast([128, 8, 32]), op=ALU.is_ge),
                                  [sel, mx], [w1_])
                            kb.op("dve", lambda e: e.scalar_tensor_tensor(out=w2_[:], in0=w1_[:], scalar=-10.0, in1=sel[:], op0=ALU.mult, op1=ALU.add), [w1_, sel], [w2_])
                            kb.op("dve", lambda e: e.tensor_reduce(out=mx2[:], in_=w2_[:].rearrange("p (g c) -> p g c", g=8), axis=AX.X, op=ALU.max), [w2_], [mx2])
                            yield
                            kb.op("dve", lambda e: e.tensor_tensor(out=gs[:], in0=mx[:], in1=mx2[:], op=ALU.add), [mx, mx2], [gs])
                            kb.op("dve", lambda e: e.max(out=gsort[:], in_=gs[:]), [gs], [gsort])
                            kb.op("dve", lambda e: e.tensor_scalar(out=gs[:], in0=gs[:], scalar1=gsort[:, 3:4], scalar2=None, op0=ALU.is_ge), [gs, gsort], [gs])
                            kb.op("dve", lambda e: e.scalar_tensor_tensor(out=w1_[:].rearrange("p (g c) -> p g c", g=8), in0=sel3, scalar=2.0,
                                                                          in1=gs[:].unsqueeze(2).to_broadcast([128, 8, 32]), op0=ALU.add, op1=ALU.mult), [sel, gs], [w1_])
                            yield
                            top8 = m8r.get()
                            kb.op("dve", lambda e: e.max(out=top8[:], in_=w1_[:]), [w1_], [top8])
                            kb.op("dve", lambda e: e.tensor_scalar(out=w2_[:], in0=w1_[:], scalar1=top8[:, 7:8], scalar2=None, op0=ALU.is_ge), [w1_, top8], [w2_])
                            kb.op("pool", lambda e: e.tensor_copy(out=mask_all[:, i, :], in_=w2_[:]), [w2_], [mask_all])
                            yield
                            st = small_rot.get()
                            kb.op("dve", lambda e: e.tensor_tensor(out=w1_[:], in0=w2_[:], in1=sc[:], op=ALU.mult), [w2_, sc], [w1_])
                            kb.op("dve", lambda e: e.tensor_reduce(out=st[:, 0:1], in_=w1_[:], axis=AX.X, op=ALU.add), [w1_], [st])
                            kb.op("dve", lambda e: e.reciprocal(out=st[:, 1:2], in_=st[:, 0:1]), [st], [st])
                            kb.op("dve", lambda e: e.tensor_scalar(out=G_all[:, i, :], in0=w1_[:], scalar1=st[:, 1:2], scalar2=2.5, op0=ALU.mult, op1=ALU.mult), [w1_, st], [G_all])
                            yield
                            pc = ps1.get()
                            kb.op("pe", lambda e: e.matmul(pc[:, 0:256], lhsT=tri[:, 1, :], rhs=mask_all[:, i, :], start=True, stop=True), [tri, mask_all], [pc])
                            kb.op("pe", lambda e: e.matmul(pc[:, 256:512], lhsT=ones_b[:], rhs=mask_all[:, i, :], start=True, stop=True), [ones_b, mask_all], [pc])
                            kb.op("dve", lambda e: e.tensor_tensor(out=POS_all[:, i, :], in0=pc[:, 0:256], in1=cnt[:], op=ALU.add), [pc, cnt], [POS_all])
                            kb.op("dve", lambda e: e.tensor_tensor(out=cnt[:], in0=pc[:, 256:512], in1=cnt[:], op=ALU.add), [pc, cnt], [cnt])
                    run_interleaved((tileA(i) for i in range(NT)), 3)
                if "G_dbg" in dbg:
                    kb.dma("sp", dbg_d["G_dbg"][:, :, 0:256], G_all[:, 0:16, :], reads=[G_all], writes=[dbg_d["G_dbg"]])
                base_r = kb.sb("base_r", [128, 256], F32)
                with kb.scope():
                    ind = kb.sb("ind", [128, 256], BF16)
                    kb.op("dve", lambda e: e.tensor_scalar(out=ind[:], in0=cnt[:], scalar1=iota_p[:, 1:2], scalar2=None, op0=ALU.is_gt), [cnt, iota_p], [ind])
                    pn = ps1.get()
                    kb.op("pe", lambda e: e.matmul(pn[:, 0:256], lhsT=ones_b[:], rhs=ind[:], start=True, stop=True), [ones_b, ind], [pn])
                    nblk = kb.sb("nblk", [128, 256], BF16)
                    kb.op("dve", lambda e: e.tensor_copy(out=nblk[:], in_=pn[:, 0:256]), [pn], [nblk])
                    nbT = kb.sb("nbT", [128, 2, 128], BF16)
                    transposes(nblk, [nblk[:, 0:128], nblk[:, 128:256]], nbT[:], nbT)
                    pb = ps1.get()
                    kb.op("pe", lambda e: e.matmul(pb[:, 0:128], lhsT=nbT[:, 0, :], rhs=tri[:, 1, :], start=True, stop=True), [nbT, tri], [pb])
                    kb.op("pe", lambda e: e.matmul(pb[:, 128:256], lhsT=nbT[:, 0, :], rhs=ones_b[:], start=True, stop=False), [nbT, ones_b], [pb], inc=False)
                    kb.op("pe", lambda e: e.matmul(pb[:, 128:256], lhsT=nbT[:, 1, :], rhs=tri[:, 1, :], start=False, stop=True), [nbT, tri], [pb])
                    kb.op("dve", lambda e: e.tensor_copy(out=base_r[:], in_=pb[:, 0:256]), [pb], [base_r])
                    pe_ = ps1.get()
                    kb.op("pe", lambda e: e.matmul(pe_[:, 0:1], lhsT=tri[:, 0, :], rhs=nbT[:, 0, 0:1], start=True, stop=True), [nbT, tri], [pe_])
                    kb.op("pe", lambda e: e.matmul(pe_[:, 1:2], lhsT=ones_b[:], rhs=nbT[:, 0, 0:1], start=True, stop=False), [nbT, ones_b], [pe_], inc=False)
                    kb.op("pe", lambda e: e.matmul(pe_[:, 1:2], lhsT=tri[:, 0, :], rhs=nbT[:, 1, 0:1], start=False, stop=True), [nbT, tri], [pe_])
                    endT = kb.sb("endT", [128, 2], F32)
                    kb.op("dve", lambda e: e.tensor_copy(out=endT[:], in_=pe_[:, 0:2]), [pe_], [endT])
                    iota_b = kb.sb("iota_b", [128, NBLK], F32)
                    kb.dma("sp", iota_b[:], I["c_iotab"].ap()[:, 0:NBLK], writes=[iota_b])
                    ind2 = kb.sb("ind2", [128, 2, NBLK], BF16)
                    for c_ in range(2):
                        kb.op("dve", lambda e: e.tensor_scalar(out=ind2[:, c_, :], in0=iota_b[:], scalar1=endT[:, c_:c_ + 1], scalar2=None, op0=ALU.is_ge), [iota_b, endT], [ind2])
                    BEf = kb.sb("BEf", [128, NBLK], F32)
                    for n0 in range(0, NBLK, 512):
                        n1 = min(n0 + 512, NBLK)
                        pbe = ps1.get()
                        for c_ in range(2):
                            kb.op("pe", lambda e: e.matmul(pbe[:, 0:n1 - n0], lhsT=ones_b[:], rhs=ind2[:, c_, n0:n1], start=(c_ == 0), stop=(c_ == 1)), [ones_b, ind2], [pbe], inc=(c_ == 1))
                        kb.op("dve", lambda e: e.tensor_scalar(out=BEf[:, n0:n1], in0=pbe[:, 0:n1 - n0], scalar1=128.0, scalar2=iota_p[:, 0:1], op0=ALU.mult, op1=ALU.add), [pbe, iota_p], [BEf])
                    kb.op("dve", lambda e: e.tensor_copy(out=IDXi[:], in_=BEf[:]), [BEf], [IDXi])
                with kb.scope():
                    s256 = kb.rot("s256b", [128, 256], F32, 9)
                    m8r = kb.rot("m8b", [128, 8], F32, 6)
                    ubr = kb.rot("ubB", [128, 1024], BF16, 3)
                    def tileB(i):
                            t_ = s256.get(); V = s256.get(); eq = s256.get()
                            kb.op("dve", lambda e: e.scalar_tensor_tensor(out=t_[:], in0=base_r[:], scalar=256.0, in1=POS_all[:, i, :], op0=ALU.mult, op1=ALU.add), [base_r, POS_all], [t_])
                            kb.op("dve", lambda e: e.scalar_tensor_tensor(out=V[:], in0=t_[:], scalar=1.0, in1=mask_all[:, i, :], op0=ALU.add, op1=ALU.mult), [t_, mask_all], [V])
                            yield
                            top8 = m8r.get(); d8 = m8r.get()
                            kb.op("dve", lambda e: e.max(out=top8[:], in_=V[:]), [V], [top8])
                            kb.op("dve", lambda e: e.tensor_scalar(out=d8[:], in0=top8[:], scalar1=-1.0, scalar2=None, op0=ALU.add), [top8], [d8])
                            kb.op("dve", lambda e: e.tensor_copy(out=dest_all[:, i, :], in_=d8[:]), [d8], [dest_all])
                            yield
                            for k in range(8):
                                kb.op("dve", lambda e: e.tensor_scalar(out=eq[:], in0=V[:], scalar1=top8[:, k:k + 1], scalar2=None, op0=ALU.is_equal), [V, top8], [eq])
                                kb.op("pool", lambda e: e.tensor_tensor(out=eq[:], in0=eq[:], in1=G_all[:, i, :], op=ALU.mult), [eq, G_all], [eq])
                                kb.op("dve", lambda e: e.tensor_reduce(out=gate_all[:, i, k:k + 1], in_=eq[:], axis=AX.X, op=ALU.add), [eq], [gate_all])
                                if k % 2 == 1:
                                    yield
                            yield
                            ub = ubr.get()
                            kb.dma("sp", ub[:], u_d[i * 128:(i + 1) * 128, :], reads=[u_r[i]], writes=[ub])
                            for k in range(8):
                                kb.idma(out=xs_d[:, :], in_=ub[:], out_off=dest_all[:, i, k:k + 1], reads=[ub, dest_all], writes=[xs_d])
                    run_interleaved((tileB(i) for i in range(NT)), 3)
            xT_rot = kb.rot("xT", [128, 8, 128], BF16, 3)
            silu_rot = kb.rot("silu", [128, 256], F32, 2)
            hb_rot = kb.rot("hb", [128, 256], BF16, 4)
            hT_rot = kb.rot("hT6", [128, 2, 128], BF16, 3)

            def ffn_a(xsb, wb13, strided):
                if strided:
                    srcs = [xsb[:].rearrange("s (p k) -> s k p", k=8)[:, k, :] for k in range(8)]
                else:
                    srcs = [xsb[:, k * 128:(k + 1) * 128] for k in range(8)]
                xT = xT_rot.get()
                transposes(xsb, srcs, xT[:], xT)
                ph = ps1.get()
                for k in range(8):
                    kb.op("pe", lambda e: e.matmul(ph[:, :], lhsT=xT[:, k, :], rhs=wb13[:, k, :, :].rearrange("p w n -> p (w n)"), start=(k == 0), stop=(k == 7)),
                          [xT, wb13], [ph], inc=(k == 7))
                sl = silu_rot.get(); hb = hb_rot.get()
                kb.op("act", lambda e: e.activation(out=sl[:], in_=ph[:, 0:256], func=AF.Silu), [ph], [sl])
                kb.op("dve", lambda e: e.tensor_tensor(out=hb[:], in0=ph[:, 256:512], in1=sl[:], op=ALU.mult), [ph, sl], [hb])
                return hb

            def ffn_b(hb, wb2, strided):
                if strided:
                    hs = [hb[:].rearrange("s (p j) -> s j p", j=2)[:, j, :] for j in range(2)]
                else:
                    hs = [hb[:, j * 128:(j + 1) * 128] for j in range(2)]
                hT = hT_rot.get()
                transposes(hb, hs, hT[:], hT, eng="dve")
                po_ = ps2.get()
                for nb_ in range(2):
                    for j in range(2):
                        kb.op("pe", lambda e: e.matmul(po_[:, nb_ * 512:(nb_ + 1) * 512], lhsT=hT[:, j, :], rhs=wb2[:, j, nb_ * 512:(nb_ + 1) * 512],
                                                       start=(j == 0), stop=(j == 1)), [hT, wb2], [po_], inc=(j == 1))
                return po_

            def ffn_block(xsb, wb13, wb2, strided):
                return ffn_b(ffn_a(xsb, wb13, strided), wb2, strided)

            with kb.scope():
                w1v = I["expert_w1"].ap().rearrange("e (p k) n -> (e p) (k n)", k=8)
                w3v = I["expert_w3"].ap().rearrange("e (p k) n -> (e p) (k n)", k=8)
                w2v = I["expert_w2"].ap().rearrange("e (p j) n -> (e p) (j n)", j=2)
                wf13 = kb.rot("wf13", [128, 2, 2048], F32, 3)
                wf2 = kb.rot("wf2", [128, 2048], F32, 3)
                wb13r = kb.rot("wb13", [128, 8, 2, 256], BF16, 3)
                wb2r = kb.rot("wb2", [128, 2, 1024], BF16, 4)
                xsr = kb.rot("xsb", [128, 1024], BF16, 8)
                ybr = kb.rot("yb", [128, 1024], BF16, 3)
                for t_ in wf13.tiles + wf2.tiles:
                    kb.op("pool", lambda e: e.memset(t_[:], 0.0), [], [t_])
                pend = {}

                def issue(bk):
                    f13 = wf13.get(); f2 = wf2.get()
                    idx = IDXi[:, bk:bk + 1]
                    kb.idma(out=f13[:, 0, :], in_=w1v, in_off=idx, reads=[IDXi], writes=[f13], bounds=32767)
                    kb.idma(out=f13[:, 1, :], in_=w3v, in_off=idx, reads=[IDXi], writes=[f13], bounds=32767)
                    kb.idma(out=f2[:], in_=w2v, in_off=idx, reads=[IDXi], writes=[f2], bounds=32767)
                    xs2 = []
                    for sbk in range(2):
                        xsb = xsr.get()
                        r0 = (bk * 2 + sbk) * 128
                        kb.dma("sp", xsb[:], xs_d[r0:r0 + 128, :], reads=[xs_d], writes=[xsb])
                        xs2.append(xsb)
                    pend[bk] = (f13, f2, xs2)

                nrun = min(NBLK, blk_limit)
                NSUB = nrun * 2
                ctx_ = {}

                def st0(n):
                    bk = n // 2
                    if n % 2 == 0:
                        if bk + 2 < nrun:
                            issue(bk + 2)
                        f13, f2, xs2 = pend.pop(bk)
                        wb13 = wb13r.get(); wb2 = wb2r.get()
                        kb.op("act", lambda e: e.copy(out=wb13[:, :, 0, :], in_=f13[:, 0, :].rearrange("p (k n) -> p k n", k=8)), [f13], [wb13])
                        kb.op("dve", lambda e: e.tensor_copy(out=wb13[:, :, 1, :], in_=f13[:, 1, :].rearrange("p (k n) -> p k n", k=8)), [f13], [wb13])
                        kb.op("act", lambda e: e.copy(out=wb2[:, 0, :], in_=f2[:, 0:1024]), [f2], [wb2])
                        kb.op("dve", lambda e: e.tensor_copy(out=wb2[:, 1, :], in_=f2[:, 1024:2048]), [f2], [wb2])
                        ctx_[("w", bk)] = (wb13, wb2, xs2)
                    wb13, wb2, xs2 = ctx_[("w", bk)]
                    xsb = xs2[n % 2]
                    srcs = [xsb[:].rearrange("s (p k) -> s k p", k=8)[:, k, :] for k in range(8)]
                    xT = xT_rot.get()
                    transposes(xsb, srcs, xT[:], xT)
                    ctx_[n] = dict(xT=xT, wb13=wb13, wb2=wb2)

                def st1(n):
                    c = ctx_[n]
                    xT, wb13 = c["xT"], c["wb13"]
                    ph = ps1.get()
                    for k in range(8):
                        kb.op("pe", lambda e: e.matmul(ph[:, :], lhsT=xT[:, k, :], rhs=wb13[:, k, :, :].rearrange("p w n -> p (w n)"), start=(k == 0), stop=(k == 7)),
                              [xT, wb13], [ph], inc=(k == 7))
                    sl = silu_rot.get(); hb = hb_rot.get()
                    kb.op("act", lambda e: e.activation(out=sl[:], in_=ph[:, 0:256], func=AF.Silu), [ph], [sl])
                    kb.op("dve", lambda e: e.tensor_tensor(out=hb[:], in0=ph[:, 256:512], in1=sl[:], op=ALU.mult), [ph, sl], [hb])
                    c["hb"] = hb

                def st2(n):
                    c = ctx_[n]
                    hb = c["hb"]
                    hs = [hb[:].rearrange("s (p j) -> s j p", j=2)[:, j, :] for j in range(2)]
                    hT = hT_rot.get()
                    transposes(hb, hs, hT[:], hT, eng="dve")
                    c["hT"] = hT

                def st3(n):
                    c = ctx_.pop(n)
                    hT, wb2 = c["hT"], c["wb2"]
                    po_ = ps2.get()
                    for nb_ in range(2):
                        for j in range(2):
                            kb.op("pe", lambda e: e.matmul(po_[:, nb_ * 512:(nb_ + 1) * 512], lhsT=hT[:, j, :], rhs=wb2[:, j, nb_ * 512:(nb_ + 1) * 512],
                                                           start=(j == 0), stop=(j == 1)), [hT, wb2], [po_], inc=(j == 1))
                    yb = ybr.get()
                    kb.op("act", lambda e: e.copy(out=yb[:, 0:512], in_=po_[:, 0:512]), [po_], [yb])
                    kb.op("dve", lambda e: e.tensor_copy(out=yb[:, 512:1024], in_=po_[:, 512:1024]), [po_], [yb])
                    kb.dma("sp", Y_d[n * 128:(n + 1) * 128, :], yb[:], reads=[yb], writes=[Y_d])

                for bk in range(min(2, nrun)):
                    issue(bk)
                stages = (st0, st1, st2, st3)
                for step in range(NSUB + 3):
                    for k_, fn_ in enumerate(stages):
                        n = step - k_
                        if 0 <= n < NSUB:
                            fn_(n)
            with kb.scope():
                wsf = kb.sb("wsf", [128, 8, 256], F32)
                ws13 = kb.sb("ws13", [128, 8, 2, 256], BF16)
                ws2f = kb.sb("ws2f", [128, 2, 1024], F32)
                ws2 = kb.sb("ws2", [128, 2, 1024], BF16)
                for w_, nm in enumerate(("shared_w1", "shared_w3")):
                    kb.dma("sp", wsf[:], I[nm].ap().rearrange("(k p) n -> p k n", p=128), reads=[wsf], writes=[wsf])
                    kb.op("dve", lambda e: e.tensor_copy(out=ws13[:, :, w_, :], in_=wsf[:]), [wsf], [ws13])
                kb.dma("sp", ws2f[:], I["shared_w2"].ap().rearrange("(j p) n -> p j n", p=128), writes=[ws2f])
                kb.op("dve", lambda e: e.tensor_copy(out=ws2[:], in_=ws2f[:]), [ws2f], [ws2])
                Gms = []
                for b in range(nbatch):
                    Gm = kb.sb("Gm", [128, 1024], F32)
                    bcast_load(Gm[:], Gm, mod_d.t.ap()[b:b + 1, 5120:6144], 128, reads=[mod_d])
                    Gms.append(Gm)
                ubr = kb.rot("ubD", [128, 1024], BF16, 3)
                ygr = kb.rot("yg", [128, 1024], BF16, 8)
                accr = kb.rot("acc", [128, 1024], F32, 3)
                xrot = kb.rot("xtD", [128, 1024], F32, 3)
                def tileD(i):
                        b = i // 16
                        ub = ubr.get()
                        kb.dma("sp", ub[:], u_d[i * 128:(i + 1) * 128, :], reads=[u_r[i]], writes=[ub])
                        yield
                        hb_ = ffn_a(ub, ws13, False)
                        yield
                        po_ = ffn_b(hb_, ws2, False)
                        acc = accr.get()
                        kb.op("act", lambda e: e.copy(out=acc[:], in_=po_[:, :]), [po_], [acc])
                        yield
                        for k in range(8):
                            yg = ygr.get()
                            kb.idma(out=yg[:], in_=Y_d[:, :], in_off=dest_all[:, i, k:k + 1], reads=[dest_all, Y_d], writes=[yg])
                            kb.op("dve", lambda e: e.scalar_tensor_tensor(out=acc[:], in0=yg[:], scalar=gate_all[:, i, k:k + 1], in1=acc[:], op0=ALU.mult, op1=ALU.add),
                                  [yg, gate_all, acc], [acc])
                            if k % 2 == 1:
                                yield
                        if "moe_dbg" in dbg and i < 16:
                            kb.dma("sp", dbg_d["moe_dbg"][:, i, :], acc[:], reads=[acc], writes=[dbg_d["moe_dbg"]])
                        yield
                        xt = xrot.get()
                        kb.dma("sp", xt[:], x1_d[i * 128:(i + 1) * 128, :], reads=[x1_r[i]], writes=[xt])
                        kb.op("dve", lambda e: e.tensor_tensor(out=acc[:], in0=acc[:], in1=Gms[b][:], op=ALU.mult), [acc, Gms[b]], [acc])
                        kb.op("pool", lambda e: e.tensor_tensor(out=xt[:], in0=xt[:], in1=acc[:], op=ALU.add), [xt, acc], [xt])
                        kb.dma("sp", out_d.ap()[b, (i % 16) * 128:(i % 16 + 1) * 128, :], xt[:], reads=[xt])
                run_interleaved((tileD(i) for i in range(NT)), 3)
        kb.finish()
        print("instructions:", kb.nins, "sems:", len(kb.sems) + len(kb.dsem))
    return nc


_CACHE = {}


def kernel(**inputs):
    n = 8
    consts = host_consts()
    sq = lambda k: np.ascontiguousarray(np.asarray(inputs[k], dtype=np.float32)[0])
    shared = {}
    for k in IN_SHAPES:
        if k in ("x", "ctx", "cT") or k.startswith("c_"):
            continue
        a = sq(k)
        shared[k] = a.reshape(IN_SHAPES[k])
    shared.update(consts)
    x = np.asarray(inputs["x"], np.float32)
    ctx = np.asarray(inputs["ctx"], np.float32)
    c = np.asarray(inputs["c"], np.float32)
    c_ctx = np.asarray(inputs["c_ctx"], np.float32)
    in_maps = []
    for i in range(n):
        m = dict(shared)
        m["x"] = np.ascontiguousarray(x[2 * i:2 * i + 2])
        m["ctx"] = np.ascontiguousarray(ctx[2 * i:2 * i + 2])
        m["cT"] = np.ascontiguousarray(np.stack([c[2 * i], c[2 * i + 1], c_ctx], axis=1))
        in_maps.append(m)
    if "nc" not in _CACHE:
        _CACHE["nc"] = build()
    res = run_bass_kernel_spmd(_CACHE["nc"], in_maps, core_ids=list(range(n)))
    return np.concatenate([np.asarray(r["out"], np.float32) for r in res.results], axis=0)
```

```python
import numpy as np
import ml_dtypes
import concourse.bass as bass
import concourse.mybir as mybir
from concourse.bass_utils import run_bass_kernel_spmd
from contextlib import ExitStack, contextmanager

F32 = mybir.dt.float32
BF16 = mybir.dt.bfloat16
AF = mybir.ActivationFunctionType
ALU = mybir.AluOpType
AX = mybir.AxisListType

C_DEC = -0.6065306597126334
EPS = 1e-6
GN_EPS = 64e-5
ATTN_SCALE = 96 ** -0.5


NAMES = {}


class Buf:
    __slots__ = ("name", "w", "r", "excl")

    def __init__(self, name=""):
        self.name = name
        self.w = None
        self.r = {}
        self.excl = False


class Tile:
    def __init__(self, t, name):
        self.t = t
        self.b = Buf(name)

    def __getitem__(self, k):
        return self.t[k]


class Rot:
    def __init__(self, tiles):
        self.tiles = tiles
        self.i = 0

    def get(self):
        t = self.tiles[self.i % len(self.tiles)]
        self.i += 1
        return t


class KB:
    ENG = ("pe", "act", "dve", "pool", "sp")
    EPOCH = 8000
    NDMA = 40

    def __init__(self, nc, stack):
        self.nc = nc
        self.gstack = stack
        self.stack = stack
        self.e = {"pe": nc.tensor, "act": nc.scalar, "dve": nc.vector,
                  "pool": nc.gpsimd, "sp": nc.sync}
        self.cnt = {k: 0 for k in self.ENG}
        self.ep = {k: 0 for k in self.ENG}
        self.sems = {}
        self.waited = {}
        for k in self.ENG:
            self._newsem(k)
        self.dsem = []
        self.dslot = []
        self.dval = []
        for i in range(self.NDMA):
            self.dsem.append(stack.enter_context(nc.semaphore(f"dq{i}")))
            self.dslot.append(i)
            self.dval.append(0)
        self.dnext = 0
        self.dwaited = {}
        self.nins = 0
        self.uid = 0
        self.promised = False
        self.bregs = {}

    def sb(self, name, shape, dtype):
        self.uid += 1
        nm = f"{name}_{self.uid}"
        NAMES[name] = nm
        return Tile(self.stack.enter_context(self.nc.sbuf_tensor(nm, list(shape), dtype)), nm)

    def ps(self, name, shape, dtype=F32):
        t = Tile(self.gstack.enter_context(self.nc.psum_tensor(name, list(shape), dtype)), name)
        t.b.excl = True
        return t

    def dram(self, name, shape, dtype, kind="Internal"):
        return Tile(self.nc.dram_tensor(name, list(shape), dtype, kind=kind), name)

    def rot(self, name, shape, dtype, n=2):
        return Rot([self.sb(f"{name}{i}", shape, dtype) for i in range(n)])

    @contextmanager
    def scope(self):
        old = self.stack
        with ExitStack() as st:
            self.stack = st
            yield
            self.barrier()
        self.stack = old

    def _newsem(self, k):
        self.sems[(k, self.ep[k])] = self.gstack.enter_context(
            self.nc.semaphore(f"s_{k}_{self.ep[k]}"))

    def _wait(self, eng, tok):
        if tok is None:
            return
        if tok[0] == "e":
            _, k, ep, c = tok
            if eng == "pe" and k == "pe":
                return
            assert not (k == "pe" and ep == self.ep["pe"] and c > self.cnt["pe"]), "wait on a promised PE token"
            key = (eng, k, ep)
            if self.waited.get(key, 0) >= c:
                return
            self.e[eng].wait_ge(self.sems[(k, ep)], c)
            self.waited[key] = c
        else:
            _, i, v = tok
            key = (eng, i)
            if self.dwaited.get(key, 0) >= v:
                return
            self.e[eng].wait_ge(self.dsem[i], v)
            self.dwaited[key] = v

    @staticmethod
    def _bufs(xs):
        out = []
        for x in xs:
            if x is None:
                continue
            out.append(x.b if isinstance(x, Tile) else x)
        return out

    def _deps(self, eng, reads, writes):
        for b in reads:
            self._wait(eng, b.w)
        for b in writes:
            self._wait(eng, b.w)
            for t in b.r.values():
                self._wait(eng, t)

    def _commit(self, tok, reads, writes):
        for b in writes:
            b.w = tok
            b.r = {}
        for b in reads:
            if b not in writes:
                key = tok[1] if tok[0] == "e" else ("d", tok[1])
                b.r[key] = tok

    def op(self, eng, fn, reads=(), writes=(), inc=True):
        reads = self._bufs(reads)
        writes = self._bufs(writes)
        writes = writes + [b for b in reads if b.excl and b not in writes]
        self._deps(eng, reads, writes)
        if inc and self.cnt[eng] >= self.EPOCH and not (eng == "pe" and self.promised):
            self.ep[eng] += 1
            self.cnt[eng] = 0
            self._newsem(eng)
        ins = fn(self.e[eng])
        if eng == "pe":
            self.promised = not inc
        if inc:
            self.cnt[eng] += 1
            ins.then_inc(self.sems[(eng, self.ep[eng])], 1)
            tok = ("e", eng, self.ep[eng], self.cnt[eng])
        else:
            assert eng == "pe"
            tok = ("e", eng, self.ep[eng], self.cnt[eng] + 1)
        self._commit(tok, reads, writes)
        self.nins += 1

    def dma(self, eng, out, in_, reads=(), writes=(), **kw):
        reads = self._bufs(reads)
        writes = self._bufs(writes)
        self._deps(eng, reads, writes)
        s = self.dnext
        self.dnext = (self.dnext + 1) % self.NDMA
        i = self.dslot[s]
        if self.dval[i] >= 8000:
            self.dsem.append(self.gstack.enter_context(self.nc.semaphore(f"dq{len(self.dsem)}")))
            self.dval.append(0)
            prev = i
            i = len(self.dsem) - 1
            self.dslot[s] = i
            self._wait(eng, ("d", prev, self.dval[prev]))
        if self.dval[i] > 0:
            self._wait(eng, ("d", i, self.dval[i]))
        self.dval[i] += 16
        self.e[eng].dma_start(out=out, in_=in_, **kw).then_inc(self.dsem[i], 16)
        tok = ("d", i, self.dval[i])
        self._commit(tok, reads, writes)
        self.nins += 1

    def idma(self, out, in_, out_off=None, in_off=None, reads=(), writes=(), bounds=None):
        eng = "pool"
        reads = self._bufs(reads)
        writes = self._bufs(writes)
        self._deps(eng, reads, writes)
        s = self.dnext
        self.dnext = (self.dnext + 1) % self.NDMA
        i = self.dslot[s]
        if self.dval[i] >= 8000:
            self.dsem.append(self.gstack.enter_context(self.nc.semaphore(f"dq{len(self.dsem)}")))
            self.dval.append(0)
            prev = i
            i = len(self.dsem) - 1
            self.dslot[s] = i
            self._wait(eng, ("d", prev, self.dval[prev]))
        if self.dval[i] > 0:
            self._wait(eng, ("d", i, self.dval[i]))
        self.dval[i] += 16
        kw = {}
        if bounds is not None:
            if bounds not in self.bregs:
                r = self.nc.gpsimd.alloc_register(f"bnd{bounds}")
                self.nc.gpsimd.reg_mov(r, bounds)
                self.bregs[bounds] = r
            kw = dict(bounds_check=self.bregs[bounds], oob_is_err=False)
        self.e[eng].indirect_dma_start(
            out=out, out_offset=None if out_off is None else bass.IndirectOffsetOnAxis(ap=out_off, axis=0),
            in_=in_, in_offset=None if in_off is None else bass.IndirectOffsetOnAxis(ap=in_off, axis=0), **kw,
        ).then_inc(self.dsem[i], 16)
        tok = ("d", i, self.dval[i])
        self._commit(tok, reads, writes)
        self.nins += 1

    def finish(self):
        for i in range(len(self.dsem)):
            if self.dval[i] > 0:
                self._wait("sp", ("d", i, self.dval[i]))
        for k in self.ENG:
            if k != "sp" and (self.cnt[k] > 0 or self.ep[k] > 0):
                self._wait("sp", ("e", k, self.ep[k], self.cnt[k]))

    def barrier(self):
        self.finish()
        self.op("sp", lambda e: e.nop(), (), ())
        tok = ("e", "sp", self.ep["sp"], self.cnt["sp"])
        for k in ("pe", "act", "dve", "pool"):
            self._wait(k, tok)


def run_interleaved(gens, width):
    it = iter(gens)
    active = []
    exhausted = False
    while True:
        while len(active) < width and not exhausted:
            try:
                active.append(next(it))
            except StopIteration:
                exhausted = True
        if not active:
            break
        for g in list(active):
            try:
                next(g)
            except StopIteration:
                active.remove(g)


def host_consts():
    s = np.arange(128)[:, None]
    t = np.arange(128)[None, :]
    le = (s <= t).astype(np.float32)
    lt = (s < t).astype(np.float32)
    ge = (s >= t).astype(np.float32)
    gt = (s > t).astype(np.float32)
    tri = np.stack([le, lt, ge, gt], axis=1)
    mask4 = np.zeros((2, 128, 512), np.float32)
    maskt = np.zeros((2, 128, 128), np.float32)
    for d, (strict, incl) in enumerate(((lt, le), (gt, ge))):
        mask4[d, :, 0:128] = -strict
        mask4[d, :, 128:256] = -incl
        mask4[d, :, 256:384] = strict
        mask4[d, :, 384:512] = incl
        maskt[d] = -strict.T
    rows = 2048 // 64
    row = np.repeat(np.arange(rows, dtype=np.float32), 64)
    col = np.tile(np.arange(64, dtype=np.float32), rows)
    inv = (10000.0 ** (-np.arange(8, dtype=np.float32) / 8)).astype(np.float32)
    ang = np.concatenate([row[:, None] * inv, col[:, None] * inv], axis=-1).astype(np.float32)
    p_ = np.arange(128, dtype=np.float32)
    iotap = np.stack([p_, 256 * p_], axis=1)
    iotab = np.tile(np.arange(512, dtype=np.float32)[None, :], (128, 1))
    return dict(c_iotap=iotap, c_iotab=iotab, c_ident=np.eye(128, dtype=np.float32), c_tri=tri, c_mask4=mask4, c_maskt=maskt,
                c_cos=np.cos(ang).astype(np.float32), c_sin=np.sin(ang).astype(np.float32))


IN_SHAPES = dict(
    x=[2, 2048, 1024], ctx=[2, 256, 1024], cT=[1024, 3],
    ada_w=[1024, 6144], ada_b=[1, 6144], norm_mix=[1, 1024], norm_ffn=[1, 1024],
    w_in=[1024, 4416], shift_conv=[3, 1952], q_lat_norm=[1, 256], w_uq=[256, 768],
    kv_lat_norm=[1, 128], w_ukv=[128, 1024], q_norm=[1, 96], k_norm=[1, 96], w_o_mla=[512, 1024],
    decay_w0=[2, 512], decay_w2=[2, 64, 512], aicl_a0=[2, 512], aicl_a2=[2, 64, 512],
    k_k=[1, 512], k_a=[1, 512], r_k=[1, 512], gn_w=[1, 512], gn_b=[1, 512], gate_g2=[160, 512],
    w_o_rwkv=[512, 1024], w_out=[1024, 1024], router_w=[1024, 256], router_bias=[1, 256],
    expert_w1=[256, 1024, 256], expert_w3=[256, 1024, 256], expert_w2=[256, 256, 1024],
    shared_w1=[1024, 256], shared_w3=[1024, 256], shared_w2=[256, 1024],
    c_ident=[128, 128], c_tri=[128, 4, 128], c_mask4=[2, 128, 512], c_maskt=[2, 128, 128],
    c_cos=[2048, 16], c_sin=[2048, 16], c_iotap=[128, 2], c_iotab=[128, 512],
)

CT0 = 1
LT0 = 259
HW = 2308


def build(upto=99, dbg=(), n_exp=256, nbatch=2, rw_tiles=99, rw_phase=9, rw_dirs=2, blk_limit=10 ** 9):
    nc = bass.Bass("TRN2", target_bir_lowering=False)
    I = {k: nc.dram_tensor(k, list(v), F32, kind="ExternalInput") for k, v in IN_SHAPES.items()
         if not (upto < 6 and k.startswith(("expert_", "shared_")))}
    out_d = nc.dram_tensor("out", [2, 2048, 1024], F32, kind="ExternalOutput")

    with ExitStack() as gst:
        kb = KB(nc, gst)
        dbgk = lambda n: ("ExternalOutput" if n in dbg else "Internal")
        ps1 = Rot([kb.ps(f"ps1_{i}", [128, 512]) for i in range(4)])
        ps2 = Rot([kb.ps(f"ps2_{i}", [128, 1024]) for i in range(2)])

        mod_d = kb.dram("mod_d", [3, 6144], F32, dbgk("mod_d"))
        mla_d = kb.dram("mla_d", [2304, 416], F32, dbgk("mla_d"))
        mla_r = [Buf() for _ in range(18)]
        rkv_d = kb.dram("rkv_d", [2304, 1536], F32, dbgk("rkv_d"))
        rkv_r = [Buf() for _ in range(18)]
        gates_d = kb.dram("gates_d", [2048, 2048], BF16, dbgk("gates_d"))
        gates_r = [[Buf() for _ in range(4)] for _ in range(16)]
        x1_d = kb.dram("x1_d", [4096, 1024], F32, dbgk("x1_d"))
        x1_r = [Buf() for _ in range(32)]
        dbg_d = {}
        for n, shp, dt in (("fm_dbg", [128, 4, 2304], BF16), ("att_dbg", [128, 4, 2048], BF16),
                           ("y_dbg", [128, 16, 512], F32), ("rw_dbg", [128, 4, 2048], BF16),
                           ("G_dbg", [128, 16, 257], F32), ("qT_dbg", [96, 8, 2048], BF16),
                           ("kT_dbg", [96, 8, 2304], BF16), ("moe_dbg", [128, 16, 1024], F32)):
            if n in dbg:
                dbg_d[n] = kb.dram(n, shp, dt, "ExternalOutput")

        ident_f = kb.sb("ident_f", [128, 128], F32)
        ident_b = kb.sb("ident_b", [128, 128], BF16)
        ones_b = kb.sb("ones_b", [128, 128], BF16)
        kb.dma("sp", ident_f[:], I["c_ident"].ap(), writes=[ident_f])
        kb.op("dve", lambda e: e.tensor_copy(out=ident_b[:], in_=ident_f[:]), [ident_f], [ident_b])
        kb.op("pool", lambda e: e.memset(ones_b[:], 1.0), [], [ones_b])

        def bcast_load(dst_ap, dst_tile, src_ap, nparts, reads=()):
            kb.dma("sp", dst_ap, src_ap.to_broadcast([nparts, src_ap.shape[-1]]), reads=reads, writes=[dst_tile])

        def transposes(src_tile, src_aps, dst_ap, dst_tile, dt=BF16, eng="act", width=128, pool=None):
            pp = (pool or ps1).get()
            pv = pp[:].bitcast(BF16) if dt == BF16 else pp[:]
            idn = ident_b if dt == BF16 else ident_f
            n = len(src_aps)
            P = src_aps[0].shape[0]
            w = src_aps[0].shape[1]
            for j, a in enumerate(src_aps):
                kb.op("pe", lambda e: e.transpose(out=pv[0:w, j * width:j * width + P], in_=a, identity=idn[0:P, 0:P]),
                      [src_tile, idn], [pp])
            src = pv[0:w, 0:n * width]
            if eng == "act":
                kb.op("act", lambda e: e.copy(out=dst_ap, in_=src.rearrange("p (j t) -> p j t", j=n) if len(dst_ap.shape) == 3 else src), [pp], [dst_tile])
            else:
                kb.op(eng, lambda e: e.tensor_copy(out=dst_ap, in_=src.rearrange("p (j t) -> p j t", j=n) if len(dst_ap.shape) == 3 else src), [pp], [dst_tile])

        def rms_rstd(ssq_ap, out_ap, tile_in, tile_out, n, eps):
            kb.op("act", lambda e: e.activation(out=out_ap, in_=ssq_ap, func=AF.Sqrt, bias=eps_t[:, 0:1] if eps == EPS else (gneps_t[:, 0:1] if eps == GN_EPS else tiny_t[:, 0:1]), scale=1.0 / n),
                  [tile_in], [tile_out])
            kb.op("dve", lambda e: e.reciprocal(out=out_ap, in_=out_ap), [tile_out], [tile_out])

        eps_t = kb.sb("eps_t", [128, 1], F32)
        gneps_t = kb.sb("gneps_t", [128, 1], F32)
        tiny_t = kb.sb("tiny_t", [128, 1], F32)
        kb.op("pool", lambda e: e.memset(eps_t[:], EPS), [], [eps_t])
        kb.op("pool", lambda e: e.memset(gneps_t[:], GN_EPS), [], [gneps_t])
        kb.op("pool", lambda e: e.memset(tiny_t[:], 1e-12), [], [tiny_t])

        with kb.scope():
            cTs = kb.sb("cTs", [128, 8, 3], F32)
            kb.dma("sp", cTs[:], I["cT"].ap().rearrange("(k p) r -> p k r", p=128), writes=[cTs])
            sT = kb.sb("sT", [128, 8, 3], F32)
            kb.op("act", lambda e: e.activation(out=sT[:], in_=cTs[:], func=AF.Silu), [cTs], [sT])
            modsb = kb.sb("modsb", [3, 6144], F32)
            adab = kb.sb("adab", [3, 6144], F32)
            bcast_load(adab[:], adab, I["ada_b"].ap()[0:1, :], 3)
            nrm = kb.sb("nrm", [3, 2, 1024], F32)
            bcast_load(nrm[:, 0, :], nrm, I["norm_mix"].ap()[0:1, :], 3)
            bcast_load(nrm[:, 1, :], nrm, I["norm_ffn"].ap()[0:1, :], 3)
            wrot = kb.rot("adaw", [128, 8, 512], F32, 2)
            for cb in range(12):
                wt = wrot.get()
                kb.dma("sp", wt[:], I["ada_w"].ap()[:, cb * 512:(cb + 1) * 512].rearrange("(k p) n -> p k n", p=128), writes=[wt])
                pp = ps1.get()
                for k in range(8):
                    kb.op("pe", lambda e: e.matmul(pp[0:3, :], lhsT=sT[:, k, :], rhs=wt[:, k, :], start=(k == 0), stop=(k == 7)),
                          [sT, wt], [pp], inc=(k == 7))
                kb.op("dve", lambda e: e.tensor_tensor(out=modsb[:, cb * 512:(cb + 1) * 512], in0=pp[0:3, :],
                                                       in1=adab[:, cb * 512:(cb + 1) * 512], op=ALU.add), [pp, adab], [modsb])
            for (c0, j) in ((1024, 0), (4096, 1)):
                kb.op("dve", lambda e: e.scalar_tensor_tensor(out=modsb[:, c0:c0 + 1024], in0=modsb[:, c0:c0 + 1024], scalar=1.0,
                                                              in1=nrm[:, j, :], op0=ALU.add, op1=ALU.mult), [modsb, nrm], [modsb])
            kb.dma("sp", mod_d[:, :], modsb[:], reads=[modsb], writes=[mod_d])
        if upto <= 0:
            kb.finish()
            return nc

        def norm_mod(xt, A, S, hb):
            junk = junk_rot.get()
            ssq = small_rot.get()
            kb.op("act", lambda e: e.activation(out=junk[:], in_=xt[:], func=AF.Square, accum_out=ssq[:, 0:1]), [xt], [junk, ssq])
            rms_rstd(ssq[:, 0:1], ssq[:, 1:2], ssq, ssq, 1024, EPS)
            kb.op("dve", lambda e: e.scalar_tensor_tensor(out=junk[:], in0=xt[:], scalar=ssq[:, 1:2], in1=A[:], op0=ALU.mult, op1=ALU.mult),
                  [xt, ssq, A], [junk])
            kb.op("pool", lambda e: e.tensor_tensor(out=hb[:], in0=junk[:], in1=S[:], op=ALU.add), [junk, S], [hb])

        for b in range(nbatch):
          with kb.scope():
            junk_rot = kb.rot("junk", [128, 1024], F32, 2)
            small_rot = kb.rot("small", [128, 16], F32, 6)
            mix_stack = ExitStack()
            fm_stack = ExitStack()
            _old = kb.stack
            kb.stack = mix_stack
            attT = kb.sb("attT", [128, 4, 2048], BF16)
            rwT = kb.sb("rwT", [128, 4, 2048], BF16)
            kb.stack = fm_stack
            fm = kb.sb("fm", [128, 4, 2304], BF16)
            kb.stack = _old

            with kb.scope():
                hT = kb.sb("hT", [128, 8, HW], BF16)
                for (c0, c1) in ((0, 1), (257, 259), (2307, 2308)):
                    kb.op("pool", lambda e: e.memset(hT[:, :, c0:c1], 0.0), [], [hT])
                with kb.scope():
                  xrot = kb.rot("xt", [128, 1024], F32, 2)
                  hbrot = kb.rot("hb", [128, 1024], BF16, 2)
                  for seg, (src, nt, off, row) in enumerate(((I["ctx"], 2, CT0, 2), (I["x"], 16, LT0, b))):
                    A1 = kb.sb("A1", [128, 1024], F32)
                    S1 = kb.sb("S1", [128, 1024], F32)
                    bcast_load(A1[:], A1, mod_d.t.ap()[row:row + 1, 1024:2048], 128, reads=[mod_d])
                    bcast_load(S1[:], S1, mod_d.t.ap()[row:row + 1, 0:1024], 128, reads=[mod_d])
                    for t in range(nt):
                        xt = xrot.get()
                        kb.dma("sp", xt[:], src.ap()[b, t * 128:(t + 1) * 128, :], writes=[xt])
                        hb = hbrot.get()
                        norm_mod(xt, A1, S1, hb)
                        transposes(hb, [hb[:, k * 128:(k + 1) * 128] for k in range(8)],
                                   hT[:, :, off + t * 128: off + (t + 1) * 128], hT)
                conv = kb.sb("conv", [128, 3, 1952], F32)
                for j in range(3):
                    bcast_load(conv[:, j, :], conv, I["shift_conv"].ap()[j:j + 1, :], 128)
                wf = kb.sb("wf", [128, 8, 512], F32)
                wbs = [kb.rot(f"wb{j}", [128, 8, 512], BF16, 2) for j in range(3)]
                osb = kb.rot("osb", [128, 512], F32, 2)
                gsb = kb.rot("gsb", [128, 512], BF16, 2)
                tok_tiles = [(CT0 + t * 128, t) for t in range(2)] + [(LT0 + t * 128, 2 + t) for t in range(16)]
                blocks = [(0, 416, "M")] + [(416 + i * 512, 512, "R") for i in range(3)] + [(1952, 416, "F")] + \
                         [(2368 + i * 512, 512, "G") for i in range(4)]
                for (c0, ncol, kind) in blocks:
                    kb.dma("sp", wf[:, :, 0:ncol], I["w_in"].ap()[:, c0:c0 + ncol].rearrange("(k p) n -> p k n", p=128), writes=[wf])
                    has_conv = kind in ("R", "F")
                    ws = []
                    for j in (range(3) if has_conv else range(1)):
                        wb = wbs[j].get()
                        if has_conv:
                            cc = c0 - 416
                            for k in range(8):
                                kb.op("pool" if k % 2 else "dve", lambda e: e.tensor_tensor(out=wb[:, k, 0:ncol], in0=wf[:, k, 0:ncol],
                                                                                           in1=conv[:, j, cc:cc + ncol], op=ALU.mult), [wf, conv], [wb])
                        else:
                            kb.op("pool", lambda e: e.tensor_copy(out=wb[:, :, 0:ncol], in_=wf[:, :, 0:ncol]), [wf], [wb])
                        ws.append(wb)
                    shifts = [(-1, 0), (0, 1), (1, 2)] if has_conv else [(0, 0)]
                    if kind in ("M", "R"):
                        for (off, ti) in tok_tiles:
                            pp = ps1.get()
                            n = len(shifts) * 8
                            i = 0
                            for (sh, j) in shifts:
                                for k in range(8):
                                    kb.op("pe", lambda e: e.matmul(pp[:, 0:ncol], lhsT=hT[:, k, off + sh:off + sh + 128], rhs=ws[j][:, k, 0:ncol],
                                                                   start=(i == 0), stop=(i == n - 1)), [hT, ws[j]], [pp], inc=(i == n - 1))
                                    i += 1
                            o = osb.get()
                            kb.op("act", lambda e: e.copy(out=o[:, 0:ncol], in_=pp[:, 0:ncol]), [pp], [o])
                            if kind == "M":
                                kb.dma("sp", mla_d[ti * 128:(ti + 1) * 128, :], o[:, 0:416], reads=[o], writes=[mla_r[ti]])
                            else:
                                cc = c0 - 416
                                kb.dma("sp", rkv_d[ti * 128:(ti + 1) * 128, cc:cc + 512], o[:, :], reads=[o], writes=[rkv_r[ti]])
                    elif kind == "F":
                        nblks = [(CT0, 0, 256)] + [(LT0 + i * 512, 256 + i * 512, 512) for i in range(4)]
                        for m, (m0, msz) in enumerate(((0, 128), (128, 128), (256, 128), (384, 32))):
                            func = (AF.Tanh, AF.Copy, AF.Sigmoid, AF.Sigmoid)[m]
                            for (off, dcol, nn) in nblks:
                                pp = ps1.get()
                                i = 0
                                for (sh, j) in shifts:
                                    for k in range(8):
                                        kb.op("pe", lambda e: e.matmul(pp[0:msz, 0:nn], lhsT=ws[j][:, k, m0:m0 + msz], rhs=hT[:, k, off + sh:off + sh + nn],
                                                                       start=(i == 0), stop=(i == 23)), [hT, ws[j]], [pp], inc=(i == 23))
                                        i += 1
                                kb.op("act", lambda e: e.activation(out=fm[0:msz, m, dcol:dcol + nn], in_=pp[0:msz, 0:nn], func=func), [pp], [fm])
                    else:
                        g0 = c0 - 2368
                        for m in range(4):
                            mt = g0 // 128 + m
                            for nb_ in range(4):
                                pp = ps1.get()
                                for k in range(8):
                                    kb.op("pe", lambda e: e.matmul(pp[:, :], lhsT=ws[0][:, k, m * 128:(m + 1) * 128],
                                                                   rhs=hT[:, k, LT0 + nb_ * 512:LT0 + (nb_ + 1) * 512], start=(k == 0), stop=(k == 7)),
                                          [hT, ws[0]], [pp], inc=(k == 7))
                                g = gsb.get()
                                kb.op("act", lambda e: e.activation(out=g[:], in_=pp[:, :], func=AF.Sigmoid), [pp], [g])
                                kb.dma("sp", gates_d[mt * 128:(mt + 1) * 128, nb_ * 512:(nb_ + 1) * 512], g[:], reads=[g], writes=[gates_r[mt][nb_]])
                if "fm_dbg" in dbg and b == 0:
                    kb.dma("sp", dbg_d["fm_dbg"][:, :, :], fm[:], reads=[fm], writes=[dbg_d["fm_dbg"]])
            if upto <= 2:
                kb.barrier(); fm_stack.close(); mix_stack.close()
                continue

            with kb.scope():
                Ybuf = kb.sb("Ybuf", [128, 16, 512], F32)
                halves = []
                for t2 in ps2.tiles:
                    for c0_ in (0, 512):
                        ht = Tile(t2.t[:, c0_:c0_ + 512], f"{t2.b.name}_h{c0_}")
                        ht.b.excl = True
                        halves.append(ht)
                pY_bank = halves[0]
                psP = Rot(halves[1:3])
                psR = Rot(ps1.tiles + halves[3:])
                bonus = kb.sb("bonus", [128, 16, 2, 8], F32)
                tri_f = kb.sb("tri_f", [128, 4, 128], F32)
                tri = kb.sb("tri", [128, 4, 128], BF16)
                kb.dma("sp", tri_f[:], I["c_tri"].ap(), writes=[tri_f])
                kb.op("dve", lambda e: e.tensor_copy(out=tri[:], in_=tri_f[:]), [tri_f], [tri])
                mask4 = kb.sb("mask4", [128, 2, 512], F32)
                maskt = kb.sb("maskt", [128, 2, 128], F32)
                for d in range(2):
                    kb.dma("sp", mask4[:, d, :], I["c_mask4"].ap()[d], writes=[mask4])
                    kb.dma("sp", maskt[:, d, :], I["c_maskt"].ap()[d], writes=[maskt])
                prm = kb.sb("prm", [128, 3, 512], F32)
                bcast_load(prm[:, 0, :], prm, I["k_k"].ap()[0:1, :], 128)
                bcast_load(prm[:, 1, :], prm, I["k_a"].ap()[0:1, :], 128)
                bcast_load(prm[:, 2, :], prm, I["r_k"].ap()[0:1, :], 128)
                w2f = kb.sb("w2f", [128, 2, 512], F32)
                w2b = kb.sb("w2b", [128, 2, 512], BF16)
                kb.dma("sp", w2f[:, 0, :], I["decay_w2"].ap().rearrange("d l c -> (d l) c"), writes=[w2f])
                kb.dma("sp", w2f[:, 1, :], I["aicl_a2"].ap().rearrange("d l c -> (d l) c"), writes=[w2f])
                kb.op("dve", lambda e: e.tensor_copy(out=w2b[:], in_=w2f[:]), [w2f], [w2b])
                b0f = kb.sb("b0f", [128, 2, 512], F32)
                b0b = kb.sb("b0b", [128, 2, 512], BF16)
                kb.op("pool", lambda e: e.memset(b0f[:], 0.0), [], [b0f])
                for d_ in range(2):
                    kb.dma("sp", b0f[d_ * 64:d_ * 64 + 1, 0, :], I["decay_w0"].ap()[d_:d_ + 1, :], writes=[b0f])
                    kb.dma("sp", b0f[d_ * 64:d_ * 64 + 1, 1, :], I["aicl_a0"].ap()[d_:d_ + 1, :], writes=[b0f])
                kb.op("dve", lambda e: e.tensor_copy(out=b0b[:], in_=b0f[:]), [b0f], [b0b])
                Hf = [kb.sb(f"Hf{p}", [128, 64], F32) for p in range(4)]
                Hb = [kb.sb(f"Hb{p}", [128, 64], BF16) for p in range(4)]
                rkv_rot = kb.rot("rkvt", [128, 1536], F32, 2)
                f512 = kb.rot("f512", [128, 512], F32, 9)
                b512 = kb.rot("b512", [128, 512], BF16, 14)
                fmT = kb.rot("fmT", [128, 4, 4, 128], BF16, 2)
                A4rot = kb.rot("A4", [128, 512], BF16, 8)
                XPa = kb.rot("XPa", [128, 512], BF16, 12)
                Wsb = kb.rot("Wsb", [128, 64], BF16, 10)
                Usb = kb.rot("Usb", [128, 512], BF16, 2)
                Vb = kb.rot("Vb", [128, 512], BF16, 2)
                ptot_rot = kb.rot("ptot", [128, 4], F32, 2)

                for d in range(rw_dirs):
                    for p in range(4):
                        kb.op("pool", lambda e: e.memset(Hf[p][:], 0.0), [], [Hf[p]])
                        kb.op("pool", lambda e: e.memset(Hb[p][:], 0.0), [], [Hb[p]])
                    order = list(range(18)) if d == 0 else [1, 0] + list(range(17, 1, -1))
                    def prep_gen(ti, c_):
                        lat = ti >= 2
                        rt = rkv_rot.get()
                        kb.dma("sp", rt[:], rkv_d[ti * 128:(ti + 1) * 128, :], reads=[rkv_r[ti]], writes=[rt])
                        r_ = rt[:, 0:512]
                        k_ = rt[:, 512:1024]
                        v_ = rt[:, 1024:1536]
                        kkr = f512.get(); sq = f512.get(); st = small_rot.get()
                        kb.op("dve", lambda e: e.tensor_tensor(out=kkr[:], in0=k_, in1=prm[:, 0, :], op=ALU.mult), [rt, prm], [kkr])
                        kb.op("act", lambda e: e.activation(out=sq[:], in_=kkr[:], func=AF.Square), [kkr], [sq])
                        kb.op("dve", lambda e: e.tensor_reduce(out=st[:, 0:8], in_=sq[:].rearrange("p (h c) -> p h c", h=8), axis=AX.X, op=ALU.add), [sq], [st])
                        rms_rstd(st[:, 0:8], st[:, 8:16], st, st, 1.0, 1e-12)
                        kap = f512.get()
                        kb.op("dve", lambda e: e.tensor_tensor(out=kap[:].rearrange("p (h c) -> p h c", h=8), in0=kkr[:].rearrange("p (h c) -> p h c", h=8),
                                                               in1=st[:, 8:16].unsqueeze(2).to_broadcast([128, 8, 64]), op=ALU.mult), [kkr, st], [kap])
                        yield
                        tcol = (ti * 128) if ti < 2 else (256 + (ti - 2) * 128)
                        pz = psP.get()
                        kb.op("pe", lambda e: e.matmul(pz[:, :], lhsT=fm[d * 64:(d + 1) * 64, 0, tcol:tcol + 128], rhs=w2b[d * 64:(d + 1) * 64, 0, :], start=True, stop=False),
                              [fm, w2b], [pz], inc=False)
                        kb.op("pe", lambda e: e.matmul(pz[:, :], lhsT=ones_b[d * 64:d * 64 + 1, 0:128], rhs=b0b[d * 64:d * 64 + 1, 0, :], start=False, stop=True), [ones_b, b0b], [pz])
                        sigb = b512.get()
                        kb.op("act", lambda e: e.activation(out=sigb[:], in_=pz[:, :], func=AF.Sigmoid), [pz], [sigb])
                        yield
                        pa = psP.get()
                        kb.op("pe", lambda e: e.matmul(pa[:, :], lhsT=fm[d * 64:(d + 1) * 64, 1, tcol:tcol + 128], rhs=w2b[d * 64:(d + 1) * 64, 1, :], start=True, stop=False),
                              [fm, w2b], [pa], inc=False)
                        kb.op("pe", lambda e: e.matmul(pa[:, :], lhsT=ones_b[d * 64:d * 64 + 1, 0:128], rhs=b0b[d * 64:d * 64 + 1, 1, :], start=False, stop=True), [ones_b, b0b], [pa])
                        a_ = f512.get()
                        kb.op("act", lambda e: e.activation(out=a_[:], in_=pa[:, :], func=AF.Sigmoid), [pa], [a_])
                        yield
                        t1 = f512.get(); ktl = f512.get(); beta = f512.get()
                        kb.op("dve", lambda e: e.scalar_tensor_tensor(out=t1[:], in0=a_[:], scalar=-1.0, in1=prm[:, 1, :], op0=ALU.add, op1=ALU.mult), [a_, prm], [t1])
                        kb.op("dve", lambda e: e.scalar_tensor_tensor(out=ktl[:], in0=t1[:], scalar=1.0, in1=k_, op0=ALU.add, op1=ALU.mult), [t1, rt], [ktl])
                        kb.op("pool", lambda e: e.tensor_tensor(out=beta[:], in0=a_[:], in1=kap[:], op=ALU.mult), [a_, kap], [beta])
                        yield
                        if lat:
                            kb.op("pool", lambda e: e.tensor_tensor(out=t1[:], in0=r_, in1=prm[:, 2, :], op=ALU.mult), [rt, prm], [t1])
                            kb.op("pool", lambda e: e.tensor_tensor(out=t1[:], in0=t1[:], in1=ktl[:], op=ALU.mult), [t1, ktl], [t1])
                            kb.op("dve", lambda e: e.tensor_reduce(out=bonus[:, ti - 2, d, :], in_=t1[:].rearrange("p (h c) -> p h c", h=8), axis=AX.X, op=ALU.add), [t1], [bonus])
                        ci, ce, cr = ((0, 1, 3) if d == 0 else (2, 3, 1))
                        eI = f512.get(); eE = f512.get(); eN = f512.get(); eR = f512.get()
                        pI = psP.get()
                        kb.op("pe", lambda e: e.matmul(pI[:, :], lhsT=tri[:, ci, :], rhs=sigb[:], start=True, stop=True), [tri, sigb], [pI])
                        kb.op("act", lambda e: e.activation(out=eI[:], in_=pI[:, :], func=AF.Exp, scale=C_DEC), [pI], [eI])
                        kb.op("act", lambda e: e.activation(out=eN[:], in_=pI[:, :], func=AF.Exp, scale=-C_DEC), [pI], [eN])
                        yield
                        pE = psP.get()
                        kb.op("pe", lambda e: e.matmul(pE[:, :], lhsT=tri[:, ce, :], rhs=sigb[:], start=True, stop=True), [tri, sigb], [pE])
                        kb.op("act", lambda e: e.activation(out=eE[:], in_=pE[:, :], func=AF.Exp, scale=C_DEC), [pE], [eE])
                        yield
                        pR = psP.get()
                        kb.op("pe", lambda e: e.matmul(pR[:, :], lhsT=tri[:, cr, :], rhs=sigb[:], start=True, stop=True), [tri, sigb], [pR])
                        kb.op("act", lambda e: e.activation(out=eR[:], in_=pR[:, :], func=AF.Exp, scale=C_DEC), [pR], [eR])
                        yield
                        pT = psP.get()
                        for p in range(4):
                            kb.op("pe", lambda e: e.matmul(pT[:, p:p + 1], lhsT=sigb[:, p * 128:(p + 1) * 128], rhs=ones_b[:, 0:1], start=True, stop=True), [sigb, ones_b], [pT])
                        ptot = ptot_rot.get()
                        kb.op("act", lambda e: e.activation(out=ptot[:], in_=pT[:, 0:4], func=AF.Exp, scale=C_DEC), [pT], [ptot])
                        yield
                        Rh = b512.get(); Ka = b512.get(); Bh = b512.get(); Kh = b512.get(); NBb = b512.get(); Kbb = b512.get()
                        kb.op("dve", lambda e: e.tensor_tensor(out=Rh[:], in0=r_, in1=eI[:], op=ALU.mult), [rt, eI], [Rh])
                        kb.op("pool", lambda e: e.tensor_tensor(out=Ka[:], in0=kap[:], in1=eE[:], op=ALU.mult), [kap, eE], [Ka])
                        yield
                        kb.op("dve", lambda e: e.tensor_tensor(out=Bh[:], in0=beta[:], in1=eN[:], op=ALU.mult), [beta, eN], [Bh])
                        kb.op("pool", lambda e: e.tensor_tensor(out=Kh[:], in0=ktl[:], in1=eN[:], op=ALU.mult), [ktl, eN], [Kh])
                        yield
                        kb.op("dve", lambda e: e.scalar_tensor_tensor(out=NBb[:], in0=beta[:], scalar=-1.0, in1=eR[:], op0=ALU.mult, op1=ALU.mult), [beta, eR], [NBb])
                        kb.op("pool", lambda e: e.tensor_tensor(out=Kbb[:], in0=ktl[:], in1=eR[:], op=ALU.mult), [ktl, eR], [Kbb])
                        vb = Vb.get()
                        kb.op("pool", lambda e: e.tensor_copy(out=vb[:], in_=v_), [rt], [vb])
                        yield
                        fT = fmT.get()
                        for wi, src in enumerate((Ka, Rh, Bh, Kh)):
                            transposes(src, [src[:, p * 128:(p + 1) * 128] for p in range(4)], fT[:, :, wi, :], fT, eng=("act" if wi % 2 else "dve"), pool=psP)
                            yield
                        c_.update(fT=fT, NBb=NBb, Kbb=Kbb, vb=vb, ptot=ptot, lat=lat)

                    def advance(g_, n_):
                        if g_ is None:
                            return
                        for _ in range(n_):
                            try:
                                next(g_)
                            except StopIteration:
                                return

                    def heads(ti, c_, nxt):
                        fT = c_['fT']; NBb = c_['NBb']; Kbb = c_['Kbb']; vb = c_['vb']; ptot = c_['ptot']; lat = c_['lat']
                        U = Usb.get()
                        pY = pY_bank if lat else None
                        HG = 8
                        for g0 in range(0, 8, HG):
                            hs = list(range(g0, g0 + HG))
                            P_ = {h: h // 2 for h in hs}
                            LO = {h: (h % 2) * 64 for h in hs}
                            HC = {h: slice(h * 64, (h + 1) * 64) for h in hs}
                            KaT = {h: fT[LO[h]:LO[h] + 64, P_[h], 0, :] for h in hs}
                            RT = {h: fT[LO[h]:LO[h] + 64, P_[h], 1, :] for h in hs}
                            BT = {h: fT[LO[h]:LO[h] + 64, P_[h], 2, :] for h in hs}
                            KT = {h: fT[LO[h]:LO[h] + 64, P_[h], 3, :] for h in hs}
                            KaRT = {h: fT[LO[h]:LO[h] + 64, P_[h], 0:2, :].rearrange("c w t -> c (w t)") for h in hs}
                            A4 = {}; Xt = {}; XP = {}; pp_ = {}; Wt = {}
                            NBK = len(psR.tiles)

                            def lockstep(pe_fn, ev_fn):
                                pend_ = []
                                for h_ in hs:
                                    if len(pend_) >= NBK:
                                        ev_fn(pend_.pop(0))
                                    pe_fn(h_)
                                    pend_.append(h_)
                                for h_ in pend_:
                                    ev_fn(h_)

                            def pe_A(h):
                                pA = psR.get(); pp_[h] = pA
                                kb.op("pe", lambda e: e.matmul(pA[:, 0:256], lhsT=BT[h], rhs=KaRT[h], start=True, stop=True), [fT], [pA], inc=False)
                                kb.op("pe", lambda e: e.matmul(pA[:, 256:512], lhsT=KT[h], rhs=KaRT[h], start=True, stop=True), [fT], [pA])

                            def ev_A(h):
                                A4[h] = A4rot.get()
                                kb.op("dve", lambda e: e.tensor_tensor(out=A4[h][:], in0=pp_[h][:, :], in1=mask4[:, d, :], op=ALU.mult), [pp_[h], mask4], [A4[h]])
                            lockstep(pe_A, ev_A)
                            advance(nxt, 2)

                            def pe_L(h):
                                pL = psR.get(); pp_[h] = pL
                                kb.op("pe", lambda e: e.matmul(pL[:, 0:128], lhsT=KaT[h], rhs=BT[h], start=True, stop=True), [fT], [pL])

                            def ev_L(h):
                                XP[h] = XPa.get()
                                kb.op("dve", lambda e: e.tensor_tensor(out=XP[h][:, 256:384], in0=pp_[h][:, 0:128], in1=maskt[:, d, :], op=ALU.mult), [pp_[h], maskt], [XP[h]])
                                kb.op("pool", lambda e: e.tensor_copy(out=XP[h][:, 0:128], in_=A4[h][:, 0:128]), [A4[h]], [XP[h]])
                                kb.op("pool", lambda e: e.tensor_tensor(out=XP[h][:, 128:256], in0=A4[h][:, 0:128], in1=ident_b[:], op=ALU.add), [A4[h], ident_b], [XP[h]])
                            lockstep(pe_L, ev_L)
                            advance(nxt, 2)

                            def xxt(ap_):
                                return ap_[:, 0:512].rearrange("p (a c) -> p a c", c=256)[:, :, 0:128] if ap_.shape[-1] >= 512 else None

                            for j in range(0, 7):
                                first = (j == 0)
                                last = (j == 6)

                                def pe_Bj(h):
                                    pB = psR.get(); pp_[h] = pB
                                    if first:
                                        kb.op("pe", lambda e: e.matmul(pB[:, 0:128], lhsT=XP[h][:, 256:384], rhs=XP[h][:, 0:128], start=True, stop=True), [XP[h]], [pB], inc=False)
                                        kb.op("pe", lambda e: e.matmul(pB[:, 256:384], lhsT=XP[h][:, 0:128], rhs=XP[h][:, 256:384], start=True, stop=True), [XP[h]], [pB])
                                    elif not last:
                                        kb.op("pe", lambda e: e.matmul(pB[:, 0:256], lhsT=XP[h][:, 256:384], rhs=XP[h][:, 0:256], start=True, stop=True), [XP[h]], [pB], inc=False)
                                        kb.op("pe", lambda e: e.matmul(pB[:, 256:384], lhsT=XP[h][:, 0:128], rhs=XP[h][:, 256:384], start=True, stop=True), [XP[h]], [pB])
                                    else:
                                        kb.op("pe", lambda e: e.matmul(pB[:, 128:256], lhsT=XP[h][:, 256:384], rhs=XP[h][:, 128:256], start=True, stop=True), [XP[h]], [pB])

                                def ev_Bj(h):
                                    pB = pp_[h]
                                    XP2 = XPa.get()
                                    if first:
                                        kb.op("pool", lambda e: e.tensor_copy(out=XP2[:, 128:256], in_=XP[h][:, 128:256]), [XP[h]], [XP2])
                                    else:
                                        kb.op("dve", lambda e: e.tensor_tensor(out=XP2[:, 128:256], in0=pB[:, 128:256], in1=XP[h][:, 128:256], op=ALU.add), [pB, XP[h]], [XP2])
                                    if not last:
                                        src_ = pB[:, 0:512].rearrange("p (a c) -> p a c", c=256)[:, :, 0:128]
                                        dst_ = XP2[:, 0:512].rearrange("p (a c) -> p a c", c=256)[:, :, 0:128]
                                        kb.op("act", lambda e: e.copy(out=dst_, in_=src_), [pB], [XP2])
                                    XP[h] = XP2
                                lockstep(pe_Bj, ev_Bj)
                                advance(nxt, 2)

                            def pe_W(h):
                                pW = psR.get(); pp_[h] = pW
                                kb.op("pe", lambda e: e.matmul(pW[:, 0:64], lhsT=KaT[h], rhs=Hb[P_[h]][LO[h]:LO[h] + 64, :], start=True, stop=False), [fT, Hb[P_[h]]], [pW], inc=False)
                                kb.op("pe", lambda e: e.matmul(pW[:, 0:64], lhsT=A4[h][:, 256:384], rhs=vb[:, HC[h]], start=False, stop=True), [A4[h], vb], [pW])

                            def ev_W(h):
                                Wt[h] = Wsb.get()
                                kb.op("act", lambda e: e.copy(out=Wt[h][:], in_=pp_[h][:, 0:64]), [pp_[h]], [Wt[h]])
                            lockstep(pe_W, ev_W)
                            advance(nxt, 1)

                            def pe_U(h):
                                pU = psR.get(); pp_[h] = pU
                                kb.op("pe", lambda e: e.matmul(pU[:, 0:64], lhsT=XP[h][:, 128:256], rhs=Wt[h][:], start=True, stop=True), [XP[h], Wt[h]], [pU])

                            def ev_U(h):
                                kb.op("dve" if h % 2 else "act", (lambda e: e.tensor_copy(out=U[:, HC[h]], in_=pp_[h][:, 0:64])) if h % 2 else
                                      (lambda e: e.copy(out=U[:, HC[h]], in_=pp_[h][:, 0:64])), [pp_[h]], [U])
                            lockstep(pe_U, ev_U)
                            advance(nxt, 1)
                            if lat:
                                for h in hs:
                                    kb.op("pe", lambda e: e.matmul(pY[:, HC[h]], lhsT=RT[h], rhs=Hb[P_[h]][LO[h]:LO[h] + 64, :], start=True, stop=False), [fT, Hb[P_[h]]], [pY], inc=False)
                                    kb.op("pe", lambda e: e.matmul(pY[:, HC[h]], lhsT=A4[h][:, 128:256], rhs=U[:, HC[h]], start=False, stop=False), [A4[h], U], [pY], inc=False)
                                    kb.op("pe", lambda e: e.matmul(pY[:, HC[h]], lhsT=A4[h][:, 384:512], rhs=vb[:, HC[h]], start=False, stop=True), [A4[h], vb], [pY])
                            pHs = {}
                            for p in sorted(set(P_.values())):
                                pc = slice(p * 128, (p + 1) * 128)
                                pH = psR.get(); pHs[p] = pH
                                kb.op("pe", lambda e: e.matmul(pH[:, 0:128], lhsT=NBb[:, pc], rhs=U[:, pc], start=True, stop=False), [NBb, U], [pH], inc=False)
                                kb.op("pe", lambda e: e.matmul(pH[:, 0:128], lhsT=Kbb[:, pc], rhs=vb[:, pc], start=False, stop=True), [Kbb, vb], [pH])
                            for p in sorted(set(P_.values())):
                                pH = pHs[p]
                                for hh in range(2):
                                    l2 = hh * 64
                                    kb.op("dve", lambda e: e.scalar_tensor_tensor(out=Hf[p][l2:l2 + 64, :], in0=Hf[p][l2:l2 + 64, :], scalar=ptot[l2:l2 + 64, p:p + 1],
                                                                                  in1=pH[l2:l2 + 64, l2:l2 + 64], op0=ALU.mult, op1=ALU.add), [Hf[p], ptot, pH], [Hf[p]])
                                kb.op("act", lambda e: e.copy(out=Hb[p][:], in_=Hf[p][:]), [Hf[p]], [Hb[p]])
                        if lat:
                            if d == 0:
                                kb.op("act", lambda e: e.copy(out=Ybuf[:, ti - 2, :], in_=pY[:, 0:512]), [pY], [Ybuf])
                            else:
                                kb.op("dve", lambda e: e.tensor_tensor(out=Ybuf[:, ti - 2, :], in0=pY[:, 0:512], in1=Ybuf[:, ti - 2, :], op=ALU.add), [pY, Ybuf], [Ybuf])

                    tl_ = order[:rw_tiles]
                    ctxs_ = [dict() for _ in tl_]
                    gens_ = [prep_gen(ti, ctxs_[n]) for n, ti in enumerate(tl_)]
                    advance(gens_[0], 10 ** 6)
                    for n, ti in enumerate(tl_):
                        nxt_ = gens_[n + 1] if n + 1 < len(tl_) else None
                        heads(ti, ctxs_[n], nxt_)
                        advance(nxt_, 10 ** 6)
                if "y_dbg" in dbg and b == 0:
                    kb.dma("sp", dbg_d["y_dbg"][:, :, :], Ybuf[:], reads=[Ybuf], writes=[dbg_d["y_dbg"]])
                gnw = kb.sb("gnw", [128, 2, 512], F32)
                bcast_load(gnw[:, 0, :], gnw, I["gn_w"].ap()[0:1, :], 128)
                bcast_load(gnw[:, 1, :], gnw, I["gn_b"].ap()[0:1, :], 128)
                g2f = w2f
                g2b = kb.sb("g2b", [128, 2, 512], BF16)
                kb.dma("sp", g2f[:, 0, :], I["gate_g2"].ap()[0:128, :], reads=[], writes=[g2f])
                kb.dma("sp", g2f[0:32, 1, :], I["gate_g2"].ap()[128:160, :], writes=[g2f])
                kb.op("dve", lambda e: e.tensor_copy(out=g2b[:, 0, :], in_=g2f[:, 0, :]), [g2f], [g2b])
                kb.op("dve", lambda e: e.tensor_copy(out=g2b[0:32, 1, :], in_=g2f[0:32, 1, :]), [g2f], [g2b])
                rwb = kb.rot("rwb", [128, 512], BF16, 2)
                def tileRO(t):
                        ti = t + 2
                        rt = rkv_rot.get()
                        kb.dma("sp", rt[:], rkv_d[ti * 128:(ti + 1) * 128, :], reads=[rkv_r[ti]], writes=[rt])
                        v_ = rt[:, 1024:1536]
                        y3 = Ybuf[:, t, :].rearrange("p (h c) -> p h c", h=8)
                        st = small_rot.get(); st2 = small_rot.get()
                        cen = f512.get(); sq = f512.get(); yn = f512.get()
                        cen3 = cen[:].rearrange("p (h c) -> p h c", h=8)
                        kb.op("dve", lambda e: e.tensor_reduce(out=st[:, 0:8], in_=y3, axis=AX.X, op=ALU.add), [Ybuf], [st])
                        kb.op("dve", lambda e: e.tensor_scalar(out=st[:, 0:8], in0=st[:, 0:8], scalar1=-1.0 / 64, scalar2=None, op0=ALU.mult), [st], [st])
                        kb.op("dve", lambda e: e.tensor_tensor(out=cen3, in0=y3, in1=st[:, 0:8].unsqueeze(2).to_broadcast([128, 8, 64]), op=ALU.add), [Ybuf, st], [cen])
                        yield
                        kb.op("act", lambda e: e.activation(out=sq[:], in_=cen[:], func=AF.Square), [cen], [sq])
                        kb.op("dve", lambda e: e.tensor_reduce(out=st2[:, 0:8], in_=sq[:].rearrange("p (h c) -> p h c", h=8), axis=AX.X, op=ALU.add), [sq], [st2])
                        yield
                        rms_rstd(st2[:, 0:8], st2[:, 8:16], st2, st2, 64, GN_EPS)
                        kb.op("dve", lambda e: e.tensor_tensor(out=yn[:].rearrange("p (h c) -> p h c", h=8), in0=cen3, in1=st2[:, 8:16].unsqueeze(2).to_broadcast([128, 8, 64]), op=ALU.mult),
                              [cen, st2], [yn])
                        kb.op("pool", lambda e: e.tensor_tensor(out=yn[:], in0=yn[:], in1=gnw[:, 0, :], op=ALU.mult), [yn, gnw], [yn])
                        kb.op("pool", lambda e: e.tensor_tensor(out=yn[:], in0=yn[:], in1=gnw[:, 1, :], op=ALU.add), [yn, gnw], [yn])
                        yield
                        kb.op("dve", lambda e: e.tensor_tensor(out=st[:, 8:16], in0=bonus[:, t, 0, :], in1=bonus[:, t, 1, :], op=ALU.add), [bonus], [st])
                        kb.op("dve", lambda e: e.tensor_tensor(out=sq[:].rearrange("p (h c) -> p h c", h=8), in0=v_.rearrange("p (h c) -> p h c", h=8),
                                                               in1=st[:, 8:16].unsqueeze(2).to_broadcast([128, 8, 64]), op=ALU.mult), [rt, st], [sq])
                        kb.op("pool", lambda e: e.tensor_tensor(out=yn[:], in0=yn[:], in1=sq[:], op=ALU.add), [yn, sq], [yn])
                        yield
                        pg = ps1.get()
                        tcol = 256 + t * 128
                        kb.op("pe", lambda e: e.matmul(pg[:, :], lhsT=fm[:, 2, tcol:tcol + 128], rhs=g2b[:, 0, :], start=True, stop=False), [fm, g2b], [pg], inc=False)
                        kb.op("pe", lambda e: e.matmul(pg[:, :], lhsT=fm[0:32, 3, tcol:tcol + 128], rhs=g2b[0:32, 1, :], start=False, stop=True), [fm, g2b], [pg])
                        ro = rwb.get()
                        kb.op("dve", lambda e: e.tensor_tensor(out=ro[:], in0=pg[:, :], in1=yn[:], op=ALU.mult), [pg, yn], [ro])
                        transposes(ro, [ro[:, k * 128:(k + 1) * 128] for k in range(4)], rwT[:, :, t * 128:(t + 1) * 128], rwT)
                run_interleaved((tileRO(t) for t in range(16)), 2)
                if "rw_dbg" in dbg and b == 0:
                    kb.dma("sp", dbg_d["rw_dbg"][:, :, :], rwT[:], reads=[rwT], writes=[dbg_d["rw_dbg"]])
            kb.barrier()
            fm_stack.close()
            if upto <= 3:
                mix_stack.close()
                continue

            with kb.scope():
                qT = kb.sb("qT", [96, 8, 2048], BF16)
                kT = kb.sb("kT", [96, 8, 2304], BF16)
                Vall = kb.sb("Vall", [128, 18, 8, 65], BF16)
                kb.op("pool", lambda e: e.memset(Vall[:, :, :, 64:65], 1.0), [], [Vall])
                qln = kb.sb("qln", [128, 256], F32)
                kvln = kb.sb("kvln", [128, 128], F32)
                qnw = kb.sb("qnw", [128, 8, 96], F32)
                knw = kb.sb("knw", [128, 8, 96], F32)
                bcast_load(qln[:], qln, I["q_lat_norm"].ap()[0:1, :], 128)
                bcast_load(kvln[:], kvln, I["kv_lat_norm"].ap()[0:1, :], 128)
                for h in range(8):
                    bcast_load(qnw[:, h, :], qnw, I["q_norm"].ap()[0:1, :], 128)
                    bcast_load(knw[:, h, :], knw, I["k_norm"].ap()[0:1, :], 128)
                kb.op("dve", lambda e: e.tensor_scalar(out=qnw[:], in0=qnw[:], scalar1=ATTN_SCALE, scalar2=None, op0=ALU.mult), [qnw], [qnw])
                wuq_f = kb.sb("wuq_f", [128, 2, 768], F32)
                wuq = kb.sb("wuq", [128, 2, 768], BF16)
                kb.dma("sp", wuq_f[:], I["w_uq"].ap().rearrange("(k p) n -> p k n", p=128), writes=[wuq_f])
                kb.op("pool", lambda e: e.tensor_copy(out=wuq[:], in_=wuq_f[:]), [wuq_f], [wuq])
                wukv_f = kb.sb("wukv_f", [128, 1024], F32)
                wukv = kb.sb("wukv", [128, 1024], BF16)
                kb.dma("sp", wukv_f[:], I["w_ukv"].ap(), writes=[wukv_f])
                kb.op("pool", lambda e: e.tensor_copy(out=wukv[:], in_=wukv_f[:]), [wukv_f], [wukv])
                mrot = kb.rot("mt", [128, 416], F32, 2)
                cs_rot = kb.rot("cs", [128, 2, 16], F32, 2)
                t768 = kb.rot("t768", [128, 8, 96], F32, 8)
                small_m = kb.rot("small_m", [128, 16], F32, 10)
                tb768 = kb.rot("tb768", [128, 8, 96], BF16, 4)
                tbn = kb.rot("tbn", [128, 256], BF16, 4)
                tTn = kb.rot("tTn", [128, 2, 128], BF16, 4)
                rope_t = kb.rot("rope_t", [128, 8, 16], F32, 16)

                def head_norm_rope(src, dst_b, gain, cs, n_extra_ssq=None):
                    sq = t768.get()
                    st = small_m.get()
                    kb.op("act", lambda e: e.activation(out=sq[:], in_=src[:], func=AF.Square), [src], [sq])
                    kb.op("dve", lambda e: e.tensor_reduce(out=st[:, 0:8], in_=sq[:], axis=AX.X, op=ALU.add), [sq], [st])
                    yield
                    rms_rstd(st[:, 0:8], st[:, 8:16], st, st, 96, EPS)
                    kb.op("dve", lambda e: e.tensor_tensor(out=sq[:], in0=src[:], in1=st[:, 8:16].unsqueeze(2).to_broadcast([128, 8, 96]), op=ALU.mult),
                          [src, st], [sq])
                    yield
                    if cs is None:
                        kb.op("pool", lambda e: e.tensor_tensor(out=dst_b[:], in0=sq[:], in1=gain[:], op=ALU.mult), [sq, gain], [dst_b])
                        return
                    kb.op("pool", lambda e: e.tensor_tensor(out=sq[:], in0=sq[:], in1=gain[:], op=ALU.mult), [sq, gain], [sq])
                    kb.op("pool", lambda e: e.tensor_copy(out=dst_b[:, :, 0:64], in_=sq[:, :, 0:64]), [sq], [dst_b])
                    cb_ = cs[:, 0, :].unsqueeze(1).to_broadcast([128, 8, 16])
                    sb_ = cs[:, 1, :].unsqueeze(1).to_broadcast([128, 8, 16])
                    x1 = sq[:, :, 64:80]
                    x2 = sq[:, :, 80:96]
                    ta = rope_t.get(); tb_ = rope_t.get(); tc_ = rope_t.get(); td = rope_t.get()
                    yield
                    kb.op("dve", lambda e: e.tensor_tensor(out=ta[:], in0=x1, in1=cb_, op=ALU.mult), [sq, cs], [ta])
                    kb.op("dve", lambda e: e.tensor_tensor(out=tb_[:], in0=x2, in1=sb_, op=ALU.mult), [sq, cs], [tb_])
                    kb.op("pool", lambda e: e.tensor_tensor(out=tc_[:], in0=x1, in1=sb_, op=ALU.mult), [sq, cs], [tc_])
                    kb.op("pool", lambda e: e.tensor_tensor(out=td[:], in0=x2, in1=cb_, op=ALU.mult), [sq, cs], [td])
                    kb.op("dve", lambda e: e.tensor_tensor(out=dst_b[:, :, 64:80], in0=ta[:], in1=tb_[:], op=ALU.subtract), [ta, tb_], [dst_b])
                    kb.op("pool", lambda e: e.tensor_tensor(out=dst_b[:, :, 80:96], in0=tc_[:], in1=td[:], op=ALU.add), [tc_, td], [dst_b])

                def mla_tile(ti):
                        lat = ti >= 2
                        mt_ = mrot.get()
                        kb.dma("sp", mt_[:], mla_d[ti * 128:(ti + 1) * 128, :], reads=[mla_r[ti]], writes=[mt_])
                        cs = None
                        if lat:
                            cs = cs_rot.get()
                            lt_ = ti - 2
                            kb.dma("sp", cs[:, 0, :], I["c_cos"].ap()[lt_ * 128:(lt_ + 1) * 128, :], writes=[cs])
                            kb.dma("sp", cs[:, 1, :], I["c_sin"].ap()[lt_ * 128:(lt_ + 1) * 128, :], writes=[cs])
                        junk = junk_rot.get()
                        st = small_m.get()
                        kb.op("act", lambda e: e.activation(out=junk[:, 0:128], in_=mt_[:, 256:384], func=AF.Square, accum_out=st[:, 0:1]), [mt_], [junk, st])
                        kb.op("act", lambda e: e.activation(out=junk[:, 128:160], in_=mt_[:, 384:416], func=AF.Square, accum_out=st[:, 2:3]), [mt_], [junk, st])
                        rms_rstd(st[:, 0:1], st[:, 1:2], st, st, 128, EPS)
                        kvn = tbn.get()
                        kb.op("dve", lambda e: e.scalar_tensor_tensor(out=kvn[:, 0:128], in0=mt_[:, 256:384], scalar=st[:, 1:2], in1=kvln[:], op0=ALU.mult, op1=ALU.mult),
                              [mt_, st, kvln], [kvn])
                        yield
                        kvT = tTn.get()
                        transposes(kvn, [kvn[:, 0:128]], kvT[:, 0, :], kvT)
                        pk = ps2.get()
                        for nb_ in range(2):
                            kb.op("pe", lambda e: e.matmul(pk[:, nb_ * 512:(nb_ + 1) * 512], lhsT=kvT[:, 0, :], rhs=wukv[:, nb_ * 512:(nb_ + 1) * 512], start=True, stop=True),
                                  [kvT, wukv], [pk])
                        pk3 = pk[:].rearrange("p (h c) -> p h c", h=8)
                        kb.op("act", lambda e: e.copy(out=Vall[:, ti, :, 0:64], in_=pk3[:, :, 64:128]), [pk], [Vall])
                        kf = t768.get()
                        kb.op("dve", lambda e: e.tensor_copy(out=kf[:, :, 0:64], in_=pk3[:, :, 0:64]), [pk], [kf])
                        kb.op("pool", lambda e: e.tensor_copy(out=kf[:, :, 64:96], in_=mt_[:, 384:416].unsqueeze(1).to_broadcast([128, 8, 32])), [mt_], [kf])
                        yield
                        kbf = tb768.get()
                        yield from head_norm_rope(kf, kbf, knw, cs)
                        yield
                        transposes(kbf, [kbf[:, h, :] for h in range(8)], kT[:, :, ti * 128:(ti + 1) * 128], kT, eng="dve")
                        yield
                        if lat:
                            st2 = small_m.get()
                            kb.op("act", lambda e: e.activation(out=junk[:, 256:512], in_=mt_[:, 0:256], func=AF.Square, accum_out=st2[:, 0:1]), [mt_], [junk, st2])
                            rms_rstd(st2[:, 0:1], st2[:, 1:2], st2, st2, 256, EPS)
                            qn = tbn.get()
                            kb.op("dve", lambda e: e.scalar_tensor_tensor(out=qn[:], in0=mt_[:, 0:256], scalar=st2[:, 1:2], in1=qln[:], op0=ALU.mult, op1=ALU.mult),
                                  [mt_, st2, qln], [qn])
                            yield
                            qnT = tTn.get()
                            transposes(qn, [qn[:, 0:128], qn[:, 128:256]], qnT[:], qnT)
                            pq = ps2.get()
                            for (n0, n1) in ((0, 512), (512, 768)):
                                for k in range(2):
                                    kb.op("pe", lambda e: e.matmul(pq[:, n0:n1], lhsT=qnT[:, k, :], rhs=wuq[:, k, n0:n1], start=(k == 0), stop=(k == 1)),
                                          [qnT, wuq], [pq], inc=(k == 1))
                            qf = t768.get()
                            kb.op("act", lambda e: e.copy(out=qf[:].rearrange("p h c -> p (h c)"), in_=pq[:, 0:768]), [pq], [qf])
                            yield
                            qbf = tb768.get()
                            yield from head_norm_rope(qf, qbf, qnw, cs)
                            yield
                            transposes(qbf, [qbf[:, h, :] for h in range(8)], qT[:, :, lt_ * 128:(lt_ + 1) * 128], qT, eng="dve")

                run_interleaved((mla_tile(ti) for ti in range(18)), 2)
                if "qT_dbg" in dbg and b == 0:
                    kb.dma("sp", dbg_d["qT_dbg"][:, :, :], qT[:], reads=[qT], writes=[dbg_d["qT_dbg"]])
                    kb.dma("sp", dbg_d["kT_dbg"][:, :, :], kT[:], reads=[kT], writes=[dbg_d["kT_dbg"]])
                Erot = kb.rot("E", [128, 512], BF16, 3)
                apair = kb.rot("apair", [128, 4, 128], BF16, 2)
                for hp in range(4):
                    for qb in range(4):
                        ap_ = apair.get()
                        for hh in range(2):
                            h = hp * 2 + hh
                            po = [ps2.get(), ps2.get()]
                            for kt in range(18):
                                pS = ps1.get()
                                kb.op("pe", lambda e: e.matmul(pS[:, :], lhsT=kT[:, h, kt * 128:(kt + 1) * 128], rhs=qT[:, h, qb * 512:(qb + 1) * 512], start=True, stop=True),
                                      [kT, qT], [pS])
                                E = Erot.get()
                                kb.op("act", lambda e: e.activation(out=E[:], in_=pS[:, :], func=AF.Exp), [pS], [E])
                                for qs in range(4):
                                    p_ = po[qs // 2]
                                    c0 = (qs % 2) * 512
                                    kb.op("pe", lambda e: e.matmul(p_[:, c0:c0 + 65], lhsT=E[:, qs * 128:(qs + 1) * 128], rhs=Vall[:, kt, h, :],
                                                                   start=(kt == 0), stop=(kt == 17)), [E, Vall], [p_], inc=(kt == 17))
                            for qs in range(4):
                                p_ = po[qs // 2]
                                c0 = (qs % 2) * 512
                                st = small_rot.get()
                                kb.op("dve", lambda e: e.reciprocal(out=st[:, 0:1], in_=p_[:, c0 + 64:c0 + 65]), [p_], [st])
                                kb.op("dve", lambda e: e.tensor_scalar(out=ap_[:, qs, hh * 64:(hh + 1) * 64], in0=p_[:, c0:c0 + 64], scalar1=st[:, 0:1], scalar2=None,
                                                                       op0=ALU.mult), [p_, st], [ap_])
                        transposes(ap_, [ap_[:, qs, :] for qs in range(4)], attT[:, hp, qb * 512:(qb + 1) * 512], attT)
                if "att_dbg" in dbg and b == 0:
                    kb.dma("sp", dbg_d["att_dbg"][:, :, :], attT[:], reads=[attT], writes=[dbg_d["att_dbg"]])
            if upto <= 4:
                mix_stack.close()
                continue

            with kb.scope():
                wo_f = kb.sb("wo_f", [128, 8, 1024], F32)
                wo1 = kb.sb("wo1", [128, 4, 1024], BF16)
                wo2 = kb.sb("wo2", [128, 4, 1024], BF16)
                wo3 = kb.sb("wo3", [128, 8, 1024], BF16)
                kb.dma("sp", wo_f[:, 0:4, :], I["w_o_mla"].ap().rearrange("(k p) n -> p k n", p=128), writes=[wo_f])
                kb.op("pool", lambda e: e.tensor_copy(out=wo1[:], in_=wo_f[:, 0:4, :]), [wo_f], [wo1])
                kb.dma("sp", wo_f[:, 0:4, :], I["w_o_rwkv"].ap().rearrange("(k p) n -> p k n", p=128), reads=[wo_f], writes=[wo_f])
                kb.op("pool", lambda e: e.tensor_copy(out=wo2[:], in_=wo_f[:, 0:4, :]), [wo_f], [wo2])
                kb.dma("sp", wo_f[:], I["w_out"].ap().rearrange("(k p) n -> p k n", p=128), reads=[wo_f], writes=[wo_f])
                kb.op("pool", lambda e: e.tensor_copy(out=wo3[:], in_=wo_f[:]), [wo_f], [wo3])
                mT = kb.sb("mT", [128, 8, 2048], BF16)
                grot = kb.rot("gt", [128, 2, 512], BF16, 2)
                trot = kb.rot("tm", [128, 512], F32, 2)
                for m in range(8):
                    for nb_ in range(4):
                        g = grot.get()
                        kb.dma("sp", g[:, 0, :], gates_d[m * 128:(m + 1) * 128, nb_ * 512:(nb_ + 1) * 512], reads=[gates_r[m][nb_]], writes=[g])
                        kb.dma("sp", g[:, 1, :], gates_d[(8 + m) * 128:(9 + m) * 128, nb_ * 512:(nb_ + 1) * 512], reads=[gates_r[8 + m][nb_]], writes=[g])
                        p1 = ps1.get(); p2 = ps1.get()
                        for k in range(4):
                            kb.op("pe", lambda e: e.matmul(p1[:, :], lhsT=wo1[:, k, m * 128:(m + 1) * 128], rhs=attT[:, k, nb_ * 512:(nb_ + 1) * 512], start=(k == 0), stop=(k == 3)),
                                  [wo1, attT], [p1], inc=(k == 3))
                        for k in range(4):
                            kb.op("pe", lambda e: e.matmul(p2[:, :], lhsT=wo2[:, k, m * 128:(m + 1) * 128], rhs=rwT[:, k, nb_ * 512:(nb_ + 1) * 512], start=(k == 0), stop=(k == 3)),
                                  [wo2, rwT], [p2], inc=(k == 3))
                        t1 = trot.get(); t2 = trot.get()
                        kb.op("dve", lambda e: e.tensor_tensor(out=t1[:], in0=p1[:, :], in1=g[:, 0, :], op=ALU.mult), [p1, g], [t1])
                        kb.op("dve", lambda e: e.tensor_tensor(out=t2[:], in0=p2[:, :], in1=g[:, 1, :], op=ALU.mult), [p2, g], [t2])
                        kb.op("pool", lambda e: e.tensor_tensor(out=mT[:, m, nb_ * 512:(nb_ + 1) * 512], in0=t1[:], in1=t2[:], op=ALU.add), [t1, t2], [mT])
                Ga = kb.sb("Ga", [128, 1024], F32)
                bcast_load(Ga[:], Ga, mod_d.t.ap()[b:b + 1, 2048:3072], 128, reads=[mod_d])
                xrot = kb.rot("xt5", [128, 1024], F32, 2)
                for t in range(16):
                    xt = xrot.get()
                    kb.dma("sp", xt[:], I["x"].ap()[b, t * 128:(t + 1) * 128, :], writes=[xt])
                    pm = ps2.get()
                    for nb_ in range(2):
                        for k in range(8):
                            kb.op("pe", lambda e: e.matmul(pm[:, nb_ * 512:(nb_ + 1) * 512], lhsT=mT[:, k, t * 128:(t + 1) * 128], rhs=wo3[:, k, nb_ * 512:(nb_ + 1) * 512],
                                                           start=(k == 0), stop=(k == 7)), [mT, wo3], [pm], inc=(k == 7))
                    junk = junk_rot.get()
                    kb.op("dve", lambda e: e.tensor_tensor(out=junk[:], in0=pm[:, :], in1=Ga[:], op=ALU.mult), [pm, Ga], [junk])
                    kb.op("pool", lambda e: e.tensor_tensor(out=xt[:], in0=junk[:], in1=xt[:], op=ALU.add), [junk, xt], [xt])
                    kb.dma("sp", x1_d[(b * 16 + t) * 128:(b * 16 + t + 1) * 128, :], xt[:], reads=[xt], writes=[x1_r[b * 16 + t]])
            kb.barrier()
            mix_stack.close()
            if upto <= 5:
                continue

            pass

        if upto > 5:
          NT = 16 * nbatch
          NBLK = NT * 4 + 256
          with kb.scope():
            I32 = mybir.dt.int32
            u_d = kb.dram("u_d", [NT * 128, 1024], BF16)
            u_r = [Buf() for _ in range(NT)]
            xs_d = kb.dram("xs_d", [NBLK * 256, 1024], BF16)
            Y_d = kb.dram("Y_d", [NBLK * 256, 1024], BF16)
            junk_rot = kb.rot("junk6", [128, 1024], F32, 3)
            small_rot = kb.rot("small6", [128, 16], F32, 8)
            dest_all = kb.sb("dest_all", [128, NT, 8], I32)
            gate_all = kb.sb("gate_all", [128, NT, 8], F32)
            IDXi = kb.sb("IDXi", [128, NBLK], I32)
            tri_f = kb.sb("tri6_f", [128, 4, 128], F32)
            tri = kb.sb("tri6", [128, 4, 128], BF16)
            kb.dma("sp", tri_f[:], I["c_tri"].ap(), writes=[tri_f])
            kb.op("dve", lambda e: e.tensor_copy(out=tri[:], in_=tri_f[:]), [tri_f], [tri])
            iota_p = kb.sb("iota_p", [128, 2], F32)
            kb.dma("sp", iota_p[:], I["c_iotap"].ap(), writes=[iota_p])
            with kb.scope():
                G_all = kb.sb("G_all", [128, NT, 256], F32)
                POS_all = kb.sb("POS_all", [128, NT, 256], F32)
                mask_all = kb.sb("mask_all", [128, NT, 256], BF16)
                cnt = kb.sb("cnt", [128, 256], F32)
                kb.op("pool", lambda e: e.memset(cnt[:], 0.0), [], [cnt])
                with kb.scope():
                    rw_f = kb.sb("rw_f", [128, 8, 256], F32)
                    kb.dma("sp", rw_f[:], I["router_w"].ap().rearrange("(k p) n -> p k n", p=128), writes=[rw_f])
                    rbias = kb.sb("rbias", [128, 256], F32)
                    bcast_load(rbias[:], rbias, I["router_bias"].ap()[0:1, :], 128)
                    xrot = kb.rot("xt6", [128, 1024], F32, 3)
                    ufr = kb.rot("uf", [128, 1024], F32, 3)
                    ubr = kb.rot("ub", [128, 1024], BF16, 3)
                    uTf = kb.rot("uTf", [128, 8, 128], F32, 3)
                    s256 = kb.rot("s256", [128, 256], F32, 12)
                    m8r = kb.rot("m8", [128, 8], F32, 16)
                    A2s = []
                    for b in range(nbatch):
                        A2 = kb.sb("A2", [128, 1024], F32)
                        S2 = kb.sb("S2", [128, 1024], F32)
                        bcast_load(A2[:], A2, mod_d.t.ap()[b:b + 1, 4096:5120], 128, reads=[mod_d])
                        bcast_load(S2[:], S2, mod_d.t.ap()[b:b + 1, 3072:4096], 128, reads=[mod_d])
                        A2s.append((A2, S2))
                    def tileA(i):
                            b = i // 16
                            A2, S2 = A2s[b]
                            xt = xrot.get()
                            kb.dma("sp", xt[:], x1_d[i * 128:(i + 1) * 128, :], reads=[x1_r[i]], writes=[xt])
                            junk = junk_rot.get(); ssq = small_rot.get()
                            kb.op("act", lambda e: e.activation(out=junk[:], in_=xt[:], func=AF.Square, accum_out=ssq[:, 0:1]), [xt], [junk, ssq])
                            rms_rstd(ssq[:, 0:1], ssq[:, 1:2], ssq, ssq, 1024, EPS)
                            yield
                            uf = ufr.get()
                            kb.op("dve", lambda e: e.scalar_tensor_tensor(out=junk[:], in0=xt[:], scalar=ssq[:, 1:2], in1=A2[:], op0=ALU.mult, op1=ALU.mult), [xt, ssq, A2], [junk])
                            kb.op("pool", lambda e: e.tensor_tensor(out=uf[:], in0=junk[:], in1=S2[:], op=ALU.add), [junk, S2], [uf])
                            ub = ubr.get()
                            kb.op("act", lambda e: e.copy(out=ub[:], in_=uf[:]), [uf], [ub])
                            kb.dma("sp", u_d[i * 128:(i + 1) * 128, :], ub[:], reads=[ub], writes=[u_r[i]])
                            yield
                            utf = uTf.get()
                            for half in range(2):
                                transposes(uf, [uf[:, (half * 4 + k) * 128:(half * 4 + k + 1) * 128] for k in range(4)], utf[:, half * 4:(half + 1) * 4, :], utf, dt=F32, eng="dve")
                            pr = ps1.get()
                            for k in range(8):
                                kb.op("pe", lambda e: e.matmul(pr[:, 0:256], lhsT=utf[:, k, :], rhs=rw_f[:, k, :], start=(k == 0), stop=(k == 7)), [utf, rw_f], [pr], inc=(k == 7))
                            sc = s256.get(); sel = s256.get(); w1_ = s256.get(); w2_ = s256.get()
                            kb.op("act", lambda e: e.activation(out=sc[:], in_=pr[:, 0:256], func=AF.Sigmoid), [pr], [sc])
                            kb.op("dve", lambda e: e.tensor_tensor(out=sel[:], in0=sc[:], in1=rbias[:], op=ALU.add), [sc, rbias], [sel])
                            yield
                            sel3 = sel[:].rearrange("p (g c) -> p g c", g=8)
                            mx = m8r.get(); mx2 = m8r.get(); gs = m8r.get(); gsort = m8r.get()
                            kb.op("dve", lambda e: e.tensor_reduce(out=mx[:], in_=sel3, axis=AX.X, op=ALU.max), [sel], [mx])
                            kb.op("dve", lambda e: e.tensor_tensor(out=w1_[:].rearrange("p (g c) -> p g c", g=8), in0=sel3, in1=mx[:].unsqueeze(2).to_broadcast([128, 8, 32]), op=ALU.is_ge),
                                  [sel, mx], [w1_])
                            kb.op("dve", lambda e: e.scalar_tensor_tensor(out=w2_[:], in0=w1_[:], scalar=-10.0, in1=sel[:], op0=ALU.mult, op1=ALU.add), [w1_, sel], [w2_])
                            kb.op("dve", lambda e: e.tensor_reduce(out=mx2[:], in_=w2_[:].rearrange("p (g c) -> p g c", g=8), axis=AX.X, op=ALU.max), [w2_], [mx2])
                            yield
                            kb.op("dve", lambda e: e.tensor_tensor(out=gs[:], in0=mx[:], in1=mx2[:], op=ALU.add), [mx, mx2], [gs])
                            kb.op("dve", lambda e: e.max(out=gsort[:], in_=gs[:]), [gs], [gsort])
                            kb.op("dve", lambda e: e.tensor_scalar(out=gs[:], in0=gs[:], scalar1=gsort[:, 3:4], scalar2=None, op0=ALU.is_ge), [gs, gsort], [gs])
                            kb.op("dve", lambda e: e.scalar_tensor_tensor(out=w1_[:].rearrange("p (g c) -> p g c", g=8), in0=sel3, scalar=2.0,
                                                                          in1=gs[:].unsqueeze(2).to_broadcast([128, 8, 32]), op0=ALU.add, op1=ALU.mult), [sel, gs], [w1_])
                            yield
                            top8 = m8r.get()
                            kb.op("dve", lambda e: e.max(out=top8[:], in_=w1_[:]), [w1_], [top8])
                            kb.op("dve", lambda e: e.tensor_scalar(out=w2_[:], in0=w1_[:], scalar1=top8[:, 7:8], scalar2=None, op0=ALU.is_ge), [w1_, top8], [w2_])
                            kb.op("pool", lambda e: e.tensor_copy(out=mask_all[:, i, :], in_=w2_[:]), [w2_], [mask_all])
                            yield
                            st = small_rot.get()
                            kb.op("dve", lambda e: e.tensor_tensor(out=w1_[:], in0=w2_[:], in1=sc[:], op=ALU.mult), [w2_, sc], [w1_])
                            kb.op("dve", lambda e: e.tensor_reduce(out=st[:, 0:1], in_=w1_[:], axis=AX.X, op=ALU.add), [w1_], [st])
                            kb.op("dve", lambda e: e.reciprocal(out=st[:, 1:2], in_=st[:, 0:1]), [st], [st])
                            kb.op("dve", lambda e: e.tensor_scalar(out=G_all[:, i, :], in0=w1_[:], scalar1=st[:, 1:2], scalar2=2.5, op0=ALU.mult, op1=ALU.mult), [w1_, st], [G_all])
                            yield
                            pc = ps1.get()
                            kb.op("pe", lambda e: e.matmul(pc[:, 0:256], lhsT=tri[:, 1, :], rhs=mask_all[:, i, :], start=True, stop=True), [tri, mask_all], [pc])
                            kb.op("pe", lambda e: e.matmul(pc[:, 256:512], lhsT=ones_b[:], rhs=mask_all[:, i, :], start=True, stop=True), [ones_b, mask_all], [pc])
                            kb.op("dve", lambda e: e.tensor_tensor(out=POS_all[:, i, :], in0=pc[:, 0:256], in1=cnt[:], op=ALU.add), [pc, cnt], [POS_all])
                            kb.op("dve", lambda e: e.tensor_tensor(out=cnt[:], in0=pc[:, 256:512], in1=cnt[:], op=ALU.add), [pc, cnt], [cnt])
                    run_interleaved((tileA(i) for i in range(NT)), 3)
                if "G_dbg" in dbg:
                    kb.dma("sp", dbg_d["G_dbg"][:, :, 0:256], G_all[:, 0:16, :], reads=[G_all], writes=[dbg_d["G_dbg"]])
                base_r = kb.sb("base_r", [128, 256], F32)
                with kb.scope():
                    ind = kb.sb("ind", [128, 256], BF16)
                    kb.op("dve", lambda e: e.tensor_scalar(out=ind[:], in0=cnt[:], scalar1=iota_p[:, 1:2], scalar2=None, op0=ALU.is_gt), [cnt, iota_p], [ind])
                    pn = ps1.get()
                    kb.op("pe", lambda e: e.matmul(pn[:, 0:256], lhsT=ones_b[:], rhs=ind[:], start=True, stop=True), [ones_b, ind], [pn])
                    nblk = kb.sb("nblk", [128, 256], BF16)
                    kb.op("dve", lambda e: e.tensor_copy(out=nblk[:], in_=pn[:, 0:256]), [pn], [nblk])
                    nbT = kb.sb("nbT", [128, 2, 128], BF16)
                    transposes(nblk, [nblk[:, 0:128], nblk[:, 128:256]], nbT[:], nbT)
                    pb = ps1.get()
                    kb.op("pe", lambda e: e.matmul(pb[:, 0:128], lhsT=nbT[:, 0, :], rhs=tri[:, 1, :], start=True, stop=True), [nbT, tri], [pb])
                    kb.op("pe", lambda e: e.matmul(pb[:, 128:256], lhsT=nbT[:, 0, :], rhs=ones_b[:], start=True, stop=False), [nbT, ones_b], [pb], inc=False)
                    kb.op("pe", lambda e: e.matmul(pb[:, 128:256], lhsT=nbT[:, 1, :], rhs=tri[:, 1, :], start=False, stop=True), [nbT, tri], [pb])
                    kb.op("dve", lambda e: e.tensor_copy(out=base_r[:], in_=pb[:, 0:256]), [pb], [base_r])
                    pe_ = ps1.get()
                    kb.op("pe", lambda e: e.matmul(pe_[:, 0:1], lhsT=tri[:, 0, :], rhs=nbT[:, 0, 0:1], start=True, stop=True), [nbT, tri], [pe_])
                    kb.op("pe", lambda e: e.matmul(pe_[:, 1:2], lhsT=ones_b[:], rhs=nbT[:, 0, 0:1], start=True, stop=False), [nbT, ones_b], [pe_], inc=False)
                    kb.op("pe", lambda e: e.matmul(pe_[:, 1:2], lhsT=tri[:, 0, :], rhs=nbT[:, 1, 0:1], start=False, stop=True), [nbT, tri], [pe_])
                    endT = kb.sb("endT", [128, 2], F32)
                    kb.op("dve", lambda e: e.tensor_copy(out=endT[:], in_=pe_[:, 0:2]), [pe_], [endT])
                    iota_b = kb.sb("iota_b", [128, NBLK], F32)
                    kb.dma("sp", iota_b[:], I["c_iotab"].ap()[:, 0:NBLK], writes=[iota_b])
                    ind2 = kb.sb("ind2", [128, 2, NBLK], BF16)
                    for c_ in range(2):
                        kb.op("dve", lambda e: e.tensor_scalar(out=ind2[:, c_, :], in0=iota_b[:], scalar1=endT[:, c_:c_ + 1], scalar2=None, op0=ALU.is_ge), [iota_b, endT], [ind2])
                    BEf = kb.sb("BEf", [128, NBLK], F32)
                    for n0 in range(0, NBLK, 512):
                        n1 = min(n0 + 512, NBLK)
                        pbe = ps1.get()
                        for c_ in range(2):
                            kb.op("pe", lambda e: e.matmul(pbe[:, 0:n1 - n0], lhsT=ones_b[:], rhs=ind2[:, c_, n0:n1], start=(c_ == 0), stop=(c_ == 1)), [ones_b, ind2], [pbe], inc=(c_ == 1))
                        kb.op("dve", lambda e: e.tensor_scalar(out=BEf[:, n0:n1], in0=pbe[:, 0:n1 - n0], scalar1=128.0, scalar2=iota_p[:, 0:1], op0=ALU.mult, op1=ALU.add), [pbe, iota_p], [BEf])
                    kb.op("dve", lambda e: e.tensor_copy(out=IDXi[:], in_=BEf[:]), [BEf], [IDXi])
                with kb.scope():
                    s256 = kb.rot("s256b", [128, 256], F32, 9)
                    m8r = kb.rot("m8b", [128, 8], F32, 6)
                    ubr = kb.rot("ubB", [128, 1024], BF16, 3)
                    def tileB(i):
                            t_ = s256.get(); V = s256.get(); eq = s256.get()
                            kb.op("dve", lambda e: e.scalar_tensor_tensor(out=t_[:], in0=base_r[:], scalar=256.0, in1=POS_all[:, i, :], op0=ALU.mult, op1=ALU.add), [base_r, POS_all], [t_])
                            kb.op("dve", lambda e: e.scalar_tensor_tensor(out=V[:], in0=t_[:], scalar=1.0, in1=mask_all[:, i, :], op0=ALU.add, op1=ALU.mult), [t_, mask_all], [V])
                            yield
                            top8 = m8r.get(); d8 = m8r.get()
                            kb.op("dve", lambda e: e.max(out=top8[:], in_=V[:]), [V], [top8])
                            kb.op("dve", lambda e: e.tensor_scalar(out=d8[:], in0=top8[:], scalar1=-1.0, scalar2=None, op0=ALU.add), [top8], [d8])
                            kb.op("dve", lambda e: e.tensor_copy(out=dest_all[:, i, :], in_=d8[:]), [d8], [dest_all])
                            yield
                            for k in range(8):
                                kb.op("dve", lambda e: e.tensor_scalar(out=eq[:], in0=V[:], scalar1=top8[:, k:k + 1], scalar2=None, op0=ALU.is_equal), [V, top8], [eq])
                                kb.op("pool", lambda e: e.tensor_tensor(out=eq[:], in0=eq[:], in1=G_all[:, i, :], op=ALU.mult), [eq, G_all], [eq])
                                kb.op("dve", lambda e: e.tensor_reduce(out=gate_all[:, i, k:k + 1], in_=eq[:], axis=AX.X, op=ALU.add), [eq], [gate_all])
                                if k % 2 == 1:
                                    yield
                            yield
                            ub = ubr.get()
                            kb.dma("sp", ub[:], u_d[i * 128:(i + 1) * 128, :], reads=[u_r[i]], writes=[ub])
                            for k in range(8):
                                kb.idma(out=xs_d[:, :], in_=ub[:], out_off=dest_all[:, i, k:k + 1], reads=[ub, dest_all], writes=[xs_d])
                    run_interleaved((tileB(i) for i in range(NT)), 3)
            xT_rot = kb.rot("xT", [128, 8, 128], BF16, 3)
            silu_rot = kb.rot("silu", [128, 256], F32, 2)
            hb_rot = kb.rot("hb", [128, 256], BF16, 4)
            hT_rot = kb.rot("hT6", [128, 2, 128], BF16, 3)

            def ffn_a(xsb, wb13, strided):
                if strided:
                    srcs = [xsb[:].rearrange("s (p k) -> s k p", k=8)[:, k, :] for k in range(8)]
                else:
                    srcs = [xsb[:, k * 128:(k + 1) * 128] for k in range(8)]
                xT = xT_rot.get()
                transposes(xsb, srcs, xT[:], xT)
                ph = ps1.get()
                for k in range(8):
                    kb.op("pe", lambda e: e.matmul(ph[:, :], lhsT=xT[:, k, :], rhs=wb13[:, k, :, :].rearrange("p w n -> p (w n)"), start=(k == 0), stop=(k == 7)),
                          [xT, wb13], [ph], inc=(k == 7))
                sl = silu_rot.get(); hb = hb_rot.get()
                kb.op("act", lambda e: e.activation(out=sl[:], in_=ph[:, 0:256], func=AF.Silu), [ph], [sl])
                kb.op("dve", lambda e: e.tensor_tensor(out=hb[:], in0=ph[:, 256:512], in1=sl[:], op=ALU.mult), [ph, sl], [hb])
                return hb

            def ffn_b(hb, wb2, strided):
                if strided:
                    hs = [hb[:].rearrange("s (p j) -> s j p", j=2)[:, j, :] for j in range(2)]
                else:
                    hs = [hb[:, j * 128:(j + 1) * 128] for j in range(2)]
                hT = hT_rot.get()
                transposes(hb, hs, hT[:], hT, eng="dve")
                po_ = ps2.get()
                for nb_ in range(2):
                    for j in range(2):
                        kb.op("pe", lambda e: e.matmul(po_[:, nb_ * 512:(nb_ + 1) * 512], lhsT=hT[:, j, :], rhs=wb2[:, j, nb_ * 512:(nb_ + 1) * 512],
                                                       start=(j == 0), stop=(j == 1)), [hT, wb2], [po_], inc=(j == 1))
                return po_

            def ffn_block(xsb, wb13, wb2, strided):
                return ffn_b(ffn_a(xsb, wb13, strided), wb2, strided)

            with kb.scope():
                w1v = I["expert_w1"].ap().rearrange("e (p k) n -> (e p) (k n)", k=8)
                w3v = I["expert_w3"].ap().rearrange("e (p k) n -> (e p) (k n)", k=8)
                w2v = I["expert_w2"].ap().rearrange("e (p j) n -> (e p) (j n)", j=2)
                wf13 = kb.rot("wf13", [128, 2, 2048], F32, 3)
                wf2 = kb.rot("wf2", [128, 2048], F32, 3)
                wb13r = kb.rot("wb13", [128, 8, 2, 256], BF16, 3)
                wb2r = kb.rot("wb2", [128, 2, 1024], BF16, 4)
                xsr = kb.rot("xsb", [128, 1024], BF16, 8)
                ybr = kb.rot("yb", [128, 1024], BF16, 3)
                for t_ in wf13.tiles + wf2.tiles:
                    kb.op("pool", lambda e: e.memset(t_[:], 0.0), [], [t_])
                pend = {}

                def issue(bk):
                    f13 = wf13.get(); f2 = wf2.get()
                    idx = IDXi[:, bk:bk + 1]
                    kb.idma(out=f13[:, 0, :], in_=w1v, in_off=idx, reads=[IDXi], writes=[f13], bounds=32767)
                    kb.idma(out=f13[:, 1, :], in_=w3v, in_off=idx, reads=[IDXi], writes=[f13], bounds=32767)
                    kb.idma(out=f2[:], in_=w2v, in_off=idx, reads=[IDXi], writes=[f2], bounds=32767)
                    xs2 = []
                    for sbk in range(2):
                        xsb = xsr.get()
                        r0 = (bk * 2 + sbk) * 128
                        kb.dma("sp", xsb[:], xs_d[r0:r0 + 128, :], reads=[xs_d], writes=[xsb])
                        xs2.append(xsb)
                    pend[bk] = (f13, f2, xs2)

                nrun = min(NBLK, blk_limit)
                NSUB = nrun * 2
                ctx_ = {}

                def st0(n):
                    bk = n // 2
                    if n % 2 == 0:
                        if bk + 2 < nrun:
                            issue(bk + 2)
                        f13, f2, xs2 = pend.pop(bk)
                        wb13 = wb13r.get(); wb2 = wb2r.get()
                        kb.op("act", lambda e: e.copy(out=wb13[:, :, 0, :], in_=f13[:, 0, :].rearrange("p (k n) -> p k n", k=8)), [f13], [wb13])
                        kb.op("dve", lambda e: e.tensor_copy(out=wb13[:, :, 1, :], in_=f13[:, 1, :].rearrange("p (k n) -> p k n", k=8)), [f13], [wb13])
                        kb.op("act", lambda e: e.copy(out=wb2[:, 0, :], in_=f2[:, 0:1024]), [f2], [wb2])
                        kb.op("dve", lambda e: e.tensor_copy(out=wb2[:, 1, :], in_=f2[:, 1024:2048]), [f2], [wb2])
                        ctx_[("w", bk)] = (wb13, wb2, xs2)
                    wb13, wb2, xs2 = ctx_[("w", bk)]
                    xsb = xs2[n % 2]
                    srcs = [xsb[:].rearrange("s (p k) -> s k p", k=8)[:, k, :] for k in range(8)]
                    xT = xT_rot.get()
                    transposes(xsb, srcs, xT[:], xT)
                    ctx_[n] = dict(xT=xT, wb13=wb13, wb2=wb2)

                def st1(n):
                    c = ctx_[n]
                    xT, wb13 = c["xT"], c["wb13"]
                    ph = ps1.get()
                    for k in range(8):
                        kb.op("pe", lambda e: e.matmul(ph[:, :], lhsT=xT[:, k, :], rhs=wb13[:, k, :, :].rearrange("p w n -> p (w n)"), start=(k == 0), stop=(k == 7)),
                              [xT, wb13], [ph], inc=(k == 7))
                    sl = silu_rot.get(); hb = hb_rot.get()
                    kb.op("act", lambda e: e.activation(out=sl[:], in_=ph[:, 0:256], func=AF.Silu), [ph], [sl])
                    kb.op("dve", lambda e: e.tensor_tensor(out=hb[:], in0=ph[:, 256:512], in1=sl[:], op=ALU.mult), [ph, sl], [hb])
                    c["hb"] = hb

                def st2(n):
                    c = ctx_[n]
                    hb = c["hb"]
                    hs = [hb[:].rearrange("s (p j) -> s j p", j=2)[:, j, :] for j in range(2)]
                    hT = hT_rot.get()
                    transposes(hb, hs, hT[:], hT, eng="dve")
                    c["hT"] = hT

                def st3(n):
                    c = ctx_.pop(n)
                    hT, wb2 = c["hT"], c["wb2"]
                    po_ = ps2.get()
                    for nb_ in range(2):
                        for j in range(2):
                            kb.op("pe", lambda e: e.matmul(po_[:, nb_ * 512:(nb_ + 1) * 512], lhsT=hT[:, j, :], rhs=wb2[:, j, nb_ * 512:(nb_ + 1) * 512],
                                                           start=(j == 0), stop=(j == 1)), [hT, wb2], [po_], inc=(j == 1))
                    yb = ybr.get()
                    kb.op("act", lambda e: e.copy(out=yb[:, 0:512], in_=po_[:, 0:512]), [po_], [yb])
                    kb.op("dve", lambda e: e.tensor_copy(out=yb[:, 512:1024], in_=po_[:, 512:1024]), [po_], [yb])
                    kb.dma("sp", Y_d[n * 128:(n + 1) * 128, :], yb[:], reads=[yb], writes=[Y_d])

                for bk in range(min(2, nrun)):
                    issue(bk)
                stages = (st0, st1, st2, st3)
                for step in range(NSUB + 3):
                    for k_, fn_ in enumerate(stages):
                        n = step - k_
                        if 0 <= n < NSUB:
                            fn_(n)
            with kb.scope():
                wsf = kb.sb("wsf", [128, 8, 256], F32)
                ws13 = kb.sb("ws13", [128, 8, 2, 256], BF16)
                ws2f = kb.sb("ws2f", [128, 2, 1024], F32)
                ws2 = kb.sb("ws2", [128, 2, 1024], BF16)
                for w_, nm in enumerate(("shared_w1", "shared_w3")):
                    kb.dma("sp", wsf[:], I[nm].ap().rearrange("(k p) n -> p k n", p=128), reads=[wsf], writes=[wsf])
                    kb.op("dve", lambda e: e.tensor_copy(out=ws13[:, :, w_, :], in_=wsf[:]), [wsf], [ws13])
                kb.dma("sp", ws2f[:], I["shared_w2"].ap().rearrange("(j p) n -> p j n", p=128), writes=[ws2f])
                kb.op("dve", lambda e: e.tensor_copy(out=ws2[:], in_=ws2f[:]), [ws2f], [ws2])
                Gms = []
                for b in range(nbatch):
                    Gm = kb.sb("Gm", [128, 1024], F32)
                    bcast_load(Gm[:], Gm, mod_d.t.ap()[b:b + 1, 5120:6144], 128, reads=[mod_d])
                    Gms.append(Gm)
                ubr = kb.rot("ubD", [128, 1024], BF16, 3)
                ygr = kb.rot("yg", [128, 1024], BF16, 8)
                accr = kb.rot("acc", [128, 1024], F32, 3)
                xrot = kb.rot("xtD", [128, 1024], F32, 3)
                def tileD(i):
                        b = i // 16
                        ub = ubr.get()
                        kb.dma("sp", ub[:], u_d[i * 128:(i + 1) * 128, :], reads=[u_r[i]], writes=[ub])
                        yield
                        hb_ = ffn_a(ub, ws13, False)
                        yield
                        po_ = ffn_b(hb_, ws2, False)
                        acc = accr.get()
                        kb.op("act", lambda e: e.copy(out=acc[:], in_=po_[:, :]), [po_], [acc])
                        yield
                        for k in range(8):
                            yg = ygr.get()
                            kb.idma(out=yg[:], in_=Y_d[:, :], in_off=dest_all[:, i, k:k + 1], reads=[dest_all, Y_d], writes=[yg])
                            kb.op("dve", lambda e: e.scalar_tensor_tensor(out=acc[:], in0=yg[:], scalar=gate_all[:, i, k:k + 1], in1=acc[:], op0=ALU.mult, op1=ALU.add),
                                  [yg, gate_all, acc], [acc])
                            if k % 2 == 1:
                                yield
                        if "moe_dbg" in dbg and i < 16:
                            kb.dma("sp", dbg_d["moe_dbg"][:, i, :], acc[:], reads=[acc], writes=[dbg_d["moe_dbg"]])
                        yield
                        xt = xrot.get()
                        kb.dma("sp", xt[:], x1_d[i * 128:(i + 1) * 128, :], reads=[x1_r[i]], writes=[xt])
                        kb.op("dve", lambda e: e.tensor_tensor(out=acc[:], in0=acc[:], in1=Gms[b][:], op=ALU.mult), [acc, Gms[b]], [acc])
                        kb.op("pool", lambda e: e.tensor_tensor(out=xt[:], in0=xt[:], in1=acc[:], op=ALU.add), [xt, acc], [xt])
                        kb.dma("sp", out_d.ap()[b, (i % 16) * 128:(i % 16 + 1) * 128, :], xt[:], reads=[xt])
                run_interleaved((tileD(i) for i in range(NT)), 3)
        kb.finish()
        print("instructions:", kb.nins, "sems:", len(kb.sems) + len(kb.dsem))
    return nc


_CACHE = {}


def kernel(**inputs):
    n = 8
    consts = host_consts()
    sq = lambda k: np.ascontiguousarray(np.asarray(inputs[k], dtype=np.float32)[0])
    shared = {}
    for k in IN_SHAPES:
        if k in ("x", "ctx", "cT") or k.startswith("c_"):
            continue
        a = sq(k)
        shared[k] = a.reshape(IN_SHAPES[k])
    shared.update(consts)
    x = np.asarray(inputs["x"], np.float32)
    ctx = np.asarray(inputs["ctx"], np.float32)
    c = np.asarray(inputs["c"], np.float32)
    c_ctx = np.asarray(inputs["c_ctx"], np.float32)
    in_maps = []
    for i in range(n):
        m = dict(shared)
        m["x"] = np.ascontiguousarray(x[2 * i:2 * i + 2])
        m["ctx"] = np.ascontiguousarray(ctx[2 * i:2 * i + 2])
        m["cT"] = np.ascontiguousarray(np.stack([c[2 * i], c[2 * i + 1], c_ctx], axis=1))
        in_maps.append(m)
    if "nc" not in _CACHE:
        _CACHE["nc"] = build()
    res = run_bass_kernel_spmd(_CACHE["nc"], in_maps, core_ids=list(range(n)))
    return np.concatenate([np.asarray(r["out"], np.float32) for r in res.results], axis=0)
```

```python
import numpy as np
import ml_dtypes
import concourse.bass as bass
import concourse.mybir as mybir
from concourse.bass_utils import run_bass_kernel_spmd
from contextlib import ExitStack, contextmanager

F32 = mybir.dt.float32
BF16 = mybir.dt.bfloat16
AF = mybir.ActivationFunctionType
ALU = mybir.AluOpType
AX = mybir.AxisListType

C_DEC = -0.6065306597126334
EPS = 1e-6
GN_EPS = 64e-5
ATTN_SCALE = 96 ** -0.5


NAMES = {}


class Buf:
    __slots__ = ("name", "w", "r", "excl")

    def __init__(self, name=""):
        self.name = name
        self.w = None
        self.r = {}
        self.excl = False


class Tile:
    def __init__(self, t, name):
        self.t = t
        self.b = Buf(name)

    def __getitem__(self, k):
        return self.t[k]


class Rot:
    def __init__(self, tiles):
        self.tiles = tiles
        self.i = 0

    def get(self):
        t = self.tiles[self.i % len(self.tiles)]
        self.i += 1
        return t


class KB:
    ENG = ("pe", "act", "dve", "pool", "sp")
    EPOCH = 8000
    NDMA = 40

    def __init__(self, nc, stack):
        self.nc = nc
        self.gstack = stack
        self.stack = stack
        self.e = {"pe": nc.tensor, "act": nc.scalar, "dve": nc.vector,
                  "pool": nc.gpsimd, "sp": nc.sync}
        self.cnt = {k: 0 for k in self.ENG}
        self.ep = {k: 0 for k in self.ENG}
        self.sems = {}
        self.waited = {}
        for k in self.ENG:
            self._newsem(k)
        self.dsem = []
        self.dslot = []
        self.dval = []
        for i in range(self.NDMA):
            self.dsem.append(stack.enter_context(nc.semaphore(f"dq{i}")))
            self.dslot.append(i)
            self.dval.append(0)
        self.dnext = 0
        self.dwaited = {}
        self.nins = 0
        self.uid = 0
        self.promised = False
        self.bregs = {}

    def sb(self, name, shape, dtype):
        self.uid += 1
        nm = f"{name}_{self.uid}"
        NAMES[name] = nm
        return Tile(self.stack.enter_context(self.nc.sbuf_tensor(nm, list(shape), dtype)), nm)

    def ps(self, name, shape, dtype=F32):
        t = Tile(self.gstack.enter_context(self.nc.psum_tensor(name, list(shape), dtype)), name)
        t.b.excl = True
        return t

    def dram(self, name, shape, dtype, kind="Internal"):
        return Tile(self.nc.dram_tensor(name, list(shape), dtype, kind=kind), name)

    def rot(self, name, shape, dtype, n=2):
        return Rot([self.sb(f"{name}{i}", shape, dtype) for i in range(n)])

    @contextmanager
    def scope(self):
        old = self.stack
        with ExitStack() as st:
            self.stack = st
            yield
            self.barrier()
        self.stack = old

    def _newsem(self, k):
        self.sems[(k, self.ep[k])] = self.gstack.enter_context(
            self.nc.semaphore(f"s_{k}_{self.ep[k]}"))

    def _wait(self, eng, tok):
        if tok is None:
            return
        if tok[0] == "e":
            _, k, ep, c = tok
            if eng == "pe" and k == "pe":
                return
            assert not (k == "pe" and ep == self.ep["pe"] and c > self.cnt["pe"]), "wait on a promised PE token"
            key = (eng, k, ep)
            if self.waited.get(key, 0) >= c:
                return
            self.e[eng].wait_ge(self.sems[(k, ep)], c)
            self.waited[key] = c
        else:
            _, i, v = tok
            key = (eng, i)
            if self.dwaited.get(key, 0) >= v:
                return
            self.e[eng].wait_ge(self.dsem[i], v)
            self.dwaited[key] = v

    @staticmethod
    def _bufs(xs):
        out = []
        for x in xs:
            if x is None:
                continue
            out.append(x.b if isinstance(x, Tile) else x)
        return out

    def _deps(self, eng, reads, writes):
        for b in reads:
            self._wait(eng, b.w)
        for b in writes:
            self._wait(eng, b.w)
            for t in b.r.values():
                self._wait(eng, t)

    def _commit(self, tok, reads, writes):
        for b in writes:
            b.w = tok
            b.r = {}
        for b in reads:
            if b not in writes:
                key = tok[1] if tok[0] == "e" else ("d", tok[1])
                b.r[key] = tok

    def op(self, eng, fn, reads=(), writes=(), inc=True):
        reads = self._bufs(reads)
        writes = self._bufs(writes)
        writes = writes + [b for b in reads if b.excl and b not in writes]
        self._deps(eng, reads, writes)
        if inc and self.cnt[eng] >= self.EPOCH and not (eng == "pe" and self.promised):
            self.ep[eng] += 1
            self.cnt[eng] = 0
            self._newsem(eng)
        ins = fn(self.e[eng])
        if eng == "pe":
            self.promised = not inc
        if inc:
            self.cnt[eng] += 1
            ins.then_inc(self.sems[(eng, self.ep[eng])], 1)
            tok = ("e", eng, self.ep[eng], self.cnt[eng])
        else:
            assert eng == "pe"
            tok = ("e", eng, self.ep[eng], self.cnt[eng] + 1)
        self._commit(tok, reads, writes)
        self.nins += 1

    def dma(self, eng, out, in_, reads=(), writes=(), **kw):
        reads = self._bufs(reads)
        writes = self._bufs(writes)
        self._deps(eng, reads, writes)
        s = self.dnext
        self.dnext = (self.dnext + 1) % self.NDMA
        i = self.dslot[s]
        if self.dval[i] >= 8000:
            self.dsem.append(self.gstack.enter_context(self.nc.semaphore(f"dq{len(self.dsem)}")))
            self.dval.append(0)
            prev = i
            i = len(self.dsem) - 1
            self.dslot[s] = i
            self._wait(eng, ("d", prev, self.dval[prev]))
        if self.dval[i] > 0:
            self._wait(eng, ("d", i, self.dval[i]))
        self.dval[i] += 16
        self.e[eng].dma_start(out=out, in_=in_, **kw).then_inc(self.dsem[i], 16)
        tok = ("d", i, self.dval[i])
        self._commit(tok, reads, writes)
        self.nins += 1

    def idma(self, out, in_, out_off=None, in_off=None, reads=(), writes=(), bounds=None):
        eng = "pool"
        reads = self._bufs(reads)
        writes = self._bufs(writes)
        self._deps(eng, reads, writes)
        s = self.dnext
        self.dnext = (self.dnext + 1) % self.NDMA
        i = self.dslot[s]
        if self.dval[i] >= 8000:
            self.dsem.append(self.gstack.enter_context(self.nc.semaphore(f"dq{len(self.dsem)}")))
            self.dval.append(0)
            prev = i
            i = len(self.dsem) - 1
            self.dslot[s] = i
            self._wait(eng, ("d", prev, self.dval[prev]))
        if self.dval[i] > 0:
            self._wait(eng, ("d", i, self.dval[i]))
        self.dval[i] += 16
        kw = {}
        if bounds is not None:
            if bounds not in self.bregs:
                r = self.nc.gpsimd.alloc_register(f"bnd{bounds}")
                self.nc.gpsimd.reg_mov(r, bounds)
                self.bregs[bounds] = r
            kw = dict(bounds_check=self.bregs[bounds], oob_is_err=False)
        self.e[eng].indirect_dma_start(
            out=out, out_offset=None if out_off is None else bass.IndirectOffsetOnAxis(ap=out_off, axis=0),
            in_=in_, in_offset=None if in_off is None else bass.IndirectOffsetOnAxis(ap=in_off, axis=0), **kw,
        ).then_inc(self.dsem[i], 16)
        tok = ("d", i, self.dval[i])
        self._commit(tok, reads, writes)
        self.nins += 1

    def finish(self):
        for i in range(len(self.dsem)):
            if self.dval[i] > 0:
                self._wait("sp", ("d", i, self.dval[i]))
        for k in self.ENG:
            if k != "sp" and (self.cnt[k] > 0 or self.ep[k] > 0):
                self._wait("sp", ("e", k, self.ep[k], self.cnt[k]))

    def barrier(self):
        self.finish()
        self.op("sp", lambda e: e.nop(), (), ())
        tok = ("e", "sp", self.ep["sp"], self.cnt["sp"])
        for k in ("pe", "act", "dve", "pool"):
            self._wait(k, tok)


def run_interleaved(gens, width):
    it = iter(gens)
    active = []
    exhausted = False
    while True:
        while len(active) < width and not exhausted:
            try:
                active.append(next(it))
            except StopIteration:
                exhausted = True
        if not active:
            break
        for g in list(active):
            try:
                next(g)
            except StopIteration:
                active.remove(g)


def host_consts():
    s = np.arange(128)[:, None]
    t = np.arange(128)[None, :]
    le = (s <= t).astype(np.float32)
    lt = (s < t).astype(np.float32)
    ge = (s >= t).astype(np.float32)
    gt = (s > t).astype(np.float32)
    tri = np.stack([le, lt, ge, gt], axis=1)
    mask4 = np.zeros((2, 128, 512), np.float32)
    maskt = np.zeros((2, 128, 128), np.float32)
    for d, (strict, incl) in enumerate(((lt, le), (gt, ge))):
        mask4[d, :, 0:128] = -strict
        mask4[d, :, 128:256] = -incl
        mask4[d, :, 256:384] = strict
        mask4[d, :, 384:512] = incl
        maskt[d] = -strict.T
    rows = 2048 // 64
    row = np.repeat(np.arange(rows, dtype=np.float32), 64)
    col = np.tile(np.arange(64, dtype=np.float32), rows)
    inv = (10000.0 ** (-np.arange(8, dtype=np.float32) / 8)).astype(np.float32)
    ang = np.concatenate([row[:, None] * inv, col[:, None] * inv], axis=-1).astype(np.float32)
    p_ = np.arange(128, dtype=np.float32)
    iotap = np.stack([p_, 256 * p_], axis=1)
    iotab = np.tile(np.arange(512, dtype=np.float32)[None, :], (128, 1))
    return dict(c_iotap=iotap, c_iotab=iotab, c_ident=np.eye(128, dtype=np.float32), c_tri=tri, c_mask4=mask4, c_maskt=maskt,
                c_cos=np.cos(ang).astype(np.float32), c_sin=np.sin(ang).astype(np.float32))


IN_SHAPES = dict(
    x=[2, 2048, 1024], ctx=[2, 256, 1024], cT=[1024, 3],
    ada_w=[1024, 6144], ada_b=[1, 6144], norm_mix=[1, 1024], norm_ffn=[1, 1024],
    w_in=[1024, 4416], shift_conv=[3, 1952], q_lat_norm=[1, 256], w_uq=[256, 768],
    kv_lat_norm=[1, 128], w_ukv=[128, 1024], q_norm=[1, 96], k_norm=[1, 96], w_o_mla=[512, 1024],
    decay_w0=[2, 512], decay_w2=[2, 64, 512], aicl_a0=[2, 512], aicl_a2=[2, 64, 512],
    k_k=[1, 512], k_a=[1, 512], r_k=[1, 512], gn_w=[1, 512], gn_b=[1, 512], gate_g2=[160, 512],
    w_o_rwkv=[512, 1024], w_out=[1024, 1024], router_w=[1024, 256], router_bias=[1, 256],
    expert_w1=[256, 1024, 256], expert_w3=[256, 1024, 256], expert_w2=[256, 256, 1024],
    shared_w1=[1024, 256], shared_w3=[1024, 256], shared_w2=[256, 1024],
    c_ident=[128, 128], c_tri=[128, 4, 128], c_mask4=[2, 128, 512], c_maskt=[2, 128, 128],
    c_cos=[2048, 16], c_sin=[2048, 16], c_iotap=[128, 2], c_iotab=[128, 512],
)

CT0 = 1
LT0 = 259
HW = 2308


def build(upto=99, dbg=(), n_exp=256, nbatch=2, rw_tiles=99, rw_phase=9, rw_dirs=2, blk_limit=10 ** 9):
    nc = bass.Bass("TRN2", target_bir_lowering=False)
    I = {k: nc.dram_tensor(k, list(v), F32, kind="ExternalInput") for k, v in IN_SHAPES.items()
         if not (upto < 6 and k.startswith(("expert_", "shared_")))}
    out_d = nc.dram_tensor("out", [2, 2048, 1024], F32, kind="ExternalOutput")

    with ExitStack() as gst:
        kb = KB(nc, gst)
        dbgk = lambda n: ("ExternalOutput" if n in dbg else "Internal")
        ps1 = Rot([kb.ps(f"ps1_{i}", [128, 512]) for i in range(4)])
        ps2 = Rot([kb.ps(f"ps2_{i}", [128, 1024]) for i in range(2)])

        mod_d = kb.dram("mod_d", [3, 6144], F32, dbgk("mod_d"))
        mla_d = kb.dram("mla_d", [2304, 416], F32, dbgk("mla_d"))
        mla_r = [Buf() for _ in range(18)]
        rkv_d = kb.dram("rkv_d", [2304, 1536], F32, dbgk("rkv_d"))
        rkv_r = [Buf() for _ in range(18)]
        gates_d = kb.dram("gates_d", [2048, 2048], BF16, dbgk("gates_d"))
        gates_r = [[Buf() for _ in range(4)] for _ in range(16)]
        x1_d = kb.dram("x1_d", [4096, 1024], F32, dbgk("x1_d"))
        x1_r = [Buf() for _ in range(32)]
        dbg_d = {}
        for n, shp, dt in (("fm_dbg", [128, 4, 2304], BF16), ("att_dbg", [128, 4, 2048], BF16),
                           ("y_dbg", [128, 16, 512], F32), ("rw_dbg", [128, 4, 2048], BF16),
                           ("G_dbg", [128, 16, 257], F32), ("qT_dbg", [96, 8, 2048], BF16),
                           ("kT_dbg", [96, 8, 2304], BF16), ("moe_dbg", [128, 16, 1024], F32)):
            if n in dbg:
                dbg_d[n] = kb.dram(n, shp, dt, "ExternalOutput")

        ident_f = kb.sb("ident_f", [128, 128], F32)
        ident_b = kb.sb("ident_b", [128, 128], BF16)
        ones_b = kb.sb("ones_b", [128, 128], BF16)
        kb.dma("sp", ident_f[:], I["c_ident"].ap(), writes=[ident_f])
        kb.op("dve", lambda e: e.tensor_copy(out=ident_b[:], in_=ident_f[:]), [ident_f], [ident_b])
        kb.op("pool", lambda e: e.memset(ones_b[:], 1.0), [], [ones_b])

        def bcast_load(dst_ap, dst_tile, src_ap, nparts, reads=()):
            kb.dma("sp", dst_ap, src_ap.to_broadcast([nparts, src_ap.shape[-1]]), reads=reads, writes=[dst_tile])

        def transposes(src_tile, src_aps, dst_ap, dst_tile, dt=BF16, eng="act", width=128, pool=None):
            pp = (pool or ps1).get()
            pv = pp[:].bitcast(BF16) if dt == BF16 else pp[:]
            idn = ident_b if dt == BF16 else ident_f
            n = len(src_aps)
            P = src_aps[0].shape[0]
            w = src_aps[0].shape[1]
            for j, a in enumerate(src_aps):
                kb.op("pe", lambda e: e.transpose(out=pv[0:w, j * width:j * width + P], in_=a, identity=idn[0:P, 0:P]),
                      [src_tile, idn], [pp])
            src = pv[0:w, 0:n * width]
            if eng == "act":
                kb.op("act", lambda e: e.copy(out=dst_ap, in_=src.rearrange("p (j t) -> p j t", j=n) if len(dst_ap.shape) == 3 else src), [pp], [dst_tile])
            else:
                kb.op(eng, lambda e: e.tensor_copy(out=dst_ap, in_=src.rearrange("p (j t) -> p j t", j=n) if len(dst_ap.shape) == 3 else src), [pp], [dst_tile])

        def rms_rstd(ssq_ap, out_ap, tile_in, tile_out, n, eps):
            kb.op("act", lambda e: e.activation(out=out_ap, in_=ssq_ap, func=AF.Sqrt, bias=eps_t[:, 0:1] if eps == EPS else (gneps_t[:, 0:1] if eps == GN_EPS else tiny_t[:, 0:1]), scale=1.0 / n),
                  [tile_in], [tile_out])
            kb.op("dve", lambda e: e.reciprocal(out=out_ap, in_=out_ap), [tile_out], [tile_out])

        eps_t = kb.sb("eps_t", [128, 1], F32)
        gneps_t = kb.sb("gneps_t", [128, 1], F32)
        tiny_t = kb.sb("tiny_t", [128, 1], F32)
        kb.op("pool", lambda e: e.memset(eps_t[:], EPS), [], [eps_t])
        kb.op("pool", lambda e: e.memset(gneps_t[:], GN_EPS), [], [gneps_t])
        kb.op("pool", lambda e: e.memset(tiny_t[:], 1e-12), [], [tiny_t])

        with kb.scope():
            cTs = kb.sb("cTs", [128, 8, 3], F32)
            kb.dma("sp", cTs[:], I["cT"].ap().rearrange("(k p) r -> p k r", p=128), writes=[cTs])
            sT = kb.sb("sT", [128, 8, 3], F32)
            kb.op("act", lambda e: e.activation(out=sT[:], in_=cTs[:], func=AF.Silu), [cTs], [sT])
            modsb = kb.sb("modsb", [3, 6144], F32)
            adab = kb.sb("adab", [3, 6144], F32)
            bcast_load(adab[:], adab, I["ada_b"].ap()[0:1, :], 3)
            nrm = kb.sb("nrm", [3, 2, 1024], F32)
            bcast_load(nrm[:, 0, :], nrm, I["norm_mix"].ap()[0:1, :], 3)
            bcast_load(nrm[:, 1, :], nrm, I["norm_ffn"].ap()[0:1, :], 3)
            wrot = kb.rot("adaw", [128, 8, 512], F32, 2)
            for cb in range(12):
                wt = wrot.get()
                kb.dma("sp", wt[:], I["ada_w"].ap()[:, cb * 512:(cb + 1) * 512].rearrange("(k p) n -> p k n", p=128), writes=[wt])
                pp = ps1.get()
                for k in range(8):
                    kb.op("pe", lambda e: e.matmul(pp[0:3, :], lhsT=sT[:, k, :], rhs=wt[:, k, :], start=(k == 0), stop=(k == 7)),
                          [sT, wt], [pp], inc=(k == 7))
                kb.op("dve", lambda e: e.tensor_tensor(out=modsb[:, cb * 512:(cb + 1) * 512], in0=pp[0:3, :],
                                                       in1=adab[:, cb * 512:(cb + 1) * 512], op=ALU.add), [pp, adab], [modsb])
            for (c0, j) in ((1024, 0), (4096, 1)):
                kb.op("dve", lambda e: e.scalar_tensor_tensor(out=modsb[:, c0:c0 + 1024], in0=modsb[:, c0:c0 + 1024], scalar=1.0,
                                                              in1=nrm[:, j, :], op0=ALU.add, op1=ALU.mult), [modsb, nrm], [modsb])
            kb.dma("sp", mod_d[:, :], modsb[:], reads=[modsb], writes=[mod_d])
        if upto <= 0:
            kb.finish()
            return nc

        def norm_mod(xt, A, S, hb):
            junk = junk_rot.get()
            ssq = small_rot.get()
            kb.op("act", lambda e: e.activation(out=junk[:], in_=xt[:], func=AF.Square, accum_out=ssq[:, 0:1]), [xt], [junk, ssq])
            rms_rstd(ssq[:, 0:1], ssq[:, 1:2], ssq, ssq, 1024, EPS)
            kb.op("dve", lambda e: e.scalar_tensor_tensor(out=junk[:], in0=xt[:], scalar=ssq[:, 1:2], in1=A[:], op0=ALU.mult, op1=ALU.mult),
                  [xt, ssq, A], [junk])
            kb.op("pool", lambda e: e.tensor_tensor(out=hb[:], in0=junk[:], in1=S[:], op=ALU.add), [junk, S], [hb])

        for b in range(nbatch):
          with kb.scope():
            junk_rot = kb.rot("junk", [128, 1024], F32, 2)
            small_rot = kb.rot("small", [128, 16], F32, 6)
            mix_stack = ExitStack()
            fm_stack = ExitStack()
            _old = kb.stack
            kb.stack = mix_stack
            attT = kb.sb("attT", [128, 4, 2048], BF16)
            rwT = kb.sb("rwT", [128, 4, 2048], BF16)
            kb.stack = fm_stack
            fm = kb.sb("fm", [128, 4, 2304], BF16)
            kb.stack = _old

            with kb.scope():
                hT = kb.sb("hT", [128, 8, HW], BF16)
                for (c0, c1) in ((0, 1), (257, 259), (2307, 2308)):
                    kb.op("pool", lambda e: e.memset(hT[:, :, c0:c1], 0.0), [], [hT])
                with kb.scope():
                  xrot = kb.rot("xt", [128, 1024], F32, 2)
                  hbrot = kb.rot("hb", [128, 1024], BF16, 2)
                  for seg, (src, nt, off, row) in enumerate(((I["ctx"], 2, CT0, 2), (I["x"], 16, LT0, b))):
                    A1 = kb.sb("A1", [128, 1024], F32)
                    S1 = kb.sb("S1", [128, 1024], F32)
                    bcast_load(A1[:], A1, mod_d.t.ap()[row:row + 1, 1024:2048], 128, reads=[mod_d])
                    bcast_load(S1[:], S1, mod_d.t.ap()[row:row + 1, 0:1024], 128, reads=[mod_d])
                    for t in range(nt):
                        xt = xrot.get()
                        kb.dma("sp", xt[:], src.ap()[b, t * 128:(t + 1) * 128, :], writes=[xt])
                        hb = hbrot.get()
                        norm_mod(xt, A1, S1, hb)
                        transposes(hb, [hb[:, k * 128:(k + 1) * 128] for k in range(8)],
                                   hT[:, :, off + t * 128: off + (t + 1) * 128], hT)
                conv = kb.sb("conv", [128, 3, 1952], F32)
                for j in range(3):
                    bcast_load(conv[:, j, :], conv, I["shift_conv"].ap()[j:j + 1, :], 128)
                wf = kb.sb("wf", [128, 8, 512], F32)
                wbs = [kb.rot(f"wb{j}", [128, 8, 512], BF16, 2) for j in range(3)]
                osb = kb.rot("osb", [128, 512], F32, 2)
                gsb = kb.rot("gsb", [128, 512], BF16, 2)
                tok_tiles = [(CT0 + t * 128, t) for t in range(2)] + [(LT0 + t * 128, 2 + t) for t in range(16)]
                blocks = [(0, 416, "M")] + [(416 + i * 512, 512, "R") for i in range(3)] + [(1952, 416, "F")] + \
                         [(2368 + i * 512, 512, "G") for i in range(4)]
                for (c0, ncol, kind) in blocks:
                    kb.dma("sp", wf[:, :, 0:ncol], I["w_in"].ap()[:, c0:c0 + ncol].rearrange("(k p) n -> p k n", p=128), writes=[wf])
                    has_conv = kind in ("R", "F")
                    ws = []
                    for j in (range(3) if has_conv else range(1)):
                        wb = wbs[j].get()
                        if has_conv:
                            cc = c0 - 416
                            for k in range(8):
                                kb.op("pool" if k % 2 else "dve", lambda e: e.tensor_tensor(out=wb[:, k, 0:ncol], in0=wf[:, k, 0:ncol],
                                                                                           in1=conv[:, j, cc:cc + ncol], op=ALU.mult), [wf, conv], [wb])
                        else:
                            kb.op("pool", lambda e: e.tensor_copy(out=wb[:, :, 0:ncol], in_=wf[:, :, 0:ncol]), [wf], [wb])
                        ws.append(wb)
                    shifts = [(-1, 0), (0, 1), (1, 2)] if has_conv else [(0, 0)]
                    if kind in ("M", "R"):
                        for (off, ti) in tok_tiles:
                            pp = ps1.get()
                            n = len(shifts) * 8
                            i = 0
                            for (sh, j) in shifts:
                                for k in range(8):
                                    kb.op("pe", lambda e: e.matmul(pp[:, 0:ncol], lhsT=hT[:, k, off + sh:off + sh + 128], rhs=ws[j][:, k, 0:ncol],
                                                                   start=(i == 0), stop=(i == n - 1)), [hT, ws[j]], [pp], inc=(i == n - 1))
                                    i += 1
                            o = osb.get()
                            kb.op("act", lambda e: e.copy(out=o[:, 0:ncol], in_=pp[:, 0:ncol]), [pp], [o])
                            if kind == "M":
                                kb.dma("sp", mla_d[ti * 128:(ti + 1) * 128, :], o[:, 0:416], reads=[o], writes=[mla_r[ti]])
                            else:
                                cc = c0 - 416
                                kb.dma("sp", rkv_d[ti * 128:(ti + 1) * 128, cc:cc + 512], o[:, :], reads=[o], writes=[rkv_r[ti]])
                    elif kind == "F":
                        nblks = [(CT0, 0, 256)] + [(LT0 + i * 512, 256 + i * 512, 512) for i in range(4)]
                        for m, (m0, msz) in enumerate(((0, 128), (128, 128), (256, 128), (384, 32))):
                            func = (AF.Tanh, AF.Copy, AF.Sigmoid, AF.Sigmoid)[m]
                            for (off, dcol, nn) in nblks:
                                pp = ps1.get()
                                i = 0
                                for (sh, j) in shifts:
                                    for k in range(8):
                                        kb.op("pe", lambda e: e.matmul(pp[0:msz, 0:nn], lhsT=ws[j][:, k, m0:m0 + msz], rhs=hT[:, k, off + sh:off + sh + nn],
                                                                       start=(i == 0), stop=(i == 23)), [hT, ws[j]], [pp], inc=(i == 23))
                                        i += 1
                                kb.op("act", lambda e: e.activation(out=fm[0:msz, m, dcol:dcol + nn], in_=pp[0:msz, 0:nn], func=func), [pp], [fm])
                    else:
                        g0 = c0 - 2368
                        for m in range(4):
                            mt = g0 // 128 + m
                            for nb_ in range(4):
                                pp = ps1.get()
                                for k in range(8):
                                    kb.op("pe", lambda e: e.matmul(pp[:, :], lhsT=ws[0][:, k, m * 128:(m + 1) * 128],
                                                                   rhs=hT[:, k, LT0 + nb_ * 512:LT0 + (nb_ + 1) * 512], start=(k == 0), stop=(k == 7)),
                                          [hT, ws[0]], [pp], inc=(k == 7))
                                g = gsb.get()
                                kb.op("act", lambda e: e.activation(out=g[:], in_=pp[:, :], func=AF.Sigmoid), [pp], [g])
                                kb.dma("sp", gates_d[mt * 128:(mt + 1) * 128, nb_ * 512:(nb_ + 1) * 512], g[:], reads=[g], writes=[gates_r[mt][nb_]])
                if "fm_dbg" in dbg and b == 0:
                    kb.dma("sp", dbg_d["fm_dbg"][:, :, :], fm[:], reads=[fm], writes=[dbg_d["fm_dbg"]])
            if upto <= 2:
                kb.barrier(); fm_stack.close(); mix_stack.close()
                continue

            with kb.scope():
                Ybuf = kb.sb("Ybuf", [128, 16, 512], F32)
                halves = []
                for t2 in ps2.tiles:
                    for c0_ in (0, 512):
                        ht = Tile(t2.t[:, c0_:c0_ + 512], f"{t2.b.name}_h{c0_}")
                        ht.b.excl = True
                        halves.append(ht)
                pY_bank = halves[0]
                psP = Rot(halves[1:3])
                psR = Rot(ps1.tiles + halves[3:])
                bonus = kb.sb("bonus", [128, 16, 2, 8], F32)
                tri_f = kb.sb("tri_f", [128, 4, 128], F32)
                tri = kb.sb("tri", [128, 4, 128], BF16)
                kb.dma("sp", tri_f[:], I["c_tri"].ap(), writes=[tri_f])
                kb.op("dve", lambda e: e.tensor_copy(out=tri[:], in_=tri_f[:]), [tri_f], [tri])
                mask4 = kb.sb("mask4", [128, 2, 512], F32)
                maskt = kb.sb("maskt", [128, 2, 128], F32)
                for d in range(2):
                    kb.dma("sp", mask4[:, d, :], I["c_mask4"].ap()[d], writes=[mask4])
                    kb.dma("sp", maskt[:, d, :], I["c_maskt"].ap()[d], writes=[maskt])
                prm = kb.sb("prm", [128, 3, 512], F32)
                bcast_load(prm[:, 0, :], prm, I["k_k"].ap()[0:1, :], 128)
                bcast_load(prm[:, 1, :], prm, I["k_a"].ap()[0:1, :], 128)
                bcast_load(prm[:, 2, :], prm, I["r_k"].ap()[0:1, :], 128)
                w2f = kb.sb("w2f", [128, 2, 512], F32)
                w2b = kb.sb("w2b", [128, 2, 512], BF16)
                kb.dma("sp", w2f[:, 0, :], I["decay_w2"].ap().rearrange("d l c -> (d l) c"), writes=[w2f])
                kb.dma("sp", w2f[:, 1, :], I["aicl_a2"].ap().rearrange("d l c -> (d l) c"), writes=[w2f])
                kb.op("dve", lambda e: e.tensor_copy(out=w2b[:], in_=w2f[:]), [w2f], [w2b])
                b0f = kb.sb("b0f", [128, 2, 512], F32)
                b0b = kb.sb("b0b", [128, 2, 512], BF16)
                kb.op("pool", lambda e: e.memset(b0f[:], 0.0), [], [b0f])
                for d_ in range(2):
                    kb.dma("sp", b0f[d_ * 64:d_ * 64 + 1, 0, :], I["decay_w0"].ap()[d_:d_ + 1, :], writes=[b0f])
                    kb.dma("sp", b0f[d_ * 64:d_ * 64 + 1, 1, :], I["aicl_a0"].ap()[d_:d_ + 1, :], writes=[b0f])
                kb.op("dve", lambda e: e.tensor_copy(out=b0b[:], in_=b0f[:]), [b0f], [b0b])
                Hf = [kb.sb(f"Hf{p}", [128, 64], F32) for p in range(4)]
                Hb = [kb.sb(f"Hb{p}", [128, 64], BF16) for p in range(4)]
                rkv_rot = kb.rot("rkvt", [128, 1536], F32, 2)
                f512 = kb.rot("f512", [128, 512], F32, 9)
                b512 = kb.rot("b512", [128, 512], BF16, 14)
                fmT = kb.rot("fmT", [128, 4, 4, 128], BF16, 2)
                A4rot = kb.rot("A4", [128, 512], BF16, 8)
                XPa = kb.rot("XPa", [128, 512], BF16, 12)
                Wsb = kb.rot("Wsb", [128, 64], BF16, 10)
                Usb = kb.rot("Usb", [128, 512], BF16, 2)
                Vb = kb.rot("Vb", [128, 512], BF16, 2)
                ptot_rot = kb.rot("ptot", [128, 4], F32, 2)

                for d in range(rw_dirs):
                    for p in range(4):
                        kb.op("pool", lambda e: e.memset(Hf[p][:], 0.0), [], [Hf[p]])
                        kb.op("pool", lambda e: e.memset(Hb[p][:], 0.0), [], [Hb[p]])
                    order = list(range(18)) if d == 0 else [1, 0] + list(range(17, 1, -1))
                    def prep_gen(ti, c_):
                        lat = ti >= 2
                        rt = rkv_rot.get()
                        kb.dma("sp", rt[:], rkv_d[ti * 128:(ti + 1) * 128, :], reads=[rkv_r[ti]], writes=[rt])
                        r_ = rt[:, 0:512]
                        k_ = rt[:, 512:1024]
                        v_ = rt[:, 1024:1536]
                        kkr = f512.get(); sq = f512.get(); st = small_rot.get()
                        kb.op("dve", lambda e: e.tensor_tensor(out=kkr[:], in0=k_, in1=prm[:, 0, :], op=ALU.mult), [rt, prm], [kkr])
                        kb.op("act", lambda e: e.activation(out=sq[:], in_=kkr[:], func=AF.Square), [kkr], [sq])
                        kb.op("dve", lambda e: e.tensor_reduce(out=st[:, 0:8], in_=sq[:].rearrange("p (h c) -> p h c", h=8), axis=AX.X, op=ALU.add), [sq], [st])
                        rms_rstd(st[:, 0:8], st[:, 8:16], st, st, 1.0, 1e-12)
                        kap = f512.get()
                        kb.op("dve", lambda e: e.tensor_tensor(out=kap[:].rearrange("p (h c) -> p h c", h=8), in0=kkr[:].rearrange("p (h c) -> p h c", h=8),
                                                               in1=st[:, 8:16].unsqueeze(2).to_broadcast([128, 8, 64]), op=ALU.mult), [kkr, st], [kap])
                        yield
                        tcol = (ti * 128) if ti < 2 else (256 + (ti - 2) * 128)
                        pz = psP.get()
                        kb.op("pe", lambda e: e.matmul(pz[:, :], lhsT=fm[d * 64:(d + 1) * 64, 0, tcol:tcol + 128], rhs=w2b[d * 64:(d + 1) * 64, 0, :], start=True, stop=False),
                              [fm, w2b], [pz], inc=False)
                        kb.op("pe", lambda e: e.matmul(pz[:, :], lhsT=ones_b[d * 64:d * 64 + 1, 0:128], rhs=b0b[d * 64:d * 64 + 1, 0, :], start=False, stop=True), [ones_b, b0b], [pz])
                        sigb = b512.get()
                        kb.op("act", lambda e: e.activation(out=sigb[:], in_=pz[:, :], func=AF.Sigmoid), [pz], [sigb])
                        yield
                        pa = psP.get()
                        kb.op("pe", lambda e: e.matmul(pa[:, :], lhsT=fm[d * 64:(d + 1) * 64, 1, tcol:tcol + 128], rhs=w2b[d * 64:(d + 1) * 64, 1, :], start=True, stop=False),
                              [fm, w2b], [pa], inc=False)
                        kb.op("pe", lambda e: e.matmul(pa[:, :], lhsT=ones_b[d * 64:d * 64 + 1, 0:128], rhs=b0b[d * 64:d * 64 + 1, 1, :], start=False, stop=True), [ones_b, b0b], [pa])
                        a_ = f512.get()
                        kb.op("act", lambda e: e.activation(out=a_[:], in_=pa[:, :], func=AF.Sigmoid), [pa], [a_])
                        yield
                        t1 = f512.get(); ktl = f512.get(); beta = f512.get()
                        kb.op("dve", lambda e: e.scalar_tensor_tensor(out=t1[:], in0=a_[:], scalar=-1.0, in1=prm[:, 1, :], op0=ALU.add, op1=ALU.mult), [a_, prm], [t1])
                        kb.op("dve", lambda e: e.scalar_tensor_tensor(out=ktl[:], in0=t1[:], scalar=1.0, in1=k_, op0=ALU.add, op1=ALU.mult), [t1, rt], [ktl])
                        kb.op("pool", lambda e: e.tensor_tensor(out=beta[:], in0=a_[:], in1=kap[:], op=ALU.mult), [a_, kap], [beta])
                        yield
                        if lat:
                            kb.op("pool", lambda e: e.tensor_tensor(out=t1[:], in0=r_, in1=prm[:, 2, :], op=ALU.mult), [rt, prm], [t1])
                            kb.op("pool", lambda e: e.tensor_tensor(out=t1[:], in0=t1[:], in1=ktl[:], op=ALU.mult), [t1, ktl], [t1])
                            kb.op("dve", lambda e: e.tensor_reduce(out=bonus[:, ti - 2, d, :], in_=t1[:].rearrange("p (h c) -> p h c", h=8), axis=AX.X, op=ALU.add), [t1], [bonus])
                        ci, ce, cr = ((0, 1, 3) if d == 0 else (2, 3, 1))
                        eI = f512.get(); eE = f512.get(); eN = f512.get(); eR = f512.get()
                        pI = psP.get()
                        kb.op("pe", lambda e: e.matmul(pI[:, :], lhsT=tri[:, ci, :], rhs=sigb[:], start=True, stop=True), [tri, sigb], [pI])
                        kb.op("act", lambda e: e.activation(out=eI[:], in_=pI[:, :], func=AF.Exp, scale=C_DEC), [pI], [eI])
                        kb.op("act", lambda e: e.activation(out=eN[:], in_=pI[:, :], func=AF.Exp, scale=-C_DEC), [pI], [eN])
                        yield
                        pE = psP.get()
                        kb.op("pe", lambda e: e.matmul(pE[:, :], lhsT=tri[:, ce, :], rhs=sigb[:], start=True, stop=True), [tri, sigb], [pE])
                        kb.op("act", lambda e: e.activation(out=eE[:], in_=pE[:, :], func=AF.Exp, scale=C_DEC), [pE], [eE])
                        yield
                        pR = psP.get()
                        kb.op("pe", lambda e: e.matmul(pR[:, :], lhsT=tri[:, cr, :], rhs=sigb[:], start=True, stop=True), [tri, sigb], [pR])
                        kb.op("act", lambda e: e.activation(out=eR[:], in_=pR[:, :], func=AF.Exp, scale=C_DEC), [pR], [eR])
                        yield
                        pT = psP.get()
                        for p in range(4):
                            kb.op("pe", lambda e: e.matmul(pT[:, p:p + 1], lhsT=sigb[:, p * 128:(p + 1) * 128], rhs=ones_b[:, 0:1], start=True, stop=True), [sigb, ones_b], [pT])
                        ptot = ptot_rot.get()
                        kb.op("act", lambda e: e.activation(out=ptot[:], in_=pT[:, 0:4], func=AF.Exp, scale=C_DEC), [pT], [ptot])
                        yield
                        Rh = b512.get(); Ka = b512.get(); Bh = b512.get(); Kh = b512.get(); NBb = b512.get(); Kbb = b512.get()
                        kb.op("dve", lambda e: e.tensor_tensor(out=Rh[:], in0=r_, in1=eI[:], op=ALU.mult), [rt, eI], [Rh])
                        kb.op("pool", lambda e: e.tensor_tensor(out=Ka[:], in0=kap[:], in1=eE[:], op=ALU.mult), [kap, eE], [Ka])
                        yield
                        kb.op("dve", lambda e: e.tensor_tensor(out=Bh[:], in0=beta[:], in1=eN[:], op=ALU.mult), [beta, eN], [Bh])
                        kb.op("pool", lambda e: e.tensor_tensor(out=Kh[:], in0=ktl[:], in1=eN[:], op=ALU.mult), [ktl, eN], [Kh])
                        yield
                        kb.op("dve", lambda e: e.scalar_tensor_tensor(out=NBb[:], in0=beta[:], scalar=-1.0, in1=eR[:], op0=ALU.mult, op1=ALU.mult), [beta, eR], [NBb])
                        kb.op("pool", lambda e: e.tensor_tensor(out=Kbb[:], in0=ktl[:], in1=eR[:], op=ALU.mult), [ktl, eR], [Kbb])
                        vb = Vb.get()
                        kb.op("pool", lambda e: e.tensor_copy(out=vb[:], in_=v_), [rt], [vb])
                        yield
                        fT = fmT.get()
                        for wi, src in enumerate((Ka, Rh, Bh, Kh)):
                            transposes(src, [src[:, p * 128:(p + 1) * 128] for p in range(4)], fT[:, :, wi, :], fT, eng=("act" if wi % 2 else "dve"), pool=psP)
                            yield
                        c_.update(fT=fT, NBb=NBb, Kbb=Kbb, vb=vb, ptot=ptot, lat=lat)

                    def advance(g_, n_):
                        if g_ is None:
                            return
                        for _ in range(n_):
                            try:
                                next(g_)
                            except StopIteration:
                                return

                    def heads(ti, c_, nxt):
                        fT = c_['fT']; NBb = c_['NBb']; Kbb = c_['Kbb']; vb = c_['vb']; ptot = c_['ptot']; lat = c_['lat']
                        U = Usb.get()
                        pY = pY_bank if lat else None
                        HG = 8
                        for g0 in range(0, 8, HG):
                            hs = list(range(g0, g0 + HG))
                            P_ = {h: h // 2 for h in hs}
                            LO = {h: (h % 2) * 64 for h in hs}
                            HC = {h: slice(h * 64, (h + 1) * 64) for h in hs}
                            KaT = {h: fT[LO[h]:LO[h] + 64, P_[h], 0, :] for h in hs}
                            RT = {h: fT[LO[h]:LO[h] + 64, P_[h], 1, :] for h in hs}
                            BT = {h: fT[LO[h]:LO[h] + 64, P_[h], 2, :] for h in hs}
                            KT = {h: fT[LO[h]:LO[h] + 64, P_[h], 3, :] for h in hs}
                            KaRT = {h: fT[LO[h]:LO[h] + 64, P_[h], 0:2, :].rearrange("c w t -> c (w t)") for h in hs}
                            A4 = {}; Xt = {}; XP = {}; pp_ = {}; Wt = {}
                            NBK = len(psR.tiles)

                            def lockstep(pe_fn, ev_fn):
                                pend_ = []
                                for h_ in hs:
                                    if len(pend_) >= NBK:
                                        ev_fn(pend_.pop(0))
                                    pe_fn(h_)
                                    pend_.append(h_)
                                for h_ in pend_:
                                    ev_fn(h_)

                            def pe_A(h):
                                pA = psR.get(); pp_[h] = pA
                                kb.op("pe", lambda e: e.matmul(pA[:, 0:256], lhsT=BT[h], rhs=KaRT[h], start=True, stop=True), [fT], [pA], inc=False)
                                kb.op("pe", lambda e: e.matmul(pA[:, 256:512], lhsT=KT[h], rhs=KaRT[h], start=True, stop=True), [fT], [pA])

                            def ev_A(h):
                                A4[h] = A4rot.get()
                                kb.op("dve", lambda e: e.tensor_tensor(out=A4[h][:], in0=pp_[h][:, :], in1=mask4[:, d, :], op=ALU.mult), [pp_[h], mask4], [A4[h]])
                            lockstep(pe_A, ev_A)
                            advance(nxt, 2)

                            def pe_L(h):
                                pL = psR.get(); pp_[h] = pL
                                kb.op("pe", lambda e: e.matmul(pL[:, 0:128], lhsT=KaT[h], rhs=BT[h], start=True, stop=True), [fT], [pL])

                            def ev_L(h):
                                XP[h] = XPa.get()
                                kb.op("dve", lambda e: e.tensor_tensor(out=XP[h][:, 256:384], in0=pp_[h][:, 0:128], in1=maskt[:, d, :], op=ALU.mult), [pp_[h], maskt], [XP[h]])
                                kb.op("pool", lambda e: e.tensor_copy(out=XP[h][:, 0:128], in_=A4[h][:, 0:128]), [A4[h]], [XP[h]])
                                kb.op("pool", lambda e: e.tensor_tensor(out=XP[h][:, 128:256], in0=A4[h][:, 0:128], in1=ident_b[:], op=ALU.add), [A4[h], ident_b], [XP[h]])
                            lockstep(pe_L, ev_L)
                            advance(nxt, 2)

                            def xxt(ap_):
                                return ap_[:, 0:512].rearrange("p (a c) -> p a c", c=256)[:, :, 0:128] if ap_.shape[-1] >= 512 else None

                            for j in range(0, 7):
                                first = (j == 0)
                                last = (j == 6)

                                def pe_Bj(h):
                                    pB = psR.get(); pp_[h] = pB
                                    if first:
                                        kb.op("pe", lambda e: e.matmul(pB[:, 0:128], lhsT=XP[h][:, 256:384], rhs=XP[h][:, 0:128], start=True, stop=True), [XP[h]], [pB], inc=False)
                                        kb.op("pe", lambda e: e.matmul(pB[:, 256:384], lhsT=XP[h][:, 0:128], rhs=XP[h][:, 256:384], start=True, stop=True), [XP[h]], [pB])
                                    elif not last:
                                        kb.op("pe", lambda e: e.matmul(pB[:, 0:256], lhsT=XP[h][:, 256:384], rhs=XP[h][:, 0:256], start=True, stop=True), [XP[h]], [pB], inc=False)
                                        kb.op("pe", lambda e: e.matmul(pB[:, 256:384], lhsT=XP[h][:, 0:128], rhs=XP[h][:, 256:384], start=True, stop=True), [XP[h]], [pB])
                                    else:
                                        kb.op("pe", lambda e: e.matmul(pB[:, 128:256], lhsT=XP[h][:, 256:384], rhs=XP[h][:, 128:256], start=True, stop=True), [XP[h]], [pB])

                                def ev_Bj(h):
                                    pB = pp_[h]
                                    XP2 = XPa.get()
                                    if first:
                                        kb.op("pool", lambda e: e.tensor_copy(out=XP2[:, 128:256], in_=XP[h][:, 128:256]), [XP[h]], [XP2])
                                    else:
                                        kb.op("dve", lambda e: e.tensor_tensor(out=XP2[:, 128:256], in0=pB[:, 128:256], in1=XP[h][:, 128:256], op=ALU.add), [pB, XP[h]], [XP2])
                                    if not last:
                                        src_ = pB[:, 0:512].rearrange("p (a c) -> p a c", c=256)[:, :, 0:128]
                                        dst_ = XP2[:, 0:512].rearrange("p (a c) -> p a c", c=256)[:, :, 0:128]
                                        kb.op("act", lambda e: e.copy(out=dst_, in_=src_), [pB], [XP2])
                                    XP[h] = XP2
                                lockstep(pe_Bj, ev_Bj)
                                advance(nxt, 2)

                            def pe_W(h):
                                pW = psR.get(); pp_[h] = pW
                                kb.op("pe", lambda e: e.matmul(pW[:, 0:64], lhsT=KaT[h], rhs=Hb[P_[h]][LO[h]:LO[h] + 64, :], start=True, stop=False), [fT, Hb[P_[h]]], [pW], inc=False)
                                kb.op("pe", lambda e: e.matmul(pW[:, 0:64], lhsT=A4[h][:, 256:384], rhs=vb[:, HC[h]], start=False, stop=True), [A4[h], vb], [pW])

                            def ev_W(h):
                                Wt[h] = Wsb.get()
                                kb.op("act", lambda e: e.copy(out=Wt[h][:], in_=pp_[h][:, 0:64]), [pp_[h]], [Wt[h]])
                            lockstep(pe_W, ev_W)
                            advance(nxt, 1)

                            def pe_U(h):
                                pU = psR.get(); pp_[h] = pU
                                kb.op("pe", lambda e: e.matmul(pU[:, 0:64], lhsT=XP[h][:, 128:256], rhs=Wt[h][:], start=True, stop=True), [XP[h], Wt[h]], [pU])

                            def ev_U(h):
                                kb.op("dve" if h % 2 else "act", (lambda e: e.tensor_copy(out=U[:, HC[h]], in_=pp_[h][:, 0:64])) if h % 2 else
                                      (lambda e: e.copy(out=U[:, HC[h]], in_=pp_[h][:, 0:64])), [pp_[h]], [U])
                            lockstep(pe_U, ev_U)
                            advance(nxt, 1)
                            if lat:
                                for h in hs:
                                    kb.op("pe", lambda e: e.matmul(pY[:, HC[h]], lhsT=RT[h], rhs=Hb[P_[h]][LO[h]:LO[h] + 64, :], start=True, stop=False), [fT, Hb[P_[h]]], [pY], inc=False)
                                    kb.op("pe", lambda e: e.matmul(pY[:, HC[h]], lhsT=A4[h][:, 128:256], rhs=U[:, HC[h]], start=False, stop=False), [A4[h], U], [pY], inc=False)
                                    kb.op("pe", lambda e: e.matmul(pY[:, HC[h]], lhsT=A4[h][:, 384:512], rhs=vb[:, HC[h]], start=False, stop=True), [A4[h], vb], [pY])
                            pHs = {}
                            for p in sorted(set(P_.values())):
                                pc = slice(p * 128, (p + 1) * 128)
                                pH = psR.get(); pHs[p] = pH
                                kb.op("pe", lambda e: e.matmul(pH[:, 0:128], lhsT=NBb[:, pc], rhs=U[:, pc], start=True, stop=False), [NBb, U], [pH], inc=False)
                                kb.op("pe", lambda e: e.matmul(pH[:, 0:128], lhsT=Kbb[:, pc], rhs=vb[:, pc], start=False, stop=True), [Kbb, vb], [pH])
                            for p in sorted(set(P_.values())):
                                pH = pHs[p]
                                for hh in range(2):
                                    l2 = hh * 64
                                    kb.op("dve", lambda e: e.scalar_tensor_tensor(out=Hf[p][l2:l2 + 64, :], in0=Hf[p][l2:l2 + 64, :], scalar=ptot[l2:l2 + 64, p:p + 1],
                                                                                  in1=pH[l2:l2 + 64, l2:l2 + 64], op0=ALU.mult, op1=ALU.add), [Hf[p], ptot, pH], [Hf[p]])
                                kb.op("act", lambda e: e.copy(out=Hb[p][:], in_=Hf[p][:]), [Hf[p]], [Hb[p]])
                        if lat:
                            if d == 0:
                                kb.op("act", lambda e: e.copy(out=Ybuf[:, ti - 2, :], in_=pY[:, 0:512]), [pY], [Ybuf])
                            else:
                                kb.op("dve", lambda e: e.tensor_tensor(out=Ybuf[:, ti - 2, :], in0=pY[:, 0:512], in1=Ybuf[:, ti - 2, :], op=ALU.add), [pY, Ybuf], [Ybuf])

                    tl_ = order[:rw_tiles]
                    ctxs_ = [dict() for _ in tl_]
                    gens_ = [prep_gen(ti, ctxs_[n]) for n, ti in enumerate(tl_)]
                    advance(gens_[0], 10 ** 6)
                    for n, ti in enumerate(tl_):
                        nxt_ = gens_[n + 1] if n + 1 < len(tl_) else None
                        heads(ti, ctxs_[n], nxt_)
                        advance(nxt_, 10 ** 6)
                if "y_dbg" in dbg and b == 0:
                    kb.dma("sp", dbg_d["y_dbg"][:, :, :], Ybuf[:], reads=[Ybuf], writes=[dbg_d["y_dbg"]])
                gnw = kb.sb("gnw", [128, 2, 512], F32)
                bcast_load(gnw[:, 0, :], gnw, I["gn_w"].ap()[0:1, :], 128)
                bcast_load(gnw[:, 1, :], gnw, I["gn_b"].ap()[0:1, :], 128)
                g2f = w2f
                g2b = kb.sb("g2b", [128, 2, 512], BF16)
                kb.dma("sp", g2f[:, 0, :], I["gate_g2"].ap()[0:128, :], reads=[], writes=[g2f])
                kb.dma("sp", g2f[0:32, 1, :], I["gate_g2"].ap()[128:160, :], writes=[g2f])
                kb.op("dve", lambda e: e.tensor_copy(out=g2b[:, 0, :], in_=g2f[:, 0, :]), [g2f], [g2b])
                kb.op("dve", lambda e: e.tensor_copy(out=g2b[0:32, 1, :], in_=g2f[0:32, 1, :]), [g2f], [g2b])
                rwb = kb.rot("rwb", [128, 512], BF16, 2)
                def tileRO(t):
                        ti = t + 2
                        rt = rkv_rot.get()
                        kb.dma("sp", rt[:], rkv_d[ti * 128:(ti + 1) * 128, :], reads=[rkv_r[ti]], writes=[rt])
                        v_ = rt[:, 1024:1536]
                        y3 = Ybuf[:, t, :].rearrange("p (h c) -> p h c", h=8)
                        st = small_rot.get(); st2 = small_rot.get()
                        cen = f512.get(); sq = f512.get(); yn = f512.get()
                        cen3 = cen[:].rearrange("p (h c) -> p h c", h=8)
                        kb.op("dve", lambda e: e.tensor_reduce(out=st[:, 0:8], in_=y3, axis=AX.X, op=ALU.add), [Ybuf], [st])
                        kb.op("dve", lambda e: e.tensor_scalar(out=st[:, 0:8], in0=st[:, 0:8], scalar1=-1.0 / 64, scalar2=None, op0=ALU.mult), [st], [st])
                        kb.op("dve", lambda e: e.tensor_tensor(out=cen3, in0=y3, in1=st[:, 0:8].unsqueeze(2).to_broadcast([128, 8, 64]), op=ALU.add), [Ybuf, st], [cen])
                        yield
                        kb.op("act", lambda e: e.activation(out=sq[:], in_=cen[:], func=AF.Square), [cen], [sq])
                        kb.op("dve", lambda e: e.tensor_reduce(out=st2[:, 0:8], in_=sq[:].rearrange("p (h c) -> p h c", h=8), axis=AX.X, op=ALU.add), [sq], [st2])
                        yield
                        rms_rstd(st2[:, 0:8], st2[:, 8:16], st2, st2, 64, GN_EPS)
                        kb.op("dve", lambda e: e.tensor_tensor(out=yn[:].rearrange("p (h c) -> p h c", h=8), in0=cen3, in1=st2[:, 8:16].unsqueeze(2).to_broadcast([128, 8, 64]), op=ALU.mult),
                              [cen, st2], [yn])
                        kb.op("pool", lambda e: e.tensor_tensor(out=yn[:], in0=yn[:], in1=gnw[:, 0, :], op=ALU.mult), [yn, gnw], [yn])
                        kb.op("pool", lambda e: e.tensor_tensor(out=yn[:], in0=yn[:], in1=gnw[:, 1, :], op=ALU.add), [yn, gnw], [yn])
                        yield
                        kb.op("dve", lambda e: e.tensor_tensor(out=st[:, 8:16], in0=bonus[:, t, 0, :], in1=bonus[:, t, 1, :], op=ALU.add), [bonus], [st])
                        kb.op("dve", lambda e: e.tensor_tensor(out=sq[:].rearrange("p (h c) -> p h c", h=8), in0=v_.rearrange("p (h c) -> p h c", h=8),
                                                               in1=st[:, 8:16].unsqueeze(2).to_broadcast([128, 8, 64]), op=ALU.mult), [rt, st], [sq])
                        kb.op("pool", lambda e: e.tensor_tensor(out=yn[:], in0=yn[:], in1=sq[:], op=ALU.add), [yn, sq], [yn])
                        yield
                        pg = ps1.get()
                        tcol = 256 + t * 128
                        kb.op("pe", lambda e: e.matmul(pg[:, :], lhsT=fm[:, 2, tcol:tcol + 128], rhs=g2b[:, 0, :], start=True, stop=False), [fm, g2b], [pg], inc=False)
                        kb.op("pe", lambda e: e.matmul(pg[:, :], lhsT=fm[0:32, 3, tcol:tcol + 128], rhs=g2b[0:32, 1, :], start=False, stop=True), [fm, g2b], [pg])
                        ro = rwb.get()
                        kb.op("dve", lambda e: e.tensor_tensor(out=ro[:], in0=pg[:, :], in1=yn[:], op=ALU.mult), [pg, yn], [ro])
                        transposes(ro, [ro[:, k * 128:(k + 1) * 128] for k in range(4)], rwT[:, :, t * 128:(t + 1) * 128], rwT)
                run_interleaved((tileRO(t) for t in range(16)), 2)
                if "rw_dbg" in dbg and b == 0:
                    kb.dma("sp", dbg_d["rw_dbg"][:, :, :], rwT[:], reads=[rwT], writes=[dbg_d["rw_dbg"]])
            kb.barrier()
            fm_stack.close()
            if upto <= 3:
                mix_stack.close()
                continue

            with kb.scope():
                qT = kb.sb("qT", [96, 8, 2048], BF16)
                kT = kb.sb("kT", [96, 8, 2304], BF16)
                Vall = kb.sb("Vall", [128, 18, 8, 65], BF16)
                kb.op("pool", lambda e: e.memset(Vall[:, :, :, 64:65], 1.0), [], [Vall])
                qln = kb.sb("qln", [128, 256], F32)
                kvln = kb.sb("kvln", [128, 128], F32)
                qnw = kb.sb("qnw", [128, 8, 96], F32)
                knw = kb.sb("knw", [128, 8, 96], F32)
                bcast_load(qln[:], qln, I["q_lat_norm"].ap()[0:1, :], 128)
                bcast_load(kvln[:], kvln, I["kv_lat_norm"].ap()[0:1, :], 128)
                for h in range(8):
                    bcast_load(qnw[:, h, :], qnw, I["q_norm"].ap()[0:1, :], 128)
                    bcast_load(knw[:, h, :], knw, I["k_norm"].ap()[0:1, :], 128)
                kb.op("dve", lambda e: e.tensor_scalar(out=qnw[:], in0=qnw[:], scalar1=ATTN_SCALE, scalar2=None, op0=ALU.mult), [qnw], [qnw])
                wuq_f = kb.sb("wuq_f", [128, 2, 768], F32)
                wuq = kb.sb("wuq", [128, 2, 768], BF16)
                kb.dma("sp", wuq_f[:], I["w_uq"].ap().rearrange("(k p) n -> p k n", p=128), writes=[wuq_f])
                kb.op("pool", lambda e: e.tensor_copy(out=wuq[:], in_=wuq_f[:]), [wuq_f], [wuq])
                wukv_f = kb.sb("wukv_f", [128, 1024], F32)
                wukv = kb.sb("wukv", [128, 1024], BF16)
                kb.dma("sp", wukv_f[:], I["w_ukv"].ap(), writes=[wukv_f])
                kb.op("pool", lambda e: e.tensor_copy(out=wukv[:], in_=wukv_f[:]), [wukv_f], [wukv])
                mrot = kb.rot("mt", [128, 416], F32, 2)
                cs_rot = kb.rot("cs", [128, 2, 16], F32, 2)
                t768 = kb.rot("t768", [128, 8, 96], F32, 8)
                small_m = kb.rot("small_m", [128, 16], F32, 10)
                tb768 = kb.rot("tb768", [128, 8, 96], BF16, 4)
                tbn = kb.rot("tbn", [128, 256], BF16, 4)
                tTn = kb.rot("tTn", [128, 2, 128], BF16, 4)
                rope_t = kb.rot("rope_t", [128, 8, 16], F32, 16)

                def head_norm_rope(src, dst_b, gain, cs, n_extra_ssq=None):
                    sq = t768.get()
                    st = small_m.get()
                    kb.op("act", lambda e: e.activation(out=sq[:], in_=src[:], func=AF.Square), [src], [sq])
                    kb.op("dve", lambda e: e.tensor_reduce(out=st[:, 0:8], in_=sq[:], axis=AX.X, op=ALU.add), [sq], [st])
                    yield
                    rms_rstd(st[:, 0:8], st[:, 8:16], st, st, 96, EPS)
                    kb.op("dve", lambda e: e.tensor_tensor(out=sq[:], in0=src[:], in1=st[:, 8:16].unsqueeze(2).to_broadcast([128, 8, 96]), op=ALU.mult),
                          [src, st], [sq])
                    yield
                    if cs is None:
                        kb.op("pool", lambda e: e.tensor_tensor(out=dst_b[:], in0=sq[:], in1=gain[:], op=ALU.mult), [sq, gain], [dst_b])
                        return
                    kb.op("pool", lambda e: e.tensor_tensor(out=sq[:], in0=sq[:], in1=gain[:], op=ALU.mult), [sq, gain], [sq])
                    kb.op("pool", lambda e: e.tensor_copy(out=dst_b[:, :, 0:64], in_=sq[:, :, 0:64]), [sq], [dst_b])
                    cb_ = cs[:, 0, :].unsqueeze(1).to_broadcast([128, 8, 16])
                    sb_ = cs[:, 1, :].unsqueeze(1).to_broadcast([128, 8, 16])
                    x1 = sq[:, :, 64:80]
                    x2 = sq[:, :, 80:96]
                    ta = rope_t.get(); tb_ = rope_t.get(); tc_ = rope_t.get(); td = rope_t.get()
                    yield
                    kb.op("dve", lambda e: e.tensor_tensor(out=ta[:], in0=x1, in1=cb_, op=ALU.mult), [sq, cs], [ta])
                    kb.op("dve", lambda e: e.tensor_tensor(out=tb_[:], in0=x2, in1=sb_, op=ALU.mult), [sq, cs], [tb_])
                    kb.op("pool", lambda e: e.tensor_tensor(out=tc_[:], in0=x1, in1=sb_, op=ALU.mult), [sq, cs], [tc_])
                    kb.op("pool", lambda e: e.tensor_tensor(out=td[:], in0=x2, in1=cb_, op=ALU.mult), [sq, cs], [td])
                    kb.op("dve", lambda e: e.tensor_tensor(out=dst_b[:, :, 64:80], in0=ta[:], in1=tb_[:], op=ALU.subtract), [ta, tb_], [dst_b])
                    kb.op("pool", lambda e: e.tensor_tensor(out=dst_b[:, :, 80:96], in0=tc_[:], in1=td[:], op=ALU.add), [tc_, td], [dst_b])

                def mla_tile(ti):
                        lat = ti >= 2
                        mt_ = mrot.get()
                        kb.dma("sp", mt_[:], mla_d[ti * 128:(ti + 1) * 128, :], reads=[mla_r[ti]], writes=[mt_])
                        cs = None
                        if lat:
                            cs = cs_rot.get()
                            lt_ = ti - 2
                            kb.dma("sp", cs[:, 0, :], I["c_cos"].ap()[lt_ * 128:(lt_ + 1) * 128, :], writes=[cs])
                            kb.dma("sp", cs[:, 1, :], I["c_sin"].ap()[lt_ * 128:(lt_ + 1) * 128, :], writes=[cs])
                        junk = junk_rot.get()
                        st = small_m.get()
                        kb.op("act", lambda e: e.activation(out=junk[:, 0:128], in_=mt_[:, 256:384], func=AF.Square, accum_out=st[:, 0:1]), [mt_], [junk, st])
                        kb.op("act", lambda e: e.activation(out=junk[:, 128:160], in_=mt_[:, 384:416], func=AF.Square, accum_out=st[:, 2:3]), [mt_], [junk, st])
                        rms_rstd(st[:, 0:1], st[:, 1:2], st, st, 128, EPS)
                        kvn = tbn.get()
                        kb.op("dve", lambda e: e.scalar_tensor_tensor(out=kvn[:, 0:128], in0=mt_[:, 256:384], scalar=st[:, 1:2], in1=kvln[:], op0=ALU.mult, op1=ALU.mult),
                              [mt_, st, kvln], [kvn])
                        yield
                        kvT = tTn.get()
                        transposes(kvn, [kvn[:, 0:128]], kvT[:, 0, :], kvT)
                        pk = ps2.get()
                        for nb_ in range(2):
                            kb.op("pe", lambda e: e.matmul(pk[:, nb_ * 512:(nb_ + 1) * 512], lhsT=kvT[:, 0, :], rhs=wukv[:, nb_ * 512:(nb_ + 1) * 512], start=True, stop=True),
                                  [kvT, wukv], [pk])
                        pk3 = pk[:].rearrange("p (h c) -> p h c", h=8)
                        kb.op("act", lambda e: e.copy(out=Vall[:, ti, :, 0:64], in_=pk3[:, :, 64:128]), [pk], [Vall])
                        kf = t768.get()
                        kb.op("dve", lambda e: e.tensor_copy(out=kf[:, :, 0:64], in_=pk3[:, :, 0:64]), [pk], [kf])
                        kb.op("pool", lambda e: e.tensor_copy(out=kf[:, :, 64:96], in_=mt_[:, 384:416].unsqueeze(1).to_broadcast([128, 8, 32])), [mt_], [kf])
                        yield
                        kbf = tb768.get()
                        yield from head_norm_rope(kf, kbf, knw, cs)
                        yield
                        transposes(kbf, [kbf[:, h, :] for h in range(8)], kT[:, :, ti * 128:(ti + 1) * 128], kT, eng="dve")
                        yield
                        if lat:
                            st2 = small_m.get()
                            kb.op("act", lambda e: e.activation(out=junk[:, 256:512], in_=mt_[:, 0:256], func=AF.Square, accum_out=st2[:, 0:1]), [mt_], [junk, st2])
                            rms_rstd(st2[:, 0:1], st2[:, 1:2], st2, st2, 256, EPS)
                            qn = tbn.get()
                            kb.op("dve", lambda e: e.scalar_tensor_tensor(out=qn[:], in0=mt_[:, 0:256], scalar=st2[:, 1:2], in1=qln[:], op0=ALU.mult, op1=ALU.mult),
                                  [mt_, st2, qln], [qn])
                            yield
                            qnT = tTn.get()
                            transposes(qn, [qn[:, 0:128], qn[:, 128:256]], qnT[:], qnT)
                            pq = ps2.get()
                            for (n0, n1) in ((0, 512), (512, 768)):
                                for k in range(2):
                                    kb.op("pe", lambda e: e.matmul(pq[:, n0:n1], lhsT=qnT[:, k, :], rhs=wuq[:, k, n0:n1], start=(k == 0), stop=(k == 1)),
                                          [qnT, wuq], [pq], inc=(k == 1))
                            qf = t768.get()
                            kb.op("act", lambda e: e.copy(out=qf[:].rearrange("p h c -> p (h c)"), in_=pq[:, 0:768]), [pq], [qf])
                            yield
                            qbf = tb768.get()
                            yield from head_norm_rope(qf, qbf, qnw, cs)
                            yield
                            transposes(qbf, [qbf[:, h, :] for h in range(8)], qT[:, :, lt_ * 128:(lt_ + 1) * 128], qT, eng="dve")

                run_interleaved((mla_tile(ti) for ti in range(18)), 2)
                if "qT_dbg" in dbg and b == 0:
                    kb.dma("sp", dbg_d["qT_dbg"][:, :, :], qT[:], reads=[qT], writes=[dbg_d["qT_dbg"]])
                    kb.dma("sp", dbg_d["kT_dbg"][:, :, :], kT[:], reads=[kT], writes=[dbg_d["kT_dbg"]])
                Erot = kb.rot("E", [128, 512], BF16, 3)
                apair = kb.rot("apair", [128, 4, 128], BF16, 2)
                for hp in range(4):
                    for qb in range(4):
                        ap_ = apair.get()
                        for hh in range(2):
                            h = hp * 2 + hh
                            po = [ps2.get(), ps2.get()]
                            pSd = {}

                            def emitS(kt_):
                                pS_ = ps1.get()
                                kb.op("pe", lambda e: e.matmul(pS_[:, :], lhsT=kT[:, h, kt_ * 128:(kt_ + 1) * 128], rhs=qT[:, h, qb * 512:(qb + 1) * 512], start=True, stop=True),
                                      [kT, qT], [pS_])
                                pSd[kt_] = pS_
                            emitS(0)
                            for kt in range(18):
                                if kt + 1 < 18:
                                    emitS(kt + 1)
                                pS = pSd.pop(kt)
                                E = Erot.get()
                                kb.op("act", lambda e: e.activation(out=E[:], in_=pS[:, :], func=AF.Exp), [pS], [E])
                                for qs in range(4):
                                    p_ = po[qs // 2]
                                    c0 = (qs % 2) * 512
                                    kb.op("pe", lambda e: e.matmul(p_[:, c0:c0 + 65], lhsT=E[:, qs * 128:(qs + 1) * 128], rhs=Vall[:, kt, h, :],
                                                                   start=(kt == 0), stop=(kt == 17)), [E, Vall], [p_], inc=(kt == 17))
                            for qs in range(4):
                                p_ = po[qs // 2]
                                c0 = (qs % 2) * 512
                                st = small_rot.get()
                                kb.op("dve", lambda e: e.reciprocal(out=st[:, 0:1], in_=p_[:, c0 + 64:c0 + 65]), [p_], [st])
                                kb.op("dve", lambda e: e.tensor_scalar(out=ap_[:, qs, hh * 64:(hh + 1) * 64], in0=p_[:, c0:c0 + 64], scalar1=st[:, 0:1], scalar2=None,
                                                                       op0=ALU.mult), [p_, st], [ap_])
                        transposes(ap_, [ap_[:, qs, :] for qs in range(4)], attT[:, hp, qb * 512:(qb + 1) * 512], attT)
                if "att_dbg" in dbg and b == 0:
                    kb.dma("sp", dbg_d["att_dbg"][:, :, :], attT[:], reads=[attT], writes=[dbg_d["att_dbg"]])
            if upto <= 4:
                mix_stack.close()
                continue

            with kb.scope():
                wo_f = kb.sb("wo_f", [128, 8, 1024], F32)
                wo1 = kb.sb("wo1", [128, 4, 1024], BF16)
                wo2 = kb.sb("wo2", [128, 4, 1024], BF16)
                wo3 = kb.sb("wo3", [128, 8, 1024], BF16)
                kb.dma("sp", wo_f[:, 0:4, :], I["w_o_mla"].ap().rearrange("(k p) n -> p k n", p=128), writes=[wo_f])
                kb.op("pool", lambda e: e.tensor_copy(out=wo1[:], in_=wo_f[:, 0:4, :]), [wo_f], [wo1])
                kb.dma("sp", wo_f[:, 0:4, :], I["w_o_rwkv"].ap().rearrange("(k p) n -> p k n", p=128), reads=[wo_f], writes=[wo_f])
                kb.op("pool", lambda e: e.tensor_copy(out=wo2[:], in_=wo_f[:, 0:4, :]), [wo_f], [wo2])
                kb.dma("sp", wo_f[:], I["w_out"].ap().rearrange("(k p) n -> p k n", p=128), reads=[wo_f], writes=[wo_f])
                kb.op("pool", lambda e: e.tensor_copy(out=wo3[:], in_=wo_f[:]), [wo_f], [wo3])
                mT = kb.sb("mT", [128, 8, 2048], BF16)
                grot = kb.rot("gt", [128, 2, 512], BF16, 2)
                trot = kb.rot("tm", [128, 512], F32, 2)
                for m in range(8):
                    for nb_ in range(4):
                        g = grot.get()
                        kb.dma("sp", g[:, 0, :], gates_d[m * 128:(m + 1) * 128, nb_ * 512:(nb_ + 1) * 512], reads=[gates_r[m][nb_]], writes=[g])
                        kb.dma("sp", g[:, 1, :], gates_d[(8 + m) * 128:(9 + m) * 128, nb_ * 512:(nb_ + 1) * 512], reads=[gates_r[8 + m][nb_]], writes=[g])
                        p1 = ps1.get(); p2 = ps1.get()
                        for k in range(4):
                            kb.op("pe", lambda e: e.matmul(p1[:, :], lhsT=wo1[:, k, m * 128:(m + 1) * 128], rhs=attT[:, k, nb_ * 512:(nb_ + 1) * 512], start=(k == 0), stop=(k == 3)),
                                  [wo1, attT], [p1], inc=(k == 3))
                        for k in range(4):
                            kb.op("pe", lambda e: e.matmul(p2[:, :], lhsT=wo2[:, k, m * 128:(m + 1) * 128], rhs=rwT[:, k, nb_ * 512:(nb_ + 1) * 512], start=(k == 0), stop=(k == 3)),
                                  [wo2, rwT], [p2], inc=(k == 3))
                        t1 = trot.get(); t2 = trot.get()
                        kb.op("dve", lambda e: e.tensor_tensor(out=t1[:], in0=p1[:, :], in1=g[:, 0, :], op=ALU.mult), [p1, g], [t1])
                        kb.op("dve", lambda e: e.tensor_tensor(out=t2[:], in0=p2[:, :], in1=g[:, 1, :], op=ALU.mult), [p2, g], [t2])
                        kb.op("pool", lambda e: e.tensor_tensor(out=mT[:, m, nb_ * 512:(nb_ + 1) * 512], in0=t1[:], in1=t2[:], op=ALU.add), [t1, t2], [mT])
                Ga = kb.sb("Ga", [128, 1024], F32)
                bcast_load(Ga[:], Ga, mod_d.t.ap()[b:b + 1, 2048:3072], 128, reads=[mod_d])
                xrot = kb.rot("xt5", [128, 1024], F32, 2)
                for t in range(16):
                    xt = xrot.get()
                    kb.dma("sp", xt[:], I["x"].ap()[b, t * 128:(t + 1) * 128, :], writes=[xt])
                    pm = ps2.get()
                    for nb_ in range(2):
                        for k in range(8):
                            kb.op("pe", lambda e: e.matmul(pm[:, nb_ * 512:(nb_ + 1) * 512], lhsT=mT[:, k, t * 128:(t + 1) * 128], rhs=wo3[:, k, nb_ * 512:(nb_ + 1) * 512],
                                                           start=(k == 0), stop=(k == 7)), [mT, wo3], [pm], inc=(k == 7))
                    junk = junk_rot.get()
                    kb.op("dve", lambda e: e.tensor_tensor(out=junk[:], in0=pm[:, :], in1=Ga[:], op=ALU.mult), [pm, Ga], [junk])
                    kb.op("pool", lambda e: e.tensor_tensor(out=xt[:], in0=junk[:], in1=xt[:], op=ALU.add), [junk, xt], [xt])
                    kb.dma("sp", x1_d[(b * 16 + t) * 128:(b * 16 + t + 1) * 128, :], xt[:], reads=[xt], writes=[x1_r[b * 16 + t]])
            kb.barrier()
            mix_stack.close()
            if upto <= 5:
                continue

            pass

        if upto > 5:
          NT = 16 * nbatch
          NBLK = NT * 4 + 256
          with kb.scope():
            I32 = mybir.dt.int32
            u_d = kb.dram("u_d", [NT * 128, 1024], BF16)
            u_r = [Buf() for _ in range(NT)]
            xs_d = kb.dram("xs_d", [NBLK * 256, 1024], BF16)
            Y_d = kb.dram("Y_d", [NBLK * 256, 1024], BF16)
            junk_rot = kb.rot("junk6", [128, 1024], F32, 3)
            small_rot = kb.rot("small6", [128, 16], F32, 8)
            dest_all = kb.sb("dest_all", [128, NT, 8], I32)
            gate_all = kb.sb("gate_all", [128, NT, 8], F32)
            IDXi = kb.sb("IDXi", [128, NBLK], I32)
            tri_f = kb.sb("tri6_f", [128, 4, 128], F32)
            tri = kb.sb("tri6", [128, 4, 128], BF16)
            kb.dma("sp", tri_f[:], I["c_tri"].ap(), writes=[tri_f])
            kb.op("dve", lambda e: e.tensor_copy(out=tri[:], in_=tri_f[:]), [tri_f], [tri])
            iota_p = kb.sb("iota_p", [128, 2], F32)
            kb.dma("sp", iota_p[:], I["c_iotap"].ap(), writes=[iota_p])
            with kb.scope():
                G_all = kb.sb("G_all", [128, NT, 256], F32)
                POS_all = kb.sb("POS_all", [128, NT, 256], F32)
                mask_all = kb.sb("mask_all", [128, NT, 256], BF16)
                cnt = kb.sb("cnt", [128, 256], F32)
                kb.op("pool", lambda e: e.memset(cnt[:], 0.0), [], [cnt])
                with kb.scope():
                    rw_f = kb.sb("rw_f", [128, 8, 256], F32)
                    kb.dma("sp", rw_f[:], I["router_w"].ap().rearrange("(k p) n -> p k n", p=128), writes=[rw_f])
                    rbias = kb.sb("rbias", [128, 256], F32)
                    bcast_load(rbias[:], rbias, I["router_bias"].ap()[0:1, :], 128)
                    xrot = kb.rot("xt6", [128, 1024], F32, 3)
                    ufr = kb.rot("uf", [128, 1024], F32, 3)
                    ubr = kb.rot("ub", [128, 1024], BF16, 3)
                    uTf = kb.rot("uTf", [128, 8, 128], F32, 3)
                    s256 = kb.rot("s256", [128, 256], F32, 12)
                    m8r = kb.rot("m8", [128, 8], F32, 16)
                    A2s = []
                    for b in range(nbatch):
                        A2 = kb.sb("A2", [128, 1024], F32)
                        S2 = kb.sb("S2", [128, 1024], F32)
                        bcast_load(A2[:], A2, mod_d.t.ap()[b:b + 1, 4096:5120], 128, reads=[mod_d])
                        bcast_load(S2[:], S2, mod_d.t.ap()[b:b + 1, 3072:4096], 128, reads=[mod_d])
                        A2s.append((A2, S2))
                    def tileA(i):
                            b = i // 16
                            A2, S2 = A2s[b]
                            xt = xrot.get()
                            kb.dma("sp", xt[:], x1_d[i * 128:(i + 1) * 128, :], reads=[x1_r[i]], writes=[xt])
                            junk = junk_rot.get(); ssq = small_rot.get()
                            kb.op("act", lambda e: e.activation(out=junk[:], in_=xt[:], func=AF.Square, accum_out=ssq[:, 0:1]), [xt], [junk, ssq])
                            rms_rstd(ssq[:, 0:1], ssq[:, 1:2], ssq, ssq, 1024, EPS)
                            yield
                            uf = ufr.get()
                            kb.op("dve", lambda e: e.scalar_tensor_tensor(out=junk[:], in0=xt[:], scalar=ssq[:, 1:2], in1=A2[:], op0=ALU.mult, op1=ALU.mult), [xt, ssq, A2], [junk])
                            kb.op("pool", lambda e: e.tensor_tensor(out=uf[:], in0=junk[:], in1=S2[:], op=ALU.add), [junk, S2], [uf])
                            ub = ubr.get()
                            kb.op("act", lambda e: e.copy(out=ub[:], in_=uf[:]), [uf], [ub])
                            kb.dma("sp", u_d[i * 128:(i + 1) * 128, :], ub[:], reads=[ub], writes=[u_r[i]])
                            yield
                            utf = uTf.get()
                            for half in range(2):
                                transposes(uf, [uf[:, (half * 4 + k) * 128:(half * 4 + k + 1) * 128] for k in range(4)], utf[:, half * 4:(half + 1) * 4, :], utf, dt=F32, eng="dve")
                            pr = ps1.get()
                            for k in range(8):
                                kb.op("pe", lambda e: e.matmul(pr[:, 0:256], lhsT=utf[:, k, :], rhs=rw_f[:, k, :], start=(k == 0), stop=(k == 7)), [utf, rw_f], [pr], inc=(k == 7))
                            sc = s256.get(); sel = s256.get(); w1_ = s256.get(); w2_ = s256.get()
                            kb.op("act", lambda e: e.activation(out=sc[:], in_=pr[:, 0:256], func=AF.Sigmoid), [pr], [sc])
                            kb.op("dve", lambda e: e.tensor_tensor(out=sel[:], in0=sc[:], in1=rbias[:], op=ALU.add), [sc, rbias], [sel])
                            yield
                            sel3 = sel[:].rearrange("p (g c) -> p g c", g=8)
                            mx = m8r.get(); mx2 = m8r.get(); gs = m8r.get(); gsort = m8r.get()
                            kb.op("dve", lambda e: e.tensor_reduce(out=mx[:], in_=sel3, axis=AX.X, op=ALU.max), [sel], [mx])
                            kb.op("dve", lambda e: e.tensor_tensor(out=w1_[:].rearrange("p (g c) -> p g c", g=8), in0=sel3, in1=mx[:].unsqueeze(2).to_broadcast([128, 8, 32]), op=ALU.is_ge),
                                  [sel, mx], [w1_])
                            kb.op("dve", lambda e: e.scalar_tensor_tensor(out=w2_[:], in0=w1_[:], scalar=-10.0, in1=sel[:], op0=ALU.mult, op1=ALU.add), [w1_, sel], [w2_])
                            kb.op("dve", lambda e: e.tensor_reduce(out=mx2[:], in_=w2_[:].rearrange("p (g c) -> p g c", g=8), axis=AX.X, op=ALU.max), [w2_], [mx2])
                            yield
                            kb.op("dve", lambda e: e.tensor_tensor(out=gs[:], in0=mx[:], in1=mx2[:], op=ALU.add), [mx, mx2], [gs])
                            kb.op("dve", lambda e: e.max(out=gsort[:], in_=gs[:]), [gs], [gsort])
                            kb.op("dve", lambda e: e.tensor_scalar(out=gs[:], in0=gs[:], scalar1=gsort[:, 3:4], scalar2=None, op0=ALU.is_ge), [gs, gsort], [gs])
                            kb.op("dve", lambda e: e.scalar_tensor_tensor(out=w1_[:].rearrange("p (g c) -> p g c", g=8), in0=sel3, scalar=2.0,
                                                                          in1=gs[:].unsqueeze(2).to_broadcast([128, 8, 32]), op0=ALU.add, op1=ALU.mult), [sel, gs], [w1_])
                            yield
                            top8 = m8r.get()
                            kb.op("dve", lambda e: e.max(out=top8[:], in_=w1_[:]), [w1_], [top8])
                            kb.op("dve", lambda e: e.tensor_scalar(out=w2_[:], in0=w1_[:], scalar1=top8[:, 7:8], scalar2=None, op0=ALU.is_ge), [w1_, top8], [w2_])
                            kb.op("pool", lambda e: e.tensor_copy(out=mask_all[:, i, :], in_=w2_[:]), [w2_], [mask_all])
                            yield
                            st = small_rot.get()
                            kb.op("dve", lambda e: e.tensor_tensor(out=w1_[:], in0=w2_[:], in1=sc[:], op=ALU.mult), [w2_, sc], [w1_])
                            kb.op("dve", lambda e: e.tensor_reduce(out=st[:, 0:1], in_=w1_[:], axis=AX.X, op=ALU.add), [w1_], [st])
                            kb.op("dve", lambda e: e.reciprocal(out=st[:, 1:2], in_=st[:, 0:1]), [st], [st])
                            kb.op("dve", lambda e: e.tensor_scalar(out=G_all[:, i, :], in0=w1_[:], scalar1=st[:, 1:2], scalar2=2.5, op0=ALU.mult, op1=ALU.mult), [w1_, st], [G_all])
                            yield
                            pc = ps1.get()
                            kb.op("pe", lambda e: e.matmul(pc[:, 0:256], lhsT=tri[:, 1, :], rhs=mask_all[:, i, :], start=True, stop=True), [tri, mask_all], [pc])
                            kb.op("pe", lambda e: e.matmul(pc[:, 256:512], lhsT=ones_b[:], rhs=mask_all[:, i, :], start=True, stop=True), [ones_b, mask_all], [pc])
                            kb.op("dve", lambda e: e.tensor_tensor(out=POS_all[:, i, :], in0=pc[:, 0:256], in1=cnt[:], op=ALU.add), [pc, cnt], [POS_all])
                            kb.op("dve", lambda e: e.tensor_tensor(out=cnt[:], in0=pc[:, 256:512], in1=cnt[:], op=ALU.add), [pc, cnt], [cnt])
                    run_interleaved((tileA(i) for i in range(NT)), 3)
                if "G_dbg" in dbg:
                    kb.dma("sp", dbg_d["G_dbg"][:, :, 0:256], G_all[:, 0:16, :], reads=[G_all], writes=[dbg_d["G_dbg"]])
                base_r = kb.sb("base_r", [128, 256], F32)
                with kb.scope():
                    ind = kb.sb("ind", [128, 256], BF16)
                    kb.op("dve", lambda e: e.tensor_scalar(out=ind[:], in0=cnt[:], scalar1=iota_p[:, 1:2], scalar2=None, op0=ALU.is_gt), [cnt, iota_p], [ind])
                    pn = ps1.get()
                    kb.op("pe", lambda e: e.matmul(pn[:, 0:256], lhsT=ones_b[:], rhs=ind[:], start=True, stop=True), [ones_b, ind], [pn])
                    nblk = kb.sb("nblk", [128, 256], BF16)
                    kb.op("dve", lambda e: e.tensor_copy(out=nblk[:], in_=pn[:, 0:256]), [pn], [nblk])
                    nbT = kb.sb("nbT", [128, 2, 128], BF16)
                    transposes(nblk, [nblk[:, 0:128], nblk[:, 128:256]], nbT[:], nbT)
                    pb = ps1.get()
                    kb.op("pe", lambda e: e.matmul(pb[:, 0:128], lhsT=nbT[:, 0, :], rhs=tri[:, 1, :], start=True, stop=True), [nbT, tri], [pb])
                    kb.op("pe", lambda e: e.matmul(pb[:, 128:256], lhsT=nbT[:, 0, :], rhs=ones_b[:], start=True, stop=False), [nbT, ones_b], [pb], inc=False)
                    kb.op("pe", lambda e: e.matmul(pb[:, 128:256], lhsT=nbT[:, 1, :], rhs=tri[:, 1, :], start=False, stop=True), [nbT, tri], [pb])
                    kb.op("dve", lambda e: e.tensor_copy(out=base_r[:], in_=pb[:, 0:256]), [pb], [base_r])
                    pe_ = ps1.get()
                    kb.op("pe", lambda e: e.matmul(pe_[:, 0:1], lhsT=tri[:, 0, :], rhs=nbT[:, 0, 0:1], start=True, stop=True), [nbT, tri], [pe_])
                    kb.op("pe", lambda e: e.matmul(pe_[:, 1:2], lhsT=ones_b[:], rhs=nbT[:, 0, 0:1], start=True, stop=False), [nbT, ones_b], [pe_], inc=False)
                    kb.op("pe", lambda e: e.matmul(pe_[:, 1:2], lhsT=tri[:, 0, :], rhs=nbT[:, 1, 0:1], start=False, stop=True), [nbT, tri], [pe_])
                    endT = kb.sb("endT", [128, 2], F32)
                    kb.op("dve", lambda e: e.tensor_copy(out=endT[:], in_=pe_[:, 0:2]), [pe_], [endT])
                    iota_b = kb.sb("iota_b", [128, NBLK], F32)
                    kb.dma("sp", iota_b[:], I["c_iotab"].ap()[:, 0:NBLK], writes=[iota_b])
                    ind2 = kb.sb("ind2", [128, 2, NBLK], BF16)
                    for c_ in range(2):
                        kb.op("dve", lambda e: e.tensor_scalar(out=ind2[:, c_, :], in0=iota_b[:], scalar1=endT[:, c_:c_ + 1], scalar2=None, op0=ALU.is_ge), [iota_b, endT], [ind2])
                    BEf = kb.sb("BEf", [128, NBLK], F32)
                    for n0 in range(0, NBLK, 512):
                        n1 = min(n0 + 512, NBLK)
                        pbe = ps1.get()
                        for c_ in range(2):
                            kb.op("pe", lambda e: e.matmul(pbe[:, 0:n1 - n0], lhsT=ones_b[:], rhs=ind2[:, c_, n0:n1], start=(c_ == 0), stop=(c_ == 1)), [ones_b, ind2], [pbe], inc=(c_ == 1))
                        kb.op("dve", lambda e: e.tensor_scalar(out=BEf[:, n0:n1], in0=pbe[:, 0:n1 - n0], scalar1=128.0, scalar2=iota_p[:, 0:1], op0=ALU.mult, op1=ALU.add), [pbe, iota_p], [BEf])
                    kb.op("dve", lambda e: e.tensor_copy(out=IDXi[:], in_=BEf[:]), [BEf], [IDXi])
                with kb.scope():
                    s256 = kb.rot("s256b", [128, 256], F32, 9)
                    m8r = kb.rot("m8b", [128, 8], F32, 6)
                    ubr = kb.rot("ubB", [128, 1024], BF16, 3)
                    def tileB(i):
                            t_ = s256.get(); V = s256.get(); eq = s256.get()
                            kb.op("dve", lambda e: e.scalar_tensor_tensor(out=t_[:], in0=base_r[:], scalar=256.0, in1=POS_all[:, i, :], op0=ALU.mult, op1=ALU.add), [base_r, POS_all], [t_])
                            kb.op("dve", lambda e: e.scalar_tensor_tensor(out=V[:], in0=t_[:], scalar=1.0, in1=mask_all[:, i, :], op0=ALU.add, op1=ALU.mult), [t_, mask_all], [V])
                            yield
                            top8 = m8r.get(); d8 = m8r.get()
                            kb.op("dve", lambda e: e.max(out=top8[:], in_=V[:]), [V], [top8])
                            kb.op("dve", lambda e: e.tensor_scalar(out=d8[:], in0=top8[:], scalar1=-1.0, scalar2=None, op0=ALU.add), [top8], [d8])
                            kb.op("dve", lambda e: e.tensor_copy(out=dest_all[:, i, :], in_=d8[:]), [d8], [dest_all])
                            yield
                            for k in range(8):
                                kb.op("dve", lambda e: e.tensor_scalar(out=eq[:], in0=V[:], scalar1=top8[:, k:k + 1], scalar2=None, op0=ALU.is_equal), [V, top8], [eq])
                                kb.op("pool", lambda e: e.tensor_tensor(out=eq[:], in0=eq[:], in1=G_all[:, i, :], op=ALU.mult), [eq, G_all], [eq])
                                kb.op("dve", lambda e: e.tensor_reduce(out=gate_all[:, i, k:k + 1], in_=eq[:], axis=AX.X, op=ALU.add), [eq], [gate_all])
                                if k % 2 == 1:
                                    yield
                            yield
                            ub = ubr.get()
                            kb.dma("sp", ub[:], u_d[i * 128:(i + 1) * 128, :], reads=[u_r[i]], writes=[ub])
                            for k in range(8):
                                kb.idma(out=xs_d[:, :], in_=ub[:], out_off=dest_all[:, i, k:k + 1], reads=[ub, dest_all], writes=[xs_d])
                    run_interleaved((tileB(i) for i in range(NT)), 3)
            xT_rot = kb.rot("xT", [128, 8, 128], BF16, 3)
            silu_rot = kb.rot("silu", [128, 256], F32, 2)
            hb_rot = kb.rot("hb", [128, 256], BF16, 4)
            hT_rot = kb.rot("hT6", [128, 2, 128], BF16, 3)

            def ffn_a(xsb, wb13, strided):
                if strided:
                    srcs = [xsb[:].rearrange("s (p k) -> s k p", k=8)[:, k, :] for k in range(8)]
                else:
                    srcs = [xsb[:, k * 128:(k + 1) * 128] for k in range(8)]
                xT = xT_rot.get()
                transposes(xsb, srcs, xT[:], xT)
                ph = ps1.get()
                for k in range(8):
                    kb.op("pe", lambda e: e.matmul(ph[:, :], lhsT=xT[:, k, :], rhs=wb13[:, k, :, :].rearrange("p w n -> p (w n)"), start=(k == 0), stop=(k == 7)),
                          [xT, wb13], [ph], inc=(k == 7))
                sl = silu_rot.get(); hb = hb_rot.get()
                kb.op("act", lambda e: e.activation(out=sl[:], in_=ph[:, 0:256], func=AF.Silu), [ph], [sl])
                kb.op("dve", lambda e: e.tensor_tensor(out=hb[:], in0=ph[:, 256:512], in1=sl[:], op=ALU.mult), [ph, sl], [hb])
                return hb

            def ffn_b(hb, wb2, strided):
                if strided:
                    hs = [hb[:].rearrange("s (p j) -> s j p", j=2)[:, j, :] for j in range(2)]
                else:
                    hs = [hb[:, j * 128:(j + 1) * 128] for j in range(2)]
                hT = hT_rot.get()
                transposes(hb, hs, hT[:], hT, eng="dve")
                po_ = ps2.get()
                for nb_ in range(2):
                    for j in range(2):
                        kb.op("pe", lambda e: e.matmul(po_[:, nb_ * 512:(nb_ + 1) * 512], lhsT=hT[:, j, :], rhs=wb2[:, j, nb_ * 512:(nb_ + 1) * 512],
                                                       start=(j == 0), stop=(j == 1)), [hT, wb2], [po_], inc=(j == 1))
                return po_

            def ffn_block(xsb, wb13, wb2, strided):
                return ffn_b(ffn_a(xsb, wb13, strided), wb2, strided)

            with kb.scope():
                w1v = I["expert_w1"].ap().rearrange("e (p k) n -> (e p) (k n)", k=8)
                w3v = I["expert_w3"].ap().rearrange("e (p k) n -> (e p) (k n)", k=8)
                w2v = I["expert_w2"].ap().rearrange("e (p j) n -> (e p) (j n)", j=2)
                wf13 = kb.rot("wf13", [128, 2, 2048], F32, 3)
                wf2 = kb.rot("wf2", [128, 2048], F32, 3)
                wb13r = kb.rot("wb13", [128, 8, 2, 256], BF16, 3)
                wb2r = kb.rot("wb2", [128, 2, 1024], BF16, 4)
                xsr = kb.rot("xsb", [128, 1024], BF16, 8)
                ybr = kb.rot("yb", [128, 1024], BF16, 3)
                for t_ in wf13.tiles + wf2.tiles:
                    kb.op("pool", lambda e: e.memset(t_[:], 0.0), [], [t_])
                pend = {}

                def issue(bk):
                    f13 = wf13.get(); f2 = wf2.get()
                    idx = IDXi[:, bk:bk + 1]
                    kb.idma(out=f13[:, 0, :], in_=w1v, in_off=idx, reads=[IDXi], writes=[f13], bounds=32767)
                    kb.idma(out=f13[:, 1, :], in_=w3v, in_off=idx, reads=[IDXi], writes=[f13], bounds=32767)
                    kb.idma(out=f2[:], in_=w2v, in_off=idx, reads=[IDXi], writes=[f2], bounds=32767)
                    xs2 = []
                    for sbk in range(2):
                        xsb = xsr.get()
                        r0 = (bk * 2 + sbk) * 128
                        kb.dma("sp", xsb[:], xs_d[r0:r0 + 128, :], reads=[xs_d], writes=[xsb])
                        xs2.append(xsb)
                    pend[bk] = (f13, f2, xs2)

                nrun = min(NBLK, blk_limit)
                NSUB = nrun * 2
                ctx_ = {}

                def st0(n):
                    bk = n // 2
                    if n % 2 == 0:
                        if bk + 2 < nrun:
                            issue(bk + 2)
                        f13, f2, xs2 = pend.pop(bk)
                        wb13 = wb13r.get(); wb2 = wb2r.get()
                        kb.op("act", lambda e: e.copy(out=wb13[:, :, 0, :], in_=f13[:, 0, :].rearrange("p (k n) -> p k n", k=8)), [f13], [wb13])
                        kb.op("dve", lambda e: e.tensor_copy(out=wb13[:, :, 1, :], in_=f13[:, 1, :].rearrange("p (k n) -> p k n", k=8)), [f13], [wb13])
                        kb.op("act", lambda e: e.copy(out=wb2[:, 0, :], in_=f2[:, 0:1024]), [f2], [wb2])
                        kb.op("dve", lambda e: e.tensor_copy(out=wb2[:, 1, :], in_=f2[:, 1024:2048]), [f2], [wb2])
                        ctx_[("w", bk)] = (wb13, wb2, xs2)
                    wb13, wb2, xs2 = ctx_[("w", bk)]
                    xsb = xs2[n % 2]
                    srcs = [xsb[:].rearrange("s (p k) -> s k p", k=8)[:, k, :] for k in range(8)]
                    xT = xT_rot.get()
                    transposes(xsb, srcs, xT[:], xT)
                    ctx_[n] = dict(xT=xT, wb13=wb13, wb2=wb2)

                def st1(n):
                    c = ctx_[n]
                    xT, wb13 = c["xT"], c["wb13"]
                    ph = ps1.get()
                    for k in range(8):
                        kb.op("pe", lambda e: e.matmul(ph[:, :], lhsT=xT[:, k, :], rhs=wb13[:, k, :, :].rearrange("p w n -> p (w n)"), start=(k == 0), stop=(k == 7)),
                              [xT, wb13], [ph], inc=(k == 7))
                    sl = silu_rot.get(); hb = hb_rot.get()
                    kb.op("act", lambda e: e.activation(out=sl[:], in_=ph[:, 0:256], func=AF.Silu), [ph], [sl])
                    kb.op("dve", lambda e: e.tensor_tensor(out=hb[:], in0=ph[:, 256:512], in1=sl[:], op=ALU.mult), [ph, sl], [hb])
                    c["hb"] = hb

                def st2(n):
                    c = ctx_[n]
                    hb = c["hb"]
                    hs = [hb[:].rearrange("s (p j) -> s j p", j=2)[:, j, :] for j in range(2)]
                    hT = hT_rot.get()
                    transposes(hb, hs, hT[:], hT, eng="dve")
                    c["hT"] = hT

                def st3(n):
                    c = ctx_.pop(n)
                    hT, wb2 = c["hT"], c["wb2"]
                    po_ = ps2.get()
                    for nb_ in range(2):
                        for j in range(2):
                            kb.op("pe", lambda e: e.matmul(po_[:, nb_ * 512:(nb_ + 1) * 512], lhsT=hT[:, j, :], rhs=wb2[:, j, nb_ * 512:(nb_ + 1) * 512],
                                                           start=(j == 0), stop=(j == 1)), [hT, wb2], [po_], inc=(j == 1))
                    yb = ybr.get()
                    kb.op("act", lambda e: e.copy(out=yb[:, 0:512], in_=po_[:, 0:512]), [po_], [yb])
                    kb.op("dve", lambda e: e.tensor_copy(out=yb[:, 512:1024], in_=po_[:, 512:1024]), [po_], [yb])
                    kb.dma("sp", Y_d[n * 128:(n + 1) * 128, :], yb[:], reads=[yb], writes=[Y_d])

                for bk in range(min(2, nrun)):
                    issue(bk)
                stages = (st0, st1, st2, st3)
                for step in range(NSUB + 3):
                    for k_, fn_ in enumerate(stages):
                        n = step - k_
                        if 0 <= n < NSUB:
                            fn_(n)
            with kb.scope():
                wsf = kb.sb("wsf", [128, 8, 256], F32)
                ws13 = kb.sb("ws13", [128, 8, 2, 256], BF16)
                ws2f = kb.sb("ws2f", [128, 2, 1024], F32)
                ws2 = kb.sb("ws2", [128, 2, 1024], BF16)
                for w_, nm in enumerate(("shared_w1", "shared_w3")):
                    kb.dma("sp", wsf[:], I[nm].ap().rearrange("(k p) n -> p k n", p=128), reads=[wsf], writes=[wsf])
                    kb.op("dve", lambda e: e.tensor_copy(out=ws13[:, :, w_, :], in_=wsf[:]), [wsf], [ws13])
                kb.dma("sp", ws2f[:], I["shared_w2"].ap().rearrange("(j p) n -> p j n", p=128), writes=[ws2f])
                kb.op("dve", lambda e: e.tensor_copy(out=ws2[:], in_=ws2f[:]), [ws2f], [ws2])
                Gms = []
                for b in range(nbatch):
                    Gm = kb.sb("Gm", [128, 1024], F32)
                    bcast_load(Gm[:], Gm, mod_d.t.ap()[b:b + 1, 5120:6144], 128, reads=[mod_d])
                    Gms.append(Gm)
                ubr = kb.rot("ubD", [128, 1024], BF16, 3)
                ygr = kb.rot("yg", [128, 1024], BF16, 8)
                accr = kb.rot("acc", [128, 1024], F32, 3)
                xrot = kb.rot("xtD", [128, 1024], F32, 3)
                def tileD(i):
                        b = i // 16
                        ub = ubr.get()
                        kb.dma("sp", ub[:], u_d[i * 128:(i + 1) * 128, :], reads=[u_r[i]], writes=[ub])
                        yield
                        hb_ = ffn_a(ub, ws13, False)
                        yield
                        po_ = ffn_b(hb_, ws2, False)
                        acc = accr.get()
                        kb.op("act", lambda e: e.copy(out=acc[:], in_=po_[:, :]), [po_], [acc])
                        yield
                        for k in range(8):
                            yg = ygr.get()
                            kb.idma(out=yg[:], in_=Y_d[:, :], in_off=dest_all[:, i, k:k + 1], reads=[dest_all, Y_d], writes=[yg])
                            kb.op("dve", lambda e: e.scalar_tensor_tensor(out=acc[:], in0=yg[:], scalar=gate_all[:, i, k:k + 1], in1=acc[:], op0=ALU.mult, op1=ALU.add),
                                  [yg, gate_all, acc], [acc])
                            if k % 2 == 1:
                                yield
                        if "moe_dbg" in dbg and i < 16:
                            kb.dma("sp", dbg_d["moe_dbg"][:, i, :], acc[:], reads=[acc], writes=[dbg_d["moe_dbg"]])
                        yield
                        xt = xrot.get()
                        kb.dma("sp", xt[:], x1_d[i * 128:(i + 1) * 128, :], reads=[x1_r[i]], writes=[xt])
                        kb.op("dve", lambda e: e.tensor_tensor(out=acc[:], in0=acc[:], in1=Gms[b][:], op=ALU.mult), [acc, Gms[b]], [acc])
                        kb.op("pool", lambda e: e.tensor_tensor(out=xt[:], in0=xt[:], in1=acc[:], op=ALU.add), [xt, acc], [xt])
                        kb.dma("sp", out_d.ap()[b, (i % 16) * 128:(i % 16 + 1) * 128, :], xt[:], reads=[xt])
                run_interleaved((tileD(i) for i in range(NT)), 3)
        kb.finish()
        print("instructions:", kb.nins, "sems:", len(kb.sems) + len(kb.dsem))
    return nc


_CACHE = {}


def kernel(**inputs):
    n = 8
    consts = host_consts()
    sq = lambda k: np.ascontiguousarray(np.asarray(inputs[k], dtype=np.float32)[0])
    shared = {}
    for k in IN_SHAPES:
        if k in ("x", "ctx", "cT") or k.startswith("c_"):
            continue
        a = sq(k)
        shared[k] = a.reshape(IN_SHAPES[k])
    shared.update(consts)
    x = np.asarray(inputs["x"], np.float32)
    ctx = np.asarray(inputs["ctx"], np.float32)
    c = np.asarray(inputs["c"], np.float32)
    c_ctx = np.asarray(inputs["c_ctx"], np.float32)
    in_maps = []
    for i in range(n):
        m = dict(shared)
        m["x"] = np.ascontiguousarray(x[2 * i:2 * i + 2])
        m["ctx"] = np.ascontiguousarray(ctx[2 * i:2 * i + 2])
        m["cT"] = np.ascontiguousarray(np.stack([c[2 * i], c[2 * i + 1], c_ctx], axis=1))
        in_maps.append(m)
    if "nc" not in _CACHE:
        _CACHE["nc"] = build()
    res = run_bass_kernel_spmd(_CACHE["nc"], in_maps, core_ids=list(range(n)))
    return np.concatenate([np.asarray(r["out"], np.float32) for r in res.results], axis=0)
```

```python
import numpy as np
import ml_dtypes
import concourse.bass as bass
import concourse.mybir as mybir
from concourse.bass_utils import run_bass_kernel_spmd
from contextlib import ExitStack, contextmanager

F32 = mybir.dt.float32
BF16 = mybir.dt.bfloat16
AF = mybir.ActivationFunctionType
ALU = mybir.AluOpType
AX = mybir.AxisListType

C_DEC = -0.6065306597126334
EPS = 1e-6
GN_EPS = 64e-5
ATTN_SCALE = 96 ** -0.5


NAMES = {}


class Buf:
    __slots__ = ("name", "w", "r", "excl")

    def __init__(self, name=""):
        self.name = name
        self.w = None
        self.r = {}
        self.excl = False


class Tile:
    def __init__(self, t, name):
        self.t = t
        self.b = Buf(name)

    def __getitem__(self, k):
        return self.t[k]


class Rot:
    def __init__(self, tiles):
        self.tiles = tiles
        self.i = 0

    def get(self):
        t = self.tiles[self.i % len(self.tiles)]
        self.i += 1
        return t


class KB:
    ENG = ("pe", "act", "dve", "pool", "sp")
    EPOCH = 8000
    NDMA = 40

    def __init__(self, nc, stack):
        self.nc = nc
        self.gstack = stack
        self.stack = stack
        self.e = {"pe": nc.tensor, "act": nc.scalar, "dve": nc.vector,
                  "pool": nc.gpsimd, "sp": nc.sync}
        self.cnt = {k: 0 for k in self.ENG}
        self.ep = {k: 0 for k in self.ENG}
        self.sems = {}
        self.waited = {}
        for k in self.ENG:
            self._newsem(k)
        self.dsem = []
        self.dslot = []
        self.dval = []
        for i in range(self.NDMA):
            self.dsem.append(stack.enter_context(nc.semaphore(f"dq{i}")))
            self.dslot.append(i)
            self.dval.append(0)
        self.dnext = 0
        self.dwaited = {}
        self.nins = 0
        self.uid = 0
        self.promised = False
        self.bregs = {}

    def sb(self, name, shape, dtype):
        self.uid += 1
        nm = f"{name}_{self.uid}"
        NAMES[name] = nm
        return Tile(self.stack.enter_context(self.nc.sbuf_tensor(nm, list(shape), dtype)), nm)

    def ps(self, name, shape, dtype=F32):
        t = Tile(self.gstack.enter_context(self.nc.psum_tensor(name, list(shape), dtype)), name)
        t.b.excl = True
        return t

    def dram(self, name, shape, dtype, kind="Internal"):
        return Tile(self.nc.dram_tensor(name, list(shape), dtype, kind=kind), name)

    def rot(self, name, shape, dtype, n=2):
        return Rot([self.sb(f"{name}{i}", shape, dtype) for i in range(n)])

    @contextmanager
    def scope(self):
        old = self.stack
        with ExitStack() as st:
            self.stack = st
            yield
            self.barrier()
        self.stack = old

    def _newsem(self, k):
        self.sems[(k, self.ep[k])] = self.gstack.enter_context(
            self.nc.semaphore(f"s_{k}_{self.ep[k]}"))

    def _wait(self, eng, tok):
        if tok is None:
            return
        if tok[0] == "e":
            _, k, ep, c = tok
            if eng == "pe" and k == "pe":
                return
            assert not (k == "pe" and ep == self.ep["pe"] and c > self.cnt["pe"]), "wait on a promised PE token"
            key = (eng, k, ep)
            if self.waited.get(key, 0) >= c:
                return
            self.e[eng].wait_ge(self.sems[(k, ep)], c)
            self.waited[key] = c
        else:
            _, i, v = tok
            key = (eng, i)
            if self.dwaited.get(key, 0) >= v:
                return
            self.e[eng].wait_ge(self.dsem[i], v)
            self.dwaited[key] = v

    @staticmethod
    def _bufs(xs):
        out = []
        for x in xs:
            if x is None:
                continue
            out.append(x.b if isinstance(x, Tile) else x)
        return out

    def _deps(self, eng, reads, writes):
        for b in reads:
            self._wait(eng, b.w)
        for b in writes:
            self._wait(eng, b.w)
            for t in b.r.values():
                self._wait(eng, t)

    def _commit(self, tok, reads, writes):
        for b in writes:
            b.w = tok
            b.r = {}
        for b in reads:
            if b not in writes:
                key = tok[1] if tok[0] == "e" else ("d", tok[1])
                b.r[key] = tok

    def op(self, eng, fn, reads=(), writes=(), inc=True):
        reads = self._bufs(reads)
        writes = self._bufs(writes)
        writes = writes + [b for b in reads if b.excl and b not in writes]
        self._deps(eng, reads, writes)
        if inc and self.cnt[eng] >= self.EPOCH and not (eng == "pe" and self.promised):
            self.ep[eng] += 1
            self.cnt[eng] = 0
            self._newsem(eng)
        ins = fn(self.e[eng])
        if eng == "pe":
            self.promised = not inc
        if inc:
            self.cnt[eng] += 1
            ins.then_inc(self.sems[(eng, self.ep[eng])], 1)
            tok = ("e", eng, self.ep[eng], self.cnt[eng])
        else:
            assert eng == "pe"
            tok = ("e", eng, self.ep[eng], self.cnt[eng] + 1)
        self._commit(tok, reads, writes)
        self.nins += 1

    def dma(self, eng, out, in_, reads=(), writes=(), **kw):
        reads = self._bufs(reads)
        writes = self._bufs(writes)
        self._deps(eng, reads, writes)
        s = self.dnext
        self.dnext = (self.dnext + 1) % self.NDMA
        i = self.dslot[s]
        if self.dval[i] >= 8000:
            self.dsem.append(self.gstack.enter_context(self.nc.semaphore(f"dq{len(self.dsem)}")))
            self.dval.append(0)
            prev = i
            i = len(self.dsem) - 1
            self.dslot[s] = i
            self._wait(eng, ("d", prev, self.dval[prev]))
        if self.dval[i] > 0:
            self._wait(eng, ("d", i, self.dval[i]))
        self.dval[i] += 16
        self.e[eng].dma_start(out=out, in_=in_, **kw).then_inc(self.dsem[i], 16)
        tok = ("d", i, self.dval[i])
        self._commit(tok, reads, writes)
        self.nins += 1

    def idma(self, out, in_, out_off=None, in_off=None, reads=(), writes=(), bounds=None):
        eng = "pool"
        reads = self._bufs(reads)
        writes = self._bufs(writes)
        self._deps(eng, reads, writes)
        s = self.dnext
        self.dnext = (self.dnext + 1) % self.NDMA
        i = self.dslot[s]
        if self.dval[i] >= 8000:
            self.dsem.append(self.gstack.enter_context(self.nc.semaphore(f"dq{len(self.dsem)}")))
            self.dval.append(0)
            prev = i
            i = len(self.dsem) - 1
            self.dslot[s] = i
            self._wait(eng, ("d", prev, self.dval[prev]))
        if self.dval[i] > 0:
            self._wait(eng, ("d", i, self.dval[i]))
        self.dval[i] += 16
        kw = {}
        if bounds is not None:
            if bounds not in self.bregs:
                r = self.nc.gpsimd.alloc_register(f"bnd{bounds}")
                self.nc.gpsimd.reg_mov(r, bounds)
                self.bregs[bounds] = r
            kw = dict(bounds_check=self.bregs[bounds], oob_is_err=False)
        self.e[eng].indirect_dma_start(
            out=out, out_offset=None if out_off is None else bass.IndirectOffsetOnAxis(ap=out_off, axis=0),
            in_=in_, in_offset=None if in_off is None else bass.IndirectOffsetOnAxis(ap=in_off, axis=0), **kw,
        ).then_inc(self.dsem[i], 16)
        tok = ("d", i, self.dval[i])
        self._commit(tok, reads, writes)
        self.nins += 1

    def finish(self):
        for i in range(len(self.dsem)):
            if self.dval[i] > 0:
                self._wait("sp", ("d", i, self.dval[i]))
        for k in self.ENG:
            if k != "sp" and (self.cnt[k] > 0 or self.ep[k] > 0):
                self._wait("sp", ("e", k, self.ep[k], self.cnt[k]))

    def barrier(self):
        self.finish()
        self.op("sp", lambda e: e.nop(), (), ())
        tok = ("e", "sp", self.ep["sp"], self.cnt["sp"])
        for k in ("pe", "act", "dve", "pool"):
            self._wait(k, tok)


def run_interleaved(gens, width):
    it = iter(gens)
    active = []
    exhausted = False
    while True:
        while len(active) < width and not exhausted:
            try:
                active.append(next(it))
            except StopIteration:
                exhausted = True
        if not active:
            break
        for g in list(active):
            try:
                next(g)
            except StopIteration:
                active.remove(g)


def host_consts():
    s = np.arange(128)[:, None]
    t = np.arange(128)[None, :]
    le = (s <= t).astype(np.float32)
    lt = (s < t).astype(np.float32)
    ge = (s >= t).astype(np.float32)
    gt = (s > t).astype(np.float32)
    tri = np.stack([le, lt, ge, gt], axis=1)
    mask4 = np.zeros((2, 128, 512), np.float32)
    maskt = np.zeros((2, 128, 128), np.float32)
    for d, (strict, incl) in enumerate(((lt, le), (gt, ge))):
        mask4[d, :, 0:128] = -strict
        mask4[d, :, 128:256] = -incl
        mask4[d, :, 256:384] = strict
        mask4[d, :, 384:512] = incl
        maskt[d] = -strict.T
    rows = 2048 // 64
    row = np.repeat(np.arange(rows, dtype=np.float32), 64)
    col = np.tile(np.arange(64, dtype=np.float32), rows)
    inv = (10000.0 ** (-np.arange(8, dtype=np.float32) / 8)).astype(np.float32)
    ang = np.concatenate([row[:, None] * inv, col[:, None] * inv], axis=-1).astype(np.float32)
    p_ = np.arange(128, dtype=np.float32)
    iotap = np.stack([p_, 256 * p_], axis=1)
    iotab = np.tile(np.arange(512, dtype=np.float32)[None, :], (128, 1))
    return dict(c_iotap=iotap, c_iotab=iotab, c_ident=np.eye(128, dtype=np.float32), c_tri=tri, c_mask4=mask4, c_maskt=maskt,
                c_cos=np.cos(ang).astype(np.float32), c_sin=np.sin(ang).astype(np.float32))


IN_SHAPES = dict(
    x=[2, 2048, 1024], ctx=[2, 256, 1024], cT=[1024, 3],
    ada_w=[1024, 6144], ada_b=[1, 6144], norm_mix=[1, 1024], norm_ffn=[1, 1024],
    w_in=[1024, 4416], shift_conv=[3, 1952], q_lat_norm=[1, 256], w_uq=[256, 768],
    kv_lat_norm=[1, 128], w_ukv=[128, 1024], q_norm=[1, 96], k_norm=[1, 96], w_o_mla=[512, 1024],
    decay_w0=[2, 512], decay_w2=[2, 64, 512], aicl_a0=[2, 512], aicl_a2=[2, 64, 512],
    k_k=[1, 512], k_a=[1, 512], r_k=[1, 512], gn_w=[1, 512], gn_b=[1, 512], gate_g2=[160, 512],
    w_o_rwkv=[512, 1024], w_out=[1024, 1024], router_w=[1024, 256], router_bias=[1, 256],
    expert_w1=[256, 1024, 256], expert_w3=[256, 1024, 256], expert_w2=[256, 256, 1024],
    shared_w1=[1024, 256], shared_w3=[1024, 256], shared_w2=[256, 1024],
    c_ident=[128, 128], c_tri=[128, 4, 128], c_mask4=[2, 128, 512], c_maskt=[2, 128, 128],
    c_cos=[2048, 16], c_sin=[2048, 16], c_iotap=[128, 2], c_iotab=[128, 512],
)

CT0 = 1
LT0 = 259
HW = 2308


def build(upto=99, dbg=(), n_exp=256, nbatch=2, rw_tiles=99, rw_phase=9, rw_dirs=2, blk_limit=10 ** 9):
    nc = bass.Bass("TRN2", target_bir_lowering=False)
    I = {k: nc.dram_tensor(k, list(v), F32, kind="ExternalInput") for k, v in IN_SHAPES.items()
         if not (upto < 6 and k.startswith(("expert_", "shared_")))}
    out_d = nc.dram_tensor("out", [2, 2048, 1024], F32, kind="ExternalOutput")

    with ExitStack() as gst:
        kb = KB(nc, gst)
        dbgk = lambda n: ("ExternalOutput" if n in dbg else "Internal")
        ps1 = Rot([kb.ps(f"ps1_{i}", [128, 512]) for i in range(4)])
        ps2 = Rot([kb.ps(f"ps2_{i}", [128, 1024]) for i in range(2)])

        mod_d = kb.dram("mod_d", [3, 6144], F32, dbgk("mod_d"))
        mla_d = kb.dram("mla_d", [2304, 416], F32, dbgk("mla_d"))
        mla_r = [Buf() for _ in range(18)]
        rkv_d = kb.dram("rkv_d", [2304, 1536], F32, dbgk("rkv_d"))
        rkv_r = [Buf() for _ in range(18)]
        gates_d = kb.dram("gates_d", [2048, 2048], BF16, dbgk("gates_d"))
        gates_r = [[Buf() for _ in range(4)] for _ in range(16)]
        x1_d = kb.dram("x1_d", [4096, 1024], F32, dbgk("x1_d"))
        x1_r = [Buf() for _ in range(32)]
        dbg_d = {}
        for n, shp, dt in (("fm_dbg", [128, 4, 2304], BF16), ("att_dbg", [128, 4, 2048], BF16),
                           ("y_dbg", [128, 16, 512], F32), ("rw_dbg", [128, 4, 2048], BF16),
                           ("G_dbg", [128, 16, 257], F32), ("qT_dbg", [96, 8, 2048], BF16),
                           ("kT_dbg", [96, 8, 2304], BF16), ("moe_dbg", [128, 16, 1024], F32)):
            if n in dbg:
                dbg_d[n] = kb.dram(n, shp, dt, "ExternalOutput")

        ident_f = kb.sb("ident_f", [128, 128], F32)
        ident_b = kb.sb("ident_b", [128, 128], BF16)
        ones_b = kb.sb("ones_b", [128, 128], BF16)
        kb.dma("sp", ident_f[:], I["c_ident"].ap(), writes=[ident_f])
        kb.op("dve", lambda e: e.tensor_copy(out=ident_b[:], in_=ident_f[:]), [ident_f], [ident_b])
        kb.op("pool", lambda e: e.memset(ones_b[:], 1.0), [], [ones_b])

        def bcast_load(dst_ap, dst_tile, src_ap, nparts, reads=()):
            kb.dma("sp", dst_ap, src_ap.to_broadcast([nparts, src_ap.shape[-1]]), reads=reads, writes=[dst_tile])

        def transposes(src_tile, src_aps, dst_ap, dst_tile, dt=BF16, eng="act", width=128, pool=None):
            pp = (pool or ps1).get()
            pv = pp[:].bitcast(BF16) if dt == BF16 else pp[:]
            idn = ident_b if dt == BF16 else ident_f
            n = len(src_aps)
            P = src_aps[0].shape[0]
            w = src_aps[0].shape[1]
            for j, a in enumerate(src_aps):
                kb.op("pe", lambda e: e.transpose(out=pv[0:w, j * width:j * width + P], in_=a, identity=idn[0:P, 0:P]),
                      [src_tile, idn], [pp])
            src = pv[0:w, 0:n * width]
            if eng == "act":
                kb.op("act", lambda e: e.copy(out=dst_ap, in_=src.rearrange("p (j t) -> p j t", j=n) if len(dst_ap.shape) == 3 else src), [pp], [dst_tile])
            else:
                kb.op(eng, lambda e: e.tensor_copy(out=dst_ap, in_=src.rearrange("p (j t) -> p j t", j=n) if len(dst_ap.shape) == 3 else src), [pp], [dst_tile])

        def rms_rstd(ssq_ap, out_ap, tile_in, tile_out, n, eps):
            kb.op("act", lambda e: e.activation(out=out_ap, in_=ssq_ap, func=AF.Sqrt, bias=eps_t[:, 0:1] if eps == EPS else (gneps_t[:, 0:1] if eps == GN_EPS else tiny_t[:, 0:1]), scale=1.0 / n),
                  [tile_in], [tile_out])
            kb.op("dve", lambda e: e.reciprocal(out=out_ap, in_=out_ap), [tile_out], [tile_out])

        eps_t = kb.sb("eps_t", [128, 1], F32)
        gneps_t = kb.sb("gneps_t", [128, 1], F32)
        tiny_t = kb.sb("tiny_t", [128, 1], F32)
        kb.op("pool", lambda e: e.memset(eps_t[:], EPS), [], [eps_t])
        kb.op("pool", lambda e: e.memset(gneps_t[:], GN_EPS), [], [gneps_t])
        kb.op("pool", lambda e: e.memset(tiny_t[:], 1e-12), [], [tiny_t])

        with kb.scope():
            cTs = kb.sb("cTs", [128, 8, 3], F32)
            kb.dma("sp", cTs[:], I["cT"].ap().rearrange("(k p) r -> p k r", p=128), writes=[cTs])
            sT = kb.sb("sT", [128, 8, 3], F32)
            kb.op("act", lambda e: e.activation(out=sT[:], in_=cTs[:], func=AF.Silu), [cTs], [sT])
            modsb = kb.sb("modsb", [3, 6144], F32)
            adab = kb.sb("adab", [3, 6144], F32)
            bcast_load(adab[:], adab, I["ada_b"].ap()[0:1, :], 3)
            nrm = kb.sb("nrm", [3, 2, 1024], F32)
            bcast_load(nrm[:, 0, :], nrm, I["norm_mix"].ap()[0:1, :], 3)
            bcast_load(nrm[:, 1, :], nrm, I["norm_ffn"].ap()[0:1, :], 3)
            wrot = kb.rot("adaw", [128, 8, 512], F32, 2)
            for cb in range(12):
                wt = wrot.get()
                kb.dma("sp", wt[:], I["ada_w"].ap()[:, cb * 512:(cb + 1) * 512].rearrange("(k p) n -> p k n", p=128), writes=[wt])
                pp = ps1.get()
                for k in range(8):
                    kb.op("pe", lambda e: e.matmul(pp[0:3, :], lhsT=sT[:, k, :], rhs=wt[:, k, :], start=(k == 0), stop=(k == 7)),
                          [sT, wt], [pp], inc=(k == 7))
                kb.op("dve", lambda e: e.tensor_tensor(out=modsb[:, cb * 512:(cb + 1) * 512], in0=pp[0:3, :],
                                                       in1=adab[:, cb * 512:(cb + 1) * 512], op=ALU.add), [pp, adab], [modsb])
            for (c0, j) in ((1024, 0), (4096, 1)):
                kb.op("dve", lambda e: e.scalar_tensor_tensor(out=modsb[:, c0:c0 + 1024], in0=modsb[:, c0:c0 + 1024], scalar=1.0,
                                                              in1=nrm[:, j, :], op0=ALU.add, op1=ALU.mult), [modsb, nrm], [modsb])
            kb.dma("sp", mod_d[:, :], modsb[:], reads=[modsb], writes=[mod_d])
        if upto <= 0:
            kb.finish()
            return nc

        def norm_mod(xt, A, S, hb):
            junk = junk_rot.get()
            ssq = small_rot.get()
            kb.op("act", lambda e: e.activation(out=junk[:], in_=xt[:], func=AF.Square, accum_out=ssq[:, 0:1]), [xt], [junk, ssq])
            rms_rstd(ssq[:, 0:1], ssq[:, 1:2], ssq, ssq, 1024, EPS)
            kb.op("dve", lambda e: e.scalar_tensor_tensor(out=junk[:], in0=xt[:], scalar=ssq[:, 1:2], in1=A[:], op0=ALU.mult, op1=ALU.mult),
                  [xt, ssq, A], [junk])
            kb.op("pool", lambda e: e.tensor_tensor(out=hb[:], in0=junk[:], in1=S[:], op=ALU.add), [junk, S], [hb])

        for b in range(nbatch):
          with kb.scope():
            junk_rot = kb.rot("junk", [128, 1024], F32, 2)
            small_rot = kb.rot("small", [128, 16], F32, 6)
            mix_stack = ExitStack()
            fm_stack = ExitStack()
            _old = kb.stack
            kb.stack = mix_stack
            attT = kb.sb("attT", [128, 4, 2048], BF16)
            rwT = kb.sb("rwT", [128, 4, 2048], BF16)
            kb.stack = fm_stack
            fm = kb.sb("fm", [128, 4, 2304], BF16)
            kb.stack = _old

            with kb.scope():
                hT = kb.sb("hT", [128, 8, HW], BF16)
                for (c0, c1) in ((0, 1), (257, 259), (2307, 2308)):
                    kb.op("pool", lambda e: e.memset(hT[:, :, c0:c1], 0.0), [], [hT])
                with kb.scope():
                  xrot = kb.rot("xt", [128, 1024], F32, 3)
                  hbrot = kb.rot("hb", [128, 1024], BF16, 3)
                  for seg, (src, nt, off, row) in enumerate(((I["ctx"], 2, CT0, 2), (I["x"], 16, LT0, b))):
                    A1 = kb.sb("A1", [128, 1024], F32)
                    S1 = kb.sb("S1", [128, 1024], F32)
                    bcast_load(A1[:], A1, mod_d.t.ap()[row:row + 1, 1024:2048], 128, reads=[mod_d])
                    bcast_load(S1[:], S1, mod_d.t.ap()[row:row + 1, 0:1024], 128, reads=[mod_d])
                    def tileS1(t, src=src, off=off, A1=A1, S1=S1):
                        xt = xrot.get()
                        kb.dma("sp", xt[:], src.ap()[b, t * 128:(t + 1) * 128, :], writes=[xt])
                        hb = hbrot.get()
                        norm_mod(xt, A1, S1, hb)
                        yield
                        transposes(hb, [hb[:, k * 128:(k + 1) * 128] for k in range(8)],
                                   hT[:, :, off + t * 128: off + (t + 1) * 128], hT)
                    run_interleaved((tileS1(t) for t in range(nt)), 2)
                conv = kb.sb("conv", [128, 3, 1952], F32)
                for j in range(3):
                    bcast_load(conv[:, j, :], conv, I["shift_conv"].ap()[j:j + 1, :], 128)
                wf = kb.sb("wf", [128, 8, 512], F32)
                wbs = [kb.rot(f"wb{j}", [128, 8, 512], BF16, 2) for j in range(3)]
                osb = kb.rot("osb", [128, 512], F32, 2)
                gsb = kb.rot("gsb", [128, 512], BF16, 2)
                tok_tiles = [(CT0 + t * 128, t) for t in range(2)] + [(LT0 + t * 128, 2 + t) for t in range(16)]
                blocks = [(0, 416, "M")] + [(416 + i * 512, 512, "R") for i in range(3)] + [(1952, 416, "F")] + \
                         [(2368 + i * 512, 512, "G") for i in range(4)]
                for (c0, ncol, kind) in blocks:
                    kb.dma("sp", wf[:, :, 0:ncol], I["w_in"].ap()[:, c0:c0 + ncol].rearrange("(k p) n -> p k n", p=128), writes=[wf])
                    has_conv = kind in ("R", "F")
                    ws = []
                    for j in (range(3) if has_conv else range(1)):
                        wb = wbs[j].get()
                        if has_conv:
                            cc = c0 - 416
                            for k in range(8):
                                kb.op("pool" if k % 2 else "dve", lambda e: e.tensor_tensor(out=wb[:, k, 0:ncol], in0=wf[:, k, 0:ncol],
                                                                                           in1=conv[:, j, cc:cc + ncol], op=ALU.mult), [wf, conv], [wb])
                        else:
                            kb.op("pool", lambda e: e.tensor_copy(out=wb[:, :, 0:ncol], in_=wf[:, :, 0:ncol]), [wf], [wb])
                        ws.append(wb)
                    shifts = [(-1, 0), (0, 1), (1, 2)] if has_conv else [(0, 0)]
                    if kind in ("M", "R"):
                        for (off, ti) in tok_tiles:
                            pp = ps1.get()
                            n = len(shifts) * 8
                            i = 0
                            for (sh, j) in shifts:
                                for k in range(8):
                                    kb.op("pe", lambda e: e.matmul(pp[:, 0:ncol], lhsT=hT[:, k, off + sh:off + sh + 128], rhs=ws[j][:, k, 0:ncol],
                                                                   start=(i == 0), stop=(i == n - 1)), [hT, ws[j]], [pp], inc=(i == n - 1))
                                    i += 1
                            o = osb.get()
                            kb.op("act", lambda e: e.copy(out=o[:, 0:ncol], in_=pp[:, 0:ncol]), [pp], [o])
                            if kind == "M":
                                kb.dma("sp", mla_d[ti * 128:(ti + 1) * 128, :], o[:, 0:416], reads=[o], writes=[mla_r[ti]])
                            else:
                                cc = c0 - 416
                                kb.dma("sp", rkv_d[ti * 128:(ti + 1) * 128, cc:cc + 512], o[:, :], reads=[o], writes=[rkv_r[ti]])
                    elif kind == "F":
                        nblks = [(CT0, 0, 256)] + [(LT0 + i * 512, 256 + i * 512, 512) for i in range(4)]
                        for m, (m0, msz) in enumerate(((0, 128), (128, 128), (256, 128), (384, 32))):
                            func = (AF.Tanh, AF.Copy, AF.Sigmoid, AF.Sigmoid)[m]
                            for (off, dcol, nn) in nblks:
                                pp = ps1.get()
                                i = 0
                                for (sh, j) in shifts:
                                    for k in range(8):
                                        kb.op("pe", lambda e: e.matmul(pp[0:msz, 0:nn], lhsT=ws[j][:, k, m0:m0 + msz], rhs=hT[:, k, off + sh:off + sh + nn],
                                                                       start=(i == 0), stop=(i == 23)), [hT, ws[j]], [pp], inc=(i == 23))
                                        i += 1
                                kb.op("act", lambda e: e.activation(out=fm[0:msz, m, dcol:dcol + nn], in_=pp[0:msz, 0:nn], func=func), [pp], [fm])
                    else:
                        g0 = c0 - 2368
                        for m in range(4):
                            mt = g0 // 128 + m
                            for nb_ in range(4):
                                pp = ps1.get()
                                for k in range(8):
                                    kb.op("pe", lambda e: e.matmul(pp[:, :], lhsT=ws[0][:, k, m * 128:(m + 1) * 128],
                                                                   rhs=hT[:, k, LT0 + nb_ * 512:LT0 + (nb_ + 1) * 512], start=(k == 0), stop=(k == 7)),
                                          [hT, ws[0]], [pp], inc=(k == 7))
                                g = gsb.get()
                                kb.op("act", lambda e: e.activation(out=g[:], in_=pp[:, :], func=AF.Sigmoid), [pp], [g])
                                kb.dma("sp", gates_d[mt * 128:(mt + 1) * 128, nb_ * 512:(nb_ + 1) * 512], g[:], reads=[g], writes=[gates_r[mt][nb_]])
                if "fm_dbg" in dbg and b == 0:
                    kb.dma("sp", dbg_d["fm_dbg"][:, :, :], fm[:], reads=[fm], writes=[dbg_d["fm_dbg"]])
            if upto <= 2:
                kb.barrier(); fm_stack.close(); mix_stack.close()
                continue

            with kb.scope():
                Ybuf = kb.sb("Ybuf", [128, 16, 512], F32)
                halves = []
                for t2 in ps2.tiles:
                    for c0_ in (0, 512):
                        ht = Tile(t2.t[:, c0_:c0_ + 512], f"{t2.b.name}_h{c0_}")
                        ht.b.excl = True
                        halves.append(ht)
                pY_bank = halves[0]
                psP = Rot(halves[1:3])
                psR = Rot(ps1.tiles + halves[3:])
                bonus = kb.sb("bonus", [128, 16, 2, 8], F32)
                tri_f = kb.sb("tri_f", [128, 4, 128], F32)
                tri = kb.sb("tri", [128, 4, 128], BF16)
                kb.dma("sp", tri_f[:], I["c_tri"].ap(), writes=[tri_f])
                kb.op("dve", lambda e: e.tensor_copy(out=tri[:], in_=tri_f[:]), [tri_f], [tri])
                mask4 = kb.sb("mask4", [128, 2, 512], F32)
                maskt = kb.sb("maskt", [128, 2, 128], F32)
                for d in range(2):
                    kb.dma("sp", mask4[:, d, :], I["c_mask4"].ap()[d], writes=[mask4])
                    kb.dma("sp", maskt[:, d, :], I["c_maskt"].ap()[d], writes=[maskt])
                prm = kb.sb("prm", [128, 3, 512], F32)
                bcast_load(prm[:, 0, :], prm, I["k_k"].ap()[0:1, :], 128)
                bcast_load(prm[:, 1, :], prm, I["k_a"].ap()[0:1, :], 128)
                bcast_load(prm[:, 2, :], prm, I["r_k"].ap()[0:1, :], 128)
                w2f = kb.sb("w2f", [128, 2, 512], F32)
                w2b = kb.sb("w2b", [128, 2, 512], BF16)
                kb.dma("sp", w2f[:, 0, :], I["decay_w2"].ap().rearrange("d l c -> (d l) c"), writes=[w2f])
                kb.dma("sp", w2f[:, 1, :], I["aicl_a2"].ap().rearrange("d l c -> (d l) c"), writes=[w2f])
                kb.op("dve", lambda e: e.tensor_copy(out=w2b[:], in_=w2f[:]), [w2f], [w2b])
                b0f = kb.sb("b0f", [128, 2, 512], F32)
                b0b = kb.sb("b0b", [128, 2, 512], BF16)
                kb.op("pool", lambda e: e.memset(b0f[:], 0.0), [], [b0f])
                for d_ in range(2):
                    kb.dma("sp", b0f[d_ * 64:d_ * 64 + 1, 0, :], I["decay_w0"].ap()[d_:d_ + 1, :], writes=[b0f])
                    kb.dma("sp", b0f[d_ * 64:d_ * 64 + 1, 1, :], I["aicl_a0"].ap()[d_:d_ + 1, :], writes=[b0f])
                kb.op("dve", lambda e: e.tensor_copy(out=b0b[:], in_=b0f[:]), [b0f], [b0b])
                Hf = [kb.sb(f"Hf{p}", [128, 64], F32) for p in range(4)]
                Hb = [kb.sb(f"Hb{p}", [128, 64], BF16) for p in range(4)]
                rkv_rot = kb.rot("rkvt", [128, 1536], F32, 2)
                f512 = kb.rot("f512", [128, 512], F32, 9)
                b512 = kb.rot("b512", [128, 512], BF16, 14)
                fmT = kb.rot("fmT", [128, 4, 4, 128], BF16, 2)
                A4rot = kb.rot("A4", [128, 512], BF16, 8)
                XPa = kb.rot("XPa", [128, 512], BF16, 12)
                Wsb = kb.rot("Wsb", [128, 64], BF16, 10)
                Usb = kb.rot("Usb", [128, 512], BF16, 2)
                Vb = kb.rot("Vb", [128, 512], BF16, 2)
                ptot_rot = kb.rot("ptot", [128, 4], F32, 2)

                for d in range(rw_dirs):
                    for p in range(4):
                        kb.op("pool", lambda e: e.memset(Hf[p][:], 0.0), [], [Hf[p]])
                        kb.op("pool", lambda e: e.memset(Hb[p][:], 0.0), [], [Hb[p]])
                    order = list(range(18)) if d == 0 else [1, 0] + list(range(17, 1, -1))
                    def prep_gen(ti, c_):
                        lat = ti >= 2
                        rt = rkv_rot.get()
                        kb.dma("sp", rt[:], rkv_d[ti * 128:(ti + 1) * 128, :], reads=[rkv_r[ti]], writes=[rt])
                        r_ = rt[:, 0:512]
                        k_ = rt[:, 512:1024]
                        v_ = rt[:, 1024:1536]
                        kkr = f512.get(); sq = f512.get(); st = small_rot.get()
                        kb.op("dve", lambda e: e.tensor_tensor(out=kkr[:], in0=k_, in1=prm[:, 0, :], op=ALU.mult), [rt, prm], [kkr])
                        kb.op("act", lambda e: e.activation(out=sq[:], in_=kkr[:], func=AF.Square), [kkr], [sq])
                        kb.op("dve", lambda e: e.tensor_reduce(out=st[:, 0:8], in_=sq[:].rearrange("p (h c) -> p h c", h=8), axis=AX.X, op=ALU.add), [sq], [st])
                        rms_rstd(st[:, 0:8], st[:, 8:16], st, st, 1.0, 1e-12)
                        kap = f512.get()
                        kb.op("dve", lambda e: e.tensor_tensor(out=kap[:].rearrange("p (h c) -> p h c", h=8), in0=kkr[:].rearrange("p (h c) -> p h c", h=8),
                                                               in1=st[:, 8:16].unsqueeze(2).to_broadcast([128, 8, 64]), op=ALU.mult), [kkr, st], [kap])
                        yield
                        tcol = (ti * 128) if ti < 2 else (256 + (ti - 2) * 128)
                        pz = psP.get()
                        kb.op("pe", lambda e: e.matmul(pz[:, :], lhsT=fm[d * 64:(d + 1) * 64, 0, tcol:tcol + 128], rhs=w2b[d * 64:(d + 1) * 64, 0, :], start=True, stop=False),
                              [fm, w2b], [pz], inc=False)
                        kb.op("pe", lambda e: e.matmul(pz[:, :], lhsT=ones_b[d * 64:d * 64 + 1, 0:128], rhs=b0b[d * 64:d * 64 + 1, 0, :], start=False, stop=True), [ones_b, b0b], [pz])
                        sigb = b512.get()
                        kb.op("act", lambda e: e.activation(out=sigb[:], in_=pz[:, :], func=AF.Sigmoid), [pz], [sigb])
                        yield
                        pa = psP.get()
                        kb.op("pe", lambda e: e.matmul(pa[:, :], lhsT=fm[d * 64:(d + 1) * 64, 1, tcol:tcol + 128], rhs=w2b[d * 64:(d + 1) * 64, 1, :], start=True, stop=False),
                              [fm, w2b], [pa], inc=False)
                        kb.op("pe", lambda e: e.matmul(pa[:, :], lhsT=ones_b[d * 64:d * 64 + 1, 0:128], rhs=b0b[d * 64:d * 64 + 1, 1, :], start=False, stop=True), [ones_b, b0b], [pa])
                        a_ = f512.get()
                        kb.op("act", lambda e: e.activation(out=a_[:], in_=pa[:, :], func=AF.Sigmoid), [pa], [a_])
                        yield
                        t1 = f512.get(); ktl = f512.get(); beta = f512.get()
                        kb.op("dve", lambda e: e.scalar_tensor_tensor(out=t1[:], in0=a_[:], scalar=-1.0, in1=prm[:, 1, :], op0=ALU.add, op1=ALU.mult), [a_, prm], [t1])
                        kb.op("dve", lambda e: e.scalar_tensor_tensor(out=ktl[:], in0=t1[:], scalar=1.0, in1=k_, op0=ALU.add, op1=ALU.mult), [t1, rt], [ktl])
                        kb.op("pool", lambda e: e.tensor_tensor(out=beta[:], in0=a_[:], in1=kap[:], op=ALU.mult), [a_, kap], [beta])
                        yield
                        if lat:
                            kb.op("pool", lambda e: e.tensor_tensor(out=t1[:], in0=r_, in1=prm[:, 2, :], op=ALU.mult), [rt, prm], [t1])
                            kb.op("pool", lambda e: e.tensor_tensor(out=t1[:], in0=t1[:], in1=ktl[:], op=ALU.mult), [t1, ktl], [t1])
                            kb.op("dve", lambda e: e.tensor_reduce(out=bonus[:, ti - 2, d, :], in_=t1[:].rearrange("p (h c) -> p h c", h=8), axis=AX.X, op=ALU.add), [t1], [bonus])
                        ci, ce, cr = ((0, 1, 3) if d == 0 else (2, 3, 1))
                        eI = f512.get(); eE = f512.get(); eN = f512.get(); eR = f512.get()
                        pI = psP.get()
                        kb.op("pe", lambda e: e.matmul(pI[:, :], lhsT=tri[:, ci, :], rhs=sigb[:], start=True, stop=True), [tri, sigb], [pI])
                        kb.op("act", lambda e: e.activation(out=eI[:], in_=pI[:, :], func=AF.Exp, scale=C_DEC), [pI], [eI])
                        kb.op("act", lambda e: e.activation(out=eN[:], in_=pI[:, :], func=AF.Exp, scale=-C_DEC), [pI], [eN])
                        yield
                        pE = psP.get()
                        kb.op("pe", lambda e: e.matmul(pE[:, :], lhsT=tri[:, ce, :], rhs=sigb[:], start=True, stop=True), [tri, sigb], [pE])
                        kb.op("act", lambda e: e.activation(out=eE[:], in_=pE[:, :], func=AF.Exp, scale=C_DEC), [pE], [eE])
                        yield
                        pR = psP.get()
                        kb.op("pe", lambda e: e.matmul(pR[:, :], lhsT=tri[:, cr, :], rhs=sigb[:], start=True, stop=True), [tri, sigb], [pR])
                        kb.op("act", lambda e: e.activation(out=eR[:], in_=pR[:, :], func=AF.Exp, scale=C_DEC), [pR], [eR])
                        yield
                        pT = psP.get()
                        for p in range(4):
                            kb.op("pe", lambda e: e.matmul(pT[:, p:p + 1], lhsT=sigb[:, p * 128:(p + 1) * 128], rhs=ones_b[:, 0:1], start=True, stop=True), [sigb, ones_b], [pT])
                        ptot = ptot_rot.get()
                        kb.op("act", lambda e: e.activation(out=ptot[:], in_=pT[:, 0:4], func=AF.Exp, scale=C_DEC), [pT], [ptot])
                        yield
                        Rh = b512.get(); Ka = b512.get(); Bh = b512.get(); Kh = b512.get(); NBb = b512.get(); Kbb = b512.get()
                        kb.op("dve", lambda e: e.tensor_tensor(out=Rh[:], in0=r_, in1=eI[:], op=ALU.mult), [rt, eI], [Rh])
                        kb.op("pool", lambda e: e.tensor_tensor(out=Ka[:], in0=kap[:], in1=eE[:], op=ALU.mult), [kap, eE], [Ka])
                        yield
                        kb.op("dve", lambda e: e.tensor_tensor(out=Bh[:], in0=beta[:], in1=eN[:], op=ALU.mult), [beta, eN], [Bh])
                        kb.op("pool", lambda e: e.tensor_tensor(out=Kh[:], in0=ktl[:], in1=eN[:], op=ALU.mult), [ktl, eN], [Kh])
                        yield
                        kb.op("dve", lambda e: e.scalar_tensor_tensor(out=NBb[:], in0=beta[:], scalar=-1.0, in1=eR[:], op0=ALU.mult, op1=ALU.mult), [beta, eR], [NBb])
                        kb.op("pool", lambda e: e.tensor_tensor(out=Kbb[:], in0=ktl[:], in1=eR[:], op=ALU.mult), [ktl, eR], [Kbb])
                        vb = Vb.get()
                        kb.op("pool", lambda e: e.tensor_copy(out=vb[:], in_=v_), [rt], [vb])
                        yield
                        fT = fmT.get()
                        for wi, src in enumerate((Ka, Rh, Bh, Kh)):
                            transposes(src, [src[:, p * 128:(p + 1) * 128] for p in range(4)], fT[:, :, wi, :], fT, eng=("act" if wi % 2 else "dve"), pool=psP)
                            yield
                        c_.update(fT=fT, NBb=NBb, Kbb=Kbb, vb=vb, ptot=ptot, lat=lat)

                    def advance(g_, n_):
                        if g_ is None:
                            return
                        for _ in range(n_):
                            try:
                                next(g_)
                            except StopIteration:
                                return

                    def heads(ti, c_, nxt):
                        fT = c_['fT']; NBb = c_['NBb']; Kbb = c_['Kbb']; vb = c_['vb']; ptot = c_['ptot']; lat = c_['lat']
                        U = Usb.get()
                        pY = pY_bank if lat else None
                        HG = 8
                        for g0 in range(0, 8, HG):
                            hs = list(range(g0, g0 + HG))
                            P_ = {h: h // 2 for h in hs}
                            LO = {h: (h % 2) * 64 for h in hs}
                            HC = {h: slice(h * 64, (h + 1) * 64) for h in hs}
                            KaT = {h: fT[LO[h]:LO[h] + 64, P_[h], 0, :] for h in hs}
                            RT = {h: fT[LO[h]:LO[h] + 64, P_[h], 1, :] for h in hs}
                            BT = {h: fT[LO[h]:LO[h] + 64, P_[h], 2, :] for h in hs}
                            KT = {h: fT[LO[h]:LO[h] + 64, P_[h], 3, :] for h in hs}
                            KaRT = {h: fT[LO[h]:LO[h] + 64, P_[h], 0:2, :].rearrange("c w t -> c (w t)") for h in hs}
                            A4 = {}; Xt = {}; XP = {}; pp_ = {}; Wt = {}
                            NBK = len(psR.tiles)

                            def lockstep(pe_fn, ev_fn):
                                pend_ = []
                                for h_ in hs:
                                    if len(pend_) >= NBK:
                                        ev_fn(pend_.pop(0))
                                    pe_fn(h_)
                                    pend_.append(h_)
                                for h_ in pend_:
                                    ev_fn(h_)

                            def pe_A(h):
                                pA = psR.get(); pp_[h] = pA
                                kb.op("pe", lambda e: e.matmul(pA[:, 0:256], lhsT=BT[h], rhs=KaRT[h], start=True, stop=True), [fT], [pA], inc=False)
                                kb.op("pe", lambda e: e.matmul(pA[:, 256:512], lhsT=KT[h], rhs=KaRT[h], start=True, stop=True), [fT], [pA])

                            def ev_A(h):
                                A4[h] = A4rot.get()
                                kb.op("dve", lambda e: e.tensor_tensor(out=A4[h][:], in0=pp_[h][:, :], in1=mask4[:, d, :], op=ALU.mult), [pp_[h], mask4], [A4[h]])
                            lockstep(pe_A, ev_A)
                            advance(nxt, 2)

                            def pe_L(h):
                                pL = psR.get(); pp_[h] = pL
                                kb.op("pe", lambda e: e.matmul(pL[:, 0:128], lhsT=KaT[h], rhs=BT[h], start=True, stop=True), [fT], [pL])

                            def ev_L(h):
                                XP[h] = XPa.get()
                                kb.op("dve", lambda e: e.tensor_tensor(out=XP[h][:, 256:384], in0=pp_[h][:, 0:128], in1=maskt[:, d, :], op=ALU.mult), [pp_[h], maskt], [XP[h]])
                                kb.op("pool", lambda e: e.tensor_copy(out=XP[h][:, 0:128], in_=A4[h][:, 0:128]), [A4[h]], [XP[h]])
                                kb.op("pool", lambda e: e.tensor_tensor(out=XP[h][:, 128:256], in0=A4[h][:, 0:128], in1=ident_b[:], op=ALU.add), [A4[h], ident_b], [XP[h]])
                            lockstep(pe_L, ev_L)
                            advance(nxt, 2)

                            def xxt(ap_):
                                return ap_[:, 0:512].rearrange("p (a c) -> p a c", c=256)[:, :, 0:128] if ap_.shape[-1] >= 512 else None

                            for j in range(0, 7):
                                first = (j == 0)
                                last = (j == 6)

                                def pe_Bj(h):
                                    pB = psR.get(); pp_[h] = pB
                                    if first:
                                        kb.op("pe", lambda e: e.matmul(pB[:, 0:128], lhsT=XP[h][:, 256:384], rhs=XP[h][:, 0:128], start=True, stop=True), [XP[h]], [pB], inc=False)
                                        kb.op("pe", lambda e: e.matmul(pB[:, 256:384], lhsT=XP[h][:, 0:128], rhs=XP[h][:, 256:384], start=True, stop=True), [XP[h]], [pB])
                                    elif not last:
                                        kb.op("pe", lambda e: e.matmul(pB[:, 0:256], lhsT=XP[h][:, 256:384], rhs=XP[h][:, 0:256], start=True, stop=True), [XP[h]], [pB], inc=False)
                                        kb.op("pe", lambda e: e.matmul(pB[:, 256:384], lhsT=XP[h][:, 0:128], rhs=XP[h][:, 256:384], start=True, stop=True), [XP[h]], [pB])
                                    else:
                                        kb.op("pe", lambda e: e.matmul(pB[:, 128:256], lhsT=XP[h][:, 256:384], rhs=XP[h][:, 128:256], start=True, stop=True), [XP[h]], [pB])

                                def ev_Bj(h):
                                    pB = pp_[h]
                                    XP2 = XPa.get()
                                    if first:
                                        kb.op("pool", lambda e: e.tensor_copy(out=XP2[:, 128:256], in_=XP[h][:, 128:256]), [XP[h]], [XP2])
                                    else:
                                        kb.op("dve", lambda e: e.tensor_tensor(out=XP2[:, 128:256], in0=pB[:, 128:256], in1=XP[h][:, 128:256], op=ALU.add), [pB, XP[h]], [XP2])
                                    if not last:
                                        src_ = pB[:, 0:512].rearrange("p (a c) -> p a c", c=256)[:, :, 0:128]
                                        dst_ = XP2[:, 0:512].rearrange("p (a c) -> p a c", c=256)[:, :, 0:128]
                                        kb.op("act", lambda e: e.copy(out=dst_, in_=src_), [pB], [XP2])
                                    XP[h] = XP2
                                lockstep(pe_Bj, ev_Bj)
                                advance(nxt, 2)

                            def pe_W(h):
                                pW = psR.get(); pp_[h] = pW
                                kb.op("pe", lambda e: e.matmul(pW[:, 0:64], lhsT=KaT[h], rhs=Hb[P_[h]][LO[h]:LO[h] + 64, :], start=True, stop=False), [fT, Hb[P_[h]]], [pW], inc=False)
                                kb.op("pe", lambda e: e.matmul(pW[:, 0:64], lhsT=A4[h][:, 256:384], rhs=vb[:, HC[h]], start=False, stop=True), [A4[h], vb], [pW])

                            def ev_W(h):
                                Wt[h] = Wsb.get()
                                kb.op("act", lambda e: e.copy(out=Wt[h][:], in_=pp_[h][:, 0:64]), [pp_[h]], [Wt[h]])
                            lockstep(pe_W, ev_W)
                            advance(nxt, 1)

                            def pe_U(h):
                                pU = psR.get(); pp_[h] = pU
                                kb.op("pe", lambda e: e.matmul(pU[:, 0:64], lhsT=XP[h][:, 128:256], rhs=Wt[h][:], start=True, stop=True), [XP[h], Wt[h]], [pU])

                            def ev_U(h):
                                kb.op("dve" if h % 2 else "act", (lambda e: e.tensor_copy(out=U[:, HC[h]], in_=pp_[h][:, 0:64])) if h % 2 else
                                      (lambda e: e.copy(out=U[:, HC[h]], in_=pp_[h][:, 0:64])), [pp_[h]], [U])
                            lockstep(pe_U, ev_U)
                            advance(nxt, 1)
                            if lat:
                                for h in hs:
                                    kb.op("pe", lambda e: e.matmul(pY[:, HC[h]], lhsT=RT[h], rhs=Hb[P_[h]][LO[h]:LO[h] + 64, :], start=True, stop=False), [fT, Hb[P_[h]]], [pY], inc=False)
                                    kb.op("pe", lambda e: e.matmul(pY[:, HC[h]], lhsT=A4[h][:, 128:256], rhs=U[:, HC[h]], start=False, stop=False), [A4[h], U], [pY], inc=False)
                                    kb.op("pe", lambda e: e.matmul(pY[:, HC[h]], lhsT=A4[h][:, 384:512], rhs=vb[:, HC[h]], start=False, stop=True), [A4[h], vb], [pY])
                            pHs = {}
                            for p in sorted(set(P_.values())):
                                pc = slice(p * 128, (p + 1) * 128)
                                pH = psR.get(); pHs[p] = pH
                                kb.op("pe", lambda e: e.matmul(pH[:, 0:128], lhsT=NBb[:, pc], rhs=U[:, pc], start=True, stop=False), [NBb, U], [pH], inc=False)
                                kb.op("pe", lambda e: e.matmul(pH[:, 0:128], lhsT=Kbb[:, pc], rhs=vb[:, pc], start=False, stop=True), [Kbb, vb], [pH])
                            for p in sorted(set(P_.values())):
                                pH = pHs[p]
                                for hh in range(2):
                                    l2 = hh * 64
                                    kb.op("dve", lambda e: e.scalar_tensor_tensor(out=Hf[p][l2:l2 + 64, :], in0=Hf[p][l2:l2 + 64, :], scalar=ptot[l2:l2 + 64, p:p + 1],
                                                                                  in1=pH[l2:l2 + 64, l2:l2 + 64], op0=ALU.mult, op1=ALU.add), [Hf[p], ptot, pH], [Hf[p]])
                                kb.op("act", lambda e: e.copy(out=Hb[p][:], in_=Hf[p][:]), [Hf[p]], [Hb[p]])
                        if lat:
                            if d == 0:
                                kb.op("act", lambda e: e.copy(out=Ybuf[:, ti - 2, :], in_=pY[:, 0:512]), [pY], [Ybuf])
                            else:
                                kb.op("dve", lambda e: e.tensor_tensor(out=Ybuf[:, ti - 2, :], in0=pY[:, 0:512], in1=Ybuf[:, ti - 2, :], op=ALU.add), [pY, Ybuf], [Ybuf])

                    tl_ = order[:rw_tiles]
                    ctxs_ = [dict() for _ in tl_]
                    gens_ = [prep_gen(ti, ctxs_[n]) for n, ti in enumerate(tl_)]
                    advance(gens_[0], 10 ** 6)
                    for n, ti in enumerate(tl_):
                        nxt_ = gens_[n + 1] if n + 1 < len(tl_) else None
                        heads(ti, ctxs_[n], nxt_)
                        advance(nxt_, 10 ** 6)
                if "y_dbg" in dbg and b == 0:
                    kb.dma("sp", dbg_d["y_dbg"][:, :, :], Ybuf[:], reads=[Ybuf], writes=[dbg_d["y_dbg"]])
                gnw = kb.sb("gnw", [128, 2, 512], F32)
                bcast_load(gnw[:, 0, :], gnw, I["gn_w"].ap()[0:1, :], 128)
                bcast_load(gnw[:, 1, :], gnw, I["gn_b"].ap()[0:1, :], 128)
                g2f = w2f
                g2b = kb.sb("g2b", [128, 2, 512], BF16)
                kb.dma("sp", g2f[:, 0, :], I["gate_g2"].ap()[0:128, :], reads=[], writes=[g2f])
                kb.dma("sp", g2f[0:32, 1, :], I["gate_g2"].ap()[128:160, :], writes=[g2f])
                kb.op("dve", lambda e: e.tensor_copy(out=g2b[:, 0, :], in_=g2f[:, 0, :]), [g2f], [g2b])
                kb.op("dve", lambda e: e.tensor_copy(out=g2b[0:32, 1, :], in_=g2f[0:32, 1, :]), [g2f], [g2b])
                rwb = kb.rot("rwb", [128, 512], BF16, 2)
                def tileRO(t):
                        ti = t + 2
                        rt = rkv_rot.get()
                        kb.dma("sp", rt[:], rkv_d[ti * 128:(ti + 1) * 128, :], reads=[rkv_r[ti]], writes=[rt])
                        v_ = rt[:, 1024:1536]
                        y3 = Ybuf[:, t, :].rearrange("p (h c) -> p h c", h=8)
                        st = small_rot.get(); st2 = small_rot.get()
                        cen = f512.get(); sq = f512.get(); yn = f512.get()
                        cen3 = cen[:].rearrange("p (h c) -> p h c", h=8)
                        kb.op("dve", lambda e: e.tensor_reduce(out=st[:, 0:8], in_=y3, axis=AX.X, op=ALU.add), [Ybuf], [st])
                        kb.op("dve", lambda e: e.tensor_scalar(out=st[:, 0:8], in0=st[:, 0:8], scalar1=-1.0 / 64, scalar2=None, op0=ALU.mult), [st], [st])
                        kb.op("dve", lambda e: e.tensor_tensor(out=cen3, in0=y3, in1=st[:, 0:8].unsqueeze(2).to_broadcast([128, 8, 64]), op=ALU.add), [Ybuf, st], [cen])
                        yield
                        kb.op("act", lambda e: e.activation(out=sq[:], in_=cen[:], func=AF.Square), [cen], [sq])
                        kb.op("dve", lambda e: e.tensor_reduce(out=st2[:, 0:8], in_=sq[:].rearrange("p (h c) -> p h c", h=8), axis=AX.X, op=ALU.add), [sq], [st2])
                        yield
                        rms_rstd(st2[:, 0:8], st2[:, 8:16], st2, st2, 64, GN_EPS)
                        kb.op("dve", lambda e: e.tensor_tensor(out=yn[:].rearrange("p (h c) -> p h c", h=8), in0=cen3, in1=st2[:, 8:16].unsqueeze(2).to_broadcast([128, 8, 64]), op=ALU.mult),
                              [cen, st2], [yn])
                        kb.op("pool", lambda e: e.tensor_tensor(out=yn[:], in0=yn[:], in1=gnw[:, 0, :], op=ALU.mult), [yn, gnw], [yn])
                        kb.op("pool", lambda e: e.tensor_tensor(out=yn[:], in0=yn[:], in1=gnw[:, 1, :], op=ALU.add), [yn, gnw], [yn])
                        yield
                        kb.op("dve", lambda e: e.tensor_tensor(out=st[:, 8:16], in0=bonus[:, t, 0, :], in1=bonus[:, t, 1, :], op=ALU.add), [bonus], [st])
                        kb.op("dve", lambda e: e.tensor_tensor(out=sq[:].rearrange("p (h c) -> p h c", h=8), in0=v_.rearrange("p (h c) -> p h c", h=8),
                                                               in1=st[:, 8:16].unsqueeze(2).to_broadcast([128, 8, 64]), op=ALU.mult), [rt, st], [sq])
                        kb.op("pool", lambda e: e.tensor_tensor(out=yn[:], in0=yn[:], in1=sq[:], op=ALU.add), [yn, sq], [yn])
                        yield
                        pg = ps1.get()
                        tcol = 256 + t * 128
                        kb.op("pe", lambda e: e.matmul(pg[:, :], lhsT=fm[:, 2, tcol:tcol + 128], rhs=g2b[:, 0, :], start=True, stop=False), [fm, g2b], [pg], inc=False)
                        kb.op("pe", lambda e: e.matmul(pg[:, :], lhsT=fm[0:32, 3, tcol:tcol + 128], rhs=g2b[0:32, 1, :], start=False, stop=True), [fm, g2b], [pg])
                        ro = rwb.get()
                        kb.op("dve", lambda e: e.tensor_tensor(out=ro[:], in0=pg[:, :], in1=yn[:], op=ALU.mult), [pg, yn], [ro])
                        transposes(ro, [ro[:, k * 128:(k + 1) * 128] for k in range(4)], rwT[:, :, t * 128:(t + 1) * 128], rwT)
                run_interleaved((tileRO(t) for t in range(16)), 2)
                if "rw_dbg" in dbg and b == 0:
                    kb.dma("sp", dbg_d["rw_dbg"][:, :, :], rwT[:], reads=[rwT], writes=[dbg_d["rw_dbg"]])
            kb.barrier()
            fm_stack.close()
            if upto <= 3:
                mix_stack.close()
                continue

            with kb.scope():
                qT = kb.sb("qT", [96, 8, 2048], BF16)
                kT = kb.sb("kT", [96, 8, 2304], BF16)
                Vall = kb.sb("Vall", [128, 18, 8, 65], BF16)
                kb.op("pool", lambda e: e.memset(Vall[:, :, :, 64:65], 1.0), [], [Vall])
                qln = kb.sb("qln", [128, 256], F32)
                kvln = kb.sb("kvln", [128, 128], F32)
                qnw = kb.sb("qnw", [128, 8, 96], F32)
                knw = kb.sb("knw", [128, 8, 96], F32)
                bcast_load(qln[:], qln, I["q_lat_norm"].ap()[0:1, :], 128)
                bcast_load(kvln[:], kvln, I["kv_lat_norm"].ap()[0:1, :], 128)
                for h in range(8):
                    bcast_load(qnw[:, h, :], qnw, I["q_norm"].ap()[0:1, :], 128)
                    bcast_load(knw[:, h, :], knw, I["k_norm"].ap()[0:1, :], 128)
                kb.op("dve", lambda e: e.tensor_scalar(out=qnw[:], in0=qnw[:], scalar1=ATTN_SCALE, scalar2=None, op0=ALU.mult), [qnw], [qnw])
                wuq_f = kb.sb("wuq_f", [128, 2, 768], F32)
                wuq = kb.sb("wuq", [128, 2, 768], BF16)
                kb.dma("sp", wuq_f[:], I["w_uq"].ap().rearrange("(k p) n -> p k n", p=128), writes=[wuq_f])
                kb.op("pool", lambda e: e.tensor_copy(out=wuq[:], in_=wuq_f[:]), [wuq_f], [wuq])
                wukv_f = kb.sb("wukv_f", [128, 1024], F32)
                wukv = kb.sb("wukv", [128, 1024], BF16)
                kb.dma("sp", wukv_f[:], I["w_ukv"].ap(), writes=[wukv_f])
                kb.op("pool", lambda e: e.tensor_copy(out=wukv[:], in_=wukv_f[:]), [wukv_f], [wukv])
                mrot = kb.rot("mt", [128, 416], F32, 2)
                cs_rot = kb.rot("cs", [128, 2, 16], F32, 2)
                t768 = kb.rot("t768", [128, 8, 96], F32, 8)
                small_m = kb.rot("small_m", [128, 16], F32, 10)
                tb768 = kb.rot("tb768", [128, 8, 96], BF16, 4)
                tbn = kb.rot("tbn", [128, 256], BF16, 4)
                tTn = kb.rot("tTn", [128, 2, 128], BF16, 4)
                rope_t = kb.rot("rope_t", [128, 8, 16], F32, 16)

                def head_norm_rope(src, dst_b, gain, cs, n_extra_ssq=None):
                    sq = t768.get()
                    st = small_m.get()
                    kb.op("act", lambda e: e.activation(out=sq[:], in_=src[:], func=AF.Square), [src], [sq])
                    kb.op("dve", lambda e: e.tensor_reduce(out=st[:, 0:8], in_=sq[:], axis=AX.X, op=ALU.add), [sq], [st])
                    yield
                    rms_rstd(st[:, 0:8], st[:, 8:16], st, st, 96, EPS)
                    kb.op("dve", lambda e: e.tensor_tensor(out=sq[:], in0=src[:], in1=st[:, 8:16].unsqueeze(2).to_broadcast([128, 8, 96]), op=ALU.mult),
                          [src, st], [sq])
                    yield
                    if cs is None:
                        kb.op("pool", lambda e: e.tensor_tensor(out=dst_b[:], in0=sq[:], in1=gain[:], op=ALU.mult), [sq, gain], [dst_b])
                        return
                    kb.op("pool", lambda e: e.tensor_tensor(out=sq[:], in0=sq[:], in1=gain[:], op=ALU.mult), [sq, gain], [sq])
                    kb.op("pool", lambda e: e.tensor_copy(out=dst_b[:, :, 0:64], in_=sq[:, :, 0:64]), [sq], [dst_b])
                    cb_ = cs[:, 0, :].unsqueeze(1).to_broadcast([128, 8, 16])
                    sb_ = cs[:, 1, :].unsqueeze(1).to_broadcast([128, 8, 16])
                    x1 = sq[:, :, 64:80]
                    x2 = sq[:, :, 80:96]
                    ta = rope_t.get(); tb_ = rope_t.get(); tc_ = rope_t.get(); td = rope_t.get()
                    yield
                    kb.op("dve", lambda e: e.tensor_tensor(out=ta[:], in0=x1, in1=cb_, op=ALU.mult), [sq, cs], [ta])
                    kb.op("dve", lambda e: e.tensor_tensor(out=tb_[:], in0=x2, in1=sb_, op=ALU.mult), [sq, cs], [tb_])
                    kb.op("pool", lambda e: e.tensor_tensor(out=tc_[:], in0=x1, in1=sb_, op=ALU.mult), [sq, cs], [tc_])
                    kb.op("pool", lambda e: e.tensor_tensor(out=td[:], in0=x2, in1=cb_, op=ALU.mult), [sq, cs], [td])
                    kb.op("dve", lambda e: e.tensor_tensor(out=dst_b[:, :, 64:80], in0=ta[:], in1=tb_[:], op=ALU.subtract), [ta, tb_], [dst_b])
                    kb.op("pool", lambda e: e.tensor_tensor(out=dst_b[:, :, 80:96], in0=tc_[:], in1=td[:], op=ALU.add), [tc_, td], [dst_b])

                def mla_tile(ti):
                        lat = ti >= 2
                        mt_ = mrot.get()
                        kb.dma("sp", mt_[:], mla_d[ti * 128:(ti + 1) * 128, :], reads=[mla_r[ti]], writes=[mt_])
                        cs = None
                        if lat:
                            cs = cs_rot.get()
                            lt_ = ti - 2
                            kb.dma("sp", cs[:, 0, :], I["c_cos"].ap()[lt_ * 128:(lt_ + 1) * 128, :], writes=[cs])
                            kb.dma("sp", cs[:, 1, :], I["c_sin"].ap()[lt_ * 128:(lt_ + 1) * 128, :], writes=[cs])
                        junk = junk_rot.get()
                        st = small_m.get()
                        kb.op("act", lambda e: e.activation(out=junk[:, 0:128], in_=mt_[:, 256:384], func=AF.Square, accum_out=st[:, 0:1]), [mt_], [junk, st])
                        kb.op("act", lambda e: e.activation(out=junk[:, 128:160], in_=mt_[:, 384:416], func=AF.Square, accum_out=st[:, 2:3]), [mt_], [junk, st])
                        rms_rstd(st[:, 0:1], st[:, 1:2], st, st, 128, EPS)
                        kvn = tbn.get()
                        kb.op("dve", lambda e: e.scalar_tensor_tensor(out=kvn[:, 0:128], in0=mt_[:, 256:384], scalar=st[:, 1:2], in1=kvln[:], op0=ALU.mult, op1=ALU.mult),
                              [mt_, st, kvln], [kvn])
                        yield
                        kvT = tTn.get()
                        transposes(kvn, [kvn[:, 0:128]], kvT[:, 0, :], kvT)
                        pk = ps2.get()
                        for nb_ in range(2):
                            kb.op("pe", lambda e: e.matmul(pk[:, nb_ * 512:(nb_ + 1) * 512], lhsT=kvT[:, 0, :], rhs=wukv[:, nb_ * 512:(nb_ + 1) * 512], start=True, stop=True),
                                  [kvT, wukv], [pk])
                        pk3 = pk[:].rearrange("p (h c) -> p h c", h=8)
                        kb.op("act", lambda e: e.copy(out=Vall[:, ti, :, 0:64], in_=pk3[:, :, 64:128]), [pk], [Vall])
                        kf = t768.get()
                        kb.op("dve", lambda e: e.tensor_copy(out=kf[:, :, 0:64], in_=pk3[:, :, 0:64]), [pk], [kf])
                        kb.op("pool", lambda e: e.tensor_copy(out=kf[:, :, 64:96], in_=mt_[:, 384:416].unsqueeze(1).to_broadcast([128, 8, 32])), [mt_], [kf])
                        yield
                        kbf = tb768.get()
                        yield from head_norm_rope(kf, kbf, knw, cs)
                        yield
                        transposes(kbf, [kbf[:, h, :] for h in range(8)], kT[:, :, ti * 128:(ti + 1) * 128], kT, eng="dve")
                        yield
                        if lat:
                            st2 = small_m.get()
                            kb.op("act", lambda e: e.activation(out=junk[:, 256:512], in_=mt_[:, 0:256], func=AF.Square, accum_out=st2[:, 0:1]), [mt_], [junk, st2])
                            rms_rstd(st2[:, 0:1], st2[:, 1:2], st2, st2, 256, EPS)
                            qn = tbn.get()
                            kb.op("dve", lambda e: e.scalar_tensor_tensor(out=qn[:], in0=mt_[:, 0:256], scalar=st2[:, 1:2], in1=qln[:], op0=ALU.mult, op1=ALU.mult),
                                  [mt_, st2, qln], [qn])
                            yield
                            qnT = tTn.get()
                            transposes(qn, [qn[:, 0:128], qn[:, 128:256]], qnT[:], qnT)
                            pq = ps2.get()
                            for (n0, n1) in ((0, 512), (512, 768)):
                                for k in range(2):
                                    kb.op("pe", lambda e: e.matmul(pq[:, n0:n1], lhsT=qnT[:, k, :], rhs=wuq[:, k, n0:n1], start=(k == 0), stop=(k == 1)),
                                          [qnT, wuq], [pq], inc=(k == 1))
                            qf = t768.get()
                            kb.op("act", lambda e: e.copy(out=qf[:].rearrange("p h c -> p (h c)"), in_=pq[:, 0:768]), [pq], [qf])
                            yield
                            qbf = tb768.get()
                            yield from head_norm_rope(qf, qbf, qnw, cs)
                            yield
                            transposes(qbf, [qbf[:, h, :] for h in range(8)], qT[:, :, lt_ * 128:(lt_ + 1) * 128], qT, eng="dve")

                run_interleaved((mla_tile(ti) for ti in range(18)), 2)
                if "qT_dbg" in dbg and b == 0:
                    kb.dma("sp", dbg_d["qT_dbg"][:, :, :], qT[:], reads=[qT], writes=[dbg_d["qT_dbg"]])
                    kb.dma("sp", dbg_d["kT_dbg"][:, :, :], kT[:], reads=[kT], writes=[dbg_d["kT_dbg"]])
                Erot = kb.rot("E", [128, 512], BF16, 3)
                apair = kb.rot("apair", [128, 4, 128], BF16, 2)
                for hp in range(4):
                    for qb in range(4):
                        ap_ = apair.get()
                        for hh in range(2):
                            h = hp * 2 + hh
                            po = [ps2.get(), ps2.get()]
                            pSd = {}

                            def emitS(kt_):
                                pS_ = ps1.get()
                                kb.op("pe", lambda e: e.matmul(pS_[:, :], lhsT=kT[:, h, kt_ * 128:(kt_ + 1) * 128], rhs=qT[:, h, qb * 512:(qb + 1) * 512], start=True, stop=True),
                                      [kT, qT], [pS_])
                                pSd[kt_] = pS_
                            emitS(0)
                            for kt in range(18):
                                if kt + 1 < 18:
                                    emitS(kt + 1)
                                pS = pSd.pop(kt)
                                E = Erot.get()
                                kb.op("act", lambda e: e.activation(out=E[:], in_=pS[:, :], func=AF.Exp), [pS], [E])
                                for qs in range(4):
                                    p_ = po[qs // 2]
                                    c0 = (qs % 2) * 512
                                    kb.op("pe", lambda e: e.matmul(p_[:, c0:c0 + 65], lhsT=E[:, qs * 128:(qs + 1) * 128], rhs=Vall[:, kt, h, :],
                                                                   start=(kt == 0), stop=(kt == 17)), [E, Vall], [p_], inc=(kt == 17))
                            for qs in range(4):
                                p_ = po[qs // 2]
                                c0 = (qs % 2) * 512
                                st = small_rot.get()
                                kb.op("dve", lambda e: e.reciprocal(out=st[:, 0:1], in_=p_[:, c0 + 64:c0 + 65]), [p_], [st])
                                kb.op("dve", lambda e: e.tensor_scalar(out=ap_[:, qs, hh * 64:(hh + 1) * 64], in0=p_[:, c0:c0 + 64], scalar1=st[:, 0:1], scalar2=None,
                                                                       op0=ALU.mult), [p_, st], [ap_])
                        transposes(ap_, [ap_[:, qs, :] for qs in range(4)], attT[:, hp, qb * 512:(qb + 1) * 512], attT)
                if "att_dbg" in dbg and b == 0:
                    kb.dma("sp", dbg_d["att_dbg"][:, :, :], attT[:], reads=[attT], writes=[dbg_d["att_dbg"]])
            if upto <= 4:
                mix_stack.close()
                continue

            with kb.scope():
                wo_f = kb.sb("wo_f", [128, 8, 1024], F32)
                wo1 = kb.sb("wo1", [128, 4, 1024], BF16)
                wo2 = kb.sb("wo2", [128, 4, 1024], BF16)
                wo3 = kb.sb("wo3", [128, 8, 1024], BF16)
                kb.dma("sp", wo_f[:, 0:4, :], I["w_o_mla"].ap().rearrange("(k p) n -> p k n", p=128), writes=[wo_f])
                kb.op("pool", lambda e: e.tensor_copy(out=wo1[:], in_=wo_f[:, 0:4, :]), [wo_f], [wo1])
                kb.dma("sp", wo_f[:, 0:4, :], I["w_o_rwkv"].ap().rearrange("(k p) n -> p k n", p=128), reads=[wo_f], writes=[wo_f])
                kb.op("pool", lambda e: e.tensor_copy(out=wo2[:], in_=wo_f[:, 0:4, :]), [wo_f], [wo2])
                kb.dma("sp", wo_f[:], I["w_out"].ap().rearrange("(k p) n -> p k n", p=128), reads=[wo_f], writes=[wo_f])
                kb.op("pool", lambda e: e.tensor_copy(out=wo3[:], in_=wo_f[:]), [wo_f], [wo3])
                mT = kb.sb("mT", [128, 8, 2048], BF16)
                grot = kb.rot("gt", [128, 2, 512], BF16, 2)
                trot = kb.rot("tm", [128, 512], F32, 2)
                for m in range(8):
                    for nb_ in range(4):
                        g = grot.get()
                        kb.dma("sp", g[:, 0, :], gates_d[m * 128:(m + 1) * 128, nb_ * 512:(nb_ + 1) * 512], reads=[gates_r[m][nb_]], writes=[g])
                        kb.dma("sp", g[:, 1, :], gates_d[(8 + m) * 128:(9 + m) * 128, nb_ * 512:(nb_ + 1) * 512], reads=[gates_r[8 + m][nb_]], writes=[g])
                        p1 = ps1.get(); p2 = ps1.get()
                        for k in range(4):
                            kb.op("pe", lambda e: e.matmul(p1[:, :], lhsT=wo1[:, k, m * 128:(m + 1) * 128], rhs=attT[:, k, nb_ * 512:(nb_ + 1) * 512], start=(k == 0), stop=(k == 3)),
                                  [wo1, attT], [p1], inc=(k == 3))
                        for k in range(4):
                            kb.op("pe", lambda e: e.matmul(p2[:, :], lhsT=wo2[:, k, m * 128:(m + 1) * 128], rhs=rwT[:, k, nb_ * 512:(nb_ + 1) * 512], start=(k == 0), stop=(k == 3)),
                                  [wo2, rwT], [p2], inc=(k == 3))
                        t1 = trot.get(); t2 = trot.get()
                        kb.op("dve", lambda e: e.tensor_tensor(out=t1[:], in0=p1[:, :], in1=g[:, 0, :], op=ALU.mult), [p1, g], [t1])
                        kb.op("dve", lambda e: e.tensor_tensor(out=t2[:], in0=p2[:, :], in1=g[:, 1, :], op=ALU.mult), [p2, g], [t2])
                        kb.op("pool", lambda e: e.tensor_tensor(out=mT[:, m, nb_ * 512:(nb_ + 1) * 512], in0=t1[:], in1=t2[:], op=ALU.add), [t1, t2], [mT])
                Ga = kb.sb("Ga", [128, 1024], F32)
                bcast_load(Ga[:], Ga, mod_d.t.ap()[b:b + 1, 2048:3072], 128, reads=[mod_d])
                xrot = kb.rot("xt5", [128, 1024], F32, 2)
                for t in range(16):
                    xt = xrot.get()
                    kb.dma("sp", xt[:], I["x"].ap()[b, t * 128:(t + 1) * 128, :], writes=[xt])
                    pm = ps2.get()
                    for nb_ in range(2):
                        for k in range(8):
                            kb.op("pe", lambda e: e.matmul(pm[:, nb_ * 512:(nb_ + 1) * 512], lhsT=mT[:, k, t * 128:(t + 1) * 128], rhs=wo3[:, k, nb_ * 512:(nb_ + 1) * 512],
                                                           start=(k == 0), stop=(k == 7)), [mT, wo3], [pm], inc=(k == 7))
                    junk = junk_rot.get()
                    kb.op("dve", lambda e: e.tensor_tensor(out=junk[:], in0=pm[:, :], in1=Ga[:], op=ALU.mult), [pm, Ga], [junk])
                    kb.op("pool", lambda e: e.tensor_tensor(out=xt[:], in0=junk[:], in1=xt[:], op=ALU.add), [junk, xt], [xt])
                    kb.dma("sp", x1_d[(b * 16 + t) * 128:(b * 16 + t + 1) * 128, :], xt[:], reads=[xt], writes=[x1_r[b * 16 + t]])
            kb.barrier()
            mix_stack.close()
            if upto <= 5:
                continue

            pass

        if upto > 5:
          NT = 16 * nbatch
          NBLK = NT * 4 + 256
          with kb.scope():
            I32 = mybir.dt.int32
            u_d = kb.dram("u_d", [NT * 128, 1024], BF16)
            u_r = [Buf() for _ in range(NT)]
            xs_d = kb.dram("xs_d", [NBLK * 256, 1024], BF16)
            Y_d = kb.dram("Y_d", [NBLK * 256, 1024], BF16)
            junk_rot = kb.rot("junk6", [128, 1024], F32, 3)
            small_rot = kb.rot("small6", [128, 16], F32, 8)
            dest_all = kb.sb("dest_all", [128, NT, 8], I32)
            gate_all = kb.sb("gate_all", [128, NT, 8], F32)
            IDXi = kb.sb("IDXi", [128, NBLK], I32)
            tri_f = kb.sb("tri6_f", [128, 4, 128], F32)
            tri = kb.sb("tri6", [128, 4, 128], BF16)
            kb.dma("sp", tri_f[:], I["c_tri"].ap(), writes=[tri_f])
            kb.op("dve", lambda e: e.tensor_copy(out=tri[:], in_=tri_f[:]), [tri_f], [tri])
            iota_p = kb.sb("iota_p", [128, 2], F32)
            kb.dma("sp", iota_p[:], I["c_iotap"].ap(), writes=[iota_p])
            with kb.scope():
                G_all = kb.sb("G_all", [128, NT, 256], F32)
                POS_all = kb.sb("POS_all", [128, NT, 256], F32)
                mask_all = kb.sb("mask_all", [128, NT, 256], BF16)
                cnt = kb.sb("cnt", [128, 256], F32)
                kb.op("pool", lambda e: e.memset(cnt[:], 0.0), [], [cnt])
                with kb.scope():
                    rw_f = kb.sb("rw_f", [128, 8, 256], F32)
                    kb.dma("sp", rw_f[:], I["router_w"].ap().rearrange("(k p) n -> p k n", p=128), writes=[rw_f])
                    rbias = kb.sb("rbias", [128, 256], F32)
                    bcast_load(rbias[:], rbias, I["router_bias"].ap()[0:1, :], 128)
                    xrot = kb.rot("xt6", [128, 1024], F32, 3)
                    ufr = kb.rot("uf", [128, 1024], F32, 3)
                    ubr = kb.rot("ub", [128, 1024], BF16, 3)
                    uTf = kb.rot("uTf", [128, 8, 128], F32, 3)
                    s256 = kb.rot("s256", [128, 256], F32, 12)
                    m8r = kb.rot("m8", [128, 8], F32, 16)
                    A2s = []
                    for b in range(nbatch):
                        A2 = kb.sb("A2", [128, 1024], F32)
                        S2 = kb.sb("S2", [128, 1024], F32)
                        bcast_load(A2[:], A2, mod_d.t.ap()[b:b + 1, 4096:5120], 128, reads=[mod_d])
                        bcast_load(S2[:], S2, mod_d.t.ap()[b:b + 1, 3072:4096], 128, reads=[mod_d])
                        A2s.append((A2, S2))
                    def tileA(i):
                            b = i // 16
                            A2, S2 = A2s[b]
                            xt = xrot.get()
                            kb.dma("sp", xt[:], x1_d[i * 128:(i + 1) * 128, :], reads=[x1_r[i]], writes=[xt])
                            junk = junk_rot.get(); ssq = small_rot.get()
                            kb.op("act", lambda e: e.activation(out=junk[:], in_=xt[:], func=AF.Square, accum_out=ssq[:, 0:1]), [xt], [junk, ssq])
                            rms_rstd(ssq[:, 0:1], ssq[:, 1:2], ssq, ssq, 1024, EPS)
                            yield
                            uf = ufr.get()
                            kb.op("dve", lambda e: e.scalar_tensor_tensor(out=junk[:], in0=xt[:], scalar=ssq[:, 1:2], in1=A2[:], op0=ALU.mult, op1=ALU.mult), [xt, ssq, A2], [junk])
                            kb.op("pool", lambda e: e.tensor_tensor(out=uf[:], in0=junk[:], in1=S2[:], op=ALU.add), [junk, S2], [uf])
                            ub = ubr.get()
                            kb.op("act", lambda e: e.copy(out=ub[:], in_=uf[:]), [uf], [ub])
                            kb.dma("sp", u_d[i * 128:(i + 1) * 128, :], ub[:], reads=[ub], writes=[u_r[i]])
                            yield
                            utf = uTf.get()
                            for half in range(2):
                                transposes(uf, [uf[:, (half * 4 + k) * 128:(half * 4 + k + 1) * 128] for k in range(4)], utf[:, half * 4:(half + 1) * 4, :], utf, dt=F32, eng="dve")
                            pr = ps1.get()
                            for k in range(8):
                                kb.op("pe", lambda e: e.matmul(pr[:, 0:256], lhsT=utf[:, k, :], rhs=rw_f[:, k, :], start=(k == 0), stop=(k == 7)), [utf, rw_f], [pr], inc=(k == 7))
                            sc = s256.get(); sel = s256.get(); w1_ = s256.get(); w2_ = s256.get()
                            kb.op("act", lambda e: e.activation(out=sc[:], in_=pr[:, 0:256], func=AF.Sigmoid), [pr], [sc])
                            kb.op("pool", lambda e: e.tensor_tensor(out=sel[:], in0=sc[:], in1=rbias[:], op=ALU.add), [sc, rbias], [sel])
                            yield
                            sel3 = sel[:].rearrange("p (g c) -> p g c", g=8)
                            mx = m8r.get(); mx2 = m8r.get(); gs = m8r.get(); gsort = m8r.get()
                            kb.op("dve", lambda e: e.tensor_reduce(out=mx[:], in_=sel3, axis=AX.X, op=ALU.max), [sel], [mx])
                            kb.op("dve", lambda e: e.tensor_tensor(out=w1_[:].rearrange("p (g c) -> p g c", g=8), in0=sel3, in1=mx[:].unsqueeze(2).to_broadcast([128, 8, 32]), op=ALU.is_ge),
                                  [sel, mx], [w1_])
                            kb.op("dve", lambda e: e.scalar_tensor_tensor(out=w2_[:], in0=w1_[:], scalar=-10.0, in1=sel[:], op0=ALU.mult, op1=ALU.add), [w1_, sel], [w2_])
                            kb.op("dve", lambda e: e.tensor_reduce(out=mx2[:], in_=w2_[:].rearrange("p (g c) -> p g c", g=8), axis=AX.X, op=ALU.max), [w2_], [mx2])
                            yield
                            kb.op("dve", lambda e: e.tensor_tensor(out=gs[:], in0=mx[:], in1=mx2[:], op=ALU.add), [mx, mx2], [gs])
                            kb.op("dve", lambda e: e.max(out=gsort[:], in_=gs[:]), [gs], [gsort])
                            kb.op("dve", lambda e: e.tensor_scalar(out=gs[:], in0=gs[:], scalar1=gsort[:, 3:4], scalar2=None, op0=ALU.is_ge), [gs, gsort], [gs])
                            kb.op("dve", lambda e: e.scalar_tensor_tensor(out=w1_[:].rearrange("p (g c) -> p g c", g=8), in0=sel3, scalar=2.0,
                                                                          in1=gs[:].unsqueeze(2).to_broadcast([128, 8, 32]), op0=ALU.add, op1=ALU.mult), [sel, gs], [w1_])
                            yield
                            top8 = m8r.get()
                            kb.op("dve", lambda e: e.max(out=top8[:], in_=w1_[:]), [w1_], [top8])
                            kb.op("dve", lambda e: e.tensor_scalar(out=w2_[:], in0=w1_[:], scalar1=top8[:, 7:8], scalar2=None, op0=ALU.is_ge), [w1_, top8], [w2_])
                            kb.op("pool", lambda e: e.tensor_copy(out=mask_all[:, i, :], in_=w2_[:]), [w2_], [mask_all])
                            yield
                            st = small_rot.get()
                            kb.op("pool", lambda e: e.tensor_tensor(out=w1_[:], in0=w2_[:], in1=sc[:], op=ALU.mult), [w2_, sc], [w1_])
                            kb.op("dve", lambda e: e.tensor_reduce(out=st[:, 0:1], in_=w1_[:], axis=AX.X, op=ALU.add), [w1_], [st])
                            kb.op("dve", lambda e: e.reciprocal(out=st[:, 1:2], in_=st[:, 0:1]), [st], [st])
                            kb.op("dve", lambda e: e.tensor_scalar(out=G_all[:, i, :], in0=w1_[:], scalar1=st[:, 1:2], scalar2=2.5, op0=ALU.mult, op1=ALU.mult), [w1_, st], [G_all])
                            yield
                            pc = ps1.get()
                            kb.op("pe", lambda e: e.matmul(pc[:, 0:256], lhsT=tri[:, 1, :], rhs=mask_all[:, i, :], start=True, stop=True), [tri, mask_all], [pc])
                            kb.op("pe", lambda e: e.matmul(pc[:, 256:512], lhsT=ones_b[:], rhs=mask_all[:, i, :], start=True, stop=True), [ones_b, mask_all], [pc])
                            kb.op("dve", lambda e: e.tensor_tensor(out=POS_all[:, i, :], in0=pc[:, 0:256], in1=cnt[:], op=ALU.add), [pc, cnt], [POS_all])
                            kb.op("dve", lambda e: e.tensor_tensor(out=cnt[:], in0=pc[:, 256:512], in1=cnt[:], op=ALU.add), [pc, cnt], [cnt])
                    run_interleaved((tileA(i) for i in range(NT)), 3)
                if "G_dbg" in dbg:
                    kb.dma("sp", dbg_d["G_dbg"][:, :, 0:256], G_all[:, 0:16, :], reads=[G_all], writes=[dbg_d["G_dbg"]])
                base_r = kb.sb("base_r", [128, 256], F32)
                with kb.scope():
                    ind = kb.sb("ind", [128, 256], BF16)
                    kb.op("dve", lambda e: e.tensor_scalar(out=ind[:], in0=cnt[:], scalar1=iota_p[:, 1:2], scalar2=None, op0=ALU.is_gt), [cnt, iota_p], [ind])
                    pn = ps1.get()
                    kb.op("pe", lambda e: e.matmul(pn[:, 0:256], lhsT=ones_b[:], rhs=ind[:], start=True, stop=True), [ones_b, ind], [pn])
                    nblk = kb.sb("nblk", [128, 256], BF16)
                    kb.op("dve", lambda e: e.tensor_copy(out=nblk[:], in_=pn[:, 0:256]), [pn], [nblk])
                    nbT = kb.sb("nbT", [128, 2, 128], BF16)
                    transposes(nblk, [nblk[:, 0:128], nblk[:, 128:256]], nbT[:], nbT)
                    pb = ps1.get()
                    kb.op("pe", lambda e: e.matmul(pb[:, 0:128], lhsT=nbT[:, 0, :], rhs=tri[:, 1, :], start=True, stop=True), [nbT, tri], [pb])
                    kb.op("pe", lambda e: e.matmul(pb[:, 128:256], lhsT=nbT[:, 0, :], rhs=ones_b[:], start=True, stop=False), [nbT, ones_b], [pb], inc=False)
                    kb.op("pe", lambda e: e.matmul(pb[:, 128:256], lhsT=nbT[:, 1, :], rhs=tri[:, 1, :], start=False, stop=True), [nbT, tri], [pb])
                    kb.op("dve", lambda e: e.tensor_copy(out=base_r[:], in_=pb[:, 0:256]), [pb], [base_r])
                    pe_ = ps1.get()
                    kb.op("pe", lambda e: e.matmul(pe_[:, 0:1], lhsT=tri[:, 0, :], rhs=nbT[:, 0, 0:1], start=True, stop=True), [nbT, tri], [pe_])
                    kb.op("pe", lambda e: e.matmul(pe_[:, 1:2], lhsT=ones_b[:], rhs=nbT[:, 0, 0:1], start=True, stop=False), [nbT, ones_b], [pe_], inc=False)
                    kb.op("pe", lambda e: e.matmul(pe_[:, 1:2], lhsT=tri[:, 0, :], rhs=nbT[:, 1, 0:1], start=False, stop=True), [nbT, tri], [pe_])
                    endT = kb.sb("endT", [128, 2], F32)
                    kb.op("dve", lambda e: e.tensor_copy(out=endT[:], in_=pe_[:, 0:2]), [pe_], [endT])
                    iota_b = kb.sb("iota_b", [128, NBLK], F32)
                    kb.dma("sp", iota_b[:], I["c_iotab"].ap()[:, 0:NBLK], writes=[iota_b])
                    ind2 = kb.sb("ind2", [128, 2, NBLK], BF16)
                    for c_ in range(2):
                        kb.op("dve", lambda e: e.tensor_scalar(out=ind2[:, c_, :], in0=iota_b[:], scalar1=endT[:, c_:c_ + 1], scalar2=None, op0=ALU.is_ge), [iota_b, endT], [ind2])
                    BEf = kb.sb("BEf", [128, NBLK], F32)
                    for n0 in range(0, NBLK, 512):
                        n1 = min(n0 + 512, NBLK)
                        pbe = ps1.get()
                        for c_ in range(2):
                            kb.op("pe", lambda e: e.matmul(pbe[:, 0:n1 - n0], lhsT=ones_b[:], rhs=ind2[:, c_, n0:n1], start=(c_ == 0), stop=(c_ == 1)), [ones_b, ind2], [pbe], inc=(c_ == 1))
                        kb.op("dve", lambda e: e.tensor_scalar(out=BEf[:, n0:n1], in0=pbe[:, 0:n1 - n0], scalar1=128.0, scalar2=iota_p[:, 0:1], op0=ALU.mult, op1=ALU.add), [pbe, iota_p], [BEf])
                    kb.op("dve", lambda e: e.tensor_copy(out=IDXi[:], in_=BEf[:]), [BEf], [IDXi])
                with kb.scope():
                    s256 = kb.rot("s256b", [128, 256], F32, 9)
                    m8r = kb.rot("m8b", [128, 8], F32, 6)
                    ubr = kb.rot("ubB", [128, 1024], BF16, 3)
                    def tileB(i):
                            t_ = s256.get(); V = s256.get(); eq = s256.get()
                            kb.op("dve", lambda e: e.scalar_tensor_tensor(out=t_[:], in0=base_r[:], scalar=256.0, in1=POS_all[:, i, :], op0=ALU.mult, op1=ALU.add), [base_r, POS_all], [t_])
                            kb.op("dve", lambda e: e.scalar_tensor_tensor(out=V[:], in0=t_[:], scalar=1.0, in1=mask_all[:, i, :], op0=ALU.add, op1=ALU.mult), [t_, mask_all], [V])
                            yield
                            top8 = m8r.get(); d8 = m8r.get()
                            kb.op("dve", lambda e: e.max(out=top8[:], in_=V[:]), [V], [top8])
                            kb.op("dve", lambda e: e.tensor_scalar(out=d8[:], in0=top8[:], scalar1=-1.0, scalar2=None, op0=ALU.add), [top8], [d8])
                            kb.op("dve", lambda e: e.tensor_copy(out=dest_all[:, i, :], in_=d8[:]), [d8], [dest_all])
                            yield
                            for k in range(8):
                                kb.op("dve", lambda e: e.tensor_scalar(out=eq[:], in0=V[:], scalar1=top8[:, k:k + 1], scalar2=None, op0=ALU.is_equal), [V, top8], [eq])
                                kb.op("pool", lambda e: e.tensor_tensor(out=eq[:], in0=eq[:], in1=G_all[:, i, :], op=ALU.mult), [eq, G_all], [eq])
                                kb.op("dve", lambda e: e.tensor_reduce(out=gate_all[:, i, k:k + 1], in_=eq[:], axis=AX.X, op=ALU.add), [eq], [gate_all])
                                if k % 2 == 1:
                                    yield
                            yield
                            ub = ubr.get()
                            kb.dma("sp", ub[:], u_d[i * 128:(i + 1) * 128, :], reads=[u_r[i]], writes=[ub])
                            for k in range(8):
                                kb.idma(out=xs_d[:, :], in_=ub[:], out_off=dest_all[:, i, k:k + 1], reads=[ub, dest_all], writes=[xs_d])
                    run_interleaved((tileB(i) for i in range(NT)), 3)
            xT_rot = kb.rot("xT", [128, 8, 128], BF16, 3)
            silu_rot = kb.rot("silu", [128, 256], F32, 2)
            hb_rot = kb.rot("hb", [128, 256], BF16, 4)
            hT_rot = kb.rot("hT6", [128, 2, 128], BF16, 3)

            def ffn_a(xsb, wb13, strided):
                if strided:
                    srcs = [xsb[:].rearrange("s (p k) -> s k p", k=8)[:, k, :] for k in range(8)]
                else:
                    srcs = [xsb[:, k * 128:(k + 1) * 128] for k in range(8)]
                xT = xT_rot.get()
                transposes(xsb, srcs, xT[:], xT)
                ph = ps1.get()
                for k in range(8):
                    kb.op("pe", lambda e: e.matmul(ph[:, :], lhsT=xT[:, k, :], rhs=wb13[:, k, :, :].rearrange("p w n -> p (w n)"), start=(k == 0), stop=(k == 7)),
                          [xT, wb13], [ph], inc=(k == 7))
                sl = silu_rot.get(); hb = hb_rot.get()
                kb.op("act", lambda e: e.activation(out=sl[:], in_=ph[:, 0:256], func=AF.Silu), [ph], [sl])
                kb.op("dve", lambda e: e.tensor_tensor(out=hb[:], in0=ph[:, 256:512], in1=sl[:], op=ALU.mult), [ph, sl], [hb])
                return hb

            def ffn_b(hb, wb2, strided):
                if strided:
                    hs = [hb[:].rearrange("s (p j) -> s j p", j=2)[:, j, :] for j in range(2)]
                else:
                    hs = [hb[:, j * 128:(j + 1) * 128] for j in range(2)]
                hT = hT_rot.get()
                transposes(hb, hs, hT[:], hT, eng="dve")
                po_ = ps2.get()
                for nb_ in range(2):
                    for j in range(2):
                        kb.op("pe", lambda e: e.matmul(po_[:, nb_ * 512:(nb_ + 1) * 512], lhsT=hT[:, j, :], rhs=wb2[:, j, nb_ * 512:(nb_ + 1) * 512],
                                                       start=(j == 0), stop=(j == 1)), [hT, wb2], [po_], inc=(j == 1))
                return po_

            def ffn_block(xsb, wb13, wb2, strided):
                return ffn_b(ffn_a(xsb, wb13, strided), wb2, strided)

            with kb.scope():
                w1v = I["expert_w1"].ap().rearrange("e (p k) n -> (e p) (k n)", k=8)
                w3v = I["expert_w3"].ap().rearrange("e (p k) n -> (e p) (k n)", k=8)
                w2v = I["expert_w2"].ap().rearrange("e (p j) n -> (e p) (j n)", j=2)
                wf13 = kb.rot("wf13", [128, 2, 2048], F32, 3)
                wf2 = kb.rot("wf2", [128, 2048], F32, 3)
                wb13r = kb.rot("wb13", [128, 8, 2, 256], BF16, 3)
                wb2r = kb.rot("wb2", [128, 2, 1024], BF16, 4)
                xsr = kb.rot("xsb", [128, 1024], BF16, 8)
                ybr = kb.rot("yb", [128, 1024], BF16, 3)
                for t_ in wf13.tiles + wf2.tiles:
                    kb.op("pool", lambda e: e.memset(t_[:], 0.0), [], [t_])
                pend = {}

                def issue(bk):
                    f13 = wf13.get(); f2 = wf2.get()
                    idx = IDXi[:, bk:bk + 1]
                    kb.idma(out=f13[:, 0, :], in_=w1v, in_off=idx, reads=[IDXi], writes=[f13], bounds=32767)
                    kb.idma(out=f13[:, 1, :], in_=w3v, in_off=idx, reads=[IDXi], writes=[f13], bounds=32767)
                    kb.idma(out=f2[:], in_=w2v, in_off=idx, reads=[IDXi], writes=[f2], bounds=32767)
                    xs2 = []
                    for sbk in range(2):
                        xsb = xsr.get()
                        r0 = (bk * 2 + sbk) * 128
                        kb.dma("sp", xsb[:], xs_d[r0:r0 + 128, :], reads=[xs_d], writes=[xsb])
                        xs2.append(xsb)
                    pend[bk] = (f13, f2, xs2)

                nrun = min(NBLK, blk_limit)
                NSUB = nrun * 2
                ctx_ = {}

                def st0(n):
                    bk = n // 2
                    if n % 2 == 0:
                        if bk + 2 < nrun:
                            issue(bk + 2)
                        f13, f2, xs2 = pend.pop(bk)
                        wb13 = wb13r.get(); wb2 = wb2r.get()
                        kb.op("act", lambda e: e.copy(out=wb13[:, :, 0, :], in_=f13[:, 0, :].rearrange("p (k n) -> p k n", k=8)), [f13], [wb13])
                        kb.op("dve", lambda e: e.tensor_copy(out=wb13[:, :, 1, :], in_=f13[:, 1, :].rearrange("p (k n) -> p k n", k=8)), [f13], [wb13])
                        kb.op("act", lambda e: e.copy(out=wb2[:, 0, :], in_=f2[:, 0:1024]), [f2], [wb2])
                        kb.op("dve", lambda e: e.tensor_copy(out=wb2[:, 1, :], in_=f2[:, 1024:2048]), [f2], [wb2])
                        ctx_[("w", bk)] = (wb13, wb2, xs2)
                    wb13, wb2, xs2 = ctx_[("w", bk)]
                    xsb = xs2[n % 2]
                    srcs = [xsb[:].rearrange("s (p k) -> s k p", k=8)[:, k, :] for k in range(8)]
                    xT = xT_rot.get()
                    transposes(xsb, srcs, xT[:], xT)
                    ctx_[n] = dict(xT=xT, wb13=wb13, wb2=wb2)

                def st1(n):
                    c = ctx_[n]
                    xT, wb13 = c["xT"], c["wb13"]
                    ph = ps1.get()
                    for k in range(8):
                        kb.op("pe", lambda e: e.matmul(ph[:, :], lhsT=xT[:, k, :], rhs=wb13[:, k, :, :].rearrange("p w n -> p (w n)"), start=(k == 0), stop=(k == 7)),
                              [xT, wb13], [ph], inc=(k == 7))
                    sl = silu_rot.get(); hb = hb_rot.get()
                    kb.op("act", lambda e: e.activation(out=sl[:], in_=ph[:, 0:256], func=AF.Silu), [ph], [sl])
                    kb.op("dve", lambda e: e.tensor_tensor(out=hb[:], in0=ph[:, 256:512], in1=sl[:], op=ALU.mult), [ph, sl], [hb])
                    c["hb"] = hb

                def st2(n):
                    c = ctx_[n]
                    hb = c["hb"]
                    hs = [hb[:].rearrange("s (p j) -> s j p", j=2)[:, j, :] for j in range(2)]
                    hT = hT_rot.get()
                    transposes(hb, hs, hT[:], hT, eng="dve")
                    c["hT"] = hT

                def st3(n):
                    c = ctx_.pop(n)
                    hT, wb2 = c["hT"], c["wb2"]
                    po_ = ps2.get()
                    for nb_ in range(2):
                        for j in range(2):
                            kb.op("pe", lambda e: e.matmul(po_[:, nb_ * 512:(nb_ + 1) * 512], lhsT=hT[:, j, :], rhs=wb2[:, j, nb_ * 512:(nb_ + 1) * 512],
                                                           start=(j == 0), stop=(j == 1)), [hT, wb2], [po_], inc=(j == 1))
                    yb = ybr.get()
                    kb.op("act", lambda e: e.copy(out=yb[:, 0:512], in_=po_[:, 0:512]), [po_], [yb])
                    kb.op("dve", lambda e: e.tensor_copy(out=yb[:, 512:1024], in_=po_[:, 512:1024]), [po_], [yb])
                    kb.dma("sp", Y_d[n * 128:(n + 1) * 128, :], yb[:], reads=[yb], writes=[Y_d])

                for bk in range(min(2, nrun)):
                    issue(bk)
                stages = (st0, st1, st2, st3)
                for step in range(NSUB + 3):
                    for k_, fn_ in enumerate(stages):
                        n = step - k_
                        if 0 <= n < NSUB:
                            fn_(n)
            with kb.scope():
                wsf = kb.sb("wsf", [128, 8, 256], F32)
                ws13 = kb.sb("ws13", [128, 8, 2, 256], BF16)
                ws2f = kb.sb("ws2f", [128, 2, 1024], F32)
                ws2 = kb.sb("ws2", [128, 2, 1024], BF16)
                for w_, nm in enumerate(("shared_w1", "shared_w3")):
                    kb.dma("sp", wsf[:], I[nm].ap().rearrange("(k p) n -> p k n", p=128), reads=[wsf], writes=[wsf])
                    kb.op("dve", lambda e: e.tensor_copy(out=ws13[:, :, w_, :], in_=wsf[:]), [wsf], [ws13])
                kb.dma("sp", ws2f[:], I["shared_w2"].ap().rearrange("(j p) n -> p j n", p=128), writes=[ws2f])
                kb.op("dve", lambda e: e.tensor_copy(out=ws2[:], in_=ws2f[:]), [ws2f], [ws2])
                Gms = []
                for b in range(nbatch):
                    Gm = kb.sb("Gm", [128, 1024], F32)
                    bcast_load(Gm[:], Gm, mod_d.t.ap()[b:b + 1, 5120:6144], 128, reads=[mod_d])
                    Gms.append(Gm)
                ubr = kb.rot("ubD", [128, 1024], BF16, 3)
                ygr = kb.rot("yg", [128, 1024], BF16, 8)
                accr = kb.rot("acc", [128, 1024], F32, 3)
                xrot = kb.rot("xtD", [128, 1024], F32, 3)
                def tileD(i):
                        b = i // 16
                        ub = ubr.get()
                        kb.dma("sp", ub[:], u_d[i * 128:(i + 1) * 128, :], reads=[u_r[i]], writes=[ub])
                        yield
                        hb_ = ffn_a(ub, ws13, False)
                        yield
                        po_ = ffn_b(hb_, ws2, False)
                        acc = accr.get()
                        kb.op("act", lambda e: e.copy(out=acc[:], in_=po_[:, :]), [po_], [acc])
                        yield
                        for k in range(8):
                            yg = ygr.get()
                            kb.idma(out=yg[:], in_=Y_d[:, :], in_off=dest_all[:, i, k:k + 1], reads=[dest_all, Y_d], writes=[yg])
                            kb.op("dve", lambda e: e.scalar_tensor_tensor(out=acc[:], in0=yg[:], scalar=gate_all[:, i, k:k + 1], in1=acc[:], op0=ALU.mult, op1=ALU.add),
                                  [yg, gate_all, acc], [acc])
                            if k % 2 == 1:
                                yield
                        if "moe_dbg" in dbg and i < 16:
                            kb.dma("sp", dbg_d["moe_dbg"][:, i, :], acc[:], reads=[acc], writes=[dbg_d["moe_dbg"]])
                        yield
                        xt = xrot.get()
                        kb.dma("sp", xt[:], x1_d[i * 128:(i + 1) * 128, :], reads=[x1_r[i]], writes=[xt])
                        kb.op("dve", lambda e: e.tensor_tensor(out=acc[:], in0=acc[:], in1=Gms[b][:], op=ALU.mult), [acc, Gms[b]], [acc])
                        kb.op("pool", lambda e: e.tensor_tensor(out=xt[:], in0=xt[:], in1=acc[:], op=ALU.add), [xt, acc], [xt])
                        kb.dma("sp", out_d.ap()[b, (i % 16) * 128:(i % 16 + 1) * 128, :], xt[:], reads=[xt])
                run_interleaved((tileD(i) for i in range(NT)), 3)
        kb.finish()
        print("instructions:", kb.nins, "sems:", len(kb.sems) + len(kb.dsem))
    return nc


_CACHE = {}


def kernel(**inputs):
    n = 8
    consts = host_consts()
    sq = lambda k: np.ascontiguousarray(np.asarray(inputs[k], dtype=np.float32)[0])
    shared = {}
    for k in IN_SHAPES:
        if k in ("x", "ctx", "cT") or k.startswith("c_"):
            continue
        a = sq(k)
        shared[k] = a.reshape(IN_SHAPES[k])
    shared.update(consts)
    x = np.asarray(inputs["x"], np.float32)
    ctx = np.asarray(inputs["ctx"], np.float32)
    c = np.asarray(inputs["c"], np.float32)
    c_ctx = np.asarray(inputs["c_ctx"], np.float32)
    in_maps = []
    for i in range(n):
        m = dict(shared)
        m["x"] = np.ascontiguousarray(x[2 * i:2 * i + 2])
        m["ctx"] = np.ascontiguousarray(ctx[2 * i:2 * i + 2])
        m["cT"] = np.ascontiguousarray(np.stack([c[2 * i], c[2 * i + 1], c_ctx], axis=1))
        in_maps.append(m)
    if "nc" not in _CACHE:
        _CACHE["nc"] = build()
    res = run_bass_kernel_spmd(_CACHE["nc"], in_maps, core_ids=list(range(n)))
    return np.concatenate([np.asarray(r["out"], np.float32) for r in res.results], axis=0)
```
